# Optimizing a Trainium2 kernel written in Bass

```python
import jax
import jax.numpy as jnp
from jax import lax
import numpy as np

D_MODEL = 1024
BATCH = 4
SEQ = 4096
DEPTH = 1

GRID_W = 64
CTX_LEN = 256
MIX_W = D_MODEL
NA_W = MIX_W // 2
NA_HEADS = 8
NA_HEAD_DIM = NA_W // NA_HEADS
NA_WIN_ROWS = 8
NA_WIN_COLS = 16
GLA_HEADS = 4
GLA_VAL_W = MIX_W - NA_W
GLA_DV = GLA_VAL_W // GLA_HEADS
GLA_DK = GLA_DV // 2
GLA_KEY_W = GLA_HEADS * GLA_DK
GLA_GATE_RANK = 16
GLA_GATE_TAU = 16.0
GLA_CHUNK = 16
ROPE_BASE = 10000.0
N_EXPERTS = 16
EC_CAPACITY_FACTOR = 2
D_EXPERT = 2816
NORM_EPS = 1e-6

KV_SIZES = (NA_W, NA_W, GLA_KEY_W, GLA_VAL_W, 2 * GLA_GATE_RANK)
Q_SIZES = (NA_W, GLA_KEY_W, GLA_VAL_W)
IN_SIZES = KV_SIZES + Q_SIZES
KV_COLS = sum(KV_SIZES)
IN_COLS = sum(IN_SIZES)

kernel_name = 'hybrid_na_gla_ec_diffusion_block'


def _split(t, sizes):
    cuts = [sum(sizes[:i + 1]) for i in range(len(sizes) - 1)]
    return jnp.split(t, cuts, axis=-1)


def _to_heads(t, n_heads):
    b, n, _ = t.shape
    return t.reshape(b, n, n_heads, -1).transpose(0, 2, 1, 3)


def _merge_heads(t):
    b, h, n, d = t.shape
    return t.transpose(0, 2, 1, 3).reshape(b, n, h * d)


def _rms_norm(x, g):
    xf = x.astype(jnp.float32)
    y = xf * lax.rsqrt(jnp.mean(xf * xf, axis=-1, keepdims=True) + NORM_EPS)
    return (y * g.astype(jnp.float32)).astype(x.dtype)


def _modulate(x, g, shift, scale):
    return _rms_norm(x, g) * (1 + scale) + shift


def _rope_1d(x, pos):
    half = x.shape[-1] // 2
    freqs = ROPE_BASE ** (-jnp.arange(half, dtype=jnp.float32) / half)
    ang = pos.astype(jnp.float32)[:, None] * freqs
    cos, sin = jnp.cos(ang).astype(x.dtype), jnp.sin(ang).astype(x.dtype)
    x1, x2 = x[..., :half], x[..., half:]
    return jnp.concatenate([x1 * cos - x2 * sin, x1 * sin + x2 * cos], axis=-1)


def _axial_rope(x, pos_row, pos_col):
    half = x.shape[-1] // 2
    return jnp.concatenate([_rope_1d(x[..., :half], pos_row), _rope_1d(x[..., half:], pos_col)], axis=-1)


def _neighbourhood_attention(q, k, v, k_ctx, v_ctx, rpb):
    b, h, n, dh = q.shape
    rows = n // GRID_W
    kr = min(NA_WIN_ROWS, rows)
    qg = q.reshape(b, h, rows, GRID_W, dh)
    kg = k.reshape(b, h, rows, GRID_W, dh)
    vg = v.reshape(b, h, rows, GRID_W, dh)
    r = jnp.arange(rows)
    row_start = jnp.clip(r - kr // 2, 0, rows - kr)
    key_rows = row_start[:, None] + jnp.arange(kr)
    k_blk = kg[:, :, key_rows]
    v_blk = vg[:, :, key_rows]
    col = jnp.arange(GRID_W)
    col_start = jnp.clip(col - NA_WIN_COLS // 2, 0, GRID_W - NA_WIN_COLS)
    in_win = (col[None, :] >= col_start[:, None]) & (col[None, :] < col_start[:, None] + NA_WIN_COLS)
    dr = key_rows - r[:, None] + NA_WIN_ROWS - 1
    dc = jnp.clip(col[None, :] - col[:, None] + NA_WIN_COLS - 1, 0, 2 * NA_WIN_COLS - 2)
    bias = rpb[:, dr[:, None, :, None], dc[None, :, None, :]]
    scale = dh ** -0.5
    s_win = jnp.einsum('bhrqd,bhrkwd->bhrqkw', qg, k_blk).astype(jnp.float32) * scale + bias.astype(jnp.float32)
    s_win = jnp.where(in_win[:, None, :], s_win, -jnp.inf)
    s_ctx = jnp.einsum('bhrqd,bhcd->bhrqc', qg, k_ctx).astype(jnp.float32) * scale
    scores = jnp.concatenate([s_win.reshape(b, h, rows, GRID_W, kr * GRID_W), s_ctx], axis=-1)
    p = jax.nn.softmax(scores, axis=-1).astype(v.dtype)
    p_win = p[..., :kr * GRID_W].reshape(b, h, rows, GRID_W, kr, GRID_W)
    p_ctx = p[..., kr * GRID_W:]
    o = jnp.einsum('bhrqkw,bhrkwd->bhrqd', p_win, v_blk) + jnp.einsum('bhrqc,bhcd->bhrqd', p_ctx, v_ctx)
    return o.reshape(b, h, n, dh)


def _context_attention(q, k, v):
    s = jnp.einsum('bhqd,bhkd->bhqk', q, k).astype(jnp.float32) * q.shape[-1] ** -0.5
    p = jax.nn.softmax(s, axis=-1).astype(v.dtype)
    return jnp.einsum('bhqk,bhkd->bhqd', p, v)


def _gla_log_decay(a_down, a_up, a_bias, direction):
    z = a_down[..., direction * GLA_GATE_RANK:(direction + 1) * GLA_GATE_RANK] @ a_up[direction] + a_bias[direction]
    return _to_heads(jax.nn.log_sigmoid(z.astype(jnp.float32)) / GLA_GATE_TAU, GLA_HEADS)


def _chunk(t):
    b, h, n, d = t.shape
    return t.reshape(b, h, n // GLA_CHUNK, GLA_CHUNK, d)


def _gla_states(kc, vc, lam, s0, keep_states):
    lam_last = lam[:, :, :, -1:, :]
    u = jnp.einsum('bhcld,bhcle->bhcde', kc * jnp.exp(lam_last - lam), vc)
    a_tot = jnp.exp(lam_last[:, :, :, 0, :])

    def step(s, xs):
        a_c, u_c = xs
        return a_c[..., None] * s + u_c, (s if keep_states else None)

    s_final, s_before = lax.scan(step, s0, (jnp.moveaxis(a_tot, 2, 0), jnp.moveaxis(u, 2, 0)))
    if keep_states:
        s_before = jnp.moveaxis(s_before, 0, 2)
    return s_before, s_final


def _gla_readout(qc, kc, vc, lam, s_before):
    ln = qc.shape[3]
    causal = jnp.tril(jnp.ones((ln, ln), dtype=bool))[:, :, None]
    diff = lam[:, :, :, :, None, :] - lam[:, :, :, None, :, :]
    decay = jnp.where(causal, jnp.exp(jnp.where(causal, diff, 0.0)), 0.0)
    scores = jnp.einsum('bhcid,bhcjd,bhcijd->bhcij', qc, kc, decay)
    o_intra = jnp.einsum('bhcij,bhcje->bhcie', scores, vc)
    o_inter = jnp.einsum('bhcid,bhcde->bhcie', qc * jnp.exp(lam), s_before)
    return o_intra + o_inter


def _gla_direction(q, k, v, log_a, s0, reverse):
    if reverse:
        q, k, v, log_a = (jnp.flip(t, axis=2) for t in (q, k, v, log_a))
    b, h, n, _ = v.shape
    qc, kc, vc = _chunk(q), _chunk(k), _chunk(v)
    lam = jnp.cumsum(_chunk(log_a), axis=3)
    s_before, s_final = _gla_states(kc, vc, lam, s0, True)
    o = _gla_readout(qc, kc, vc, lam, s_before).reshape(b, h, n, -1)
    return (jnp.flip(o, axis=2) if reverse else o), s_final


def _gla_final_state(k, v, log_a, s0, reverse):
    if reverse:
        k, v, log_a = (jnp.flip(t, axis=2) for t in (k, v, log_a))
    lam = jnp.cumsum(_chunk(log_a), axis=3)
    return _gla_states(_chunk(k), _chunk(v), lam, s0, False)[1]


def _gla_merge(o, g_norm, gate):
    o = _rms_norm(o, g_norm)
    return _merge_heads(o).astype(gate.dtype) * jax.nn.silu(gate)


def _expert_choice_ffn(h, router, w_gate, w_up, w_down):
    b, n, d = h.shape
    cap = EC_CAPACITY_FACTOR * n // N_EXPERTS
    affinity = jax.nn.softmax((h @ router).astype(jnp.float32), axis=-1)
    gates, idx = lax.top_k(jnp.swapaxes(affinity, 1, 2), cap)
    xs = jax.vmap(lambda hb, ib: hb[ib])(h, idx)
    hid = jax.nn.silu(jnp.einsum('becd,edf->becf', xs, w_gate)) * jnp.einsum('becd,edf->becf', xs, w_up)
    y = jnp.einsum('becf,efd->becd', hid, w_down) * gates[..., None].astype(h.dtype)
    return jax.vmap(lambda yb, ib: jnp.zeros((n, d), h.dtype).at[ib.reshape(-1)].add(yb.reshape(-1, d)))(y, idx)


def _normal(key, shape, scale):
    return jax.random.normal(key, shape, jnp.float32) * scale


def setup_inputs(seed: int = 0) -> dict:
    key = jax.random.key(seed)
    ks = jax.random.split(key, 20)
    D, L = D_MODEL, DEPTH
    return {
        'x': _normal(ks[0], (BATCH, SEQ, D), 1.0),
        'c': _normal(ks[1], (BATCH, D), 1.0),
        'ctx': _normal(ks[2], (BATCH, CTX_LEN, D), 1.0),
        'c_ctx': _normal(ks[3], (D,), 1.0),
        'w_mod': _normal(ks[4], (L, D, 6 * D), 0.5 * D ** -0.5),
        'b_mod': _normal(ks[5], (L, 6 * D), 0.01),
        'norm_mix_pre': 1.0 + _normal(ks[6], (L, D), 0.02),
        'norm_mix_post': 1.0 + _normal(ks[7], (L, D), 0.02),
        'norm_ffn_pre': 1.0 + _normal(ks[8], (L, D), 0.02),
        'norm_ffn_post': 1.0 + _normal(ks[9], (L, D), 0.02),
        'w_in': _normal(ks[10], (L, D, IN_COLS), D ** -0.5),
        'na_rpb': _normal(ks[11], (L, NA_HEADS, 2 * NA_WIN_ROWS - 1, 2 * NA_WIN_COLS - 1), 0.1),
        'gla_a_up': _normal(ks[12], (L, 2, GLA_GATE_RANK, GLA_KEY_W), GLA_GATE_RANK ** -0.5),
        'gla_a_bias': _normal(ks[13], (L, 2, GLA_KEY_W), 0.1),
        'gla_norm': 1.0 + _normal(ks[14], (L, GLA_DV), 0.02),
        'w_out': _normal(ks[15], (L, MIX_W, D), MIX_W ** -0.5),
        'router': _normal(ks[16], (L, D, N_EXPERTS), D ** -0.5),
        'w_gate': _normal(ks[17], (L, N_EXPERTS, D, D_EXPERT), D ** -0.5),
        'w_up': _normal(ks[18], (L, N_EXPERTS, D, D_EXPERT), D ** -0.5),
        'w_down': _normal(ks[19], (L, N_EXPERTS, D_EXPERT, D), D_EXPERT ** -0.5),
    }


def reference(x, c, ctx, c_ctx, w_mod, b_mod, norm_mix_pre, norm_mix_post, norm_ffn_pre, norm_ffn_post,
              w_in, na_rpb, gla_a_up, gla_a_bias, gla_norm, w_out, router, w_gate, w_up, w_down):
    b, n, d = x.shape
    t = jnp.arange(n)
    pos_row, pos_col = t // GRID_W, t % GRID_W
    s_zero = jnp.zeros((b, GLA_HEADS, GLA_DK, GLA_DV), jnp.float32)
    for layer in range(DEPTH):
        last = layer == DEPTH - 1
        mod = jax.nn.silu(c) @ w_mod[layer] + b_mod[layer]
        sh_m, sc_m, g_m, sh_f, sc_f, g_f = jnp.split(mod[:, None, :], 6, axis=-1)
        n_ctx_mods = 2 if last else 6
        mod_ctx = jax.nn.silu(c_ctx) @ w_mod[layer][:, :n_ctx_mods * d] + b_mod[layer][:n_ctx_mods * d]
        ctx_mods = jnp.split(mod_ctx, n_ctx_mods, axis=-1)

        h = _modulate(x, norm_mix_pre[layer], sh_m, sc_m)
        h_ctx = _modulate(ctx, norm_mix_pre[layer], ctx_mods[0], ctx_mods[1])
        na_k, na_v, gla_k, gla_v, a_down, na_q, gla_q, gla_g = _split(h @ w_in[layer], IN_SIZES)
        parts_ctx = _split(h_ctx @ w_in[layer][:, :(KV_COLS if last else IN_COLS)], KV_SIZES if last else IN_SIZES)
        c_na_k, c_na_v, c_gla_k, c_gla_v, c_a_down = parts_ctx[:5]
        c_na_k, c_na_v = _to_heads(c_na_k, NA_HEADS), _to_heads(c_na_v, NA_HEADS)

        o_na = _neighbourhood_attention(_to_heads(na_q, NA_HEADS), _to_heads(na_k, NA_HEADS),
                                        _to_heads(na_v, NA_HEADS), c_na_k, c_na_v, na_rpb[layer])

        c_k = _to_heads(c_gla_k, GLA_HEADS).astype(jnp.float32)
        c_v = _to_heads(c_gla_v, GLA_HEADS).astype(jnp.float32)
        c_la_f = _gla_log_decay(c_a_down, gla_a_up[layer], gla_a_bias[layer], 0)
        c_la_b = _gla_log_decay(c_a_down, gla_a_up[layer], gla_a_bias[layer], 1)
        if last:
            s_ctx_f = _gla_final_state(c_k, c_v, c_la_f, s_zero, False)
            s_ctx_b = _gla_final_state(c_k, c_v, c_la_b, s_zero, True)
        else:
            c_na_q, c_gla_q, c_gla_g = parts_ctx[5:]
            c_q = _to_heads(c_gla_q, GLA_HEADS).astype(jnp.float32) * GLA_DK ** -0.5
            oc_f, s_ctx_f = _gla_direction(c_q, c_k, c_v, c_la_f, s_zero, False)
            oc_b, s_ctx_b = _gla_direction(c_q, c_k, c_v, c_la_b, s_zero, True)
        q_l = _axial_rope(_to_heads(gla_q, GLA_HEADS).astype(jnp.float32), pos_row, pos_col) * GLA_DK ** -0.5
        k_l = _axial_rope(_to_heads(gla_k, GLA_HEADS).astype(jnp.float32), pos_row, pos_col)
        v_l = _to_heads(gla_v, GLA_HEADS).astype(jnp.float32)
        la_f = _gla_log_decay(a_down, gla_a_up[layer], gla_a_bias[layer], 0)
        la_b = _gla_log_decay(a_down, gla_a_up[layer], gla_a_bias[layer], 1)
        o_f, _ = _gla_direction(q_l, k_l, v_l, la_f, s_ctx_f, False)
        o_b, _ = _gla_direction(q_l, k_l, v_l, la_b, s_ctx_b, True)
        o_gla = _gla_merge(o_f + o_b, gla_norm[layer], gla_g)

        mix = jnp.concatenate([_merge_heads(o_na), o_gla], axis=-1) @ w_out[layer]
        x_new = x + g_m * _rms_norm(mix, norm_mix_post[layer])

        hf = _modulate(x_new, norm_ffn_pre[layer], sh_f, sc_f)
        y = _expert_choice_ffn(hf, router[layer], w_gate[layer], w_up[layer], w_down[layer])
        x_new = x_new + g_f * _rms_norm(y, norm_ffn_post[layer])

        if not last:
            o_na_c = _context_attention(_to_heads(c_na_q, NA_HEADS), c_na_k, c_na_v)
            o_gla_c = _gla_merge(oc_f + oc_b, gla_norm[layer], c_gla_g)
            mix_c = jnp.concatenate([_merge_heads(o_na_c), o_gla_c], axis=-1) @ w_out[layer]
            ctx = ctx + ctx_mods[2] * _rms_norm(mix_c, norm_mix_post[layer])
            hc = _modulate(ctx, norm_ffn_pre[layer], ctx_mods[3], ctx_mods[4])
            yc = _expert_choice_ffn(hc, router[layer], w_gate[layer], w_up[layer], w_down[layer])
            ctx = ctx + ctx_mods[5] * _rms_norm(yc, norm_ffn_post[layer])
        x = x_new
    return x
```

```python
import contextlib
import numpy as np
import ml_dtypes
import concourse.bass as bass
import concourse.mybir as mybir
from concourse.bass_utils import run_bass_kernel_spmd

F32 = mybir.dt.float32
BF16 = mybir.dt.bfloat16
I32 = mybir.dt.int32
AF = mybir.ActivationFunctionType
ALU = mybir.AluOpType

D = 1024
SEQ = 4096
CTX = 256
NTOK = SEQ + CTX
NT = NTOK // 128
INC = 3104
NE = 16
DE = 2816
NFC = DE // 128
CAP = 512
EPS = 1e-6
NEG = -30000.0


class T:
    __slots__ = ("ap", "w", "r", "name")

    def __init__(self, ap, name=""):
        self.ap = ap
        self.w = None
        self.r = {}
        self.name = name

    def __getitem__(self, idx):
        return self.ap[idx]


class Sync:
    def __init__(self, nc, n_sp=20, n_pool=8, n_act=6):
        self.nc = nc
        self.E = {"pe": nc.tensor, "act": nc.scalar, "dve": nc.vector, "pool": nc.gpsimd, "sp": nc.sync}
        self.sem = {}
        self.cnt = {}
        for k in ("pe", "act", "dve", "pool"):
            self.sem[k] = nc.alloc_semaphore("s_" + k)
            self.cnt[k] = 0
        self.seen = {k: {} for k in self.E}
        self.dq = {}
        for q, n in (("sp", n_sp), ("pool", n_pool), ("act", n_act)):
            lst = []
            for i in range(n):
                key = "d_%s%d" % (q, i)
                self.sem[key] = nc.alloc_semaphore(key)
                self.cnt[key] = 0
                lst.append(key)
            self.dq[q] = [lst, 0]

    def _wait(self, ek, key, val):
        if val <= 0:
            return
        if self.seen[ek].get(key, 0) >= val:
            return
        self.E[ek].wait_ge(self.sem[key], val)
        self.seen[ek][key] = val

    def _deps(self, ek, reads, writes):
        need = {}
        for t in reads:
            if t.w is not None:
                k, v = t.w
                need[k] = max(need.get(k, 0), v)
        for t in writes:
            if t.w is not None:
                k, v = t.w
                if not (k == "pe" and ek == "pe"):
                    need[k] = max(need.get(k, 0), v)
            for k, v in t.r.items():
                if not (k == "pe" and ek == "pe"):
                    need[k] = max(need.get(k, 0), v)
        for k, v in need.items():
            self._wait(ek, k, v)

    def op(self, ek, fn, reads=(), writes=()):
        self._deps(ek, reads, writes)
        ins = fn()
        self.cnt[ek] += 1
        ins.then_inc(self.sem[ek], 1)
        me = (ek, self.cnt[ek])
        for t in reads:
            t.r[ek] = self.cnt[ek]
        for t in writes:
            t.w = me
            t.r = {}
        return ins

    def dma(self, q, out, in_, reads=(), writes=(), fn=None, **kw):
        lst, i = self.dq[q]
        key = lst[i % len(lst)]
        self.dq[q][1] = i + 1
        self._wait(q, key, self.cnt[key])
        self._deps(q, reads, writes)
        if fn is None:
            ins = self.E[q].dma_start(out=out, in_=in_, **kw)
        else:
            ins = fn()
        self.cnt[key] += 16
        ins.then_inc(self.sem[key], 16)
        for t in reads:
            t.r[key] = self.cnt[key]
        for t in writes:
            t.w = (key, self.cnt[key])
            t.r = {}
        return ins

    def barrier(self):
        for ek in self.E:
            for k, v in self.cnt.items():
                if not (k == ek == "pe"):
                    self._wait(ek, k, v)

    def finish(self):
        for k, v in self.cnt.items():
            self._wait("sp", k, v)


class Ctx:
    def __init__(self, nc):
        self.nc = nc
        self.S = Sync(nc)
        self.uid = 0

    def sb(self, es, shape, dt, name):
        self.uid += 1
        return T(es.enter_context(self.nc.sbuf_tensor("%s_%d" % (name, self.uid), list(shape), dt)), name)

    def ps(self, es, shape, dt, name):
        self.uid += 1
        return T(es.enter_context(self.nc.psum_tensor("%s_%d" % (name, self.uid), list(shape), dt)), name)


def _rope_tables():
    half = 16
    freqs = (np.float32(10000.0) ** (-np.arange(half, dtype=np.float32) / np.float32(half))).astype(np.float32)
    t = np.arange(SEQ)
    pr = (t // 64).astype(np.float32)[:, None] * freqs
    pc = (t % 64).astype(np.float32)[:, None] * freqs
    ang = np.concatenate([pr, pc], axis=1).astype(np.float32)
    return np.cos(ang).astype(np.float32), np.sin(ang).astype(np.float32)


def _na_key_tile0(T_):
    return min(max(T_ - 2, 0), 27)


def _na_bias_tables(rpb):
    out = np.full((5, 8, 128, 5, 128), NEG, np.float32)
    for si, T_ in enumerate((0, 1, 2, 30, 31)):
        kt0 = _na_key_tile0(T_)
        for qi in range(128):
            r = 2 * T_ + qi // 64
            qc = qi % 64
            rs = min(max(r - 4, 0), 56)
            cs = min(max(qc - 8, 0), 48)
            for kr in range(rs, rs + 8):
                kt = kr // 2 - kt0
                assert 0 <= kt < 5
                dr = kr - r + 7
                kc = np.arange(cs, cs + 16)
                dc = np.clip(kc - qc + 15, 0, 30)
                j = (kr % 2) * 64 + kc
                out[si, :, j, kt, qi] = rpb[:, dr, dc].T
    return out


def _consts():
    c = {}
    c["ident_f"] = np.eye(128, dtype=np.float32)
    c["ident_b"] = np.eye(128, dtype=np.float32).astype(ml_dtypes.bfloat16)
    tt = np.arange(128)
    c["tri_f"] = np.where(tt[:, None] <= tt[None, :], -1.0 / 16.0, 0.0).astype(np.float32)
    c["tri_b"] = np.where(tt[:, None] >= tt[None, :], -1.0 / 16.0, 0.0).astype(np.float32)
    c["mask_f"] = (tt[:, None] <= tt[None, :]).astype(np.float32)
    c["mask_b"] = (tt[:, None] >= tt[None, :]).astype(np.float32)
    c["ones_col"] = np.full((128, 1), -1.0 / 16.0, np.float32)
    c["ones_row"] = np.ones((1, 128), np.float32)
    g = tt // 8
    c["blk_ones"] = (g[:, None] == g[None, :]).astype(np.float32)
    c["blk_lt"] = ((g[:, None] == g[None, :]) & (tt[:, None] < tt[None, :])).astype(np.float32)
    c["iota512"] = np.broadcast_to(np.arange(512, dtype=np.float32)[None, :], (128, 512)).copy()
    return c


def build_program(upto="all", debug=False):
    nc = bass.Bass("TRN2", target_bir_lowering=False)
    K = Ctx(nc)
    S = K.S
    pe, act, dve, pool = nc.tensor, nc.scalar, nc.vector, nc.gpsimd

    def din(name, shape, dt=F32):
        return nc.dram_tensor(name, list(shape), dt, kind="ExternalInput").ap()

    import os as _os
    _ext = set(_os.environ.get("EXTSET", "").split(","))

    def dscr(name, shape, dt=F32):
        return nc.dram_tensor(name, list(shape), dt, kind=("ExternalOutput" if (debug or name in _ext) else "Internal")).ap()

    xc = din("xc", [NTOK, D])
    cvec = din("cvec", [128, 16])
    w_mod = din("w_mod", [D, 6 * D])
    b_mod = din("b_mod", [1, 6 * D])
    norms = din("norms", [4, D])
    gla_norm = din("gla_norm", [1, 128])
    w_in = din("w_in", [D, INC])
    aup_bd = din("aup_bd", [32, 512])
    abias = din("abias", [1, 512])
    rope_cos = din("rope_cos", [SEQ, 32])
    rope_sin = din("rope_sin", [SEQ, 32])
    na_bias = din("na_bias", [5, 8, 128, 640])
    w_out = din("w_out", [D, D])
    router = din("router", [D, NE])
    if upto in ("F", "G", "all"):
        w_gate = din("w_gate", [NE, 11, 128, 8 * 256])
        w_up = din("w_up", [NE, 11, 128, 8 * 256])
        w_down = din("w_down", [NE, 4, 128, NFC * 256])
    ident_f = din("ident_f", [128, 128])
    ident_b = din("ident_b", [128, 128], BF16)
    tri_f = din("tri_f", [128, 128])
    tri_b = din("tri_b", [128, 128])
    mask_f = din("mask_f", [128, 128])
    mask_b = din("mask_b", [128, 128])
    ones_col = din("ones_col", [128, 1])
    ones_row = din("ones_row", [1, 128])
    blk_ones = din("blk_ones", [128, 128])
    blk_lt = din("blk_lt", [128, 128])
    iota512 = din("iota512", [128, 512])
    own_idx = din("own_idx", [128, 16], I32)
    own_flag = din("own_flag", [128, 1])
    sel = din("sel", [128, 64])
    out = nc.dram_tensor("out", [SEQ // 2, D], F32, kind="ExternalOutput").ap()

    mods_d = dscr("mods_d", [128, 8, D])
    KT_d = dscr("KT_d", [512, NTOK], BF16)
    QT_d = dscr("QT_d", [512, SEQ], BF16)
    V_d = dscr("V_d", [NTOK, 512], BF16)
    GV_d = dscr("GV_d", [NTOK, 512], BF16)
    GKQ_d = dscr("GKQ_d", [NTOK, 544])
    GG_d = dscr("GG_d", [SEQ, 512])
    OF_d = dscr("OF_d", [SEQ, 512])
    MIX_d = dscr("MIX_d", [SEQ, D], BF16)
    XN_d = dscr("XN_d", [SEQ, D])
    HF_d = dscr("HF_d", [SEQ, D], BF16)
    AFFT_d = dscr("AFFT_d", [NE, SEQ])

    glob = contextlib.ExitStack()
    idf = K.sb(glob, [128, 128], F32, "idf")
    idb = K.sb(glob, [128, 128], BF16, "idb")
    onesr = K.sb(glob, [1, 128], F32, "onesr")
    S.dma("sp", idf[:, :], ident_f, writes=[idf])
    S.dma("sp", idb[:, :], ident_b, writes=[idb])
    S.dma("sp", onesr[:, :], ones_row, writes=[onesr])

    def stage0():
        with contextlib.ExitStack() as es:
            cv = K.sb(es, [128, 16], F32, "cv")
            csil = K.sb(es, [128, 16], F32, "csil")
            crep = K.sb(es, [128, 16, 128], F32, "crep")
            bm = K.sb(es, [1, 6 * D], F32, "bm")
            modA = K.sb(es, [128, 6 * D], F32, "modA")
            modC = K.sb(es, [128, 2 * D], F32, "modC")
            nbc = K.sb(es, [128, 4, D], F32, "nbc")
            wm = [K.sb(es, [128, 8, 512], F32, "wm%d" % i) for i in range(2)]
            mods = K.sb(es, [128, 8, D], F32, "mods")
            psA = [K.ps(es, [128, 512], F32, "psA%d" % i) for i in range(2)]
            psC = [K.ps(es, [128, 512], F32, "psC%d" % i) for i in range(2)]
            S.dma("sp", cv[:, :], cvec, writes=[cv])
            S.dma("sp", bm[:, :], b_mod, writes=[bm])
            S.dma("sp", nbc[:, :, :], norms.partition_broadcast(128), writes=[nbc])
            S.op("act", lambda: act.activation(out=csil[:, :], in_=cv[:, :], func=AF.Silu), [cv], [csil])
            S.op("dve", lambda: dve.tensor_copy(out=crep[:, :, :], in_=csil[:, :].unsqueeze(2).broadcast_to([128, 16, 128])), [csil], [crep])
            wmv = w_mod.rearrange("(c p) n -> p c n", p=128)
            for nb in range(12):
                w = wm[nb % 2]
                S.dma("sp", w[:, :, :], wmv[:, :, nb * 512:(nb + 1) * 512], writes=[w])
                for which in range(2 if nb < 4 else 1):
                    ps = (psA, psC)[which][nb % 2]
                    for c in range(8):
                        S.op("pe", lambda c=c, ps=ps, w=w, which=which: pe.matmul(ps[:, :], lhsT=crep[:, which * 8 + c, :], rhs=w[:, c, :], start=(c == 0), stop=False), [crep, w], [ps])
                    S.op("pe", lambda ps=ps, nb=nb: pe.matmul(ps[:, :], lhsT=onesr[:, :], rhs=bm[:, nb * 512:(nb + 1) * 512], start=False, stop=True), [onesr, bm], [ps])
                    dst = (modA, modC)[which]
                    if which == 0:
                        S.op("act", lambda ps=ps, dst=dst, nb=nb: act.copy(out=dst[:, nb * 512:(nb + 1) * 512], in_=ps[:, :]), [ps], [dst])
                    else:
                        S.op("dve", lambda ps=ps, dst=dst, nb=nb: dve.tensor_copy(out=dst[:, nb * 512:(nb + 1) * 512], in_=ps[:, :]), [ps], [dst])
            import os
            if os.environ.get("STOP0") == "1":
                S.dma("sp", mods_d[:, 0:6, :], modA[:, :].rearrange("p (a b) -> p a b", a=6), reads=[modA], writes=[T_mods])
                return
            sl = lambda t, i: t[:, i * D:(i + 1) * D]
            S.op("dve", lambda: dve.scalar_tensor_tensor(out=mods[:, 0, :], in0=sl(modA, 1), scalar=1.0, in1=nbc[:, 0, :], op0=ALU.add, op1=ALU.mult), [modA, nbc], [mods])
            S.op("dve", lambda: dve.tensor_copy(out=mods[:, 1, :], in_=sl(modA, 0)), [modA], [mods])
            S.op("dve", lambda: dve.scalar_tensor_tensor(out=mods[:, 2, :], in0=sl(modC, 1), scalar=1.0, in1=nbc[:, 0, :], op0=ALU.add, op1=ALU.mult), [modC, nbc], [mods])
            S.op("dve", lambda: dve.tensor_copy(out=mods[:, 3, :], in_=sl(modC, 0)), [modC], [mods])
            S.op("dve", lambda: dve.tensor_tensor(out=mods[:, 4, :], in0=sl(modA, 2), in1=nbc[:, 1, :], op=ALU.mult), [modA, nbc], [mods])
            S.op("dve", lambda: dve.scalar_tensor_tensor(out=mods[:, 5, :], in0=sl(modA, 4), scalar=1.0, in1=nbc[:, 2, :], op0=ALU.add, op1=ALU.mult), [modA, nbc], [mods])
            S.op("dve", lambda: dve.tensor_copy(out=mods[:, 6, :], in_=sl(modA, 3)), [modA], [mods])
            S.op("dve", lambda: dve.tensor_tensor(out=mods[:, 7, :], in0=sl(modA, 5), in1=nbc[:, 3, :], op=ALU.mult), [modA, nbc], [mods])
            S.dma("sp", mods_d, mods[:, :, :], reads=[mods], writes=[T_mods])

    T_mods = T(None, "mods_d")
    T_KT, T_QT, T_V, T_GV, T_GKQ, T_GG = (T(None, n) for n in ("KT", "QT", "V", "GV", "GKQ", "GG"))

    def rms_rstd(es_tiles, src, src_t, ss, rstd, junk):
        S.op("act", lambda: act.activation(out=junk[:, :], in_=src, func=AF.Square, accum_out=ss[:, :]), [src_t], [junk, ss])
        S.op("act", lambda: act.activation(out=rstd[:, :], in_=ss[:, :], func=AF.Sqrt, scale=1.0 / D, bias=epsb[:, :]), [ss, epsb], [rstd])
        S.op("dve", lambda: dve.reciprocal(out=rstd[:, :], in_=rstd[:, :]), [rstd], [rstd])

    epsb = K.sb(glob, [128, 1], F32, "epsb")
    S.op("pool", lambda: pool.memset(epsb[:, :], EPS), [], [epsb])

    def stageA():
        with contextlib.ExitStack() as es:
            win = K.sb(es, [128, 8, INC], BF16, "win")
            wst = [K.sb(es, [128, 8, 512], F32, "wst%d" % i) for i in range(2)]
            gs = K.sb(es, [128, 4, D], F32, "gs")
            S.dma("sp", gs[:, :, :], mods_d[:, 0:4, :], reads=[T_mods], writes=[gs])
            wiv = w_in.rearrange("(c p) n -> p c n", p=128)
            for b in range(7):
                c0 = b * 512
                w_ = min(512, INC - c0)
                st = wst[b % 2]
                S.dma("sp", st[:, :, 0:w_], wiv[:, :, c0:c0 + w_], writes=[st])
                if b % 2 == 0:
                    S.op("act", lambda st=st, c0=c0, w_=w_: act.copy(out=win[:, :, c0:c0 + w_], in_=st[:, :, 0:w_]), [st], [win])
                else:
                    S.op("dve", lambda st=st, c0=c0, w_=w_: dve.tensor_copy(out=win[:, :, c0:c0 + w_], in_=st[:, :, 0:w_]), [st], [win])
            import os
            STOPA = int(os.environ.get("STOPA", "99"))
            if STOPA <= 1:
                return
            xt = [K.sb(es, [128, D], F32, "xt%d" % i) for i in range(2)]
            junk = K.sb(es, [128, D], BF16, "junk")
            ss = [K.sb(es, [128, 1], F32, "ss%d" % i) for i in range(2)]
            rstd = [K.sb(es, [128, 1], F32, "rstd%d" % i) for i in range(2)]
            h1 = [K.sb(es, [128, D], F32, "h1_%d" % i) for i in range(2)]
            hb = [K.sb(es, [128, D], BF16, "hb%d" % i) for i in range(2)]
            hT = [K.sb(es, [128, 8, 512], BF16, "hT%d" % i) for i in range(2)]
            ktq = [K.sb(es, [128, 512], BF16, "ktq%d" % i) for i in range(2)]
            vo = [K.sb(es, [128, 512], BF16, "vo%d" % i) for i in range(2)]
            gvo = [K.sb(es, [128, 512], BF16, "gvo%d" % i) for i in range(2)]
            gkq = [K.sb(es, [128, 544], F32, "gkq%d" % i) for i in range(2)]
            ggo = [K.sb(es, [128, 512], F32, "ggo%d" % i) for i in range(2)]
            pst = [K.ps(es, [128, 8 * 128], BF16, "pst%d" % i) for i in range(2)]
            psp = [K.ps(es, [128, 512], F32, "psp%d" % i) for i in range(4)]
            npp = [0]

            def nextps():
                npp[0] += 1
                return psp[npp[0] % 4]

            ev = [0]

            def evac(dst_t, dst_ap, ps, ps_ap):
                if psp.index(ps) % 2 == 0:
                    S.op("act", lambda: act.copy(out=dst_ap, in_=ps_ap), [ps], [dst_t])
                else:
                    S.op("dve", lambda: dve.tensor_copy(out=dst_ap, in_=ps_ap), [ps], [dst_t])

            groups = [(0, 2)] + [(2 + 4 * g, 4) for g in range(8)]
            ti_glob = 0
            for gi, (t0, ntl) in enumerate(groups):
                is_ctx = gi == 0
                hTg = hT[gi % 2]
                for k in range(ntl):
                    ti = t0 + k
                    b2 = ti_glob % 2
                    ti_glob += 1
                    x_ = xt[b2]
                    S.dma("sp", x_[:, :], xc[ti * 128:(ti + 1) * 128, :], writes=[x_])
                    rms_rstd(None, x_[:, :], x_, ss[b2], rstd[b2], junk)
                    go = 2 if is_ctx else 0
                    h_ = h1[b2]
                    S.op("dve", lambda x_=x_, h_=h_, b2=b2, go=go: dve.scalar_tensor_tensor(out=h_[:, :], in0=x_[:, :], scalar=rstd[b2][:, 0:1], in1=gs[:, go, :], op0=ALU.mult, op1=ALU.mult), [x_, rstd[b2], gs], [h_])
                    S.op("pool", lambda h_=h_, b2=b2, go=go: pool.tensor_tensor(out=hb[b2][:, :], in0=h_[:, :], in1=gs[:, go + 1, :], op=ALU.add), [h_, gs], [hb[b2]])
                    pt = pst[b2]
                    for c in range(8):
                        S.op("pe", lambda c=c, pt=pt, b2=b2: pe.transpose(out=pt[:, c * 128:(c + 1) * 128], in_=hb[b2][:, c * 128:(c + 1) * 128], identity=idb[:, :]), [hb[b2], idb], [pt])
                    S.op("act", lambda pt=pt, hTg=hTg, k=k: act.copy(out=hTg[:, :, k * 128:(k + 1) * 128], in_=pt[:, :].rearrange("p (c t) -> p c t", c=8)), [pt], [hTg])
                ntok = ntl * 128
                tok0 = t0 * 128
                if STOPA <= 2:
                    return
                for which, c0 in ((0, 0), (1, 1824)):
                    if which == 1 and is_ctx:
                        continue
                    for fb in range(4):
                        ps = nextps()
                        for c in range(8):
                            S.op("pe", lambda c=c, ps=ps, fb=fb, c0=c0: pe.matmul(ps[:, 0:ntok], lhsT=win[:, c, c0 + fb * 128:c0 + (fb + 1) * 128], rhs=hTg[:, c, 0:ntok], start=(c == 0), stop=(c == 7)), [win, hTg], [ps])
                        kb = ktq[(which * 4 + fb) % 2]
                        evac(kb, kb[:, 0:ntok], ps, ps[:, 0:ntok])
                        if which == 0:
                            S.dma("sp", KT_d[fb * 128:(fb + 1) * 128, tok0:tok0 + ntok], kb[:, 0:ntok], reads=[kb], writes=[T_KT])
                        else:
                            S.dma("sp", QT_d[fb * 128:(fb + 1) * 128, tok0 - CTX:tok0 - CTX + ntok], kb[:, 0:ntok], reads=[kb], writes=[T_QT])
                if STOPA <= 3:
                    return
                for k in range(ntl):
                    ti = t0 + k
                    b2 = ti % 2
                    r0 = ti * 128

                    def mm(c0, w_, k=k):
                        ps = nextps()
                        for c in range(8):
                            S.op("pe", lambda c=c, ps=ps: pe.matmul(ps[:, 0:w_], lhsT=hTg[:, c, k * 128:(k + 1) * 128], rhs=win[:, c, c0:c0 + w_], start=(c == 0), stop=(c == 7)), [win, hTg], [ps])
                        return ps

                    ps = mm(512, 512)
                    evac(vo[b2], vo[b2][:, :], ps, ps[:, :])
                    S.dma("sp", V_d[r0:r0 + 128, :], vo[b2][:, :], reads=[vo[b2]], writes=[T_V])
                    TM = int(os.environ.get("TM", "9"))
                    if TM <= 1:
                        continue
                    ps = mm(1024, 512)
                    EV = int(os.environ.get("EV", "3"))
                    if EV & 1:
                        evac(gkq[b2], gkq[b2][:, 0:256], ps, ps[:, 0:256])
                    if EV & 2:
                        evac(gvo[b2], gvo[b2][:, 0:256], ps, ps[:, 256:512])
                    if TM <= 2:
                        continue
                    ps = mm(1536, 288)
                    evac(gvo[b2], gvo[b2][:, 256:512], ps, ps[:, 0:256])
                    evac(gkq[b2], gkq[b2][:, 256:288], ps, ps[:, 256:288])
                    S.dma("sp", GV_d[r0:r0 + 128, :], gvo[b2][:, :], reads=[gvo[b2]], writes=[T_GV])
                    if TM <= 3:
                        continue
                    if not is_ctx:
                        ps = mm(2336, 512)
                        evac(gkq[b2], gkq[b2][:, 288:544], ps, ps[:, 0:256])
                        evac(ggo[b2], ggo[b2][:, 0:256], ps, ps[:, 256:512])
                        ps = mm(2848, 256)
                        evac(ggo[b2], ggo[b2][:, 256:512], ps, ps[:, 0:256])
                        S.dma("sp", GG_d[r0 - CTX:r0 - CTX + 128, :], ggo[b2][:, :], reads=[ggo[b2]], writes=[T_GG])
                        S.dma("sp", GKQ_d[r0:r0 + 128, :], gkq[b2][:, :], reads=[gkq[b2]], writes=[T_GKQ])
                    else:
                        S.dma("sp", GKQ_d[r0:r0 + 128, 0:288], gkq[b2][:, 0:288], reads=[gkq[b2]], writes=[T_GKQ])
                if STOPA <= 4:
                    return


    T_MIXna = T(None, "MIXna")

    def stageB():
        with contextlib.ExitStack() as es:
            NB = 3
            qT = [K.sb(es, [128, 4, 128], BF16, "qT%d" % i) for i in range(2)]
            kT = [K.sb(es, [128, 4, 896], BF16, "kT%d" % i) for i in range(2)]
            va = [K.sb(es, [128, 7, 8, 65], BF16, "va%d" % i) for i in range(2)]
            bint = K.sb(es, [128, 8, 640], F32, "bint")
            bt = [K.sb(es, [128, 640], F32, "bt%d" % i) for i in range(3)]
            tmp = [K.sb(es, [128, 896], F32, "tmp%d" % i) for i in range(NB)]
            pT = [K.sb(es, [128, 896], BF16, "pT%d" % i) for i in range(NB + 1)]
            ona = [K.sb(es, [128, 512], BF16, "ona%d" % i) for i in range(2)]
            rec = [K.sb(es, [128, 1], F32, "rec%d" % i) for i in range(2)]
            sT = [K.ps(es, [128, 1024], F32, "sT%d" % i) for i in range(NB)]
            pv = [K.ps(es, [128, 512], F32, "pv%d" % i) for i in range(2)]
            for v in va:
                S.op("pool", lambda v=v: pool.memset(v[:, :, :, :], 1.0), [], [v])
            S.dma("sp", bint[:, :, :], na_bias[2].rearrange("h j c -> j h c"), writes=[bint])
            QTv = QT_d.rearrange("(c p) t -> p c t", p=128)
            KTv = KT_d.rearrange("(c p) t -> p c t", p=128)
            nbt = [0]

            def loads(T_):
                b = T_ % 2
                kt0 = _na_key_tile0(T_)
                S.dma("sp", qT[b][:, :, :], QTv[:, :, T_ * 128:(T_ + 1) * 128], reads=[T_QT], writes=[qT[b]])
                S.dma("sp", kT[b][:, :, 0:640], KTv[:, :, CTX + kt0 * 128:CTX + kt0 * 128 + 640], reads=[T_KT], writes=[kT[b]])
                S.dma("sp", kT[b][:, :, 640:896], KTv[:, :, 0:CTX], reads=[T_KT], writes=[kT[b]])
                r0 = CTX + kt0 * 128
                for kk in range(5):
                    S.dma("sp", va[b][:, kk, :, 0:64], V_d[r0 + kk * 128:r0 + (kk + 1) * 128, :].rearrange("t (h d) -> t h d", h=8), reads=[T_V], writes=[va[b]])
                for kk in range(2):
                    S.dma("sp", va[b][:, 5 + kk, :, 0:64], V_d[kk * 128:(kk + 1) * 128, :].rearrange("t (h d) -> t h d", h=8), reads=[T_V], writes=[va[b]])

            def phase1(n):
                T_, h = n // 8, n % 8
                b = T_ % 2
                if h == 0:
                    loads(T_)
                si = {0: 0, 1: 1, 30: 3, 31: 4}.get(T_, 2)
                pr, P0 = h // 2, (h % 2) * 64
                s_, tm, pt = sT[n % NB], tmp[n % NB], pT[n % (NB + 1)]
                if si == 2:
                    bb_t, bb = bint, bint[:, h, :]
                else:
                    bb_t = bt[nbt[0] % 3]
                    nbt[0] += 1
                    bb = bb_t[:, :]
                    S.dma("sp", bb, na_bias[si, h], writes=[bb_t])
                for kk in range(7):
                    S.op("pe", lambda kk=kk: pe.matmul(s_[:, kk * 128:(kk + 1) * 128], lhsT=kT[b][P0:P0 + 64, pr, kk * 128:(kk + 1) * 128], rhs=qT[b][P0:P0 + 64, pr, :], start=True, stop=True), [kT[b], qT[b]], [s_])
                S.op("dve", lambda: dve.scalar_tensor_tensor(out=tm[:, 0:512], in0=s_[:, 0:512], scalar=0.125, in1=bb[:, 0:512], op0=ALU.mult, op1=ALU.add), [s_, bb_t], [tm])
                S.op("dve", lambda: dve.scalar_tensor_tensor(out=tm[:, 512:640], in0=s_[:, 512:640], scalar=0.125, in1=bb[:, 512:640], op0=ALU.mult, op1=ALU.add), [s_, bb_t], [tm])
                S.op("dve", lambda: dve.tensor_scalar(out=tm[:, 640:896], in0=s_[:, 640:896], scalar1=0.125, scalar2=None, op0=ALU.mult), [s_], [tm])
                S.op("act", lambda: act.activation(out=pt[:, :], in_=tm[:, :], func=AF.Exp), [tm], [pt])

            def phase2(n):
                T_, h = n // 8, n % 8
                b = T_ % 2
                pt, p_, rc = pT[n % (NB + 1)], pv[n % 2], rec[n % 2]
                for kk in range(7):
                    S.op("pe", lambda kk=kk: pe.matmul(p_[:, 0:65], lhsT=pt[:, kk * 128:(kk + 1) * 128], rhs=va[b][:, kk, h, :], start=(kk == 0), stop=(kk == 6)), [pt, va[b]], [p_])
                S.op("dve", lambda: dve.reciprocal(out=rc[:, :], in_=p_[:, 64:65]), [p_], [rc])
                S.op("dve", lambda: dve.tensor_scalar(out=ona[b][:, h * 64:(h + 1) * 64], in0=p_[:, 0:64], scalar1=rc[:, 0:1], scalar2=None, op0=ALU.mult), [p_, rc], [ona[b]])
                if h == 7:
                    S.dma("sp", MIX_d[T_ * 128:(T_ + 1) * 128, 0:512], ona[b][:, :], reads=[ona[b]], writes=[T_MIXna])

            NIT = 32 * 8
            LOOK = NB - 1
            for n in range(min(LOOK, NIT)):
                phase1(n)
            for n in range(NIT):
                if n + LOOK < NIT:
                    phase1(n + LOOK)
                phase2(n)

    T_OF = T(None, "OF")
    T_MIXgla = T(None, "MIXgla")

    def stageC():
        import os
        with contextlib.ExitStack() as es:
            tri = [K.sb(es, [128, 128], F32, "tri%d" % i) for i in range(2)]
            msk = [K.sb(es, [128, 512], F32, "msk%d" % i) for i in range(2)]
            onec = K.sb(es, [128, 1], F32, "onec")
            aup = K.sb(es, [32, 512], F32, "aup")
            abi = K.sb(es, [1, 512], F32, "abi")
            gnb = K.sb(es, [128, 128], F32, "gnb")
            for t_, src in ((tri[0], tri_f), (tri[1], tri_b), (onec, ones_col), (aup, aup_bd), (abi, abias)):
                S.dma("sp", t_[:, :], src, writes=[t_])
            for h in range(4):
                S.dma("sp", msk[0][:, h * 128:(h + 1) * 128], mask_f, writes=[msk[0]])
                S.dma("sp", msk[1][:, h * 128:(h + 1) * 128], mask_b, writes=[msk[1]])
            S.dma("sp", gnb[:, :], gla_norm.partition_broadcast(128).rearrange("p a b -> p (a b)"), writes=[gnb])
            gkq = [K.sb(es, [128, 544], F32, "gkq%d" % i) for i in range(2)]
            gv = [K.sb(es, [128, 512], BF16, "gv%d" % i) for i in range(2)]
            cs = [K.sb(es, [128, 32], F32, "cs%d" % i) for i in range(2)]
            sn = [K.sb(es, [128, 32], F32, "sn%d" % i) for i in range(2)]
            gg = [K.sb(es, [128, 512], F32, "gg%d" % i) for i in range(2)]
            ofl = [K.sb(es, [128, 512], F32, "ofl%d" % i) for i in range(2)]
            adT = K.sb(es, [32, 128], F32, "adT")
            ez = K.sb(es, [128, 256], F32, "ez")
            lz = K.sb(es, [128, 256], F32, "lz")
            E1 = K.sb(es, [128, 256], F32, "E1")
            E2 = K.sb(es, [128, 256], F32, "E2")
            at = K.sb(es, [128, 2], F32, "at")
            kr = K.sb(es, [128, 256], F32, "kr")
            qr = K.sb(es, [128, 256], F32, "qr")
            rt = [K.sb(es, [128, 128], F32, "rt%d" % i) for i in range(4)]
            qk = K.sb(es, [128, 512], BF16, "qk")
            qTz = K.sb(es, [128, 4, 128], BF16, "qTz")
            S.op("pool", lambda: pool.memset(qTz[:, :, :], 0.0), [], [qTz])
            kTc = K.sb(es, [128, 2, 128], BF16, "kTc")
            ATs = K.sb(es, [128, 4, 128], BF16, "ATs")
            Sst = [K.sb(es, [128, 128], F32, "Sst%d" % i) for i in range(2)]
            Sbf = [K.sb(es, [128, 128], BF16, "Sbf%d" % i) for i in range(2)]
            tU = K.sb(es, [128, 128], F32, "tU")
            osb = K.sb(es, [128, 512], F32, "osb")
            junk = K.sb(es, [128, 128], F32, "junkc")
            ss4 = K.sb(es, [128, 4], F32, "ss4")
            rs4 = K.sb(es, [128, 4], F32, "rs4")
            sg = K.sb(es, [128, 512], F32, "sg")
            ogl = [K.sb(es, [128, 512], BF16, "ogl%d" % i) for i in range(2)]
            ps_ad = K.ps(es, [128, 512], F32, "ps_ad")
            ps_z = K.ps(es, [128, 512], F32, "ps_z")
            ps_lam = K.ps(es, [128, 512], F32, "ps_lam")
            ps_t = K.ps(es, [128, 1024], BF16, "ps_t")
            ps_AT = K.ps(es, [128, 512], F32, "ps_AT")
            ps_o = K.ps(es, [128, 512], F32, "ps_o")
            ps_U = K.ps(es, [128, 512], F32, "ps_U")

            def rope(eng, ek, src_t, src_ap, dst_t, tmps, c_, s_):
                X = src_ap.rearrange("p (h a b f) -> p h a b f", h=4, a=2, b=2)
                O = dst_t[:, :].rearrange("p (h a b f) -> p h a b f", h=4, a=2, b=2)
                C = c_[:, :].rearrange("p (a f) -> p a f", a=2).unsqueeze(1).broadcast_to([128, 4, 2, 16])
                Sn = s_[:, :].rearrange("p (a f) -> p a f", a=2).unsqueeze(1).broadcast_to([128, 4, 2, 16])
                v = lambda t_: t_[:, :].rearrange("p (h a f) -> p h a f", h=4, a=2)
                X1, X2 = X[:, :, :, 0, :], X[:, :, :, 1, :]
                t1, t2, t3, t4 = tmps
                S.op(ek, lambda: eng.tensor_tensor(out=v(t1), in0=X1, in1=C, op=ALU.mult), [src_t, c_], [t1])
                S.op(ek, lambda: eng.tensor_tensor(out=v(t2), in0=X2, in1=Sn, op=ALU.mult), [src_t, s_], [t2])
                S.op(ek, lambda: eng.tensor_tensor(out=O[:, :, :, 0, :], in0=v(t1), in1=v(t2), op=ALU.subtract), [t1, t2], [dst_t])
                S.op(ek, lambda: eng.tensor_tensor(out=v(t3), in0=X1, in1=Sn, op=ALU.mult), [src_t, s_], [t3])
                S.op(ek, lambda: eng.tensor_tensor(out=v(t4), in0=X2, in1=C, op=ALU.mult), [src_t, c_], [t4])
                S.op(ek, lambda: eng.tensor_tensor(out=O[:, :, :, 1, :], in0=v(t3), in1=v(t4), op=ALU.add), [t3, t4], [dst_t])

            n = 0
            for dr in range(2):
                for pr in range(2):
                    S.op("pool", lambda pr=pr: pool.memset(Sst[pr][:, :], 0.0), [], [Sst[pr]])
                    S.op("pool", lambda pr=pr: pool.memset(Sbf[pr][:, :], 0.0), [], [Sbf[pr]])
                order = list(range(NT)) if dr == 0 else [1, 0] + list(range(NT - 1, 1, -1))
                for ti in order:
                    lat = ti >= 2
                    li = ti - 2
                    b = n % 2
                    n += 1
                    g_, v_ = gkq[b], gv[b]
                    gw = 544 if lat else 288
                    S.dma("sp", g_[:, 0:gw], GKQ_d[ti * 128:(ti + 1) * 128, 0:gw], reads=[T_GKQ], writes=[g_])
                    S.dma("sp", v_[:, :], GV_d[ti * 128:(ti + 1) * 128, :], reads=[T_GV], writes=[v_])
                    if lat:
                        S.dma("sp", cs[b][:, :], rope_cos[li * 128:(li + 1) * 128, :], writes=[cs[b]])
                        S.dma("sp", sn[b][:, :], rope_sin[li * 128:(li + 1) * 128, :], writes=[sn[b]])
                        if dr == 1:
                            S.dma("sp", gg[b][:, :], GG_d[li * 128:(li + 1) * 128, :], reads=[T_GG], writes=[gg[b]])
                            S.dma("sp", ofl[b][:, :], OF_d[li * 128:(li + 1) * 128, :], reads=[T_OF], writes=[ofl[b]])
                    S.op("pe", lambda g_=g_: pe.transpose(out=ps_ad[0:32, 0:128], in_=g_[:, 256:288], identity=idf[:, :]), [g_, idf], [ps_ad])
                    S.op("act", lambda: act.copy(out=adT[:, :], in_=ps_ad[0:32, 0:128]), [ps_ad], [adT])
                    STOPC = int(os.environ.get("STOPC", "99"))
                    if STOPC <= 1:
                        return
                    S.op("pe", lambda dr=dr: pe.matmul(ps_z[:, 0:256], lhsT=adT[:, :], rhs=aup[:, dr * 256:(dr + 1) * 256], start=True, stop=False), [adT, aup], [ps_z])
                    S.op("pe", lambda dr=dr: pe.matmul(ps_z[:, 0:256], lhsT=onesr[:, :], rhs=abi[:, dr * 256:(dr + 1) * 256], start=False, stop=True), [onesr, abi], [ps_z])
                    S.op("act", lambda: act.activation(out=ez[:, :], in_=ps_z[:, 0:256], func=AF.Exp, scale=-1.0), [ps_z], [ez])
                    S.op("act", lambda: act.activation(out=lz[:, :], in_=ez[:, :], func=AF.Ln, bias=1.0), [ez], [lz])
                    if STOPC <= 2:
                        return
                    S.op("pe", lambda dr=dr: pe.matmul(ps_lam[:, 0:256], lhsT=tri[dr][:, :], rhs=lz[:, :], start=True, stop=True), [tri[dr], lz], [ps_lam])
                    for pr in range(2):
                        S.op("pe", lambda pr=pr: pe.matmul(ps_lam[:, 256 + pr:257 + pr], lhsT=lz[:, pr * 128:(pr + 1) * 128], rhs=onec[:, :], start=True, stop=True), [lz, onec], [ps_lam])
                    if lat:
                        S.op("act", lambda: act.activation(out=E1[:, :], in_=ps_lam[:, 0:256], func=AF.Exp), [ps_lam], [E1])
                    S.op("act", lambda: act.activation(out=E2[:, :], in_=ps_lam[:, 0:256], func=AF.Exp, scale=-1.0), [ps_lam], [E2])
                    S.op("act", lambda: act.activation(out=at[:, :], in_=ps_lam[:, 256:258], func=AF.Exp), [ps_lam], [at])
                    if STOPC <= 3:
                        return
                    if lat:
                        rope(dve, "dve", g_, g_[:, 0:256], kr, rt, cs[b], sn[b])
                        rope(dve, "dve", g_, g_[:, 288:544], qr, rt, cs[b], sn[b])
                        S.op("dve", lambda: dve.scalar_tensor_tensor(out=qk[:, 0:256], in0=qr[:, :], scalar=0.125, in1=E1[:, :], op0=ALU.mult, op1=ALU.mult), [qr, E1], [qk])
                        S.op("dve", lambda: dve.tensor_tensor(out=qk[:, 256:512], in0=kr[:, :], in1=E2[:, :], op=ALU.mult), [kr, E2], [qk])
                        if STOPC == 71:
                            return
                    else:
                        S.op("dve", lambda g_=g_: dve.tensor_tensor(out=qk[:, 256:512], in0=g_[:, 0:256], in1=E2[:, :], op=ALU.mult), [g_, E2], [qk])
                    for j in (range(4) if lat else range(2, 4)):
                        S.op("pe", lambda j=j: pe.transpose(out=ps_t[:, j * 128:(j + 1) * 128], in_=qk[:, j * 128:(j + 1) * 128], identity=idb[:, :]), [qk, idb], [ps_t])
                    if lat:
                        for h in range(4):
                            S.op("act", lambda h=h: act.copy(out=qTz[(h % 2) * 64:(h % 2) * 64 + 64, h, :], in_=ps_t[(h % 2) * 64:(h % 2) * 64 + 64, (h // 2) * 128:(h // 2 + 1) * 128]), [ps_t], [qTz])
                        S.op("act", lambda: act.copy(out=kTc[:, :, :], in_=ps_t[:, 256:512].rearrange("p (c t) -> p c t", c=2)), [ps_t], [kTc])
                        if STOPC == 72:
                            return
                    else:
                        S.op("act", lambda: act.copy(out=kTc[:, :, :], in_=ps_t[:, 256:512].rearrange("p (c t) -> p c t", c=2)), [ps_t], [kTc])
                    if STOPC <= 5:
                        return
                    if lat:
                        for h in range(4):
                            pr, P0 = h // 2, (h % 2) * 64
                            S.op("pe", lambda h=h, pr=pr, P0=P0: pe.matmul(ps_AT[:, h * 128:(h + 1) * 128], lhsT=kTc[:, pr, :], rhs=qTz[:, h, :], start=True, stop=True), [kTc, qTz], [ps_AT])
                        ATV = os.environ.get("ATV", "dve")
                        if ATV == "dve":
                            S.op("dve", lambda dr=dr: dve.tensor_tensor(out=ATs[:, :, :].rearrange("p h t -> p (h t)"), in0=ps_AT[:, :], in1=msk[dr][:, :], op=ALU.mult), [ps_AT, msk[dr]], [ATs])
                        elif ATV == "act":
                            S.op("act", lambda: act.copy(out=ATs[:, :, :].rearrange("p h t -> p (h t)"), in_=ps_AT[:, :]), [ps_AT], [ATs])
                        elif ATV == "f32":
                            S.op("dve", lambda dr=dr: dve.tensor_tensor(out=osb[:, :], in0=ps_AT[:, :], in1=msk[dr][:, :], op=ALU.mult), [ps_AT, msk[dr]], [osb])
                        if STOPC == 73:
                            return
                        for h in range(4):
                            pr, P0 = h // 2, (h % 2) * 64
                            S.op("pe", lambda h=h, v_=v_: pe.matmul(ps_o[:, h * 128:(h + 1) * 128], lhsT=ATs[:, h, :], rhs=v_[:, h * 128:(h + 1) * 128], start=True, stop=False), [ATs, v_], [ps_o])
                            S.op("pe", lambda h=h, pr=pr, P0=P0: pe.matmul(ps_o[:, h * 128:(h + 1) * 128], lhsT=qTz[:, h, :], rhs=Sbf[pr][:, :], start=False, stop=True), [qTz, Sbf[pr]], [ps_o])
                    for h in range(4):
                        pr = h // 2
                        S.op("pe", lambda h=h, pr=pr, v_=v_: pe.matmul(ps_U[:, h * 128:(h + 1) * 128], lhsT=qk[:, 256 + pr * 128:256 + (pr + 1) * 128], rhs=v_[:, h * 128:(h + 1) * 128], start=True, stop=True), [qk, v_], [ps_U])
                    for h in range(4):
                        pr, P0 = h // 2, (h % 2) * 64
                        S.op("dve", lambda h=h, pr=pr, P0=P0: dve.tensor_scalar(out=tU[P0:P0 + 64, :], in0=ps_U[P0:P0 + 64, h * 128:(h + 1) * 128], scalar1=at[P0:P0 + 64, pr:pr + 1], scalar2=None, op0=ALU.mult), [ps_U, at], [tU])
                        S.op("dve", lambda pr=pr, P0=P0: dve.scalar_tensor_tensor(out=Sst[pr][P0:P0 + 64, :], in0=Sst[pr][P0:P0 + 64, :], scalar=at[P0:P0 + 64, pr:pr + 1], in1=tU[P0:P0 + 64, :], op0=ALU.mult, op1=ALU.add), [Sst[pr], at, tU], [Sst[pr]])
                        S.op("dve", lambda pr=pr, P0=P0: dve.tensor_copy(out=Sbf[pr][P0:P0 + 64, :], in_=Sst[pr][P0:P0 + 64, :]), [Sst[pr]], [Sbf[pr]])
                    if STOPC <= 6:
                        return
                    if STOPC <= 7 and n >= 3:
                        return
                    if not lat:
                        continue
                    if dr == 0:
                        S.op("dve", lambda: dve.tensor_copy(out=osb[:, :], in_=ps_o[:, :]), [ps_o], [osb])
                        S.dma("sp", OF_d[li * 128:(li + 1) * 128, :], osb[:, :], reads=[osb], writes=[T_OF])
                    else:
                        S.op("dve", lambda b=b: dve.tensor_tensor(out=osb[:, :], in0=ps_o[:, :], in1=ofl[b][:, :], op=ALU.add), [ps_o, ofl[b]], [osb])
                        for h in range(4):
                            S.op("act", lambda h=h: act.activation(out=junk[:, :], in_=osb[:, h * 128:(h + 1) * 128], func=AF.Square, accum_out=ss4[:, h:h + 1]), [osb], [junk, ss4])
                        S.op("act", lambda: act.activation(out=rs4[:, :], in_=ss4[:, :], func=AF.Sqrt, scale=1.0 / 128.0, bias=epsb[:, :]), [ss4, epsb], [rs4])
                        S.op("dve", lambda: dve.reciprocal(out=rs4[:, :], in_=rs4[:, :]), [rs4], [rs4])
                        S.op("act", lambda b=b: act.activation(out=sg[:, :], in_=gg[b][:, :], func=AF.Silu), [gg[b]], [sg])
                        o3 = osb[:, :].rearrange("p (h e) -> p h e", h=4)
                        S.op("dve", lambda o3=o3: dve.tensor_tensor(out=o3, in0=o3, in1=rs4[:, :].unsqueeze(2).broadcast_to([128, 4, 128]), op=ALU.mult), [osb, rs4], [osb])
                        S.op("dve", lambda o3=o3: dve.tensor_tensor(out=o3, in0=o3, in1=gnb[:, :].unsqueeze(1).broadcast_to([128, 4, 128]), op=ALU.mult), [osb, gnb], [osb])
                        S.op("dve", lambda b=b: dve.tensor_tensor(out=ogl[b][:, :], in0=osb[:, :], in1=sg[:, :], op=ALU.mult), [osb, sg], [ogl[b]])
                        S.dma("sp", MIX_d[li * 128:(li + 1) * 128, 512:1024], ogl[b][:, :], reads=[ogl[b]], writes=[T_MIXgla])


    T_XN = T(None, "XN")
    T_HF = T(None, "HF")
    T_AFFT = T(None, "AFFT")
    AX = mybir.AxisListType.X

    def stageD():
        with contextlib.ExitStack() as es:
            wo = K.sb(es, [128, 8, D], BF16, "wo")
            wst = [K.sb(es, [128, 8, 512], F32, "wstd%d" % i) for i in range(2)]
            wov = w_out.rearrange("(c p) n -> p c n", p=128)
            for b in range(2):
                S.dma("sp", wst[b][:, :, :], wov[:, :, b * 512:(b + 1) * 512], writes=[wst[b]])
                S.op("act", lambda b=b: act.copy(out=wo[:, :, b * 512:(b + 1) * 512], in_=wst[b][:, :, :]), [wst[b]], [wo])
            rt = K.sb(es, [128, 8, NE], F32, "rt")
            S.dma("sp", rt[:, :, :], router.rearrange("(c p) e -> p c e", p=128), writes=[rt])
            md = K.sb(es, [128, 3, D], F32, "md")
            S.dma("sp", md[:, :, :], mods_d[:, 4:7, :], reads=[T_mods], writes=[md])
            affT = K.sb(es, [NE, SEQ], F32, "affT")
            mixb = [K.sb(es, [128, D], BF16, "mixb%d" % i) for i in range(2)]
            xt = [K.sb(es, [128, D], F32, "xtd%d" % i) for i in range(2)]
            mixT = K.sb(es, [128, 8, 128], BF16, "mixT")
            mixs = K.sb(es, [128, D], F32, "mixs")
            junk = K.sb(es, [128, D], BF16, "junkd")
            ss = K.sb(es, [128, 1], F32, "ssd")
            rstd = K.sb(es, [128, 1], F32, "rstdd")
            t1 = K.sb(es, [128, D], F32, "t1d")
            xn = [K.sb(es, [128, D], F32, "xn%d" % i) for i in range(2)]
            hf32 = K.sb(es, [128, D], F32, "hf32")
            hfb = [K.sb(es, [128, D], BF16, "hfb%d" % i) for i in range(2)]
            hfT = K.sb(es, [128, 8, 128], F32, "hfT")
            lg = K.sb(es, [128, NE], F32, "lg")
            mx = K.sb(es, [128, 1], F32, "mx")
            ex = K.sb(es, [128, NE], F32, "ex")
            sm = K.sb(es, [128, 1], F32, "sm")
            af = K.sb(es, [128, NE], F32, "af")
            ps_t = K.ps(es, [128, 1024], BF16, "psd_t")
            ps_m = [K.ps(es, [128, 512], F32, "psd_m%d" % i) for i in range(2)]
            ps_r = [K.ps(es, [128, 512], F32, "psd_r%d" % i) for i in range(2)]
            ps_l = K.ps(es, [128, 512], F32, "psd_l")
            ps_a = K.ps(es, [128, 512], F32, "psd_a")
            for T_ in range(32):
                b = T_ % 2
                mb, x_ = mixb[b], xt[b]
                S.dma("sp", mb[:, :], MIX_d[T_ * 128:(T_ + 1) * 128, :], reads=[T_MIXna, T_MIXgla], writes=[mb])
                S.dma("sp", x_[:, :], xc[CTX + T_ * 128:CTX + (T_ + 1) * 128, :], writes=[x_])
                for c in range(8):
                    S.op("pe", lambda c=c, mb=mb: pe.transpose(out=ps_t[:, c * 128:(c + 1) * 128], in_=mb[:, c * 128:(c + 1) * 128], identity=idb[:, :]), [mb, idb], [ps_t])
                S.op("act", lambda: act.copy(out=mixT[:, :, :], in_=ps_t[:, :].rearrange("p (c t) -> p c t", c=8)), [ps_t], [mixT])
                for hlf in range(2):
                    for c in range(8):
                        S.op("pe", lambda c=c, hlf=hlf: pe.matmul(ps_m[hlf][:, :], lhsT=mixT[:, c, :], rhs=wo[:, c, hlf * 512:(hlf + 1) * 512], start=(c == 0), stop=(c == 7)), [mixT, wo], [ps_m[hlf]])
                    S.op("act", lambda hlf=hlf: act.copy(out=mixs[:, hlf * 512:(hlf + 1) * 512], in_=ps_m[hlf][:, :]), [ps_m[hlf]], [mixs])
                rms_rstd(None, mixs[:, :], mixs, ss, rstd, junk)
                S.op("dve", lambda: dve.scalar_tensor_tensor(out=t1[:, :], in0=mixs[:, :], scalar=rstd[:, 0:1], in1=md[:, 0, :], op0=ALU.mult, op1=ALU.mult), [mixs, rstd, md], [t1])
                xn_ = xn[b]
                S.op("pool", lambda x_=x_, xn_=xn_: pool.tensor_tensor(out=xn_[:, :], in0=t1[:, :], in1=x_[:, :], op=ALU.add), [t1, x_], [xn_])
                S.dma("sp", XN_d[T_ * 128:(T_ + 1) * 128, :], xn_[:, :], reads=[xn_], writes=[T_XN])
                rms_rstd(None, xn_[:, :], xn_, ss, rstd, junk)
                S.op("dve", lambda xn_=xn_: dve.scalar_tensor_tensor(out=t1[:, :], in0=xn_[:, :], scalar=rstd[:, 0:1], in1=md[:, 1, :], op0=ALU.mult, op1=ALU.mult), [xn_, rstd, md], [t1])
                S.op("pool", lambda: pool.tensor_tensor(out=hf32[:, :], in0=t1[:, :], in1=md[:, 2, :], op=ALU.add), [t1, md], [hf32])
                hb_ = hfb[b]
                S.op("act", lambda hb_=hb_: act.copy(out=hb_[:, :], in_=hf32[:, :]), [hf32], [hb_])
                S.dma("sp", HF_d[T_ * 128:(T_ + 1) * 128, :], hb_[:, :], reads=[hb_], writes=[T_HF])
                for c in range(8):
                    S.op("pe", lambda c=c: pe.transpose(out=ps_r[c // 4][:, (c % 4) * 128:(c % 4 + 1) * 128], in_=hf32[:, c * 128:(c + 1) * 128], identity=idf[:, :]), [hf32, idf], [ps_r[c // 4]])
                for i in range(2):
                    S.op("act", lambda i=i: act.copy(out=hfT[:, i * 4:(i + 1) * 4, :], in_=ps_r[i][:, :].rearrange("p (c t) -> p c t", c=4)), [ps_r[i]], [hfT])
                for c in range(8):
                    S.op("pe", lambda c=c: pe.matmul(ps_l[:, 0:NE], lhsT=hfT[:, c, :], rhs=rt[:, c, :], start=(c == 0), stop=(c == 7)), [hfT, rt], [ps_l])
                S.op("dve", lambda: dve.tensor_copy(out=lg[:, :], in_=ps_l[:, 0:NE]), [ps_l], [lg])
                S.op("dve", lambda: dve.reduce_max(out=mx[:, :], in_=lg[:, :], axis=AX), [lg], [mx])
                S.op("dve", lambda: dve.tensor_scalar(out=mx[:, :], in0=mx[:, :], scalar1=-1.0, scalar2=None, op0=ALU.mult), [mx], [mx])
                S.op("act", lambda: act.activation(out=ex[:, :], in_=lg[:, :], func=AF.Exp, bias=mx[:, :], accum_out=sm[:, :]), [lg, mx], [ex, sm])
                S.op("dve", lambda: dve.reciprocal(out=sm[:, :], in_=sm[:, :]), [sm], [sm])
                S.op("dve", lambda: dve.tensor_scalar(out=af[:, :], in0=ex[:, :], scalar1=sm[:, 0:1], scalar2=None, op0=ALU.mult), [ex, sm], [af])
                S.op("pe", lambda: pe.transpose(out=ps_a[0:NE, 0:128], in_=af[:, :], identity=idf[:, :]), [af, idf], [ps_a])
                S.op("act", lambda T_=T_: act.copy(out=affT[:, T_ * 128:(T_ + 1) * 128], in_=ps_a[0:NE, 0:128]), [ps_a], [affT])
            S.dma("sp", AFFT_d, affT[:, :], reads=[affT], writes=[T_AFFT])


    posT_d = dscr("posT_d", [128, 4, 64])
    gateT_d = dscr("gateT_d", [128, 4, 64])
    HFO_d = dscr("HFO_d", [SEQ // 2, D], BF16)
    T_HFO = T(None, "HFO")

    def stageEFG():
        import os
        with contextlib.ExitStack() as es:
            posT = K.sb(es, [128, 4, 64], F32, "posT")
            gateT = K.sb(es, [128, 4, 64], F32, "gateT")
            iot = K.sb(es, [128, 512], F32, "iot")
            S.dma("sp", iot[:, :], iota512, writes=[iot])
            PB = [K.ps(es, [128, 512], F32, "PB%d" % i) for i in range(8)]
            with contextlib.ExitStack() as e2:
                A = K.sb(e2, [128, 512], F32, "A")
                S.dma("sp", A[:, :], AFFT_d.rearrange("e (s t) -> (e s) t", s=8), reads=[T_AFFT], writes=[A])
                b1 = K.sb(e2, [128, 128], F32, "b1")
                blt = K.sb(e2, [128, 128], F32, "blt")
                ownf = K.sb(e2, [128, 1], F32, "ownf")
                selm = K.sb(e2, [128, 64], F32, "selm")
                for t_, src in ((b1, blk_ones), (blt, blk_lt), (ownf, own_flag), (selm, sel)):
                    S.dma("sp", t_[:, :], src, writes=[t_])
                lo = K.sb(e2, [128, 1], F32, "lo")
                hi = K.sb(e2, [128, 1], F32, "hi")
                mid = K.sb(e2, [128, 1], F32, "mid")
                cnt = K.sb(e2, [128, 1], F32, "cnt")
                cond = K.sb(e2, [128, 1], F32, "cond")
                d1 = K.sb(e2, [128, 1], F32, "d1")
                jk = K.sb(e2, [128, 512], F32, "jk")
                onesT = K.sb(e2, [128, 512], F32, "onesT")
                M_ = K.sb(e2, [128, 512], F32, "M_")
                posi = K.sb(e2, [128, 512], F32, "posi")
                pm = K.sb(e2, [128, 512], F32, "pm")
                offc = K.sb(e2, [128, 1], F32, "offc")
                S.op("pool", lambda: pool.memset(lo[:, :], 0.0), [], [lo])
                S.op("pool", lambda: pool.memset(hi[:, :], 1.0), [], [hi])
                S.op("pool", lambda: pool.memset(onesT[:, :], 1.0), [], [onesT])
                pc = PB[0]
                for it in range(30):
                    S.op("dve", lambda: dve.tensor_tensor(out=mid[:, :], in0=lo[:, :], in1=hi[:, :], op=ALU.add), [lo, hi], [mid])
                    S.op("dve", lambda: dve.tensor_scalar(out=mid[:, :], in0=mid[:, :], scalar1=0.5, scalar2=None, op0=ALU.mult), [mid], [mid])
                    S.op("dve", lambda: dve.tensor_scalar(out=jk[:, :], in0=A[:, :], scalar1=mid[:, 0:1], scalar2=0.0, op0=ALU.is_gt, op1=ALU.add, accum_out=cnt[:, 0:1]), [A, mid], [jk, cnt])
                    S.op("pe", lambda: pe.matmul(pc[:, 0:1], lhsT=b1[:, :], rhs=cnt[:, 0:1], start=True, stop=True), [b1, cnt], [pc])
                    S.op("dve", lambda: dve.tensor_scalar(out=cond[:, :], in0=pc[:, 0:1], scalar1=float(CAP) - 0.5, scalar2=None, op0=ALU.is_gt), [pc], [cond])
                    S.op("dve", lambda: dve.tensor_tensor(out=d1[:, :], in0=mid[:, :], in1=lo[:, :], op=ALU.subtract), [mid, lo], [d1])
                    S.op("dve", lambda: dve.scalar_tensor_tensor(out=lo[:, :], in0=d1[:, :], scalar=cond[:, 0:1], in1=lo[:, :], op0=ALU.mult, op1=ALU.add), [d1, cond, lo], [lo])
                    S.op("dve", lambda: dve.tensor_tensor(out=d1[:, :], in0=hi[:, :], in1=mid[:, :], op=ALU.subtract), [hi, mid], [d1])
                    S.op("dve", lambda: dve.scalar_tensor_tensor(out=hi[:, :], in0=d1[:, :], scalar=cond[:, 0:1], in1=mid[:, :], op0=ALU.mult, op1=ALU.add), [d1, cond, mid], [hi])
                S.op("dve", lambda: dve.tensor_scalar(out=M_[:, :], in0=A[:, :], scalar1=lo[:, 0:1], scalar2=ownf[:, 0:1], op0=ALU.is_gt, op1=ALU.mult), [A, lo, ownf], [M_])
                S.op("dve", lambda: dve.tensor_tensor_scan(out=posi[:, :], data0=onesT[:, :], data1=M_[:, :], initial=0.0, op0=ALU.mult, op1=ALU.add), [onesT, M_], [posi])
                S.op("pe", lambda: pe.matmul(pc[:, 0:1], lhsT=blt[:, :], rhs=posi[:, 511:512], start=True, stop=True), [blt, posi], [pc])
                S.op("dve", lambda: dve.tensor_scalar(out=offc[:, :], in0=pc[:, 0:1], scalar1=-10000.0, scalar2=None, op0=ALU.add), [pc], [offc])
                S.op("dve", lambda: dve.tensor_scalar(out=pm[:, :], in0=posi[:, :], scalar1=offc[:, 0:1], scalar2=None, op0=ALU.add), [posi, offc], [pm])
                S.op("dve", lambda: dve.tensor_tensor(out=pm[:, :], in0=pm[:, :], in1=M_[:, :], op=ALU.mult), [pm, M_], [pm])
                S.op("dve", lambda: dve.tensor_scalar(out=pm[:, :], in0=pm[:, :], scalar1=9999.0, scalar2=None, op0=ALU.add), [pm], [pm])
                for src, dst, pb in ((pm, posT, PB[1]), (A, gateT, PB[2])):
                    for blk in range(4):
                        S.op("pe", lambda src=src, pb=pb, blk=blk: pe.matmul(pb[:, blk * 64:(blk + 1) * 64], lhsT=src[:, blk * 128:(blk + 1) * 128], rhs=selm[:, :], start=True, stop=True), [src, selm], [pb])
                    S.op("act", lambda dst=dst, pb=pb: act.copy(out=dst[:, :, :], in_=pb[:, 0:256].rearrange("p (b c) -> p b c", b=4)), [pb], [dst])
                if debug:
                    S.dma("sp", posT_d, posT[:, :, :], reads=[posT])
                    S.dma("sp", gateT_d, gateT[:, :, :], reads=[gateT])
                S.barrier()
            S.barrier()
            if upto == "E":
                return
            oi = K.sb(es, [128, 16], I32, "oi")
            S.dma("sp", oi[:, :], own_idx, writes=[oi])
            hfj = [K.sb(es, [128, D], BF16, "hfj%d" % i) for i in range(3)]
            for j in range(16):
                hj = hfj[j % 3]
                S.dma("pool", None, None, reads=[T_HF, oi], writes=[hj], fn=lambda hj=hj, j=j: pool.indirect_dma_start(
                    out=hj[:, :], out_offset=None, in_=HF_d, in_offset=bass.IndirectOffsetOnAxis(ap=oi[:, j:j + 1], axis=0)))
                S.dma("sp", HFO_d[j * 128:(j + 1) * 128, :], hj[:, :], reads=[hj], writes=[T_HFO])
            yacc = K.sb(es, [128, 16, D], F32, "yacc")
            S.op("pool", lambda: pool.memset(yacc[:, :, :], 0.0), [], [yacc])
            e3 = contextlib.ExitStack()
            xsT = K.sb(e3, [128, 8, 512], BF16, "xsT")
            hidT = K.sb(e3, [128, NFC, 512], BF16, "hidT")
            ysb = K.sb(e3, [128, 4, D], BF16, "ysb")
            Ej = [K.sb(e3, [128, 512], BF16, "Ej%d" % i) for i in range(2)]
            Gj = [K.sb(e3, [128, 512], BF16, "Gj%d" % i) for i in range(2)]
            GTa = K.sb(e3, [128, 16, 4, 128], BF16, "GTa")
            GTt = [T(None, "GTt%d" % i) for i in range(16)]
            sgt = [K.sb(e3, [128, 512], BF16, "sgt%d" % i) for i in range(2)]
            stg = [K.sb(e3, [128, 2048], F32, "stg%d" % i) for i in range(4)]
            nst = [0]

            def stage_in(src_ap, ncols):
                st = stg[nst[0] % 4]
                nst[0] += 1
                S.dma("sp", st[:, 0:ncols], src_ap, writes=[st])
                return st
            wgb = [K.sb(e3, [128, 8, 256], BF16, "wgb%d" % i) for i in range(2)]
            wub = [K.sb(e3, [128, 8, 256], BF16, "wub%d" % i) for i in range(2)]
            wdb = K.sb(e3, [128, NFC, 256], BF16, "wdb")
            wdbT = [T(None, "wdbT%d" % i) for i in range(3)]
            NEX = int(os.environ.get("NEX", str(NE)))
            ng = 0
            PH = int(os.environ.get("PH", "15"))
            for e in range(NEX):
                for j in (range(16) if PH & 1 else []):
                    osg, blk = j // 4, j % 4
                    hj = hfj[j % 3]
                    S.dma("sp", hj[:, :], HFO_d[j * 128:(j + 1) * 128, :], reads=[T_HFO], writes=[hj])
                    E_ = Ej[j % 2]
                    S.op("dve", lambda E_=E_, blk=blk, col=e * 4 + osg: dve.tensor_scalar(out=E_[:, :], in0=iot[:, :], scalar1=posT[:, blk, col:col + 1], scalar2=None, op0=ALU.is_equal), [iot, posT], [E_])
                    for c in range(8):
                        S.op("pe", lambda c=c, hj=hj, E_=E_, j=j: pe.matmul(PB[c][:, :], lhsT=hj[:, c * 128:(c + 1) * 128], rhs=E_[:, :], start=(j == 0), stop=(j == 15)), [hj, E_], [PB[c]])
                for c in (range(8) if PH & 1 else []):
                    if c % 2 == 0:
                        S.op("act", lambda c=c: act.copy(out=xsT[:, c, :], in_=PB[c][:, :]), [PB[c]], [xsT])
                    else:
                        S.op("dve", lambda c=c: dve.tensor_copy(out=xsT[:, c, :], in_=PB[c][:, :]), [PB[c]], [xsT])

                def prep(j):
                    osg, blk = j // 4, j % 4
                    col = e * 4 + osg
                    G_ = Gj[j % 2]
                    S.op("dve", lambda: dve.tensor_scalar(out=G_[:, :], in0=iot[:, :], scalar1=posT[:, blk, col:col + 1], scalar2=gateT[:, blk, col:col + 1], op0=ALU.is_equal, op1=ALU.mult), [iot, posT, gateT], [G_])
                    pt = PB[6 + j % 2]
                    ptb = pt[:, :].bitcast(BF16)
                    for sc in range(4):
                        S.op("pe", lambda sc=sc: pe.transpose(out=ptb[:, sc * 128:(sc + 1) * 128], in_=G_[:, sc * 128:(sc + 1) * 128], identity=idb[:, :]), [G_, idb], [pt])
                    S.op("act", lambda: act.copy(out=GTa[:, j, :, :], in_=ptb[:, 0:512].rearrange("p (s t) -> p s t", s=4)), [pt], [GTt[j]])

                jn = 0
                for g in (range(11) if PH & 2 else []):
                    gb = ng % 2
                    ng += 1
                    sg_st = stage_in(w_gate[e, g], 2048)
                    su_st = stage_in(w_up[e, g], 2048)
                    S.op("act", lambda gb=gb, sg_st=sg_st: act.copy(out=wgb[gb][:, :, :].rearrange("p c f -> p (c f)"), in_=sg_st[:, :]), [sg_st], [wgb[gb]])
                    S.op("dve", lambda gb=gb, su_st=su_st: dve.tensor_copy(out=wub[gb][:, :, :].rearrange("p c f -> p (c f)"), in_=su_st[:, :]), [su_st], [wub[gb]])
                    for fl in range(2):
                        fc = g * 2 + fl
                        pg, pu = PB[(fc % 2) * 2], PB[(fc % 2) * 2 + 1]
                        for c in range(8):
                            S.op("pe", lambda c=c, pg=pg, gb=gb, fl=fl: pe.matmul(pg[:, :], lhsT=wgb[gb][:, c, fl * 128:(fl + 1) * 128], rhs=xsT[:, c, :], start=(c == 0), stop=(c == 7)), [wgb[gb], xsT], [pg])
                        for c in range(8):
                            S.op("pe", lambda c=c, pu=pu, gb=gb, fl=fl: pe.matmul(pu[:, :], lhsT=wub[gb][:, c, fl * 128:(fl + 1) * 128], rhs=xsT[:, c, :], start=(c == 0), stop=(c == 7)), [wub[gb], xsT], [pu])
                        sg_ = sgt[fc % 2]
                        S.op("act", lambda pg=pg, sg_=sg_: act.activation(out=sg_[:, :], in_=pg[:, :], func=AF.Silu), [pg], [sg_])
                        S.op("dve", lambda pu=pu, sg_=sg_, fc=fc: dve.tensor_tensor(out=hidT[:, fc, :], in0=sg_[:, :], in1=pu[:, :], op=ALU.mult), [sg_, pu], [hidT])
                    for _ in range(2 if g < 5 else 1):
                        if jn < 16:
                            prep(jn)
                            jn += 1
                while jn < 16 and (PH & 8):
                    prep(jn)
                    jn += 1
                pieces = ((0, 8), (8, 16), (16, NFC))
                for cb in (range(4) if PH & 4 else []):
                    for k, (f0, f1) in enumerate(pieces):
                        st = stage_in(w_down[e, cb, :, f0 * 256:f1 * 256], (f1 - f0) * 256)
                        S.op("act", lambda st=st, f0=f0, f1=f1: act.copy(out=wdb[:, f0:f1, :].rearrange("p c n -> p (c n)"), in_=st[:, 0:(f1 - f0) * 256]), [st], [wdbT[k]])
                    for k, (f0, f1) in enumerate(pieces):
                        for fc in range(f0, f1):
                            for sc in range(4):
                                S.op("pe", lambda fc=fc, sc=sc: pe.matmul(PB[4 + sc][:, 0:256], lhsT=hidT[:, fc, sc * 128:(sc + 1) * 128], rhs=wdb[:, fc, :], start=(fc == 0), stop=(fc == NFC - 1)), [hidT, wdbT[k]], [PB[4 + sc]])
                    for sc in range(4):
                        if sc % 2 == 0:
                            S.op("act", lambda sc=sc, cb=cb: act.copy(out=ysb[:, sc, cb * 256:(cb + 1) * 256], in_=PB[4 + sc][:, 0:256]), [PB[4 + sc]], [ysb])
                        else:
                            S.op("dve", lambda sc=sc, cb=cb: dve.tensor_copy(out=ysb[:, sc, cb * 256:(cb + 1) * 256], in_=PB[4 + sc][:, 0:256]), [PB[4 + sc]], [ysb])
                for j in (range(16) if PH & 8 else []):
                    for hlf in range(2):
                        pyc = PB[(j % 2) * 2 + hlf]
                        for sc in range(4):
                            S.op("pe", lambda sc=sc, pyc=pyc, hlf=hlf, j=j: pe.matmul(pyc[:, :], lhsT=GTa[:, j, sc, :], rhs=ysb[:, sc, hlf * 512:(hlf + 1) * 512], start=(sc == 0), stop=(sc == 3)), [GTt[j], ysb], [pyc])
                        S.op("dve", lambda pyc=pyc, j=j, hlf=hlf: dve.tensor_tensor(out=yacc[:, j, hlf * 512:(hlf + 1) * 512], in0=yacc[:, j, hlf * 512:(hlf + 1) * 512], in1=pyc[:, :], op=ALU.add), [yacc, pyc], [yacc])
            S.barrier()
            e3.close()
            gF = K.sb(es, [128, D], F32, "gF")
            S.dma("sp", gF[:, :], mods_d[:, 7, :], reads=[T_mods], writes=[gF])
            xno = [K.sb(es, [128, D], F32, "xno%d" % i) for i in range(2)]
            ob = [K.sb(es, [128, D], F32, "ob%d" % i) for i in range(2)]
            junk = K.sb(es, [128, D], BF16, "junkg")
            ss = K.sb(es, [128, 1], F32, "ssg")
            rstd = K.sb(es, [128, 1], F32, "rstdg")
            for j in range(16):
                b = j % 2
                S.dma("pool", None, None, reads=[T_XN, oi], writes=[xno[b]], fn=lambda b=b, j=j: pool.indirect_dma_start(
                    out=xno[b][:, :], out_offset=None, in_=XN_d, in_offset=bass.IndirectOffsetOnAxis(ap=oi[:, j:j + 1], axis=0)))
                S.op("act", lambda j=j: act.activation(out=junk[:, :], in_=yacc[:, j, :], func=AF.Square, accum_out=ss[:, :]), [yacc], [junk, ss])
                S.op("act", lambda: act.activation(out=rstd[:, :], in_=ss[:, :], func=AF.Sqrt, scale=1.0 / D, bias=epsb[:, :]), [ss, epsb], [rstd])
                S.op("dve", lambda: dve.reciprocal(out=rstd[:, :], in_=rstd[:, :]), [rstd], [rstd])
                S.op("dve", lambda j=j, b=b: dve.scalar_tensor_tensor(out=ob[b][:, :], in0=yacc[:, j, :], scalar=rstd[:, 0:1], in1=gF[:, :], op0=ALU.mult, op1=ALU.mult), [yacc, rstd, gF], [ob[b]])
                S.op("pool", lambda b=b: pool.tensor_tensor(out=ob[b][:, :], in0=ob[b][:, :], in1=xno[b][:, :], op=ALU.add), [ob[b], xno[b]], [ob[b]])
                S.dma("sp", out[j * 128:(j + 1) * 128, :], ob[b][:, :], reads=[ob[b]])

    stages = [("0", stage0), ("A", stageA), ("B", stageB), ("C", stageC), ("D", stageD), ("E", stageEFG)]
    if upto in ("F", "G", "all"):
        stages[-1] = (upto, stageEFG)
    for name, fn in stages:
        fn()
        S.barrier()
        if upto == name:
            break
    S.finish()
    glob.close()
    return nc


def prep_inputs(inputs, cores=range(8), with_experts=True):
    f = lambda k: np.ascontiguousarray(np.asarray(inputs[k], dtype=np.float32))
    x, c, ctx, c_ctx = f("x"), f("c"), f("ctx"), f("c_ctx")
    consts = _consts()
    cos, sin = _rope_tables()
    rpb = f("na_rpb")[0]
    nab = np.ascontiguousarray(_na_bias_tables(rpb).reshape(5, 8, 128, 640))
    a_up = f("gla_a_up")[0]
    a_bias = f("gla_a_bias")[0]
    aup_bd = np.zeros((32, 512), np.float32)
    aup_bd[0:16, 0:256] = a_up[0]
    aup_bd[16:32, 256:512] = a_up[1]
    abias = np.ascontiguousarray(a_bias.reshape(1, 512))
    norms = np.ascontiguousarray(np.stack([f("norm_mix_pre")[0], f("norm_mix_post")[0], f("norm_ffn_pre")[0], f("norm_ffn_post")[0]]))
    shared = dict(
        w_mod=f("w_mod")[0], b_mod=f("b_mod"), norms=norms, gla_norm=f("gla_norm"), w_in=f("w_in")[0],
        aup_bd=aup_bd, abias=abias, rope_cos=cos, rope_sin=sin, na_bias=nab, w_out=f("w_out")[0],
        router=f("router")[0], **consts)
    if with_experts:
        tile_gu = lambda w: np.ascontiguousarray(w.reshape(NE, 8, 128, 11, 256).transpose(0, 3, 2, 1, 4)).reshape(NE, 11, 128, 8 * 256)
        shared.update(w_gate=tile_gu(f("w_gate")[0]), w_up=tile_gu(f("w_up")[0]),
                      w_down=np.ascontiguousarray(f("w_down")[0].reshape(NE, NFC, 128, 4, 256).transpose(0, 3, 2, 1, 4)).reshape(NE, 4, 128, NFC * 256))
    maps = []
    pp = np.arange(128)
    for core in cores:
        s, p = core // 2, core % 2
        m = dict(shared)
        m["xc"] = np.ascontiguousarray(np.concatenate([ctx[s], x[s]], axis=0))
        cv = np.zeros((128, 16), np.float32)
        cv[:, 0:8] = c[s].reshape(8, 128).T
        cv[:, 8:16] = c_ctx.reshape(8, 128).T
        m["cvec"] = cv
        m["own_idx"] = np.ascontiguousarray((p * 2048 + np.arange(16)[None, :] * 128 + pp[:, None]).astype(np.int32))
        seg = pp % 8
        m["own_flag"] = ((seg // 4) == p).astype(np.float32).reshape(128, 1)
        selm = np.zeros((128, 64), np.float32)
        for e in range(16):
            for os_ in range(4):
                selm[e * 8 + p * 4 + os_, e * 4 + os_] = 1.0
        m["sel"] = selm
        maps.append(m)
    return maps


_PROGRAM = None


def kernel(**inputs):
    global _PROGRAM
    if _PROGRAM is None:
        _PROGRAM = build_program()
    maps = prep_inputs(inputs)
    res = run_bass_kernel_spmd(_PROGRAM, maps, core_ids=list(range(8)))
    out = np.zeros((4, SEQ, D), np.float32)
    for core in range(8):
        s, p = core // 2, core % 2
        out[s, p * 2048:(p + 1) * 2048] = res.results[core]["out"]
    return out
```

```python
import contextlib
import numpy as np
import ml_dtypes
import concourse.bass as bass
import concourse.mybir as mybir
from concourse.bass_utils import run_bass_kernel_spmd

F32 = mybir.dt.float32
BF16 = mybir.dt.bfloat16
I32 = mybir.dt.int32
AF = mybir.ActivationFunctionType
ALU = mybir.AluOpType

D = 1024
SEQ = 4096
CTX = 256
NTOK = SEQ + CTX
NT = NTOK // 128
INC = 3104
NE = 16
DE = 2816
NFC = DE // 128
CAP = 512
EPS = 1e-6
NEG = -30000.0


class T:
    __slots__ = ("ap", "w", "r", "name")

    def __init__(self, ap, name=""):
        self.ap = ap
        self.w = None
        self.r = {}
        self.name = name

    def __getitem__(self, idx):
        return self.ap[idx]


class Sync:
    def __init__(self, nc, n_sp=20, n_pool=8, n_act=6):
        self.nc = nc
        self.E = {"pe": nc.tensor, "act": nc.scalar, "dve": nc.vector, "pool": nc.gpsimd, "sp": nc.sync}
        self.sem = {}
        self.cnt = {}
        for k in ("pe", "act", "dve", "pool"):
            self.sem[k] = nc.alloc_semaphore("s_" + k)
            self.cnt[k] = 0
        self.seen = {k: {} for k in self.E}
        self.dq = {}
        for q, n in (("sp", n_sp), ("pool", n_pool), ("act", n_act)):
            lst = []
            for i in range(n):
                key = "d_%s%d" % (q, i)
                self.sem[key] = nc.alloc_semaphore(key)
                self.cnt[key] = 0
                lst.append(key)
            self.dq[q] = [lst, 0]

    def _wait(self, ek, key, val):
        if val <= 0:
            return
        if self.seen[ek].get(key, 0) >= val:
            return
        self.E[ek].wait_ge(self.sem[key], val)
        self.seen[ek][key] = val

    def _deps(self, ek, reads, writes):
        need = {}
        for t in reads:
            if t.w is not None:
                k, v = t.w
                need[k] = max(need.get(k, 0), v)
        for t in writes:
            if t.w is not None:
                k, v = t.w
                if not (k == "pe" and ek == "pe"):
                    need[k] = max(need.get(k, 0), v)
            for k, v in t.r.items():
                if not (k == "pe" and ek == "pe"):
                    need[k] = max(need.get(k, 0), v)
        for k, v in need.items():
            self._wait(ek, k, v)

    def op(self, ek, fn, reads=(), writes=()):
        self._deps(ek, reads, writes)
        ins = fn()
        self.cnt[ek] += 1
        ins.then_inc(self.sem[ek], 1)
        me = (ek, self.cnt[ek])
        for t in reads:
            t.r[ek] = self.cnt[ek]
        for t in writes:
            t.w = me
            t.r = {}
        return ins

    def dma(self, q, out, in_, reads=(), writes=(), fn=None, **kw):
        lst, i = self.dq[q]
        key = lst[i % len(lst)]
        self.dq[q][1] = i + 1
        self._wait(q, key, self.cnt[key])
        self._deps(q, reads, writes)
        if fn is None:
            ins = self.E[q].dma_start(out=out, in_=in_, **kw)
        else:
            ins = fn()
        self.cnt[key] += 16
        ins.then_inc(self.sem[key], 16)
        for t in reads:
            t.r[key] = self.cnt[key]
        for t in writes:
            t.w = (key, self.cnt[key])
            t.r = {}
        return ins

    def barrier(self):
        for ek in self.E:
            for k, v in self.cnt.items():
                if not (k == ek == "pe"):
                    self._wait(ek, k, v)

    def finish(self):
        for k, v in self.cnt.items():
            self._wait("sp", k, v)


class Ctx:
    def __init__(self, nc):
        self.nc = nc
        self.S = Sync(nc)
        self.uid = 0

    def sb(self, es, shape, dt, name):
        self.uid += 1
        return T(es.enter_context(self.nc.sbuf_tensor("%s_%d" % (name, self.uid), list(shape), dt)), name)

    def ps(self, es, shape, dt, name):
        self.uid += 1
        return T(es.enter_context(self.nc.psum_tensor("%s_%d" % (name, self.uid), list(shape), dt)), name)


def _rope_tables():
    half = 16
    freqs = (np.float32(10000.0) ** (-np.arange(half, dtype=np.float32) / np.float32(half))).astype(np.float32)
    t = np.arange(SEQ)
    pr = (t // 64).astype(np.float32)[:, None] * freqs
    pc = (t % 64).astype(np.float32)[:, None] * freqs
    ang = np.concatenate([pr, pc], axis=1).astype(np.float32)
    return np.cos(ang).astype(np.float32), np.sin(ang).astype(np.float32)


def _na_key_tile0(T_):
    return min(max(T_ - 2, 0), 27)


def _na_bias_tables(rpb):
    out = np.full((5, 8, 128, 5, 128), NEG, np.float32)
    for si, T_ in enumerate((0, 1, 2, 30, 31)):
        kt0 = _na_key_tile0(T_)
        for qi in range(128):
            r = 2 * T_ + qi // 64
            qc = qi % 64
            rs = min(max(r - 4, 0), 56)
            cs = min(max(qc - 8, 0), 48)
            for kr in range(rs, rs + 8):
                kt = kr // 2 - kt0
                assert 0 <= kt < 5
                dr = kr - r + 7
                kc = np.arange(cs, cs + 16)
                dc = np.clip(kc - qc + 15, 0, 30)
                j = (kr % 2) * 64 + kc
                out[si, :, j, kt, qi] = rpb[:, dr, dc].T
    return out


def _consts():
    c = {}
    c["ident_f"] = np.eye(128, dtype=np.float32)
    c["ident_b"] = np.eye(128, dtype=np.float32).astype(ml_dtypes.bfloat16)
    tt = np.arange(128)
    c["tri_f"] = np.where(tt[:, None] <= tt[None, :], -1.0 / 16.0, 0.0).astype(np.float32)
    c["tri_b"] = np.where(tt[:, None] >= tt[None, :], -1.0 / 16.0, 0.0).astype(np.float32)
    c["mask_f"] = (tt[:, None] <= tt[None, :]).astype(np.float32)
    c["mask_b"] = (tt[:, None] >= tt[None, :]).astype(np.float32)
    c["ones_col"] = np.full((128, 1), -1.0 / 16.0, np.float32)
    c["ones_row"] = np.ones((1, 128), np.float32)
    g = tt // 8
    c["blk_ones"] = (g[:, None] == g[None, :]).astype(np.float32)
    c["blk_lt"] = ((g[:, None] == g[None, :]) & (tt[:, None] < tt[None, :])).astype(np.float32)
    c["iota512"] = np.broadcast_to(np.arange(512, dtype=np.float32)[None, :], (128, 512)).copy()
    return c


def build_program(upto="all", debug=False):
    nc = bass.Bass("TRN2", target_bir_lowering=False)
    K = Ctx(nc)
    S = K.S
    pe, act, dve, pool = nc.tensor, nc.scalar, nc.vector, nc.gpsimd

    def din(name, shape, dt=F32):
        return nc.dram_tensor(name, list(shape), dt, kind="ExternalInput").ap()

    import os as _os
    _ext = set(_os.environ.get("EXTSET", "").split(","))

    def dscr(name, shape, dt=F32):
        return nc.dram_tensor(name, list(shape), dt, kind=("ExternalOutput" if (debug or name in _ext) else "Internal")).ap()

    xc = din("xc", [NTOK, D])
    cvec = din("cvec", [128, 16])
    w_mod = din("w_mod", [D, 6 * D])
    b_mod = din("b_mod", [1, 6 * D])
    norms = din("norms", [4, D])
    gla_norm = din("gla_norm", [1, 128])
    w_in = din("w_in", [D, INC])
    aup_bd = din("aup_bd", [32, 512])
    abias = din("abias", [1, 512])
    rope_cos = din("rope_cos", [SEQ, 32])
    rope_sin = din("rope_sin", [SEQ, 32])
    na_bias = din("na_bias", [5, 8, 128, 640])
    w_out = din("w_out", [D, D])
    router = din("router", [D, NE])
    if upto in ("F", "G", "all"):
        w_gate = din("w_gate", [NE, 11, 128, 8 * 256])
        w_up = din("w_up", [NE, 11, 128, 8 * 256])
        w_down = din("w_down", [NE, 4, 128, NFC * 256])
    ident_f = din("ident_f", [128, 128])
    ident_b = din("ident_b", [128, 128], BF16)
    tri_f = din("tri_f", [128, 128])
    tri_b = din("tri_b", [128, 128])
    mask_f = din("mask_f", [128, 128])
    mask_b = din("mask_b", [128, 128])
    ones_col = din("ones_col", [128, 1])
    ones_row = din("ones_row", [1, 128])
    blk_ones = din("blk_ones", [128, 128])
    blk_lt = din("blk_lt", [128, 128])
    iota512 = din("iota512", [128, 512])
    own_idx = din("own_idx", [128, 16], I32)
    own_flag = din("own_flag", [128, 1])
    sel = din("sel", [128, 64])
    out = nc.dram_tensor("out", [SEQ // 2, D], F32, kind="ExternalOutput").ap()

    mods_d = dscr("mods_d", [128, 8, D])
    KT_d = dscr("KT_d", [512, NTOK], BF16)
    QT_d = dscr("QT_d", [512, SEQ], BF16)
    V_d = dscr("V_d", [NTOK, 512], BF16)
    GV_d = dscr("GV_d", [NTOK, 512], BF16)
    GKQ_d = dscr("GKQ_d", [NTOK, 544])
    GG_d = dscr("GG_d", [SEQ, 512])
    OF_d = dscr("OF_d", [SEQ, 512])
    MIX_d = dscr("MIX_d", [SEQ, D], BF16)
    XN_d = dscr("XN_d", [SEQ, D])
    HF_d = dscr("HF_d", [SEQ, D], BF16)
    AFFT_d = dscr("AFFT_d", [NE, SEQ])

    glob = contextlib.ExitStack()
    idf = K.sb(glob, [128, 128], F32, "idf")
    idb = K.sb(glob, [128, 128], BF16, "idb")
    onesr = K.sb(glob, [1, 128], F32, "onesr")
    S.dma("sp", idf[:, :], ident_f, writes=[idf])
    S.dma("sp", idb[:, :], ident_b, writes=[idb])
    S.dma("sp", onesr[:, :], ones_row, writes=[onesr])

    def stage0():
        with contextlib.ExitStack() as es:
            cv = K.sb(es, [128, 16], F32, "cv")
            csil = K.sb(es, [128, 16], F32, "csil")
            crep = K.sb(es, [128, 16, 128], F32, "crep")
            bm = K.sb(es, [1, 6 * D], F32, "bm")
            modA = K.sb(es, [128, 6 * D], F32, "modA")
            modC = K.sb(es, [128, 2 * D], F32, "modC")
            nbc = K.sb(es, [128, 4, D], F32, "nbc")
            wm = [K.sb(es, [128, 8, 512], F32, "wm%d" % i) for i in range(2)]
            mods = K.sb(es, [128, 8, D], F32, "mods")
            psA = [K.ps(es, [128, 512], F32, "psA%d" % i) for i in range(2)]
            psC = [K.ps(es, [128, 512], F32, "psC%d" % i) for i in range(2)]
            S.dma("sp", cv[:, :], cvec, writes=[cv])
            S.dma("sp", bm[:, :], b_mod, writes=[bm])
            S.dma("sp", nbc[:, :, :], norms.partition_broadcast(128), writes=[nbc])
            S.op("act", lambda: act.activation(out=csil[:, :], in_=cv[:, :], func=AF.Silu), [cv], [csil])
            S.op("dve", lambda: dve.tensor_copy(out=crep[:, :, :], in_=csil[:, :].unsqueeze(2).broadcast_to([128, 16, 128])), [csil], [crep])
            wmv = w_mod.rearrange("(c p) n -> p c n", p=128)
            for nb in range(12):
                w = wm[nb % 2]
                S.dma("sp", w[:, :, :], wmv[:, :, nb * 512:(nb + 1) * 512], writes=[w])
                for which in range(2 if nb < 4 else 1):
                    ps = (psA, psC)[which][nb % 2]
                    for c in range(8):
                        S.op("pe", lambda c=c, ps=ps, w=w, which=which: pe.matmul(ps[:, :], lhsT=crep[:, which * 8 + c, :], rhs=w[:, c, :], start=(c == 0), stop=False), [crep, w], [ps])
                    S.op("pe", lambda ps=ps, nb=nb: pe.matmul(ps[:, :], lhsT=onesr[:, :], rhs=bm[:, nb * 512:(nb + 1) * 512], start=False, stop=True), [onesr, bm], [ps])
                    dst = (modA, modC)[which]
                    if which == 0:
                        S.op("act", lambda ps=ps, dst=dst, nb=nb: act.copy(out=dst[:, nb * 512:(nb + 1) * 512], in_=ps[:, :]), [ps], [dst])
                    else:
                        S.op("dve", lambda ps=ps, dst=dst, nb=nb: dve.tensor_copy(out=dst[:, nb * 512:(nb + 1) * 512], in_=ps[:, :]), [ps], [dst])
            import os
            if os.environ.get("STOP0") == "1":
                S.dma("sp", mods_d[:, 0:6, :], modA[:, :].rearrange("p (a b) -> p a b", a=6), reads=[modA], writes=[T_mods])
                return
            sl = lambda t, i: t[:, i * D:(i + 1) * D]
            S.op("dve", lambda: dve.scalar_tensor_tensor(out=mods[:, 0, :], in0=sl(modA, 1), scalar=1.0, in1=nbc[:, 0, :], op0=ALU.add, op1=ALU.mult), [modA, nbc], [mods])
            S.op("dve", lambda: dve.tensor_copy(out=mods[:, 1, :], in_=sl(modA, 0)), [modA], [mods])
            S.op("dve", lambda: dve.scalar_tensor_tensor(out=mods[:, 2, :], in0=sl(modC, 1), scalar=1.0, in1=nbc[:, 0, :], op0=ALU.add, op1=ALU.mult), [modC, nbc], [mods])
            S.op("dve", lambda: dve.tensor_copy(out=mods[:, 3, :], in_=sl(modC, 0)), [modC], [mods])
            S.op("dve", lambda: dve.tensor_tensor(out=mods[:, 4, :], in0=sl(modA, 2), in1=nbc[:, 1, :], op=ALU.mult), [modA, nbc], [mods])
            S.op("dve", lambda: dve.scalar_tensor_tensor(out=mods[:, 5, :], in0=sl(modA, 4), scalar=1.0, in1=nbc[:, 2, :], op0=ALU.add, op1=ALU.mult), [modA, nbc], [mods])
            S.op("dve", lambda: dve.tensor_copy(out=mods[:, 6, :], in_=sl(modA, 3)), [modA], [mods])
            S.op("dve", lambda: dve.tensor_tensor(out=mods[:, 7, :], in0=sl(modA, 5), in1=nbc[:, 3, :], op=ALU.mult), [modA, nbc], [mods])
            S.dma("sp", mods_d, mods[:, :, :], reads=[mods], writes=[T_mods])

    T_mods = T(None, "mods_d")
    T_KT, T_QT, T_V, T_GV, T_GKQ, T_GG = (T(None, n) for n in ("KT", "QT", "V", "GV", "GKQ", "GG"))

    def rms_rstd(es_tiles, src, src_t, ss, rstd, junk):
        S.op("act", lambda: act.activation(out=junk[:, :], in_=src, func=AF.Square, accum_out=ss[:, :]), [src_t], [junk, ss])
        S.op("act", lambda: act.activation(out=rstd[:, :], in_=ss[:, :], func=AF.Sqrt, scale=1.0 / D, bias=epsb[:, :]), [ss, epsb], [rstd])
        S.op("dve", lambda: dve.reciprocal(out=rstd[:, :], in_=rstd[:, :]), [rstd], [rstd])

    epsb = K.sb(glob, [128, 1], F32, "epsb")
    S.op("pool", lambda: pool.memset(epsb[:, :], EPS), [], [epsb])

    def stageA():
        with contextlib.ExitStack() as es:
            win = K.sb(es, [128, 8, INC], BF16, "win")
            wst = [K.sb(es, [128, 8, 512], F32, "wst%d" % i) for i in range(2)]
            gs = K.sb(es, [128, 4, D], F32, "gs")
            S.dma("sp", gs[:, :, :], mods_d[:, 0:4, :], reads=[T_mods], writes=[gs])
            wiv = w_in.rearrange("(c p) n -> p c n", p=128)
            for b in range(7):
                c0 = b * 512
                w_ = min(512, INC - c0)
                st = wst[b % 2]
                S.dma("sp", st[:, :, 0:w_], wiv[:, :, c0:c0 + w_], writes=[st])
                if b % 2 == 0:
                    S.op("act", lambda st=st, c0=c0, w_=w_: act.copy(out=win[:, :, c0:c0 + w_], in_=st[:, :, 0:w_]), [st], [win])
                else:
                    S.op("dve", lambda st=st, c0=c0, w_=w_: dve.tensor_copy(out=win[:, :, c0:c0 + w_], in_=st[:, :, 0:w_]), [st], [win])
            import os
            STOPA = int(os.environ.get("STOPA", "99"))
            if STOPA <= 1:
                return
            xt = [K.sb(es, [128, D], F32, "xt%d" % i) for i in range(2)]
            junk = K.sb(es, [128, D], BF16, "junk")
            ss = [K.sb(es, [128, 1], F32, "ss%d" % i) for i in range(2)]
            rstd = [K.sb(es, [128, 1], F32, "rstd%d" % i) for i in range(2)]
            h1 = [K.sb(es, [128, D], F32, "h1_%d" % i) for i in range(2)]
            hb = [K.sb(es, [128, D], BF16, "hb%d" % i) for i in range(2)]
            hT = [K.sb(es, [128, 8, 512], BF16, "hT%d" % i) for i in range(2)]
            ktq = [K.sb(es, [128, 512], BF16, "ktq%d" % i) for i in range(2)]
            vo = [K.sb(es, [128, 512], BF16, "vo%d" % i) for i in range(2)]
            gvo = [K.sb(es, [128, 512], BF16, "gvo%d" % i) for i in range(2)]
            gkq = [K.sb(es, [128, 544], F32, "gkq%d" % i) for i in range(2)]
            ggo = [K.sb(es, [128, 512], F32, "ggo%d" % i) for i in range(2)]
            pst = [K.ps(es, [128, 8 * 128], BF16, "pst%d" % i) for i in range(2)]
            psp = [K.ps(es, [128, 512], F32, "psp%d" % i) for i in range(4)]
            npp = [0]

            def nextps():
                npp[0] += 1
                return psp[npp[0] % 4]

            ev = [0]

            def evac(dst_t, dst_ap, ps, ps_ap):
                if psp.index(ps) % 2 == 0:
                    S.op("act", lambda: act.copy(out=dst_ap, in_=ps_ap), [ps], [dst_t])
                else:
                    S.op("dve", lambda: dve.tensor_copy(out=dst_ap, in_=ps_ap), [ps], [dst_t])

            groups = [(0, 2)] + [(2 + 4 * g, 4) for g in range(8)]
            ti_glob = 0
            for gi, (t0, ntl) in enumerate(groups):
                is_ctx = gi == 0
                hTg = hT[gi % 2]
                for k in range(ntl):
                    ti = t0 + k
                    b2 = ti_glob % 2
                    ti_glob += 1
                    x_ = xt[b2]
                    S.dma("sp", x_[:, :], xc[ti * 128:(ti + 1) * 128, :], writes=[x_])
                    rms_rstd(None, x_[:, :], x_, ss[b2], rstd[b2], junk)
                    go = 2 if is_ctx else 0
                    h_ = h1[b2]
                    S.op("dve", lambda x_=x_, h_=h_, b2=b2, go=go: dve.scalar_tensor_tensor(out=h_[:, :], in0=x_[:, :], scalar=rstd[b2][:, 0:1], in1=gs[:, go, :], op0=ALU.mult, op1=ALU.mult), [x_, rstd[b2], gs], [h_])
                    S.op("pool", lambda h_=h_, b2=b2, go=go: pool.tensor_tensor(out=hb[b2][:, :], in0=h_[:, :], in1=gs[:, go + 1, :], op=ALU.add), [h_, gs], [hb[b2]])
                    pt = pst[b2]
                    for c in range(8):
                        S.op("pe", lambda c=c, pt=pt, b2=b2: pe.transpose(out=pt[:, c * 128:(c + 1) * 128], in_=hb[b2][:, c * 128:(c + 1) * 128], identity=idb[:, :]), [hb[b2], idb], [pt])
                    S.op("act", lambda pt=pt, hTg=hTg, k=k: act.copy(out=hTg[:, :, k * 128:(k + 1) * 128], in_=pt[:, :].rearrange("p (c t) -> p c t", c=8)), [pt], [hTg])
                ntok = ntl * 128
                tok0 = t0 * 128
                if STOPA <= 2:
                    return
                for which, c0 in ((0, 0), (1, 1824)):
                    if which == 1 and is_ctx:
                        continue
                    for fb in range(4):
                        ps = nextps()
                        for c in range(8):
                            S.op("pe", lambda c=c, ps=ps, fb=fb, c0=c0: pe.matmul(ps[:, 0:ntok], lhsT=win[:, c, c0 + fb * 128:c0 + (fb + 1) * 128], rhs=hTg[:, c, 0:ntok], start=(c == 0), stop=(c == 7)), [win, hTg], [ps])
                        kb = ktq[(which * 4 + fb) % 2]
                        evac(kb, kb[:, 0:ntok], ps, ps[:, 0:ntok])
                        if which == 0:
                            S.dma("sp", KT_d[fb * 128:(fb + 1) * 128, tok0:tok0 + ntok], kb[:, 0:ntok], reads=[kb], writes=[T_KT])
                        else:
                            S.dma("sp", QT_d[fb * 128:(fb + 1) * 128, tok0 - CTX:tok0 - CTX + ntok], kb[:, 0:ntok], reads=[kb], writes=[T_QT])
                if STOPA <= 3:
                    return
                for k in range(ntl):
                    ti = t0 + k
                    b2 = ti % 2
                    r0 = ti * 128

                    def mm(c0, w_, k=k):
                        ps = nextps()
                        for c in range(8):
                            S.op("pe", lambda c=c, ps=ps: pe.matmul(ps[:, 0:w_], lhsT=hTg[:, c, k * 128:(k + 1) * 128], rhs=win[:, c, c0:c0 + w_], start=(c == 0), stop=(c == 7)), [win, hTg], [ps])
                        return ps

                    ps = mm(512, 512)
                    evac(vo[b2], vo[b2][:, :], ps, ps[:, :])
                    S.dma("sp", V_d[r0:r0 + 128, :], vo[b2][:, :], reads=[vo[b2]], writes=[T_V])
                    TM = int(os.environ.get("TM", "9"))
                    if TM <= 1:
                        continue
                    ps = mm(1024, 512)
                    EV = int(os.environ.get("EV", "3"))
                    if EV & 1:
                        evac(gkq[b2], gkq[b2][:, 0:256], ps, ps[:, 0:256])
                    if EV & 2:
                        evac(gvo[b2], gvo[b2][:, 0:256], ps, ps[:, 256:512])
                    if TM <= 2:
                        continue
                    ps = mm(1536, 288)
                    evac(gvo[b2], gvo[b2][:, 256:512], ps, ps[:, 0:256])
                    evac(gkq[b2], gkq[b2][:, 256:288], ps, ps[:, 256:288])
                    S.dma("sp", GV_d[r0:r0 + 128, :], gvo[b2][:, :], reads=[gvo[b2]], writes=[T_GV])
                    if TM <= 3:
                        continue
                    if not is_ctx:
                        ps = mm(2336, 512)
                        evac(gkq[b2], gkq[b2][:, 288:544], ps, ps[:, 0:256])
                        evac(ggo[b2], ggo[b2][:, 0:256], ps, ps[:, 256:512])
                        ps = mm(2848, 256)
                        evac(ggo[b2], ggo[b2][:, 256:512], ps, ps[:, 0:256])
                        S.dma("sp", GG_d[r0 - CTX:r0 - CTX + 128, :], ggo[b2][:, :], reads=[ggo[b2]], writes=[T_GG])
                        S.dma("sp", GKQ_d[r0:r0 + 128, :], gkq[b2][:, :], reads=[gkq[b2]], writes=[T_GKQ])
                    else:
                        S.dma("sp", GKQ_d[r0:r0 + 128, 0:288], gkq[b2][:, 0:288], reads=[gkq[b2]], writes=[T_GKQ])
                if STOPA <= 4:
                    return


    T_MIXna = T(None, "MIXna")

    def stageB():
        with contextlib.ExitStack() as es:
            NB = 3
            qT = [K.sb(es, [128, 4, 128], BF16, "qT%d" % i) for i in range(2)]
            kT = [K.sb(es, [128, 4, 896], BF16, "kT%d" % i) for i in range(2)]
            va = [K.sb(es, [128, 7, 8, 65], BF16, "va%d" % i) for i in range(2)]
            bint = K.sb(es, [128, 8, 640], F32, "bint")
            bt = [K.sb(es, [128, 640], F32, "bt%d" % i) for i in range(3)]
            tmp = [K.sb(es, [128, 896], F32, "tmp%d" % i) for i in range(NB)]
            pT = [K.sb(es, [128, 896], BF16, "pT%d" % i) for i in range(NB + 1)]
            ona = [K.sb(es, [128, 512], BF16, "ona%d" % i) for i in range(2)]
            rec = [K.sb(es, [128, 1], F32, "rec%d" % i) for i in range(2)]
            sT = [K.ps(es, [128, 1024], F32, "sT%d" % i) for i in range(NB)]
            pv = [K.ps(es, [128, 512], F32, "pv%d" % i) for i in range(2)]
            for v in va:
                S.op("pool", lambda v=v: pool.memset(v[:, :, :, :], 1.0), [], [v])
            S.dma("sp", bint[:, :, :], na_bias[2].rearrange("h j c -> j h c"), writes=[bint])
            QTv = QT_d.rearrange("(c p) t -> p c t", p=128)
            KTv = KT_d.rearrange("(c p) t -> p c t", p=128)
            nbt = [0]
            tmR = [[T(None, "tmR") for _ in range(3)] for _ in range(NB)]
            onaR = [[T(None, "onaR") for _ in range(8)] for _ in range(2)]

            def loads(T_):
                b = T_ % 2
                kt0 = _na_key_tile0(T_)
                S.dma("sp", qT[b][:, :, :], QTv[:, :, T_ * 128:(T_ + 1) * 128], reads=[T_QT], writes=[qT[b]])
                S.dma("sp", kT[b][:, :, 0:640], KTv[:, :, CTX + kt0 * 128:CTX + kt0 * 128 + 640], reads=[T_KT], writes=[kT[b]])
                S.dma("sp", kT[b][:, :, 640:896], KTv[:, :, 0:CTX], reads=[T_KT], writes=[kT[b]])
                r0 = CTX + kt0 * 128
                for kk in range(5):
                    S.dma("sp", va[b][:, kk, :, 0:64], V_d[r0 + kk * 128:r0 + (kk + 1) * 128, :].rearrange("t (h d) -> t h d", h=8), reads=[T_V], writes=[va[b]])
                for kk in range(2):
                    S.dma("sp", va[b][:, 5 + kk, :, 0:64], V_d[kk * 128:(kk + 1) * 128, :].rearrange("t (h d) -> t h d", h=8), reads=[T_V], writes=[va[b]])

            def phase1(n):
                T_, h = n // 8, n % 8
                b = T_ % 2
                if h == 0:
                    loads(T_)
                si = {0: 0, 1: 1, 30: 3, 31: 4}.get(T_, 2)
                pr, P0 = h // 2, (h % 2) * 64
                s_, tm, pt = sT[n % NB], tmp[n % NB], pT[n % (NB + 1)]
                if si == 2:
                    bb_t, bb = bint, bint[:, h, :]
                else:
                    bb_t = bt[nbt[0] % 3]
                    nbt[0] += 1
                    bb = bb_t[:, :]
                    S.dma("sp", bb, na_bias[si, h], writes=[bb_t])
                for kk in range(7):
                    S.op("pe", lambda kk=kk: pe.matmul(s_[:, kk * 128:(kk + 1) * 128], lhsT=kT[b][P0:P0 + 64, pr, kk * 128:(kk + 1) * 128], rhs=qT[b][P0:P0 + 64, pr, :], start=True, stop=True), [kT[b], qT[b]], [s_])
                ta, tb, tc = tmR[n % NB]
                S.op("dve", lambda: dve.scalar_tensor_tensor(out=tm[:, 0:512], in0=s_[:, 0:512], scalar=0.125, in1=bb[:, 0:512], op0=ALU.mult, op1=ALU.add), [s_, bb_t], [ta])
                S.op("dve", lambda: dve.scalar_tensor_tensor(out=tm[:, 512:640], in0=s_[:, 512:640], scalar=0.125, in1=bb[:, 512:640], op0=ALU.mult, op1=ALU.add), [s_, bb_t], [tb])
                S.op("dve", lambda: dve.tensor_scalar(out=tm[:, 640:896], in0=s_[:, 640:896], scalar1=0.125, scalar2=None, op0=ALU.mult), [s_], [tc])
                S.op("act", lambda: act.activation(out=pt[:, :], in_=tm[:, :], func=AF.Exp), [ta, tb, tc], [pt])

            def phase2(n):
                T_, h = n // 8, n % 8
                b = T_ % 2
                pt, p_, rc = pT[n % (NB + 1)], pv[n % 2], rec[n % 2]
                for kk in range(7):
                    S.op("pe", lambda kk=kk: pe.matmul(p_[:, 0:65], lhsT=pt[:, kk * 128:(kk + 1) * 128], rhs=va[b][:, kk, h, :], start=(kk == 0), stop=(kk == 6)), [pt, va[b]], [p_])
                S.op("dve", lambda: dve.reciprocal(out=rc[:, :], in_=p_[:, 64:65]), [p_], [rc])
                S.op("dve", lambda: dve.tensor_scalar(out=ona[b][:, h * 64:(h + 1) * 64], in0=p_[:, 0:64], scalar1=rc[:, 0:1], scalar2=None, op0=ALU.mult), [p_, rc], [onaR[b][h]])
                if h == 7:
                    S.dma("sp", MIX_d[T_ * 128:(T_ + 1) * 128, 0:512], ona[b][:, :], reads=onaR[b], writes=[T_MIXna])

            NIT = 32 * 8
            LOOK = NB - 1
            for n in range(min(LOOK, NIT)):
                phase1(n)
            for n in range(NIT):
                if n + LOOK < NIT:
                    phase1(n + LOOK)
                phase2(n)

    T_OF = T(None, "OF")
    T_MIXgla = T(None, "MIXgla")

    OB_d = dscr("OB_d", [SEQ, 512])
    T_OB = T(None, "OB")

    def stageC():
        with contextlib.ExitStack() as es:
            tri = [K.sb(es, [128, 128], F32, "tri%d" % i) for i in range(2)]
            msk = [K.sb(es, [128, 512], F32, "msk%d" % i) for i in range(2)]
            onec = K.sb(es, [128, 1], F32, "onec")
            aup = K.sb(es, [32, 512], F32, "aup")
            abi = K.sb(es, [1, 512], F32, "abi")
            gnb = K.sb(es, [128, 128], F32, "gnb")
            for t_, src in ((tri[0], tri_f), (tri[1], tri_b), (onec, ones_col), (aup, aup_bd), (abi, abias)):
                S.dma("sp", t_[:, :], src, writes=[t_])
            for h in range(4):
                S.dma("sp", msk[0][:, h * 128:(h + 1) * 128], mask_f, writes=[msk[0]])
                S.dma("sp", msk[1][:, h * 128:(h + 1) * 128], mask_b, writes=[msk[1]])
            S.dma("sp", gnb[:, :], gla_norm.partition_broadcast(128).rearrange("p a b -> p (a b)"), writes=[gnb])
            ps_ad = K.ps(es, [128, 512], F32, "ps_ad")
            ps_z = K.ps(es, [128, 512], F32, "ps_z")
            ps_lam = K.ps(es, [128, 512], F32, "ps_lam")
            ps_t = K.ps(es, [128, 1024], BF16, "ps_t")
            ps_AT = K.ps(es, [128, 512], F32, "ps_AT")
            ps_o = K.ps(es, [128, 512], F32, "ps_o")
            ps_U = K.ps(es, [128, 512], F32, "ps_U")

            class B:
                pass

            def alloc(ci):
                b = B()
                n_ = lambda x: "%s_c%d" % (x, ci)
                b.gkq = [K.sb(es, [128, 544], F32, n_("gkq%d" % i)) for i in range(2)]
                b.gv = [K.sb(es, [128, 512], BF16, n_("gv%d" % i)) for i in range(2)]
                b.cs = [K.sb(es, [128, 32], F32, n_("cs%d" % i)) for i in range(2)]
                b.sn = [K.sb(es, [128, 32], F32, n_("sn%d" % i)) for i in range(2)]
                b.adT = K.sb(es, [32, 128], F32, n_("adT"))
                b.ez = K.sb(es, [128, 256], F32, n_("ez"))
                b.lz = K.sb(es, [128, 256], F32, n_("lz"))
                b.E1 = K.sb(es, [128, 256], F32, n_("E1"))
                b.E2 = K.sb(es, [128, 256], F32, n_("E2"))
                b.at = K.sb(es, [128, 2], F32, n_("at"))
                b.kr = K.sb(es, [128, 256], F32, n_("kr"))
                b.qr = K.sb(es, [128, 256], F32, n_("qr"))
                b.rt = [K.sb(es, [128, 128], F32, n_("rt%d" % i)) for i in range(4)]
                b.qk = K.sb(es, [128, 512], BF16, n_("qk"))
                b.qTz = K.sb(es, [128, 4, 128], BF16, n_("qTz"))
                S.op("pool", lambda: pool.memset(b.qTz[:, :, :], 0.0), [], [b.qTz])
                b.kTc = K.sb(es, [128, 2, 128], BF16, n_("kTc"))
                b.ATs = K.sb(es, [128, 4, 128], BF16, n_("ATs"))
                b.Sst = [K.sb(es, [128, 128], F32, n_("Sst%d" % i)) for i in range(2)]
                b.Sbf = [K.sb(es, [128, 128], BF16, n_("Sbf%d" % i)) for i in range(2)]
                b.tU = K.sb(es, [128, 128], F32, n_("tU"))
                b.osb = [K.sb(es, [128, 512], F32, n_("osb%d" % i)) for i in range(2)]
                return b

            def rope(eng, ek, src_t, src_ap, dst_t, tmps, c_, s_):
                X = src_ap.rearrange("p (h a b f) -> p h a b f", h=4, a=2, b=2)
                O = dst_t[:, :].rearrange("p (h a b f) -> p h a b f", h=4, a=2, b=2)
                C = c_[:, :].rearrange("p (a f) -> p a f", a=2).unsqueeze(1).broadcast_to([128, 4, 2, 16])
                Sn = s_[:, :].rearrange("p (a f) -> p a f", a=2).unsqueeze(1).broadcast_to([128, 4, 2, 16])
                v = lambda t_: t_[:, :].rearrange("p (h a f) -> p h a f", h=4, a=2)
                X1, X2 = X[:, :, :, 0, :], X[:, :, :, 1, :]
                t1, t2, t3, t4 = tmps
                S.op(ek, lambda: eng.tensor_tensor(out=v(t1), in0=X1, in1=C, op=ALU.mult), [src_t, c_], [t1])
                S.op(ek, lambda: eng.tensor_tensor(out=v(t2), in0=X2, in1=Sn, op=ALU.mult), [src_t, s_], [t2])
                S.op(ek, lambda: eng.tensor_tensor(out=O[:, :, :, 0, :], in0=v(t1), in1=v(t2), op=ALU.subtract), [t1, t2], [dst_t])
                S.op(ek, lambda: eng.tensor_tensor(out=v(t3), in0=X1, in1=Sn, op=ALU.mult), [src_t, s_], [t3])
                S.op(ek, lambda: eng.tensor_tensor(out=v(t4), in0=X2, in1=C, op=ALU.mult), [src_t, c_], [t4])
                S.op(ek, lambda: eng.tensor_tensor(out=O[:, :, :, 1, :], in0=v(t3), in1=v(t4), op=ALU.add), [t3, t4], [dst_t])

            def chain(dr, c):
                for pr in range(2):
                    S.op("pool", lambda pr=pr: pool.memset(c.Sst[pr][:, :], 0.0), [], [c.Sst[pr]])
                    S.op("pool", lambda pr=pr: pool.memset(c.Sbf[pr][:, :], 0.0), [], [c.Sbf[pr]])
                order = list(range(NT)) if dr == 0 else [1, 0] + list(range(NT - 1, 1, -1))
                n = 0
                for ti in order:
                    lat = ti >= 2
                    li = ti - 2
                    b = n % 2
                    n += 1
                    g_, v_ = c.gkq[b], c.gv[b]
                    gw = 544 if lat else 288
                    S.dma("sp", g_[:, 0:gw], GKQ_d[ti * 128:(ti + 1) * 128, 0:gw], reads=[T_GKQ], writes=[g_])
                    S.dma("sp", v_[:, :], GV_d[ti * 128:(ti + 1) * 128, :], reads=[T_GV], writes=[v_])
                    if lat:
                        S.dma("sp", c.cs[b][:, :], rope_cos[li * 128:(li + 1) * 128, :], writes=[c.cs[b]])
                        S.dma("sp", c.sn[b][:, :], rope_sin[li * 128:(li + 1) * 128, :], writes=[c.sn[b]])
                    yield
                    S.op("pe", lambda: pe.transpose(out=ps_ad[0:32, 0:128], in_=g_[:, 256:288], identity=idf[:, :]), [g_, idf], [ps_ad])
                    S.op("act", lambda: act.copy(out=c.adT[:, :], in_=ps_ad[0:32, 0:128]), [ps_ad], [c.adT])
                    S.op("pe", lambda: pe.matmul(ps_z[:, 0:256], lhsT=c.adT[:, :], rhs=aup[:, dr * 256:(dr + 1) * 256], start=True, stop=False), [c.adT, aup], [ps_z])
                    S.op("pe", lambda: pe.matmul(ps_z[:, 0:256], lhsT=onesr[:, :], rhs=abi[:, dr * 256:(dr + 1) * 256], start=False, stop=True), [onesr, abi], [ps_z])
                    S.op("act", lambda: act.activation(out=c.ez[:, :], in_=ps_z[:, 0:256], func=AF.Exp, scale=-1.0), [ps_z], [c.ez])
                    yield
                    S.op("act", lambda: act.activation(out=c.lz[:, :], in_=c.ez[:, :], func=AF.Ln, bias=1.0), [c.ez], [c.lz])
                    S.op("pe", lambda: pe.matmul(ps_lam[:, 0:256], lhsT=tri[dr][:, :], rhs=c.lz[:, :], start=True, stop=True), [tri[dr], c.lz], [ps_lam])
                    for pr in range(2):
                        S.op("pe", lambda pr=pr: pe.matmul(ps_lam[:, 256 + pr:257 + pr], lhsT=c.lz[:, pr * 128:(pr + 1) * 128], rhs=onec[:, :], start=True, stop=True), [c.lz, onec], [ps_lam])
                    if lat:
                        S.op("act", lambda: act.activation(out=c.E1[:, :], in_=ps_lam[:, 0:256], func=AF.Exp), [ps_lam], [c.E1])
                    S.op("act", lambda: act.activation(out=c.E2[:, :], in_=ps_lam[:, 0:256], func=AF.Exp, scale=-1.0), [ps_lam], [c.E2])
                    S.op("act", lambda: act.activation(out=c.at[:, :], in_=ps_lam[:, 256:258], func=AF.Exp), [ps_lam], [c.at])
                    yield
                    if lat:
                        rope(dve, "dve", g_, g_[:, 0:256], c.kr, c.rt, c.cs[b], c.sn[b])
                        yield
                        rope(dve, "dve", g_, g_[:, 288:544], c.qr, c.rt, c.cs[b], c.sn[b])
                        yield
                        S.op("dve", lambda: dve.scalar_tensor_tensor(out=c.qk[:, 0:256], in0=c.qr[:, :], scalar=0.125, in1=c.E1[:, :], op0=ALU.mult, op1=ALU.mult), [c.qr, c.E1], [c.qk])
                        S.op("dve", lambda: dve.tensor_tensor(out=c.qk[:, 256:512], in0=c.kr[:, :], in1=c.E2[:, :], op=ALU.mult), [c.kr, c.E2], [c.qk])
                    else:
                        S.op("dve", lambda: dve.tensor_tensor(out=c.qk[:, 256:512], in0=g_[:, 0:256], in1=c.E2[:, :], op=ALU.mult), [g_, c.E2], [c.qk])
                    for j in (range(4) if lat else range(2, 4)):
                        S.op("pe", lambda j=j: pe.transpose(out=ps_t[:, j * 128:(j + 1) * 128], in_=c.qk[:, j * 128:(j + 1) * 128], identity=idb[:, :]), [c.qk, idb], [ps_t])
                    if lat:
                        for h in range(4):
                            S.op("act", lambda h=h: act.copy(out=c.qTz[(h % 2) * 64:(h % 2) * 64 + 64, h, :], in_=ps_t[(h % 2) * 64:(h % 2) * 64 + 64, (h // 2) * 128:(h // 2 + 1) * 128]), [ps_t], [c.qTz])
                    S.op("act", lambda: act.copy(out=c.kTc[:, :, :], in_=ps_t[:, 256:512].rearrange("p (c t) -> p c t", c=2)), [ps_t], [c.kTc])
                    yield
                    if lat:
                        for h in range(4):
                            pr = h // 2
                            S.op("pe", lambda h=h, pr=pr: pe.matmul(ps_AT[:, h * 128:(h + 1) * 128], lhsT=c.kTc[:, pr, :], rhs=c.qTz[:, h, :], start=True, stop=True), [c.kTc, c.qTz], [ps_AT])
                        S.op("dve", lambda: dve.tensor_tensor(out=c.ATs[:, :, :].rearrange("p h t -> p (h t)"), in0=ps_AT[:, :], in1=msk[dr][:, :], op=ALU.mult), [ps_AT, msk[dr]], [c.ATs])
                        for h in range(4):
                            pr = h // 2
                            S.op("pe", lambda h=h: pe.matmul(ps_o[:, h * 128:(h + 1) * 128], lhsT=c.ATs[:, h, :], rhs=v_[:, h * 128:(h + 1) * 128], start=True, stop=False), [c.ATs, v_], [ps_o])
                            S.op("pe", lambda h=h, pr=pr: pe.matmul(ps_o[:, h * 128:(h + 1) * 128], lhsT=c.qTz[:, h, :], rhs=c.Sbf[pr][:, :], start=False, stop=True), [c.qTz, c.Sbf[pr]], [ps_o])
                        ob_ = c.osb[b]
                        S.op("dve", lambda: dve.tensor_copy(out=ob_[:, :], in_=ps_o[:, :]), [ps_o], [ob_])
                        if dr == 0:
                            S.dma("sp", OF_d[li * 128:(li + 1) * 128, :], ob_[:, :], reads=[ob_], writes=[T_OF])
                        else:
                            S.dma("sp", OB_d[li * 128:(li + 1) * 128, :], ob_[:, :], reads=[ob_], writes=[T_OB])
                        yield
                    for h in range(4):
                        pr = h // 2
                        S.op("pe", lambda h=h, pr=pr: pe.matmul(ps_U[:, h * 128:(h + 1) * 128], lhsT=c.qk[:, 256 + pr * 128:256 + (pr + 1) * 128], rhs=v_[:, h * 128:(h + 1) * 128], start=True, stop=True), [c.qk, v_], [ps_U])
                    for h in range(4):
                        pr, P0 = h // 2, (h % 2) * 64
                        S.op("dve", lambda h=h, pr=pr, P0=P0: dve.tensor_scalar(out=c.tU[P0:P0 + 64, :], in0=ps_U[P0:P0 + 64, h * 128:(h + 1) * 128], scalar1=c.at[P0:P0 + 64, pr:pr + 1], scalar2=None, op0=ALU.mult), [ps_U, c.at], [c.tU])
                        S.op("dve", lambda pr=pr, P0=P0: dve.scalar_tensor_tensor(out=c.Sst[pr][P0:P0 + 64, :], in0=c.Sst[pr][P0:P0 + 64, :], scalar=c.at[P0:P0 + 64, pr:pr + 1], in1=c.tU[P0:P0 + 64, :], op0=ALU.mult, op1=ALU.add), [c.Sst[pr], c.at, c.tU], [c.Sst[pr]])
                        S.op("dve", lambda pr=pr, P0=P0: dve.tensor_copy(out=c.Sbf[pr][P0:P0 + 64, :], in_=c.Sst[pr][P0:P0 + 64, :]), [c.Sst[pr]], [c.Sbf[pr]])
                    yield

            gens = [chain(0, alloc(0)), chain(1, alloc(1))]
            while gens:
                for g in list(gens):
                    try:
                        next(g)
                    except StopIteration:
                        gens.remove(g)

            gg = [K.sb(es, [128, 512], F32, "gg%d" % i) for i in range(2)]
            ofl = [K.sb(es, [128, 512], F32, "ofl%d" % i) for i in range(2)]
            obl = [K.sb(es, [128, 512], F32, "obl%d" % i) for i in range(2)]
            osm = [K.sb(es, [128, 512], F32, "osm%d" % i) for i in range(2)]
            junk = [K.sb(es, [128, 128], F32, "junkc%d" % i) for i in range(2)]
            ss4 = [K.sb(es, [128, 4], F32, "ss4_%d" % i) for i in range(2)]
            rs4 = [K.sb(es, [128, 4], F32, "rs4_%d" % i) for i in range(2)]
            sg = [K.sb(es, [128, 512], F32, "sg%d" % i) for i in range(2)]
            ogl = [K.sb(es, [128, 512], BF16, "ogl%d" % i) for i in range(2)]

            def merge(li):
                b = li % 2
                S.dma("sp", gg[b][:, :], GG_d[li * 128:(li + 1) * 128, :], reads=[T_GG], writes=[gg[b]])
                S.dma("sp", ofl[b][:, :], OF_d[li * 128:(li + 1) * 128, :], reads=[T_OF], writes=[ofl[b]])
                S.dma("sp", obl[b][:, :], OB_d[li * 128:(li + 1) * 128, :], reads=[T_OB], writes=[obl[b]])
                yield
                S.op("pool", lambda: pool.tensor_tensor(out=osm[b][:, :], in0=ofl[b][:, :], in1=obl[b][:, :], op=ALU.add), [ofl[b], obl[b]], [osm[b]])
                S.op("act", lambda: act.activation(out=sg[b][:, :], in_=gg[b][:, :], func=AF.Silu), [gg[b]], [sg[b]])
                yield
                for h in range(4):
                    S.op("act", lambda h=h: act.activation(out=junk[b][:, :], in_=osm[b][:, h * 128:(h + 1) * 128], func=AF.Square, accum_out=ss4[b][:, h:h + 1]), [osm[b]], [junk[b], ss4[b]])
                S.op("act", lambda: act.activation(out=rs4[b][:, :], in_=ss4[b][:, :], func=AF.Sqrt, scale=1.0 / 128.0, bias=epsb[:, :]), [ss4[b], epsb], [rs4[b]])
                yield
                S.op("dve", lambda: dve.reciprocal(out=rs4[b][:, :], in_=rs4[b][:, :]), [rs4[b]], [rs4[b]])
                o3 = osm[b][:, :].rearrange("p (h e) -> p h e", h=4)
                S.op("dve", lambda: dve.tensor_tensor(out=o3, in0=o3, in1=rs4[b][:, :].unsqueeze(2).broadcast_to([128, 4, 128]), op=ALU.mult), [osm[b], rs4[b]], [osm[b]])
                yield
                S.op("pool", lambda: pool.tensor_tensor(out=o3, in0=o3, in1=gnb[:, :].unsqueeze(1).broadcast_to([128, 4, 128]), op=ALU.mult), [osm[b], gnb], [osm[b]])
                S.op("dve", lambda: dve.tensor_tensor(out=ogl[b][:, :], in0=osm[b][:, :], in1=sg[b][:, :], op=ALU.mult), [osm[b], sg[b]], [ogl[b]])
                S.dma("sp", MIX_d[li * 128:(li + 1) * 128, 512:1024], ogl[b][:, :], reads=[ogl[b]], writes=[T_MIXgla])
                yield

            pend = []
            for li in range(32):
                pend.append(merge(li))
                if len(pend) == 2 or li == 31:
                    live = list(pend)
                    while live:
                        for g in list(live):
                            try:
                                next(g)
                            except StopIteration:
                                live.remove(g)
                    pend = []

    T_XN = T(None, "XN")
    T_HF = T(None, "HF")
    T_AFFT = T(None, "AFFT")
    AX = mybir.AxisListType.X

    def stageD():
        with contextlib.ExitStack() as es:
            wo = K.sb(es, [128, 8, D], BF16, "wo")
            wst = [K.sb(es, [128, 8, 512], F32, "wstd%d" % i) for i in range(2)]
            wov = w_out.rearrange("(c p) n -> p c n", p=128)
            for b in range(2):
                S.dma("sp", wst[b][:, :, :], wov[:, :, b * 512:(b + 1) * 512], writes=[wst[b]])
                S.op("act", lambda b=b: act.copy(out=wo[:, :, b * 512:(b + 1) * 512], in_=wst[b][:, :, :]), [wst[b]], [wo])
            rt = K.sb(es, [128, 8, NE], F32, "rt")
            S.dma("sp", rt[:, :, :], router.rearrange("(c p) e -> p c e", p=128), writes=[rt])
            md = K.sb(es, [128, 3, D], F32, "md")
            S.dma("sp", md[:, :, :], mods_d[:, 4:7, :], reads=[T_mods], writes=[md])
            affT = K.sb(es, [NE, SEQ], F32, "affT")
            affR = [T(None, "affR%d" % i) for i in range(32)]
            dbl = lambda shape, dt, nm: [K.sb(es, shape, dt, "%s%d" % (nm, i)) for i in range(2)]
            mixb = dbl([128, D], BF16, "mixb")
            xt = dbl([128, D], F32, "xtd")
            mixT = dbl([128, 8, 128], BF16, "mixT")
            mixs = dbl([128, D], F32, "mixs")
            junk = dbl([128, D], BF16, "junkd")
            ss = dbl([128, 1], F32, "ssd")
            rstd = dbl([128, 1], F32, "rstdd")
            t1 = dbl([128, D], F32, "t1d")
            xn = dbl([128, D], F32, "xn")
            hf32 = dbl([128, D], F32, "hf32")
            hfb = dbl([128, D], BF16, "hfb")
            hfT = dbl([128, 8, 128], F32, "hfT")
            lg = dbl([128, NE], F32, "lg")
            mx = dbl([128, 1], F32, "mx")
            ex = dbl([128, NE], F32, "ex")
            sm = dbl([128, 1], F32, "sm")
            af = dbl([128, NE], F32, "af")
            ps_t = K.ps(es, [128, 1024], BF16, "psd_t")
            ps_m = [K.ps(es, [128, 512], F32, "psd_m%d" % i) for i in range(2)]
            ps_r = [K.ps(es, [128, 512], F32, "psd_r%d" % i) for i in range(2)]
            ps_l = K.ps(es, [128, 512], F32, "psd_l")
            ps_a = K.ps(es, [128, 512], F32, "psd_a")

            def tile_prog(T_):
                b = T_ % 2
                mb, x_ = mixb[b], xt[b]
                S.dma("sp", mb[:, :], MIX_d[T_ * 128:(T_ + 1) * 128, :], reads=[T_MIXna, T_MIXgla], writes=[mb])
                S.dma("sp", x_[:, :], xc[CTX + T_ * 128:CTX + (T_ + 1) * 128, :], writes=[x_])
                yield
                for c in range(8):
                    S.op("pe", lambda c=c: pe.transpose(out=ps_t[:, c * 128:(c + 1) * 128], in_=mb[:, c * 128:(c + 1) * 128], identity=idb[:, :]), [mb, idb], [ps_t])
                S.op("act", lambda: act.copy(out=mixT[b][:, :, :], in_=ps_t[:, :].rearrange("p (c t) -> p c t", c=8)), [ps_t], [mixT[b]])
                yield
                for hlf in range(2):
                    for c in range(8):
                        S.op("pe", lambda c=c, hlf=hlf: pe.matmul(ps_m[hlf][:, :], lhsT=mixT[b][:, c, :], rhs=wo[:, c, hlf * 512:(hlf + 1) * 512], start=(c == 0), stop=(c == 7)), [mixT[b], wo], [ps_m[hlf]])
                    S.op("act", lambda hlf=hlf: act.copy(out=mixs[b][:, hlf * 512:(hlf + 1) * 512], in_=ps_m[hlf][:, :]), [ps_m[hlf]], [mixs[b]])
                yield
                rms_rstd(None, mixs[b][:, :], mixs[b], ss[b], rstd[b], junk[b])
                yield
                S.op("dve", lambda: dve.scalar_tensor_tensor(out=t1[b][:, :], in0=mixs[b][:, :], scalar=rstd[b][:, 0:1], in1=md[:, 0, :], op0=ALU.mult, op1=ALU.mult), [mixs[b], rstd[b], md], [t1[b]])
                xn_ = xn[b]
                S.op("pool", lambda: pool.tensor_tensor(out=xn_[:, :], in0=t1[b][:, :], in1=x_[:, :], op=ALU.add), [t1[b], x_], [xn_])
                S.dma("sp", XN_d[T_ * 128:(T_ + 1) * 128, :], xn_[:, :], reads=[xn_], writes=[T_XN])
                yield
                rms_rstd(None, xn_[:, :], xn_, ss[b], rstd[b], junk[b])
                yield
                S.op("dve", lambda: dve.scalar_tensor_tensor(out=t1[b][:, :], in0=xn_[:, :], scalar=rstd[b][:, 0:1], in1=md[:, 1, :], op0=ALU.mult, op1=ALU.mult), [xn_, rstd[b], md], [t1[b]])
                S.op("pool", lambda: pool.tensor_tensor(out=hf32[b][:, :], in0=t1[b][:, :], in1=md[:, 2, :], op=ALU.add), [t1[b], md], [hf32[b]])
                yield
                hb_ = hfb[b]
                S.op("act", lambda: act.copy(out=hb_[:, :], in_=hf32[b][:, :]), [hf32[b]], [hb_])
                S.dma("sp", HF_d[T_ * 128:(T_ + 1) * 128, :], hb_[:, :], reads=[hb_], writes=[T_HF])
                for c in range(8):
                    S.op("pe", lambda c=c: pe.transpose(out=ps_r[c // 4][:, (c % 4) * 128:(c % 4 + 1) * 128], in_=hf32[b][:, c * 128:(c + 1) * 128], identity=idf[:, :]), [hf32[b], idf], [ps_r[c // 4]])
                for i in range(2):
                    S.op("act", lambda i=i: act.copy(out=hfT[b][:, i * 4:(i + 1) * 4, :], in_=ps_r[i][:, :].rearrange("p (c t) -> p c t", c=4)), [ps_r[i]], [hfT[b]])
                yield
                for c in range(8):
                    S.op("pe", lambda c=c: pe.matmul(ps_l[:, 0:NE], lhsT=hfT[b][:, c, :], rhs=rt[:, c, :], start=(c == 0), stop=(c == 7)), [hfT[b], rt], [ps_l])
                S.op("dve", lambda: dve.tensor_copy(out=lg[b][:, :], in_=ps_l[:, 0:NE]), [ps_l], [lg[b]])
                yield
                S.op("dve", lambda: dve.reduce_max(out=mx[b][:, :], in_=lg[b][:, :], axis=AX), [lg[b]], [mx[b]])
                S.op("dve", lambda: dve.tensor_scalar(out=mx[b][:, :], in0=mx[b][:, :], scalar1=-1.0, scalar2=None, op0=ALU.mult), [mx[b]], [mx[b]])
                yield
                S.op("act", lambda: act.activation(out=ex[b][:, :], in_=lg[b][:, :], func=AF.Exp, bias=mx[b][:, :], accum_out=sm[b][:, :]), [lg[b], mx[b]], [ex[b], sm[b]])
                yield
                S.op("dve", lambda: dve.reciprocal(out=sm[b][:, :], in_=sm[b][:, :]), [sm[b]], [sm[b]])
                S.op("dve", lambda: dve.tensor_scalar(out=af[b][:, :], in0=ex[b][:, :], scalar1=sm[b][:, 0:1], scalar2=None, op0=ALU.mult), [ex[b], sm[b]], [af[b]])
                yield
                S.op("pe", lambda: pe.transpose(out=ps_a[0:NE, 0:128], in_=af[b][:, :], identity=idf[:, :]), [af[b], idf], [ps_a])
                S.op("act", lambda: act.copy(out=affT[:, T_ * 128:(T_ + 1) * 128], in_=ps_a[0:NE, 0:128]), [ps_a], [affR[T_]])
                yield

            for T0 in range(0, 32, 2):
                live = [tile_prog(T0), tile_prog(T0 + 1)]
                while live:
                    for g in list(live):
                        try:
                            next(g)
                        except StopIteration:
                            live.remove(g)
            S.dma("sp", AFFT_d, affT[:, :], reads=affR, writes=[T_AFFT])


    posT_d = dscr("posT_d", [128, 4, 64])
    gateT_d = dscr("gateT_d", [128, 4, 64])
    HFO_d = dscr("HFO_d", [SEQ // 2, D], BF16)
    T_HFO = T(None, "HFO")

    def stageEFG():
        import os
        with contextlib.ExitStack() as es:
            posT = K.sb(es, [128, 4, 64], F32, "posT")
            gateT = K.sb(es, [128, 4, 64], F32, "gateT")
            iot = K.sb(es, [128, 512], F32, "iot")
            S.dma("sp", iot[:, :], iota512, writes=[iot])
            PB = [K.ps(es, [128, 512], F32, "PB%d" % i) for i in range(8)]
            with contextlib.ExitStack() as e2:
                A = K.sb(e2, [128, 512], F32, "A")
                S.dma("sp", A[:, :], AFFT_d.rearrange("e (s t) -> (e s) t", s=8), reads=[T_AFFT], writes=[A])
                b1 = K.sb(e2, [128, 128], F32, "b1")
                blt = K.sb(e2, [128, 128], F32, "blt")
                ownf = K.sb(e2, [128, 1], F32, "ownf")
                selm = K.sb(e2, [128, 64], F32, "selm")
                for t_, src in ((b1, blk_ones), (blt, blk_lt), (ownf, own_flag), (selm, sel)):
                    S.dma("sp", t_[:, :], src, writes=[t_])
                lo = K.sb(e2, [128, 1], F32, "lo")
                hi = K.sb(e2, [128, 1], F32, "hi")
                mid = K.sb(e2, [128, 1], F32, "mid")
                cnt = K.sb(e2, [128, 1], F32, "cnt")
                cond = K.sb(e2, [128, 1], F32, "cond")
                d1 = K.sb(e2, [128, 1], F32, "d1")
                jk = K.sb(e2, [128, 512], F32, "jk")
                onesT = K.sb(e2, [128, 512], F32, "onesT")
                M_ = K.sb(e2, [128, 512], F32, "M_")
                posi = K.sb(e2, [128, 512], F32, "posi")
                pm = K.sb(e2, [128, 512], F32, "pm")
                offc = K.sb(e2, [128, 1], F32, "offc")
                S.op("pool", lambda: pool.memset(lo[:, :], 0.0), [], [lo])
                S.op("pool", lambda: pool.memset(hi[:, :], 1.0), [], [hi])
                S.op("pool", lambda: pool.memset(onesT[:, :], 1.0), [], [onesT])
                pc = PB[0]
                for it in range(30):
                    S.op("dve", lambda: dve.tensor_tensor(out=mid[:, :], in0=lo[:, :], in1=hi[:, :], op=ALU.add), [lo, hi], [mid])
                    S.op("dve", lambda: dve.tensor_scalar(out=mid[:, :], in0=mid[:, :], scalar1=0.5, scalar2=None, op0=ALU.mult), [mid], [mid])
                    S.op("dve", lambda: dve.tensor_scalar(out=jk[:, :], in0=A[:, :], scalar1=mid[:, 0:1], scalar2=0.0, op0=ALU.is_gt, op1=ALU.add, accum_out=cnt[:, 0:1]), [A, mid], [jk, cnt])
                    S.op("pe", lambda: pe.matmul(pc[:, 0:1], lhsT=b1[:, :], rhs=cnt[:, 0:1], start=True, stop=True), [b1, cnt], [pc])
                    S.op("dve", lambda: dve.tensor_scalar(out=cond[:, :], in0=pc[:, 0:1], scalar1=float(CAP) - 0.5, scalar2=None, op0=ALU.is_gt), [pc], [cond])
                    S.op("dve", lambda: dve.tensor_tensor(out=d1[:, :], in0=mid[:, :], in1=lo[:, :], op=ALU.subtract), [mid, lo], [d1])
                    S.op("dve", lambda: dve.scalar_tensor_tensor(out=lo[:, :], in0=d1[:, :], scalar=cond[:, 0:1], in1=lo[:, :], op0=ALU.mult, op1=ALU.add), [d1, cond, lo], [lo])
                    S.op("dve", lambda: dve.tensor_tensor(out=d1[:, :], in0=hi[:, :], in1=mid[:, :], op=ALU.subtract), [hi, mid], [d1])
                    S.op("dve", lambda: dve.scalar_tensor_tensor(out=hi[:, :], in0=d1[:, :], scalar=cond[:, 0:1], in1=mid[:, :], op0=ALU.mult, op1=ALU.add), [d1, cond, mid], [hi])
                S.op("dve", lambda: dve.tensor_scalar(out=M_[:, :], in0=A[:, :], scalar1=lo[:, 0:1], scalar2=ownf[:, 0:1], op0=ALU.is_gt, op1=ALU.mult), [A, lo, ownf], [M_])
                S.op("dve", lambda: dve.tensor_tensor_scan(out=posi[:, :], data0=onesT[:, :], data1=M_[:, :], initial=0.0, op0=ALU.mult, op1=ALU.add), [onesT, M_], [posi])
                S.op("pe", lambda: pe.matmul(pc[:, 0:1], lhsT=blt[:, :], rhs=posi[:, 511:512], start=True, stop=True), [blt, posi], [pc])
                S.op("dve", lambda: dve.tensor_scalar(out=offc[:, :], in0=pc[:, 0:1], scalar1=-10000.0, scalar2=None, op0=ALU.add), [pc], [offc])
                S.op("dve", lambda: dve.tensor_scalar(out=pm[:, :], in0=posi[:, :], scalar1=offc[:, 0:1], scalar2=None, op0=ALU.add), [posi, offc], [pm])
                S.op("dve", lambda: dve.tensor_tensor(out=pm[:, :], in0=pm[:, :], in1=M_[:, :], op=ALU.mult), [pm, M_], [pm])
                S.op("dve", lambda: dve.tensor_scalar(out=pm[:, :], in0=pm[:, :], scalar1=9999.0, scalar2=None, op0=ALU.add), [pm], [pm])
                for src, dst, pb in ((pm, posT, PB[1]), (A, gateT, PB[2])):
                    for blk in range(4):
                        S.op("pe", lambda src=src, pb=pb, blk=blk: pe.matmul(pb[:, blk * 64:(blk + 1) * 64], lhsT=src[:, blk * 128:(blk + 1) * 128], rhs=selm[:, :], start=True, stop=True), [src, selm], [pb])
                    S.op("act", lambda dst=dst, pb=pb: act.copy(out=dst[:, :, :], in_=pb[:, 0:256].rearrange("p (b c) -> p b c", b=4)), [pb], [dst])
                if debug:
                    S.dma("sp", posT_d, posT[:, :, :], reads=[posT])
                    S.dma("sp", gateT_d, gateT[:, :, :], reads=[gateT])
                S.barrier()
            S.barrier()
            if upto == "E":
                return
            oi = K.sb(es, [128, 16], I32, "oi")
            S.dma("sp", oi[:, :], own_idx, writes=[oi])
            hfj = [K.sb(es, [128, D], BF16, "hfj%d" % i) for i in range(3)]
            for j in range(16):
                hj = hfj[j % 3]
                S.dma("pool", None, None, reads=[T_HF, oi], writes=[hj], fn=lambda hj=hj, j=j: pool.indirect_dma_start(
                    out=hj[:, :], out_offset=None, in_=HF_d, in_offset=bass.IndirectOffsetOnAxis(ap=oi[:, j:j + 1], axis=0)))
                S.dma("sp", HFO_d[j * 128:(j + 1) * 128, :], hj[:, :], reads=[hj], writes=[T_HFO])
            yacc = K.sb(es, [128, 16, D], F32, "yacc")
            S.op("pool", lambda: pool.memset(yacc[:, :, :], 0.0), [], [yacc])
            e3 = contextlib.ExitStack()
            xsT = K.sb(e3, [128, 8, 512], BF16, "xsT")
            hidT = K.sb(e3, [128, NFC, 512], BF16, "hidT")
            ysb = K.sb(e3, [128, 4, D], BF16, "ysb")
            Ej = [K.sb(e3, [128, 512], BF16, "Ej%d" % i) for i in range(2)]
            Gj = [K.sb(e3, [128, 512], BF16, "Gj%d" % i) for i in range(2)]
            GTa = K.sb(e3, [128, 16, 4, 128], BF16, "GTa")
            GTt = [T(None, "GTt%d" % i) for i in range(16)]
            sgt = [K.sb(e3, [128, 512], BF16, "sgt%d" % i) for i in range(2)]
            stg = [K.sb(e3, [128, 2048], F32, "stg%d" % i) for i in range(4)]
            nst = [0]

            def stage_in(src_ap, ncols):
                st = stg[nst[0] % 4]
                q = os.environ.get("WQ", "sp").split(",")
                q = q[nst[0] % len(q)]
                nst[0] += 1
                S.dma(q, st[:, 0:ncols], src_ap, writes=[st])
                return st
            wgb = [K.sb(e3, [128, 8, 256], BF16, "wgb%d" % i) for i in range(2)]
            wub = [K.sb(e3, [128, 8, 256], BF16, "wub%d" % i) for i in range(2)]
            wdb = K.sb(e3, [128, NFC, 256], BF16, "wdb")
            wdbT = [T(None, "wdbT%d" % i) for i in range(3)]
            NEX = int(os.environ.get("NEX", str(NE)))
            ng = 0
            PH = int(os.environ.get("PH", "15"))
            for e in range(NEX):
                for j in (range(16) if PH & 1 else []):
                    osg, blk = j // 4, j % 4
                    hj = hfj[j % 3]
                    S.dma("sp", hj[:, :], HFO_d[j * 128:(j + 1) * 128, :], reads=[T_HFO], writes=[hj])
                    E_ = Ej[j % 2]
                    S.op("dve", lambda E_=E_, blk=blk, col=e * 4 + osg: dve.tensor_scalar(out=E_[:, :], in0=iot[:, :], scalar1=posT[:, blk, col:col + 1], scalar2=None, op0=ALU.is_equal), [iot, posT], [E_])
                    for c in range(8):
                        S.op("pe", lambda c=c, hj=hj, E_=E_, j=j: pe.matmul(PB[c][:, :], lhsT=hj[:, c * 128:(c + 1) * 128], rhs=E_[:, :], start=(j == 0), stop=(j == 15)), [hj, E_], [PB[c]])
                for c in (range(8) if PH & 1 else []):
                    if c % 2 == 0:
                        S.op("act", lambda c=c: act.copy(out=xsT[:, c, :], in_=PB[c][:, :]), [PB[c]], [xsT])
                    else:
                        S.op("dve", lambda c=c: dve.tensor_copy(out=xsT[:, c, :], in_=PB[c][:, :]), [PB[c]], [xsT])

                def prep(j):
                    osg, blk = j // 4, j % 4
                    col = e * 4 + osg
                    G_ = Gj[j % 2]
                    S.op("dve", lambda: dve.tensor_scalar(out=G_[:, :], in0=iot[:, :], scalar1=posT[:, blk, col:col + 1], scalar2=gateT[:, blk, col:col + 1], op0=ALU.is_equal, op1=ALU.mult), [iot, posT, gateT], [G_])
                    pt = PB[6 + j % 2]
                    ptb = pt[:, :].bitcast(BF16)
                    for sc in range(4):
                        S.op("pe", lambda sc=sc: pe.transpose(out=ptb[:, sc * 128:(sc + 1) * 128], in_=G_[:, sc * 128:(sc + 1) * 128], identity=idb[:, :]), [G_, idb], [pt])
                    S.op("act", lambda: act.copy(out=GTa[:, j, :, :], in_=ptb[:, 0:512].rearrange("p (s t) -> p s t", s=4)), [pt], [GTt[j]])

                jn = 0
                for g in (range(11) if PH & 2 else []):
                    gb = ng % 2
                    ng += 1
                    sg_st = stage_in(w_gate[e, g], 2048)
                    su_st = stage_in(w_up[e, g], 2048)
                    GUX = int(os.environ.get("GUX", "0"))
                    if GUX < 2:
                        S.op("act", lambda gb=gb, sg_st=sg_st: act.copy(out=wgb[gb][:, :, :].rearrange("p c f -> p (c f)"), in_=sg_st[:, :]), [sg_st], [wgb[gb]])
                        S.op("dve", lambda gb=gb, su_st=su_st: dve.tensor_copy(out=wub[gb][:, :, :].rearrange("p c f -> p (c f)"), in_=su_st[:, :]), [su_st], [wub[gb]])
                    for fl in (range(2) if GUX == 0 else []):
                        fc = g * 2 + fl
                        pg, pu = PB[(fc % 2) * 2], PB[(fc % 2) * 2 + 1]
                        for c in range(8):
                            S.op("pe", lambda c=c, pg=pg, gb=gb, fl=fl: pe.matmul(pg[:, :], lhsT=wgb[gb][:, c, fl * 128:(fl + 1) * 128], rhs=xsT[:, c, :], start=(c == 0), stop=(c == 7)), [wgb[gb], xsT], [pg])
                        for c in range(8):
                            S.op("pe", lambda c=c, pu=pu, gb=gb, fl=fl: pe.matmul(pu[:, :], lhsT=wub[gb][:, c, fl * 128:(fl + 1) * 128], rhs=xsT[:, c, :], start=(c == 0), stop=(c == 7)), [wub[gb], xsT], [pu])
                        sg_ = sgt[fc % 2]
                        S.op("act", lambda pg=pg, sg_=sg_: act.activation(out=sg_[:, :], in_=pg[:, :], func=AF.Silu), [pg], [sg_])
                        S.op("dve", lambda pu=pu, sg_=sg_, fc=fc: dve.tensor_tensor(out=hidT[:, fc, :], in0=sg_[:, :], in1=pu[:, :], op=ALU.mult), [sg_, pu], [hidT])
                    for _ in range(2 if g < 5 else 1):
                        if jn < 16:
                            prep(jn)
                            jn += 1
                while jn < 16 and (PH & 8):
                    prep(jn)
                    jn += 1
                pieces = ((0, 8), (8, 16), (16, NFC))
                for cb in (range(4) if PH & 4 else []):
                    for k, (f0, f1) in enumerate(pieces):
                        st = stage_in(w_down[e, cb, :, f0 * 256:f1 * 256], (f1 - f0) * 256)
                        S.op("act", lambda st=st, f0=f0, f1=f1: act.copy(out=wdb[:, f0:f1, :].rearrange("p c n -> p (c n)"), in_=st[:, 0:(f1 - f0) * 256]), [st], [wdbT[k]])
                    for k, (f0, f1) in enumerate(pieces):
                        for fc in range(f0, f1):
                            for sc in range(4):
                                S.op("pe", lambda fc=fc, sc=sc: pe.matmul(PB[4 + sc][:, 0:256], lhsT=hidT[:, fc, sc * 128:(sc + 1) * 128], rhs=wdb[:, fc, :], start=(fc == 0), stop=(fc == NFC - 1)), [hidT, wdbT[k]], [PB[4 + sc]])
                    for sc in range(4):
                        if sc % 2 == 0:
                            S.op("act", lambda sc=sc, cb=cb: act.copy(out=ysb[:, sc, cb * 256:(cb + 1) * 256], in_=PB[4 + sc][:, 0:256]), [PB[4 + sc]], [ysb])
                        else:
                            S.op("dve", lambda sc=sc, cb=cb: dve.tensor_copy(out=ysb[:, sc, cb * 256:(cb + 1) * 256], in_=PB[4 + sc][:, 0:256]), [PB[4 + sc]], [ysb])
                for j in (range(16) if PH & 8 else []):
                    for hlf in range(2):
                        pyc = PB[(j % 2) * 2 + hlf]
                        for sc in range(4):
                            S.op("pe", lambda sc=sc, pyc=pyc, hlf=hlf, j=j: pe.matmul(pyc[:, :], lhsT=GTa[:, j, sc, :], rhs=ysb[:, sc, hlf * 512:(hlf + 1) * 512], start=(sc == 0), stop=(sc == 3)), [GTt[j], ysb], [pyc])
                        S.op("dve", lambda pyc=pyc, j=j, hlf=hlf: dve.tensor_tensor(out=yacc[:, j, hlf * 512:(hlf + 1) * 512], in0=yacc[:, j, hlf * 512:(hlf + 1) * 512], in1=pyc[:, :], op=ALU.add), [yacc, pyc], [yacc])
            S.barrier()
            e3.close()
            gF = K.sb(es, [128, D], F32, "gF")
            S.dma("sp", gF[:, :], mods_d[:, 7, :], reads=[T_mods], writes=[gF])
            xno = [K.sb(es, [128, D], F32, "xno%d" % i) for i in range(2)]
            ob = [K.sb(es, [128, D], F32, "ob%d" % i) for i in range(2)]
            junk = K.sb(es, [128, D], BF16, "junkg")
            ss = K.sb(es, [128, 1], F32, "ssg")
            rstd = K.sb(es, [128, 1], F32, "rstdg")
            for j in range(16):
                b = j % 2
                S.dma("pool", None, None, reads=[T_XN, oi], writes=[xno[b]], fn=lambda b=b, j=j: pool.indirect_dma_start(
                    out=xno[b][:, :], out_offset=None, in_=XN_d, in_offset=bass.IndirectOffsetOnAxis(ap=oi[:, j:j + 1], axis=0)))
                S.op("act", lambda j=j: act.activation(out=junk[:, :], in_=yacc[:, j, :], func=AF.Square, accum_out=ss[:, :]), [yacc], [junk, ss])
                S.op("act", lambda: act.activation(out=rstd[:, :], in_=ss[:, :], func=AF.Sqrt, scale=1.0 / D, bias=epsb[:, :]), [ss, epsb], [rstd])
                S.op("dve", lambda: dve.reciprocal(out=rstd[:, :], in_=rstd[:, :]), [rstd], [rstd])
                S.op("dve", lambda j=j, b=b: dve.scalar_tensor_tensor(out=ob[b][:, :], in0=yacc[:, j, :], scalar=rstd[:, 0:1], in1=gF[:, :], op0=ALU.mult, op1=ALU.mult), [yacc, rstd, gF], [ob[b]])
                S.op("pool", lambda b=b: pool.tensor_tensor(out=ob[b][:, :], in0=ob[b][:, :], in1=xno[b][:, :], op=ALU.add), [ob[b], xno[b]], [ob[b]])
                S.dma("sp", out[j * 128:(j + 1) * 128, :], ob[b][:, :], reads=[ob[b]])

    stages = [("0", stage0), ("A", stageA), ("B", stageB), ("C", stageC), ("D", stageD), ("E", stageEFG)]
    if upto in ("F", "G", "all"):
        stages[-1] = (upto, stageEFG)
    for name, fn in stages:
        fn()
        S.barrier()
        if upto == name:
            break
    S.finish()
    glob.close()
    return nc


def prep_inputs(inputs, cores=range(8), with_experts=True):
    f = lambda k: np.ascontiguousarray(np.asarray(inputs[k], dtype=np.float32))
    x, c, ctx, c_ctx = f("x"), f("c"), f("ctx"), f("c_ctx")
    consts = _consts()
    cos, sin = _rope_tables()
    rpb = f("na_rpb")[0]
    nab = np.ascontiguousarray(_na_bias_tables(rpb).reshape(5, 8, 128, 640))
    a_up = f("gla_a_up")[0]
    a_bias = f("gla_a_bias")[0]
    aup_bd = np.zeros((32, 512), np.float32)
    aup_bd[0:16, 0:256] = a_up[0]
    aup_bd[16:32, 256:512] = a_up[1]
    abias = np.ascontiguousarray(a_bias.reshape(1, 512))
    norms = np.ascontiguousarray(np.stack([f("norm_mix_pre")[0], f("norm_mix_post")[0], f("norm_ffn_pre")[0], f("norm_ffn_post")[0]]))
    shared = dict(
        w_mod=f("w_mod")[0], b_mod=f("b_mod"), norms=norms, gla_norm=f("gla_norm"), w_in=f("w_in")[0],
        aup_bd=aup_bd, abias=abias, rope_cos=cos, rope_sin=sin, na_bias=nab, w_out=f("w_out")[0],
        router=f("router")[0], **consts)
    if with_experts:
        tile_gu = lambda w: np.ascontiguousarray(w.reshape(NE, 8, 128, 11, 256).transpose(0, 3, 2, 1, 4)).reshape(NE, 11, 128, 8 * 256)
        shared.update(w_gate=tile_gu(f("w_gate")[0]), w_up=tile_gu(f("w_up")[0]),
                      w_down=np.ascontiguousarray(f("w_down")[0].reshape(NE, NFC, 128, 4, 256).transpose(0, 3, 2, 1, 4)).reshape(NE, 4, 128, NFC * 256))
    maps = []
    pp = np.arange(128)
    for core in cores:
        s, p = core // 2, core % 2
        m = dict(shared)
        m["xc"] = np.ascontiguousarray(np.concatenate([ctx[s], x[s]], axis=0))
        cv = np.zeros((128, 16), np.float32)
        cv[:, 0:8] = c[s].reshape(8, 128).T
        cv[:, 8:16] = c_ctx.reshape(8, 128).T
        m["cvec"] = cv
        m["own_idx"] = np.ascontiguousarray((p * 2048 + np.arange(16)[None, :] * 128 + pp[:, None]).astype(np.int32))
        seg = pp % 8
        m["own_flag"] = ((seg // 4) == p).astype(np.float32).reshape(128, 1)
        selm = np.zeros((128, 64), np.float32)
        for e in range(16):
            for os_ in range(4):
                selm[e * 8 + p * 4 + os_, e * 4 + os_] = 1.0
        m["sel"] = selm
        maps.append(m)
    return maps


_PROGRAM = None


def kernel(**inputs):
    global _PROGRAM
    if _PROGRAM is None:
        _PROGRAM = build_program()
    maps = prep_inputs(inputs)
    res = run_bass_kernel_spmd(_PROGRAM, maps, core_ids=list(range(8)))
    out = np.zeros((4, SEQ, D), np.float32)
    for core in range(8):
        s, p = core // 2, core % 2
        out[s, p * 2048:(p + 1) * 2048] = res.results[core]["out"]
    return out
```

```python
import contextlib
import numpy as np
import ml_dtypes
import concourse.bass as bass
import concourse.mybir as mybir
from concourse.bass_utils import run_bass_kernel_spmd

F32 = mybir.dt.float32
BF16 = mybir.dt.bfloat16
I32 = mybir.dt.int32
AF = mybir.ActivationFunctionType
ALU = mybir.AluOpType

D = 1024
SEQ = 4096
CTX = 256
NTOK = SEQ + CTX
NT = NTOK // 128
INC = 3104
NE = 16
DE = 2816
NFC = DE // 128
CAP = 512
EPS = 1e-6
NEG = -30000.0


class T:
    __slots__ = ("ap", "w", "r", "name")

    def __init__(self, ap, name=""):
        self.ap = ap
        self.w = None
        self.r = {}
        self.name = name

    def __getitem__(self, idx):
        return self.ap[idx]


class Sync:
    def __init__(self, nc, n_sp=20, n_pool=8, n_act=6):
        self.nc = nc
        self.E = {"pe": nc.tensor, "act": nc.scalar, "dve": nc.vector, "pool": nc.gpsimd, "sp": nc.sync}
        self.sem = {}
        self.cnt = {}
        for k in ("pe", "act", "dve", "pool"):
            self.sem[k] = nc.alloc_semaphore("s_" + k)
            self.cnt[k] = 0
        self.seen = {k: {} for k in self.E}
        self.dq = {}
        for q, n in (("sp", n_sp), ("pool", n_pool), ("act", n_act)):
            lst = []
            for i in range(n):
                key = "d_%s%d" % (q, i)
                self.sem[key] = nc.alloc_semaphore(key)
                self.cnt[key] = 0
                lst.append(key)
            self.dq[q] = [lst, 0]

    def _wait(self, ek, key, val):
        if val <= 0:
            return
        if self.seen[ek].get(key, 0) >= val:
            return
        self.E[ek].wait_ge(self.sem[key], val)
        self.seen[ek][key] = val

    def _deps(self, ek, reads, writes):
        need = {}
        for t in reads:
            if t.w is not None:
                k, v = t.w
                need[k] = max(need.get(k, 0), v)
        for t in writes:
            if t.w is not None:
                k, v = t.w
                if not (k == "pe" and ek == "pe"):
                    need[k] = max(need.get(k, 0), v)
            for k, v in t.r.items():
                if not (k == "pe" and ek == "pe"):
                    need[k] = max(need.get(k, 0), v)
        for k, v in need.items():
            self._wait(ek, k, v)

    def op(self, ek, fn, reads=(), writes=()):
        self._deps(ek, reads, writes)
        ins = fn()
        self.cnt[ek] += 1
        ins.then_inc(self.sem[ek], 1)
        me = (ek, self.cnt[ek])
        for t in reads:
            t.r[ek] = self.cnt[ek]
        for t in writes:
            t.w = me
            t.r = {}
        return ins

    def dma(self, q, out, in_, reads=(), writes=(), fn=None, **kw):
        lst, i = self.dq[q]
        key = lst[i % len(lst)]
        self.dq[q][1] = i + 1
        self._wait(q, key, self.cnt[key])
        self._deps(q, reads, writes)
        if fn is None:
            ins = self.E[q].dma_start(out=out, in_=in_, **kw)
        else:
            ins = fn()
        self.cnt[key] += 16
        ins.then_inc(self.sem[key], 16)
        for t in reads:
            t.r[key] = self.cnt[key]
        for t in writes:
            t.w = (key, self.cnt[key])
            t.r = {}
        return ins

    def barrier(self):
        for ek in self.E:
            for k, v in self.cnt.items():
                if not (k == ek == "pe"):
                    self._wait(ek, k, v)

    def finish(self):
        for k, v in self.cnt.items():
            self._wait("sp", k, v)


class Ctx:
    def __init__(self, nc):
        self.nc = nc
        self.S = Sync(nc)
        self.uid = 0

    def sb(self, es, shape, dt, name):
        self.uid += 1
        return T(es.enter_context(self.nc.sbuf_tensor("%s_%d" % (name, self.uid), list(shape), dt)), name)

    def ps(self, es, shape, dt, name):
        self.uid += 1
        return T(es.enter_context(self.nc.psum_tensor("%s_%d" % (name, self.uid), list(shape), dt)), name)


def _rope_tables():
    half = 16
    freqs = (np.float32(10000.0) ** (-np.arange(half, dtype=np.float32) / np.float32(half))).astype(np.float32)
    t = np.arange(SEQ)
    pr = (t // 64).astype(np.float32)[:, None] * freqs
    pc = (t % 64).astype(np.float32)[:, None] * freqs
    ang = np.concatenate([pr, pc], axis=1).astype(np.float32)
    return np.cos(ang).astype(np.float32), np.sin(ang).astype(np.float32)


def _na_key_tile0(T_):
    return min(max(T_ - 2, 0), 27)


def _na_bias_tables(rpb):
    out = np.full((5, 8, 128, 5, 128), NEG, np.float32)
    for si, T_ in enumerate((0, 1, 2, 30, 31)):
        kt0 = _na_key_tile0(T_)
        for qi in range(128):
            r = 2 * T_ + qi // 64
            qc = qi % 64
            rs = min(max(r - 4, 0), 56)
            cs = min(max(qc - 8, 0), 48)
            for kr in range(rs, rs + 8):
                kt = kr // 2 - kt0
                assert 0 <= kt < 5
                dr = kr - r + 7
                kc = np.arange(cs, cs + 16)
                dc = np.clip(kc - qc + 15, 0, 30)
                j = (kr % 2) * 64 + kc
                out[si, :, j, kt, qi] = rpb[:, dr, dc].T
    return out


def _consts():
    c = {}
    c["ident_f"] = np.eye(128, dtype=np.float32)
    c["ident_b"] = np.eye(128, dtype=np.float32).astype(ml_dtypes.bfloat16)
    tt = np.arange(128)
    c["tri_f"] = np.where(tt[:, None] <= tt[None, :], -1.0 / 16.0, 0.0).astype(np.float32)
    c["tri_b"] = np.where(tt[:, None] >= tt[None, :], -1.0 / 16.0, 0.0).astype(np.float32)
    c["mask_f"] = (tt[:, None] <= tt[None, :]).astype(np.float32)
    c["mask_b"] = (tt[:, None] >= tt[None, :]).astype(np.float32)
    c["ones_col"] = np.full((128, 1), -1.0 / 16.0, np.float32)
    c["ones_row"] = np.ones((1, 128), np.float32)
    g = tt // 8
    c["blk_ones"] = (g[:, None] == g[None, :]).astype(np.float32)
    c["blk_lt"] = ((g[:, None] == g[None, :]) & (tt[:, None] < tt[None, :])).astype(np.float32)
    c["iota512"] = np.broadcast_to(np.arange(512, dtype=np.float32)[None, :], (128, 512)).copy()
    own = np.arange(16)[None, :] * 128 + np.arange(128)[:, None]
    c["tid"] = np.stack([own // 64, own % 64], axis=-1).astype(np.float32).astype(ml_dtypes.bfloat16)
    return c


def build_program(upto="all", debug=False):
    nc = bass.Bass("TRN2", target_bir_lowering=False)
    K = Ctx(nc)
    S = K.S
    pe, act, dve, pool = nc.tensor, nc.scalar, nc.vector, nc.gpsimd

    def din(name, shape, dt=F32):
        return nc.dram_tensor(name, list(shape), dt, kind="ExternalInput").ap()

    import os as _os
    _ext = set(_os.environ.get("EXTSET", "").split(","))

    def dscr(name, shape, dt=F32):
        return nc.dram_tensor(name, list(shape), dt, kind=("ExternalOutput" if (debug or name in _ext) else "Internal")).ap()

    xc = din("xc", [NTOK, D])
    cvec = din("cvec", [128, 16])
    w_mod = din("w_mod", [D, 6 * D])
    b_mod = din("b_mod", [1, 6 * D])
    norms = din("norms", [4, D])
    gla_norm = din("gla_norm", [1, 128])
    w_in = din("w_in", [D, INC])
    aup_bd = din("aup_bd", [32, 512])
    abias = din("abias", [1, 512])
    rope_cos = din("rope_cos", [SEQ, 32])
    rope_sin = din("rope_sin", [SEQ, 32])
    na_bias = din("na_bias", [5, 8, 128, 640])
    w_out = din("w_out", [D, D])
    router = din("router", [D, NE])
    if upto in ("F", "G", "all"):
        w_gate = din("w_gate", [NE, 11, 128, 8 * 256])
        w_up = din("w_up", [NE, 11, 128, 8 * 256])
        w_down = din("w_down", [NE, 4, 128, NFC * 256])
    ident_f = din("ident_f", [128, 128])
    ident_b = din("ident_b", [128, 128], BF16)
    tri_f = din("tri_f", [128, 128])
    tri_b = din("tri_b", [128, 128])
    mask_f = din("mask_f", [128, 128])
    mask_b = din("mask_b", [128, 128])
    ones_col = din("ones_col", [128, 1])
    ones_row = din("ones_row", [1, 128])
    blk_ones = din("blk_ones", [128, 128])
    blk_lt = din("blk_lt", [128, 128])
    iota512 = din("iota512", [128, 512])
    tid_in = din("tid", [128, 16, 2], BF16)
    own_idx = din("own_idx", [128, 16], I32)
    own_flag = din("own_flag", [128, 1])
    sel = din("sel", [128, 64])
    out = nc.dram_tensor("out", [SEQ // 2, D], F32, kind="ExternalOutput").ap()

    mods_d = dscr("mods_d", [128, 8, D])
    KT_d = dscr("KT_d", [512, NTOK], BF16)
    QT_d = dscr("QT_d", [512, SEQ], BF16)
    V_d = dscr("V_d", [NTOK, 512], BF16)
    GV_d = dscr("GV_d", [NTOK, 512], BF16)
    GKQ_d = dscr("GKQ_d", [NTOK, 544])
    GG_d = dscr("GG_d", [SEQ, 512])
    OF_d = dscr("OF_d", [SEQ, 512])
    MIX_d = dscr("MIX_d", [SEQ, D], BF16)
    XN_d = dscr("XN_d", [SEQ, D])
    HF_d = dscr("HF_d", [SEQ, D], BF16)
    AFFT_d = dscr("AFFT_d", [NE, SEQ])

    glob = contextlib.ExitStack()
    idf = K.sb(glob, [128, 128], F32, "idf")
    idb = K.sb(glob, [128, 128], BF16, "idb")
    onesr = K.sb(glob, [1, 128], F32, "onesr")
    S.dma("sp", idf[:, :], ident_f, writes=[idf])
    S.dma("sp", idb[:, :], ident_b, writes=[idb])
    S.dma("sp", onesr[:, :], ones_row, writes=[onesr])

    def stage0():
        with contextlib.ExitStack() as es:
            cv = K.sb(es, [128, 16], F32, "cv")
            csil = K.sb(es, [128, 16], F32, "csil")
            crep = K.sb(es, [128, 16, 128], F32, "crep")
            bm = K.sb(es, [1, 6 * D], F32, "bm")
            modA = K.sb(es, [128, 6 * D], F32, "modA")
            modC = K.sb(es, [128, 2 * D], F32, "modC")
            nbc = K.sb(es, [128, 4, D], F32, "nbc")
            wm = [K.sb(es, [128, 8, 512], F32, "wm%d" % i) for i in range(2)]
            mods = K.sb(es, [128, 8, D], F32, "mods")
            psA = [K.ps(es, [128, 512], F32, "psA%d" % i) for i in range(2)]
            psC = [K.ps(es, [128, 512], F32, "psC%d" % i) for i in range(2)]
            S.dma("sp", cv[:, :], cvec, writes=[cv])
            S.dma("sp", bm[:, :], b_mod, writes=[bm])
            S.dma("sp", nbc[:, :, :], norms.partition_broadcast(128), writes=[nbc])
            S.op("act", lambda: act.activation(out=csil[:, :], in_=cv[:, :], func=AF.Silu), [cv], [csil])
            S.op("dve", lambda: dve.tensor_copy(out=crep[:, :, :], in_=csil[:, :].unsqueeze(2).broadcast_to([128, 16, 128])), [csil], [crep])
            wmv = w_mod.rearrange("(c p) n -> p c n", p=128)
            for nb in range(12):
                w = wm[nb % 2]
                S.dma("sp", w[:, :, :], wmv[:, :, nb * 512:(nb + 1) * 512], writes=[w])
                for which in range(2 if nb < 4 else 1):
                    ps = (psA, psC)[which][nb % 2]
                    for c in range(8):
                        S.op("pe", lambda c=c, ps=ps, w=w, which=which: pe.matmul(ps[:, :], lhsT=crep[:, which * 8 + c, :], rhs=w[:, c, :], start=(c == 0), stop=False), [crep, w], [ps])
                    S.op("pe", lambda ps=ps, nb=nb: pe.matmul(ps[:, :], lhsT=onesr[:, :], rhs=bm[:, nb * 512:(nb + 1) * 512], start=False, stop=True), [onesr, bm], [ps])
                    dst = (modA, modC)[which]
                    if which == 0:
                        S.op("act", lambda ps=ps, dst=dst, nb=nb: act.copy(out=dst[:, nb * 512:(nb + 1) * 512], in_=ps[:, :]), [ps], [dst])
                    else:
                        S.op("dve", lambda ps=ps, dst=dst, nb=nb: dve.tensor_copy(out=dst[:, nb * 512:(nb + 1) * 512], in_=ps[:, :]), [ps], [dst])
            import os
            if os.environ.get("STOP0") == "1":
                S.dma("sp", mods_d[:, 0:6, :], modA[:, :].rearrange("p (a b) -> p a b", a=6), reads=[modA], writes=[T_mods])
                return
            sl = lambda t, i: t[:, i * D:(i + 1) * D]
            S.op("dve", lambda: dve.scalar_tensor_tensor(out=mods[:, 0, :], in0=sl(modA, 1), scalar=1.0, in1=nbc[:, 0, :], op0=ALU.add, op1=ALU.mult), [modA, nbc], [mods])
            S.op("dve", lambda: dve.tensor_copy(out=mods[:, 1, :], in_=sl(modA, 0)), [modA], [mods])
            S.op("dve", lambda: dve.scalar_tensor_tensor(out=mods[:, 2, :], in0=sl(modC, 1), scalar=1.0, in1=nbc[:, 0, :], op0=ALU.add, op1=ALU.mult), [modC, nbc], [mods])
            S.op("dve", lambda: dve.tensor_copy(out=mods[:, 3, :], in_=sl(modC, 0)), [modC], [mods])
            S.op("dve", lambda: dve.tensor_tensor(out=mods[:, 4, :], in0=sl(modA, 2), in1=nbc[:, 1, :], op=ALU.mult), [modA, nbc], [mods])
            S.op("dve", lambda: dve.scalar_tensor_tensor(out=mods[:, 5, :], in0=sl(modA, 4), scalar=1.0, in1=nbc[:, 2, :], op0=ALU.add, op1=ALU.mult), [modA, nbc], [mods])
            S.op("dve", lambda: dve.tensor_copy(out=mods[:, 6, :], in_=sl(modA, 3)), [modA], [mods])
            S.op("dve", lambda: dve.tensor_tensor(out=mods[:, 7, :], in0=sl(modA, 5), in1=nbc[:, 3, :], op=ALU.mult), [modA, nbc], [mods])
            S.dma("sp", mods_d, mods[:, :, :], reads=[mods], writes=[T_mods])

    T_mods = T(None, "mods_d")
    T_KT, T_QT, T_V, T_GV, T_GKQ, T_GG = (T(None, n) for n in ("KT", "QT", "V", "GV", "GKQ", "GG"))

    def rms_rstd(es_tiles, src, src_t, ss, rstd, junk):
        S.op("act", lambda: act.activation(out=junk[:, :], in_=src, func=AF.Square, accum_out=ss[:, :]), [src_t], [junk, ss])
        S.op("act", lambda: act.activation(out=rstd[:, :], in_=ss[:, :], func=AF.Sqrt, scale=1.0 / D, bias=epsb[:, :]), [ss, epsb], [rstd])
        S.op("dve", lambda: dve.reciprocal(out=rstd[:, :], in_=rstd[:, :]), [rstd], [rstd])

    epsb = K.sb(glob, [128, 1], F32, "epsb")
    S.op("pool", lambda: pool.memset(epsb[:, :], EPS), [], [epsb])

    def stageA():
        with contextlib.ExitStack() as es:
            win = K.sb(es, [128, 8, INC], BF16, "win")
            wst = [K.sb(es, [128, 8, 512], F32, "wst%d" % i) for i in range(2)]
            gs = K.sb(es, [128, 4, D], F32, "gs")
            S.dma("sp", gs[:, :, :], mods_d[:, 0:4, :], reads=[T_mods], writes=[gs])
            wiv = w_in.rearrange("(c p) n -> p c n", p=128)
            for b in range(7):
                c0 = b * 512
                w_ = min(512, INC - c0)
                st = wst[b % 2]
                S.dma("sp", st[:, :, 0:w_], wiv[:, :, c0:c0 + w_], writes=[st])
                if b % 2 == 0:
                    S.op("act", lambda st=st, c0=c0, w_=w_: act.copy(out=win[:, :, c0:c0 + w_], in_=st[:, :, 0:w_]), [st], [win])
                else:
                    S.op("dve", lambda st=st, c0=c0, w_=w_: dve.tensor_copy(out=win[:, :, c0:c0 + w_], in_=st[:, :, 0:w_]), [st], [win])
            import os
            STOPA = int(os.environ.get("STOPA", "99"))
            if STOPA <= 1:
                return
            xt = [K.sb(es, [128, D], F32, "xt%d" % i) for i in range(2)]
            junk = K.sb(es, [128, D], BF16, "junk")
            ss = [K.sb(es, [128, 1], F32, "ss%d" % i) for i in range(2)]
            rstd = [K.sb(es, [128, 1], F32, "rstd%d" % i) for i in range(2)]
            h1 = [K.sb(es, [128, D], F32, "h1_%d" % i) for i in range(2)]
            hb = [K.sb(es, [128, D], BF16, "hb%d" % i) for i in range(2)]
            hT = [K.sb(es, [128, 8, 512], BF16, "hT%d" % i) for i in range(2)]
            ktq = [K.sb(es, [128, 512], BF16, "ktq%d" % i) for i in range(2)]
            vo = [K.sb(es, [128, 512], BF16, "vo%d" % i) for i in range(2)]
            gvo = [K.sb(es, [128, 512], BF16, "gvo%d" % i) for i in range(2)]
            gkq = [K.sb(es, [128, 544], F32, "gkq%d" % i) for i in range(2)]
            ggo = [K.sb(es, [128, 512], F32, "ggo%d" % i) for i in range(2)]
            pst = [K.ps(es, [128, 8 * 128], BF16, "pst%d" % i) for i in range(2)]
            psp = [K.ps(es, [128, 512], F32, "psp%d" % i) for i in range(4)]
            npp = [0]

            def nextps():
                npp[0] += 1
                return psp[npp[0] % 4]

            ev = [0]

            def evac(dst_t, dst_ap, ps, ps_ap):
                if psp.index(ps) % 2 == 0:
                    S.op("act", lambda: act.copy(out=dst_ap, in_=ps_ap), [ps], [dst_t])
                else:
                    S.op("dve", lambda: dve.tensor_copy(out=dst_ap, in_=ps_ap), [ps], [dst_t])

            groups = [(0, 2)] + [(2 + 4 * g, 4) for g in range(8)]
            ti_glob = 0
            for gi, (t0, ntl) in enumerate(groups):
                is_ctx = gi == 0
                hTg = hT[gi % 2]
                for k in range(ntl):
                    ti = t0 + k
                    b2 = ti_glob % 2
                    ti_glob += 1
                    x_ = xt[b2]
                    S.dma("sp", x_[:, :], xc[ti * 128:(ti + 1) * 128, :], writes=[x_])
                    rms_rstd(None, x_[:, :], x_, ss[b2], rstd[b2], junk)
                    go = 2 if is_ctx else 0
                    h_ = h1[b2]
                    S.op("dve", lambda x_=x_, h_=h_, b2=b2, go=go: dve.scalar_tensor_tensor(out=h_[:, :], in0=x_[:, :], scalar=rstd[b2][:, 0:1], in1=gs[:, go, :], op0=ALU.mult, op1=ALU.mult), [x_, rstd[b2], gs], [h_])
                    S.op("pool", lambda h_=h_, b2=b2, go=go: pool.tensor_tensor(out=hb[b2][:, :], in0=h_[:, :], in1=gs[:, go + 1, :], op=ALU.add), [h_, gs], [hb[b2]])
                    pt = pst[b2]
                    for c in range(8):
                        S.op("pe", lambda c=c, pt=pt, b2=b2: pe.transpose(out=pt[:, c * 128:(c + 1) * 128], in_=hb[b2][:, c * 128:(c + 1) * 128], identity=idb[:, :]), [hb[b2], idb], [pt])
                    S.op("act", lambda pt=pt, hTg=hTg, k=k: act.copy(out=hTg[:, :, k * 128:(k + 1) * 128], in_=pt[:, :].rearrange("p (c t) -> p c t", c=8)), [pt], [hTg])
                ntok = ntl * 128
                tok0 = t0 * 128
                if STOPA <= 2:
                    return
                for which, c0 in ((0, 0), (1, 1824)):
                    if which == 1 and is_ctx:
                        continue
                    for fb in range(4):
                        ps = nextps()
                        for c in range(8):
                            S.op("pe", lambda c=c, ps=ps, fb=fb, c0=c0: pe.matmul(ps[:, 0:ntok], lhsT=win[:, c, c0 + fb * 128:c0 + (fb + 1) * 128], rhs=hTg[:, c, 0:ntok], start=(c == 0), stop=(c == 7)), [win, hTg], [ps])
                        kb = ktq[(which * 4 + fb) % 2]
                        evac(kb, kb[:, 0:ntok], ps, ps[:, 0:ntok])
                        if which == 0:
                            S.dma("sp", KT_d[fb * 128:(fb + 1) * 128, tok0:tok0 + ntok], kb[:, 0:ntok], reads=[kb], writes=[T_KT])
                        else:
                            S.dma("sp", QT_d[fb * 128:(fb + 1) * 128, tok0 - CTX:tok0 - CTX + ntok], kb[:, 0:ntok], reads=[kb], writes=[T_QT])
                if STOPA <= 3:
                    return
                for k in range(ntl):
                    ti = t0 + k
                    b2 = ti % 2
                    r0 = ti * 128

                    def mm(c0, w_, k=k):
                        ps = nextps()
                        for c in range(8):
                            S.op("pe", lambda c=c, ps=ps: pe.matmul(ps[:, 0:w_], lhsT=hTg[:, c, k * 128:(k + 1) * 128], rhs=win[:, c, c0:c0 + w_], start=(c == 0), stop=(c == 7)), [win, hTg], [ps])
                        return ps

                    ps = mm(512, 512)
                    evac(vo[b2], vo[b2][:, :], ps, ps[:, :])
                    S.dma("sp", V_d[r0:r0 + 128, :], vo[b2][:, :], reads=[vo[b2]], writes=[T_V])
                    TM = int(os.environ.get("TM", "9"))
                    if TM <= 1:
                        continue
                    ps = mm(1024, 512)
                    EV = int(os.environ.get("EV", "3"))
                    if EV & 1:
                        evac(gkq[b2], gkq[b2][:, 0:256], ps, ps[:, 0:256])
                    if EV & 2:
                        evac(gvo[b2], gvo[b2][:, 0:256], ps, ps[:, 256:512])
                    if TM <= 2:
                        continue
                    ps = mm(1536, 288)
                    evac(gvo[b2], gvo[b2][:, 256:512], ps, ps[:, 0:256])
                    evac(gkq[b2], gkq[b2][:, 256:288], ps, ps[:, 256:288])
                    S.dma("sp", GV_d[r0:r0 + 128, :], gvo[b2][:, :], reads=[gvo[b2]], writes=[T_GV])
                    if TM <= 3:
                        continue
                    if not is_ctx:
                        ps = mm(2336, 512)
                        evac(gkq[b2], gkq[b2][:, 288:544], ps, ps[:, 0:256])
                        evac(ggo[b2], ggo[b2][:, 0:256], ps, ps[:, 256:512])
                        ps = mm(2848, 256)
                        evac(ggo[b2], ggo[b2][:, 256:512], ps, ps[:, 0:256])
                        S.dma("sp", GG_d[r0 - CTX:r0 - CTX + 128, :], ggo[b2][:, :], reads=[ggo[b2]], writes=[T_GG])
                        S.dma("sp", GKQ_d[r0:r0 + 128, :], gkq[b2][:, :], reads=[gkq[b2]], writes=[T_GKQ])
                    else:
                        S.dma("sp", GKQ_d[r0:r0 + 128, 0:288], gkq[b2][:, 0:288], reads=[gkq[b2]], writes=[T_GKQ])
                if STOPA <= 4:
                    return


    T_MIXna = T(None, "MIXna")

    def stageB():
        with contextlib.ExitStack() as es:
            NB = 3
            qT = [K.sb(es, [128, 4, 128], BF16, "qT%d" % i) for i in range(2)]
            kT = [K.sb(es, [128, 4, 896], BF16, "kT%d" % i) for i in range(2)]
            va = [K.sb(es, [128, 7, 8, 65], BF16, "va%d" % i) for i in range(2)]
            bint = K.sb(es, [128, 8, 640], F32, "bint")
            bt = [K.sb(es, [128, 640], F32, "bt%d" % i) for i in range(3)]
            tmp = [K.sb(es, [128, 896], F32, "tmp%d" % i) for i in range(NB)]
            pT = [K.sb(es, [128, 896], BF16, "pT%d" % i) for i in range(NB + 1)]
            ona = [K.sb(es, [128, 512], BF16, "ona%d" % i) for i in range(2)]
            rec = [K.sb(es, [128, 1], F32, "rec%d" % i) for i in range(2)]
            sT = [K.ps(es, [128, 1024], F32, "sT%d" % i) for i in range(NB)]
            pv = [K.ps(es, [128, 512], F32, "pv%d" % i) for i in range(2)]
            for v in va:
                S.op("pool", lambda v=v: pool.memset(v[:, :, :, :], 1.0), [], [v])
            S.dma("sp", bint[:, :, :], na_bias[2].rearrange("h j c -> j h c"), writes=[bint])
            QTv = QT_d.rearrange("(c p) t -> p c t", p=128)
            KTv = KT_d.rearrange("(c p) t -> p c t", p=128)
            nbt = [0]
            tmR = [[T(None, "tmR") for _ in range(3)] for _ in range(NB)]
            onaR = [[T(None, "onaR") for _ in range(8)] for _ in range(2)]

            def loads(T_):
                b = T_ % 2
                kt0 = _na_key_tile0(T_)
                S.dma("sp", qT[b][:, :, :], QTv[:, :, T_ * 128:(T_ + 1) * 128], reads=[T_QT], writes=[qT[b]])
                S.dma("sp", kT[b][:, :, 0:640], KTv[:, :, CTX + kt0 * 128:CTX + kt0 * 128 + 640], reads=[T_KT], writes=[kT[b]])
                S.dma("sp", kT[b][:, :, 640:896], KTv[:, :, 0:CTX], reads=[T_KT], writes=[kT[b]])
                r0 = CTX + kt0 * 128
                for kk in range(5):
                    S.dma("sp", va[b][:, kk, :, 0:64], V_d[r0 + kk * 128:r0 + (kk + 1) * 128, :].rearrange("t (h d) -> t h d", h=8), reads=[T_V], writes=[va[b]])
                for kk in range(2):
                    S.dma("sp", va[b][:, 5 + kk, :, 0:64], V_d[kk * 128:(kk + 1) * 128, :].rearrange("t (h d) -> t h d", h=8), reads=[T_V], writes=[va[b]])

            def phase1(n):
                T_, h = n // 8, n % 8
                b = T_ % 2
                if h == 0:
                    loads(T_)
                si = {0: 0, 1: 1, 30: 3, 31: 4}.get(T_, 2)
                pr, P0 = h // 2, (h % 2) * 64
                s_, tm, pt = sT[n % NB], tmp[n % NB], pT[n % (NB + 1)]
                if si == 2:
                    bb_t, bb = bint, bint[:, h, :]
                else:
                    bb_t = bt[nbt[0] % 3]
                    nbt[0] += 1
                    bb = bb_t[:, :]
                    S.dma("sp", bb, na_bias[si, h], writes=[bb_t])
                for kk in range(7):
                    S.op("pe", lambda kk=kk: pe.matmul(s_[:, kk * 128:(kk + 1) * 128], lhsT=kT[b][P0:P0 + 64, pr, kk * 128:(kk + 1) * 128], rhs=qT[b][P0:P0 + 64, pr, :], start=True, stop=True), [kT[b], qT[b]], [s_])
                ta, tb, tc = tmR[n % NB]
                S.op("dve", lambda: dve.scalar_tensor_tensor(out=tm[:, 0:512], in0=s_[:, 0:512], scalar=0.125, in1=bb[:, 0:512], op0=ALU.mult, op1=ALU.add), [s_, bb_t], [ta])
                S.op("dve", lambda: dve.scalar_tensor_tensor(out=tm[:, 512:640], in0=s_[:, 512:640], scalar=0.125, in1=bb[:, 512:640], op0=ALU.mult, op1=ALU.add), [s_, bb_t], [tb])
                S.op("dve", lambda: dve.tensor_scalar(out=tm[:, 640:896], in0=s_[:, 640:896], scalar1=0.125, scalar2=None, op0=ALU.mult), [s_], [tc])
                S.op("act", lambda: act.activation(out=pt[:, :], in_=tm[:, :], func=AF.Exp), [ta, tb, tc], [pt])

            def phase2(n):
                T_, h = n // 8, n % 8
                b = T_ % 2
                pt, p_, rc = pT[n % (NB + 1)], pv[n % 2], rec[n % 2]
                for kk in range(7):
                    S.op("pe", lambda kk=kk: pe.matmul(p_[:, 0:65], lhsT=pt[:, kk * 128:(kk + 1) * 128], rhs=va[b][:, kk, h, :], start=(kk == 0), stop=(kk == 6)), [pt, va[b]], [p_])
                S.op("dve", lambda: dve.reciprocal(out=rc[:, :], in_=p_[:, 64:65]), [p_], [rc])
                S.op("dve", lambda: dve.tensor_scalar(out=ona[b][:, h * 64:(h + 1) * 64], in0=p_[:, 0:64], scalar1=rc[:, 0:1], scalar2=None, op0=ALU.mult), [p_, rc], [onaR[b][h]])
                if h == 7:
                    S.dma("sp", MIX_d[T_ * 128:(T_ + 1) * 128, 0:512], ona[b][:, :], reads=onaR[b], writes=[T_MIXna])

            NIT = 32 * 8
            LOOK = NB - 1
            for n in range(min(LOOK, NIT)):
                phase1(n)
            for n in range(NIT):
                if n + LOOK < NIT:
                    phase1(n + LOOK)
                phase2(n)

    T_OF = T(None, "OF")
    T_MIXgla = T(None, "MIXgla")

    OB_d = dscr("OB_d", [SEQ, 512])
    T_OB = T(None, "OB")

    def stageC():
        with contextlib.ExitStack() as es:
            tri = [K.sb(es, [128, 128], F32, "tri%d" % i) for i in range(2)]
            msk = [K.sb(es, [128, 512], F32, "msk%d" % i) for i in range(2)]
            onec = K.sb(es, [128, 1], F32, "onec")
            aup = K.sb(es, [32, 512], F32, "aup")
            abi = K.sb(es, [1, 512], F32, "abi")
            gnb = K.sb(es, [128, 128], F32, "gnb")
            for t_, src in ((tri[0], tri_f), (tri[1], tri_b), (onec, ones_col), (aup, aup_bd), (abi, abias)):
                S.dma("sp", t_[:, :], src, writes=[t_])
            for h in range(4):
                S.dma("sp", msk[0][:, h * 128:(h + 1) * 128], mask_f, writes=[msk[0]])
                S.dma("sp", msk[1][:, h * 128:(h + 1) * 128], mask_b, writes=[msk[1]])
            S.dma("sp", gnb[:, :], gla_norm.partition_broadcast(128).rearrange("p a b -> p (a b)"), writes=[gnb])
            ps_ad = K.ps(es, [128, 512], F32, "ps_ad")
            ps_z = K.ps(es, [128, 512], F32, "ps_z")
            ps_lam = K.ps(es, [128, 512], F32, "ps_lam")
            ps_t = K.ps(es, [128, 1024], BF16, "ps_t")
            ps_AT = K.ps(es, [128, 512], F32, "ps_AT")
            ps_o = K.ps(es, [128, 512], F32, "ps_o")
            ps_U = K.ps(es, [128, 512], F32, "ps_U")

            class B:
                pass

            def alloc(ci):
                b = B()
                n_ = lambda x: "%s_c%d" % (x, ci)
                b.gkq = [K.sb(es, [128, 544], F32, n_("gkq%d" % i)) for i in range(2)]
                b.gv = [K.sb(es, [128, 512], BF16, n_("gv%d" % i)) for i in range(2)]
                b.cs = [K.sb(es, [128, 32], F32, n_("cs%d" % i)) for i in range(2)]
                b.sn = [K.sb(es, [128, 32], F32, n_("sn%d" % i)) for i in range(2)]
                b.adT = K.sb(es, [32, 128], F32, n_("adT"))
                b.ez = K.sb(es, [128, 256], F32, n_("ez"))
                b.lz = K.sb(es, [128, 256], F32, n_("lz"))
                b.E1 = K.sb(es, [128, 256], F32, n_("E1"))
                b.E2 = K.sb(es, [128, 256], F32, n_("E2"))
                b.at = K.sb(es, [128, 2], F32, n_("at"))
                b.kr = K.sb(es, [128, 256], F32, n_("kr"))
                b.qr = K.sb(es, [128, 256], F32, n_("qr"))
                b.rt = [K.sb(es, [128, 128], F32, n_("rt%d" % i)) for i in range(4)]
                b.qk = K.sb(es, [128, 512], BF16, n_("qk"))
                b.qTz = K.sb(es, [128, 4, 128], BF16, n_("qTz"))
                S.op("pool", lambda: pool.memset(b.qTz[:, :, :], 0.0), [], [b.qTz])
                b.kTc = K.sb(es, [128, 2, 128], BF16, n_("kTc"))
                b.ATs = K.sb(es, [128, 4, 128], BF16, n_("ATs"))
                b.Sst = [K.sb(es, [128, 128], F32, n_("Sst%d" % i)) for i in range(2)]
                b.Sbf = [K.sb(es, [128, 128], BF16, n_("Sbf%d" % i)) for i in range(2)]
                b.tU = K.sb(es, [128, 128], F32, n_("tU"))
                b.osb = [K.sb(es, [128, 512], F32, n_("osb%d" % i)) for i in range(2)]
                return b

            def rope(eng, ek, src_t, src_ap, dst_t, tmps, c_, s_):
                X = src_ap.rearrange("p (h a b f) -> p h a b f", h=4, a=2, b=2)
                O = dst_t[:, :].rearrange("p (h a b f) -> p h a b f", h=4, a=2, b=2)
                C = c_[:, :].rearrange("p (a f) -> p a f", a=2).unsqueeze(1).broadcast_to([128, 4, 2, 16])
                Sn = s_[:, :].rearrange("p (a f) -> p a f", a=2).unsqueeze(1).broadcast_to([128, 4, 2, 16])
                v = lambda t_: t_[:, :].rearrange("p (h a f) -> p h a f", h=4, a=2)
                X1, X2 = X[:, :, :, 0, :], X[:, :, :, 1, :]
                t1, t2, t3, t4 = tmps
                S.op(ek, lambda: eng.tensor_tensor(out=v(t1), in0=X1, in1=C, op=ALU.mult), [src_t, c_], [t1])
                S.op(ek, lambda: eng.tensor_tensor(out=v(t2), in0=X2, in1=Sn, op=ALU.mult), [src_t, s_], [t2])
                S.op(ek, lambda: eng.tensor_tensor(out=O[:, :, :, 0, :], in0=v(t1), in1=v(t2), op=ALU.subtract), [t1, t2], [dst_t])
                S.op(ek, lambda: eng.tensor_tensor(out=v(t3), in0=X1, in1=Sn, op=ALU.mult), [src_t, s_], [t3])
                S.op(ek, lambda: eng.tensor_tensor(out=v(t4), in0=X2, in1=C, op=ALU.mult), [src_t, c_], [t4])
                S.op(ek, lambda: eng.tensor_tensor(out=O[:, :, :, 1, :], in0=v(t3), in1=v(t4), op=ALU.add), [t3, t4], [dst_t])

            def chain(dr, c):
                for pr in range(2):
                    S.op("pool", lambda pr=pr: pool.memset(c.Sst[pr][:, :], 0.0), [], [c.Sst[pr]])
                    S.op("pool", lambda pr=pr: pool.memset(c.Sbf[pr][:, :], 0.0), [], [c.Sbf[pr]])
                order = list(range(NT)) if dr == 0 else [1, 0] + list(range(NT - 1, 1, -1))
                n = 0
                for ti in order:
                    lat = ti >= 2
                    li = ti - 2
                    b = n % 2
                    n += 1
                    g_, v_ = c.gkq[b], c.gv[b]
                    gw = 544 if lat else 288
                    S.dma("sp", g_[:, 0:gw], GKQ_d[ti * 128:(ti + 1) * 128, 0:gw], reads=[T_GKQ], writes=[g_])
                    S.dma("sp", v_[:, :], GV_d[ti * 128:(ti + 1) * 128, :], reads=[T_GV], writes=[v_])
                    if lat:
                        S.dma("sp", c.cs[b][:, :], rope_cos[li * 128:(li + 1) * 128, :], writes=[c.cs[b]])
                        S.dma("sp", c.sn[b][:, :], rope_sin[li * 128:(li + 1) * 128, :], writes=[c.sn[b]])
                    yield
                    S.op("pe", lambda: pe.transpose(out=ps_ad[0:32, 0:128], in_=g_[:, 256:288], identity=idf[:, :]), [g_, idf], [ps_ad])
                    S.op("act", lambda: act.copy(out=c.adT[:, :], in_=ps_ad[0:32, 0:128]), [ps_ad], [c.adT])
                    S.op("pe", lambda: pe.matmul(ps_z[:, 0:256], lhsT=c.adT[:, :], rhs=aup[:, dr * 256:(dr + 1) * 256], start=True, stop=False), [c.adT, aup], [ps_z])
                    S.op("pe", lambda: pe.matmul(ps_z[:, 0:256], lhsT=onesr[:, :], rhs=abi[:, dr * 256:(dr + 1) * 256], start=False, stop=True), [onesr, abi], [ps_z])
                    S.op("act", lambda: act.activation(out=c.ez[:, :], in_=ps_z[:, 0:256], func=AF.Exp, scale=-1.0), [ps_z], [c.ez])
                    yield
                    S.op("act", lambda: act.activation(out=c.lz[:, :], in_=c.ez[:, :], func=AF.Ln, bias=1.0), [c.ez], [c.lz])
                    S.op("pe", lambda: pe.matmul(ps_lam[:, 0:256], lhsT=tri[dr][:, :], rhs=c.lz[:, :], start=True, stop=True), [tri[dr], c.lz], [ps_lam])
                    for pr in range(2):
                        S.op("pe", lambda pr=pr: pe.matmul(ps_lam[:, 256 + pr:257 + pr], lhsT=c.lz[:, pr * 128:(pr + 1) * 128], rhs=onec[:, :], start=True, stop=True), [c.lz, onec], [ps_lam])
                    if lat:
                        S.op("act", lambda: act.activation(out=c.E1[:, :], in_=ps_lam[:, 0:256], func=AF.Exp), [ps_lam], [c.E1])
                    S.op("act", lambda: act.activation(out=c.E2[:, :], in_=ps_lam[:, 0:256], func=AF.Exp, scale=-1.0), [ps_lam], [c.E2])
                    S.op("act", lambda: act.activation(out=c.at[:, :], in_=ps_lam[:, 256:258], func=AF.Exp), [ps_lam], [c.at])
                    yield
                    if lat:
                        rope(dve, "dve", g_, g_[:, 0:256], c.kr, c.rt, c.cs[b], c.sn[b])
                        yield
                        rope(dve, "dve", g_, g_[:, 288:544], c.qr, c.rt, c.cs[b], c.sn[b])
                        yield
                        S.op("dve", lambda: dve.scalar_tensor_tensor(out=c.qk[:, 0:256], in0=c.qr[:, :], scalar=0.125, in1=c.E1[:, :], op0=ALU.mult, op1=ALU.mult), [c.qr, c.E1], [c.qk])
                        S.op("dve", lambda: dve.tensor_tensor(out=c.qk[:, 256:512], in0=c.kr[:, :], in1=c.E2[:, :], op=ALU.mult), [c.kr, c.E2], [c.qk])
                    else:
                        S.op("dve", lambda: dve.tensor_tensor(out=c.qk[:, 256:512], in0=g_[:, 0:256], in1=c.E2[:, :], op=ALU.mult), [g_, c.E2], [c.qk])
                    for j in (range(4) if lat else range(2, 4)):
                        S.op("pe", lambda j=j: pe.transpose(out=ps_t[:, j * 128:(j + 1) * 128], in_=c.qk[:, j * 128:(j + 1) * 128], identity=idb[:, :]), [c.qk, idb], [ps_t])
                    if lat:
                        for h in range(4):
                            S.op("act", lambda h=h: act.copy(out=c.qTz[(h % 2) * 64:(h % 2) * 64 + 64, h, :], in_=ps_t[(h % 2) * 64:(h % 2) * 64 + 64, (h // 2) * 128:(h // 2 + 1) * 128]), [ps_t], [c.qTz])
                    S.op("act", lambda: act.copy(out=c.kTc[:, :, :], in_=ps_t[:, 256:512].rearrange("p (c t) -> p c t", c=2)), [ps_t], [c.kTc])
                    yield
                    if lat:
                        for h in range(4):
                            pr = h // 2
                            S.op("pe", lambda h=h, pr=pr: pe.matmul(ps_AT[:, h * 128:(h + 1) * 128], lhsT=c.kTc[:, pr, :], rhs=c.qTz[:, h, :], start=True, stop=True), [c.kTc, c.qTz], [ps_AT])
                        S.op("dve", lambda: dve.tensor_tensor(out=c.ATs[:, :, :].rearrange("p h t -> p (h t)"), in0=ps_AT[:, :], in1=msk[dr][:, :], op=ALU.mult), [ps_AT, msk[dr]], [c.ATs])
                        for h in range(4):
                            pr = h // 2
                            S.op("pe", lambda h=h: pe.matmul(ps_o[:, h * 128:(h + 1) * 128], lhsT=c.ATs[:, h, :], rhs=v_[:, h * 128:(h + 1) * 128], start=True, stop=False), [c.ATs, v_], [ps_o])
                            S.op("pe", lambda h=h, pr=pr: pe.matmul(ps_o[:, h * 128:(h + 1) * 128], lhsT=c.qTz[:, h, :], rhs=c.Sbf[pr][:, :], start=False, stop=True), [c.qTz, c.Sbf[pr]], [ps_o])
                        ob_ = c.osb[b]
                        S.op("dve", lambda: dve.tensor_copy(out=ob_[:, :], in_=ps_o[:, :]), [ps_o], [ob_])
                        if dr == 0:
                            S.dma("sp", OF_d[li * 128:(li + 1) * 128, :], ob_[:, :], reads=[ob_], writes=[T_OF])
                        else:
                            S.dma("sp", OB_d[li * 128:(li + 1) * 128, :], ob_[:, :], reads=[ob_], writes=[T_OB])
                        yield
                    for h in range(4):
                        pr = h // 2
                        S.op("pe", lambda h=h, pr=pr: pe.matmul(ps_U[:, h * 128:(h + 1) * 128], lhsT=c.qk[:, 256 + pr * 128:256 + (pr + 1) * 128], rhs=v_[:, h * 128:(h + 1) * 128], start=True, stop=True), [c.qk, v_], [ps_U])
                    for h in range(4):
                        pr, P0 = h // 2, (h % 2) * 64
                        S.op("dve", lambda h=h, pr=pr, P0=P0: dve.tensor_scalar(out=c.tU[P0:P0 + 64, :], in0=ps_U[P0:P0 + 64, h * 128:(h + 1) * 128], scalar1=c.at[P0:P0 + 64, pr:pr + 1], scalar2=None, op0=ALU.mult), [ps_U, c.at], [c.tU])
                        S.op("dve", lambda pr=pr, P0=P0: dve.scalar_tensor_tensor(out=c.Sst[pr][P0:P0 + 64, :], in0=c.Sst[pr][P0:P0 + 64, :], scalar=c.at[P0:P0 + 64, pr:pr + 1], in1=c.tU[P0:P0 + 64, :], op0=ALU.mult, op1=ALU.add), [c.Sst[pr], c.at, c.tU], [c.Sst[pr]])
                        S.op("dve", lambda pr=pr, P0=P0: dve.tensor_copy(out=c.Sbf[pr][P0:P0 + 64, :], in_=c.Sst[pr][P0:P0 + 64, :]), [c.Sst[pr]], [c.Sbf[pr]])
                    yield

            gens = [chain(0, alloc(0)), chain(1, alloc(1))]
            while gens:
                for g in list(gens):
                    try:
                        next(g)
                    except StopIteration:
                        gens.remove(g)

            gg = [K.sb(es, [128, 512], F32, "gg%d" % i) for i in range(2)]
            ofl = [K.sb(es, [128, 512], F32, "ofl%d" % i) for i in range(2)]
            obl = [K.sb(es, [128, 512], F32, "obl%d" % i) for i in range(2)]
            osm = [K.sb(es, [128, 512], F32, "osm%d" % i) for i in range(2)]
            junk = [K.sb(es, [128, 128], F32, "junkc%d" % i) for i in range(2)]
            ss4 = [K.sb(es, [128, 4], F32, "ss4_%d" % i) for i in range(2)]
            rs4 = [K.sb(es, [128, 4], F32, "rs4_%d" % i) for i in range(2)]
            sg = [K.sb(es, [128, 512], F32, "sg%d" % i) for i in range(2)]
            ogl = [K.sb(es, [128, 512], BF16, "ogl%d" % i) for i in range(2)]

            def merge(li):
                b = li % 2
                S.dma("sp", gg[b][:, :], GG_d[li * 128:(li + 1) * 128, :], reads=[T_GG], writes=[gg[b]])
                S.dma("sp", ofl[b][:, :], OF_d[li * 128:(li + 1) * 128, :], reads=[T_OF], writes=[ofl[b]])
                S.dma("sp", obl[b][:, :], OB_d[li * 128:(li + 1) * 128, :], reads=[T_OB], writes=[obl[b]])
                yield
                S.op("pool", lambda: pool.tensor_tensor(out=osm[b][:, :], in0=ofl[b][:, :], in1=obl[b][:, :], op=ALU.add), [ofl[b], obl[b]], [osm[b]])
                S.op("act", lambda: act.activation(out=sg[b][:, :], in_=gg[b][:, :], func=AF.Silu), [gg[b]], [sg[b]])
                yield
                for h in range(4):
                    S.op("act", lambda h=h: act.activation(out=junk[b][:, :], in_=osm[b][:, h * 128:(h + 1) * 128], func=AF.Square, accum_out=ss4[b][:, h:h + 1]), [osm[b]], [junk[b], ss4[b]])
                S.op("act", lambda: act.activation(out=rs4[b][:, :], in_=ss4[b][:, :], func=AF.Sqrt, scale=1.0 / 128.0, bias=epsb[:, :]), [ss4[b], epsb], [rs4[b]])
                yield
                S.op("dve", lambda: dve.reciprocal(out=rs4[b][:, :], in_=rs4[b][:, :]), [rs4[b]], [rs4[b]])
                o3 = osm[b][:, :].rearrange("p (h e) -> p h e", h=4)
                S.op("dve", lambda: dve.tensor_tensor(out=o3, in0=o3, in1=rs4[b][:, :].unsqueeze(2).broadcast_to([128, 4, 128]), op=ALU.mult), [osm[b], rs4[b]], [osm[b]])
                yield
                S.op("pool", lambda: pool.tensor_tensor(out=o3, in0=o3, in1=gnb[:, :].unsqueeze(1).broadcast_to([128, 4, 128]), op=ALU.mult), [osm[b], gnb], [osm[b]])
                S.op("dve", lambda: dve.tensor_tensor(out=ogl[b][:, :], in0=osm[b][:, :], in1=sg[b][:, :], op=ALU.mult), [osm[b], sg[b]], [ogl[b]])
                S.dma("sp", MIX_d[li * 128:(li + 1) * 128, 512:1024], ogl[b][:, :], reads=[ogl[b]], writes=[T_MIXgla])
                yield

            pend = []
            for li in range(32):
                pend.append(merge(li))
                if len(pend) == 2 or li == 31:
                    live = list(pend)
                    while live:
                        for g in list(live):
                            try:
                                next(g)
                            except StopIteration:
                                live.remove(g)
                    pend = []

    T_XN = T(None, "XN")
    T_HF = T(None, "HF")
    T_AFFT = T(None, "AFFT")
    AX = mybir.AxisListType.X

    def stageD():
        with contextlib.ExitStack() as es:
            wo = K.sb(es, [128, 8, D], BF16, "wo")
            wst = [K.sb(es, [128, 8, 512], F32, "wstd%d" % i) for i in range(2)]
            wov = w_out.rearrange("(c p) n -> p c n", p=128)
            for b in range(2):
                S.dma("sp", wst[b][:, :, :], wov[:, :, b * 512:(b + 1) * 512], writes=[wst[b]])
                S.op("act", lambda b=b: act.copy(out=wo[:, :, b * 512:(b + 1) * 512], in_=wst[b][:, :, :]), [wst[b]], [wo])
            rt = K.sb(es, [128, 8, NE], F32, "rt")
            S.dma("sp", rt[:, :, :], router.rearrange("(c p) e -> p c e", p=128), writes=[rt])
            md = K.sb(es, [128, 3, D], F32, "md")
            S.dma("sp", md[:, :, :], mods_d[:, 4:7, :], reads=[T_mods], writes=[md])
            affT = K.sb(es, [NE, SEQ], F32, "affT")
            affR = [T(None, "affR%d" % i) for i in range(32)]
            dbl = lambda shape, dt, nm: [K.sb(es, shape, dt, "%s%d" % (nm, i)) for i in range(2)]
            mixb = dbl([128, D], BF16, "mixb")
            xt = dbl([128, D], F32, "xtd")
            mixT = dbl([128, 8, 128], BF16, "mixT")
            mixs = dbl([128, D], F32, "mixs")
            junk = dbl([128, D], BF16, "junkd")
            ss = dbl([128, 1], F32, "ssd")
            rstd = dbl([128, 1], F32, "rstdd")
            t1 = dbl([128, D], F32, "t1d")
            xn = dbl([128, D], F32, "xn")
            hf32 = dbl([128, D], F32, "hf32")
            hfb = dbl([128, D], BF16, "hfb")
            hfT = dbl([128, 8, 128], F32, "hfT")
            lg = dbl([128, NE], F32, "lg")
            mx = dbl([128, 1], F32, "mx")
            ex = dbl([128, NE], F32, "ex")
            sm = dbl([128, 1], F32, "sm")
            af = dbl([128, NE], F32, "af")
            ps_t = K.ps(es, [128, 1024], BF16, "psd_t")
            ps_m = [K.ps(es, [128, 512], F32, "psd_m%d" % i) for i in range(2)]
            ps_r = [K.ps(es, [128, 512], F32, "psd_r%d" % i) for i in range(2)]
            ps_l = K.ps(es, [128, 512], F32, "psd_l")
            ps_a = K.ps(es, [128, 512], F32, "psd_a")

            def tile_prog(T_):
                b = T_ % 2
                mb, x_ = mixb[b], xt[b]
                S.dma("sp", mb[:, :], MIX_d[T_ * 128:(T_ + 1) * 128, :], reads=[T_MIXna, T_MIXgla], writes=[mb])
                S.dma("sp", x_[:, :], xc[CTX + T_ * 128:CTX + (T_ + 1) * 128, :], writes=[x_])
                yield
                for c in range(8):
                    S.op("pe", lambda c=c: pe.transpose(out=ps_t[:, c * 128:(c + 1) * 128], in_=mb[:, c * 128:(c + 1) * 128], identity=idb[:, :]), [mb, idb], [ps_t])
                S.op("act", lambda: act.copy(out=mixT[b][:, :, :], in_=ps_t[:, :].rearrange("p (c t) -> p c t", c=8)), [ps_t], [mixT[b]])
                yield
                for hlf in range(2):
                    for c in range(8):
                        S.op("pe", lambda c=c, hlf=hlf: pe.matmul(ps_m[hlf][:, :], lhsT=mixT[b][:, c, :], rhs=wo[:, c, hlf * 512:(hlf + 1) * 512], start=(c == 0), stop=(c == 7)), [mixT[b], wo], [ps_m[hlf]])
                    S.op("act", lambda hlf=hlf: act.copy(out=mixs[b][:, hlf * 512:(hlf + 1) * 512], in_=ps_m[hlf][:, :]), [ps_m[hlf]], [mixs[b]])
                yield
                rms_rstd(None, mixs[b][:, :], mixs[b], ss[b], rstd[b], junk[b])
                yield
                S.op("dve", lambda: dve.scalar_tensor_tensor(out=t1[b][:, :], in0=mixs[b][:, :], scalar=rstd[b][:, 0:1], in1=md[:, 0, :], op0=ALU.mult, op1=ALU.mult), [mixs[b], rstd[b], md], [t1[b]])
                xn_ = xn[b]
                S.op("pool", lambda: pool.tensor_tensor(out=xn_[:, :], in0=t1[b][:, :], in1=x_[:, :], op=ALU.add), [t1[b], x_], [xn_])
                S.dma("sp", XN_d[T_ * 128:(T_ + 1) * 128, :], xn_[:, :], reads=[xn_], writes=[T_XN])
                yield
                rms_rstd(None, xn_[:, :], xn_, ss[b], rstd[b], junk[b])
                yield
                S.op("dve", lambda: dve.scalar_tensor_tensor(out=t1[b][:, :], in0=xn_[:, :], scalar=rstd[b][:, 0:1], in1=md[:, 1, :], op0=ALU.mult, op1=ALU.mult), [xn_, rstd[b], md], [t1[b]])
                S.op("pool", lambda: pool.tensor_tensor(out=hf32[b][:, :], in0=t1[b][:, :], in1=md[:, 2, :], op=ALU.add), [t1[b], md], [hf32[b]])
                yield
                hb_ = hfb[b]
                S.op("act", lambda: act.copy(out=hb_[:, :], in_=hf32[b][:, :]), [hf32[b]], [hb_])
                S.dma("sp", HF_d[T_ * 128:(T_ + 1) * 128, :], hb_[:, :], reads=[hb_], writes=[T_HF])
                for c in range(8):
                    S.op("pe", lambda c=c: pe.transpose(out=ps_r[c // 4][:, (c % 4) * 128:(c % 4 + 1) * 128], in_=hf32[b][:, c * 128:(c + 1) * 128], identity=idf[:, :]), [hf32[b], idf], [ps_r[c // 4]])
                for i in range(2):
                    S.op("act", lambda i=i: act.copy(out=hfT[b][:, i * 4:(i + 1) * 4, :], in_=ps_r[i][:, :].rearrange("p (c t) -> p c t", c=4)), [ps_r[i]], [hfT[b]])
                yield
                for c in range(8):
                    S.op("pe", lambda c=c: pe.matmul(ps_l[:, 0:NE], lhsT=hfT[b][:, c, :], rhs=rt[:, c, :], start=(c == 0), stop=(c == 7)), [hfT[b], rt], [ps_l])
                S.op("dve", lambda: dve.tensor_copy(out=lg[b][:, :], in_=ps_l[:, 0:NE]), [ps_l], [lg[b]])
                yield
                S.op("dve", lambda: dve.reduce_max(out=mx[b][:, :], in_=lg[b][:, :], axis=AX), [lg[b]], [mx[b]])
                S.op("dve", lambda: dve.tensor_scalar(out=mx[b][:, :], in0=mx[b][:, :], scalar1=-1.0, scalar2=None, op0=ALU.mult), [mx[b]], [mx[b]])
                yield
                S.op("act", lambda: act.activation(out=ex[b][:, :], in_=lg[b][:, :], func=AF.Exp, bias=mx[b][:, :], accum_out=sm[b][:, :]), [lg[b], mx[b]], [ex[b], sm[b]])
                yield
                S.op("dve", lambda: dve.reciprocal(out=sm[b][:, :], in_=sm[b][:, :]), [sm[b]], [sm[b]])
                S.op("dve", lambda: dve.tensor_scalar(out=af[b][:, :], in0=ex[b][:, :], scalar1=sm[b][:, 0:1], scalar2=None, op0=ALU.mult), [ex[b], sm[b]], [af[b]])
                yield
                S.op("pe", lambda: pe.transpose(out=ps_a[0:NE, 0:128], in_=af[b][:, :], identity=idf[:, :]), [af[b], idf], [ps_a])
                S.op("act", lambda: act.copy(out=affT[:, T_ * 128:(T_ + 1) * 128], in_=ps_a[0:NE, 0:128]), [ps_a], [affR[T_]])
                yield

            for T0 in range(0, 32, 2):
                live = [tile_prog(T0), tile_prog(T0 + 1)]
                while live:
                    for g in list(live):
                        try:
                            next(g)
                        except StopIteration:
                            live.remove(g)
            S.dma("sp", AFFT_d, affT[:, :], reads=affR, writes=[T_AFFT])


    posT_d = dscr("posT_d", [128, 4, 64])
    gateT_d = dscr("gateT_d", [128, 4, 64])
    HFO_d = dscr("HFO_d", [SEQ // 2, D], BF16)
    T_HFO = T(None, "HFO")

    def stageEFG():
        import os
        with contextlib.ExitStack() as es:
            posT = K.sb(es, [128, 4, 64], F32, "posT")
            gateT = K.sb(es, [128, 4, 64], F32, "gateT")
            iot = K.sb(es, [128, 512], F32, "iot")
            S.dma("sp", iot[:, :], iota512, writes=[iot])
            PB = [K.ps(es, [128, 512], F32, "PB%d" % i) for i in range(8)]
            with contextlib.ExitStack() as e2:
                A = K.sb(e2, [128, 512], F32, "A")
                S.dma("sp", A[:, :], AFFT_d.rearrange("e (s t) -> (e s) t", s=8), reads=[T_AFFT], writes=[A])
                b1 = K.sb(e2, [128, 128], F32, "b1")
                blt = K.sb(e2, [128, 128], F32, "blt")
                ownf = K.sb(e2, [128, 1], F32, "ownf")
                selm = K.sb(e2, [128, 64], F32, "selm")
                for t_, src in ((b1, blk_ones), (blt, blk_lt), (ownf, own_flag), (selm, sel)):
                    S.dma("sp", t_[:, :], src, writes=[t_])
                lo = K.sb(e2, [128, 1], F32, "lo")
                hi = K.sb(e2, [128, 1], F32, "hi")
                mid = K.sb(e2, [128, 1], F32, "mid")
                cnt = K.sb(e2, [128, 1], F32, "cnt")
                cond = K.sb(e2, [128, 1], F32, "cond")
                d1 = K.sb(e2, [128, 1], F32, "d1")
                jk = K.sb(e2, [128, 512], F32, "jk")
                onesT = K.sb(e2, [128, 512], F32, "onesT")
                M_ = K.sb(e2, [128, 512], F32, "M_")
                posi = K.sb(e2, [128, 512], F32, "posi")
                pm = K.sb(e2, [128, 512], F32, "pm")
                offc = K.sb(e2, [128, 1], F32, "offc")
                S.op("pool", lambda: pool.memset(lo[:, :], 0.0), [], [lo])
                S.op("pool", lambda: pool.memset(hi[:, :], 1.0), [], [hi])
                S.op("pool", lambda: pool.memset(onesT[:, :], 1.0), [], [onesT])
                pc = PB[0]
                for it in range(30):
                    S.op("dve", lambda: dve.tensor_tensor(out=mid[:, :], in0=lo[:, :], in1=hi[:, :], op=ALU.add), [lo, hi], [mid])
                    S.op("dve", lambda: dve.tensor_scalar(out=mid[:, :], in0=mid[:, :], scalar1=0.5, scalar2=None, op0=ALU.mult), [mid], [mid])
                    S.op("dve", lambda: dve.tensor_scalar(out=jk[:, :], in0=A[:, :], scalar1=mid[:, 0:1], scalar2=0.0, op0=ALU.is_gt, op1=ALU.add, accum_out=cnt[:, 0:1]), [A, mid], [jk, cnt])
                    S.op("pe", lambda: pe.matmul(pc[:, 0:1], lhsT=b1[:, :], rhs=cnt[:, 0:1], start=True, stop=True), [b1, cnt], [pc])
                    S.op("dve", lambda: dve.tensor_scalar(out=cond[:, :], in0=pc[:, 0:1], scalar1=float(CAP) - 0.5, scalar2=None, op0=ALU.is_gt), [pc], [cond])
                    S.op("dve", lambda: dve.tensor_tensor(out=d1[:, :], in0=mid[:, :], in1=lo[:, :], op=ALU.subtract), [mid, lo], [d1])
                    S.op("dve", lambda: dve.scalar_tensor_tensor(out=lo[:, :], in0=d1[:, :], scalar=cond[:, 0:1], in1=lo[:, :], op0=ALU.mult, op1=ALU.add), [d1, cond, lo], [lo])
                    S.op("dve", lambda: dve.tensor_tensor(out=d1[:, :], in0=hi[:, :], in1=mid[:, :], op=ALU.subtract), [hi, mid], [d1])
                    S.op("dve", lambda: dve.scalar_tensor_tensor(out=hi[:, :], in0=d1[:, :], scalar=cond[:, 0:1], in1=mid[:, :], op0=ALU.mult, op1=ALU.add), [d1, cond, mid], [hi])
                S.op("dve", lambda: dve.tensor_scalar(out=M_[:, :], in0=A[:, :], scalar1=lo[:, 0:1], scalar2=ownf[:, 0:1], op0=ALU.is_gt, op1=ALU.mult), [A, lo, ownf], [M_])
                S.op("dve", lambda: dve.tensor_tensor_scan(out=posi[:, :], data0=onesT[:, :], data1=M_[:, :], initial=0.0, op0=ALU.mult, op1=ALU.add), [onesT, M_], [posi])
                S.op("pe", lambda: pe.matmul(pc[:, 0:1], lhsT=blt[:, :], rhs=posi[:, 511:512], start=True, stop=True), [blt, posi], [pc])
                S.op("dve", lambda: dve.tensor_scalar(out=offc[:, :], in0=pc[:, 0:1], scalar1=-10000.0, scalar2=None, op0=ALU.add), [pc], [offc])
                S.op("dve", lambda: dve.tensor_scalar(out=pm[:, :], in0=posi[:, :], scalar1=offc[:, 0:1], scalar2=None, op0=ALU.add), [posi, offc], [pm])
                S.op("dve", lambda: dve.tensor_tensor(out=pm[:, :], in0=pm[:, :], in1=M_[:, :], op=ALU.mult), [pm, M_], [pm])
                S.op("dve", lambda: dve.tensor_scalar(out=pm[:, :], in0=pm[:, :], scalar1=9999.0, scalar2=None, op0=ALU.add), [pm], [pm])
                for src, dst, pb in ((pm, posT, PB[1]), (A, gateT, PB[2])):
                    for blk in range(4):
                        S.op("pe", lambda src=src, pb=pb, blk=blk: pe.matmul(pb[:, blk * 64:(blk + 1) * 64], lhsT=src[:, blk * 128:(blk + 1) * 128], rhs=selm[:, :], start=True, stop=True), [src, selm], [pb])
                    S.op("act", lambda dst=dst, pb=pb: act.copy(out=dst[:, :, :], in_=pb[:, 0:256].rearrange("p (b c) -> p b c", b=4)), [pb], [dst])
                if debug:
                    S.dma("sp", posT_d, posT[:, :, :], reads=[posT])
                    S.dma("sp", gateT_d, gateT[:, :, :], reads=[gateT])
                S.barrier()
            S.barrier()
            if upto == "E":
                return
            oi = K.sb(es, [128, 16], I32, "oi")
            S.dma("sp", oi[:, :], own_idx, writes=[oi])
            with contextlib.ExitStack() as e4:
                hfj = [K.sb(e4, [128, D], BF16, "hfj%d" % i) for i in range(3)]
                for j in range(16):
                    hj = hfj[j % 3]
                    S.dma("pool", None, None, reads=[T_HF, oi], writes=[hj], fn=lambda hj=hj, j=j: pool.indirect_dma_start(
                        out=hj[:, :], out_offset=None, in_=HF_d, in_offset=bass.IndirectOffsetOnAxis(ap=oi[:, j:j + 1], axis=0)))
                    S.dma("sp", HFO_d[j * 128:(j + 1) * 128, :], hj[:, :], reads=[hj], writes=[T_HFO])
                S.barrier()
            yacc = K.sb(es, [128, 16, D], F32, "yacc")
            S.op("pool", lambda: pool.memset(yacc[:, :, :], 0.0), [], [yacc])
            e3 = contextlib.ExitStack()
            xsT = K.sb(e3, [128, 8, 512], BF16, "xsT")
            hidT = K.sb(e3, [128, NFC, 512], BF16, "hidT")
            ysb = K.sb(e3, [128, 4, D], BF16, "ysb")
            Ej = [K.sb(e3, [128, 512], BF16, "Ej%d" % i) for i in range(2)]
            Gj = [K.sb(e3, [128, 512], BF16, "Gj%d" % i) for i in range(2)]
            tidt = K.sb(e3, [128, 16, 2], BF16, "tidt")
            S.dma("sp", tidt[:, :, :], tid_in, writes=[tidt])
            xg = [K.sb(e3, [128, 4, D], BF16, "xg%d" % i) for i in range(2)]
            xgT = [[T(None, "xgT") for _ in range(4)] for _ in range(2)]
            idxs = K.sb(e3, [128, 8], F32, "idxs")
            idxf = K.sb(e3, [128, 4], F32, "idxf")
            idxi = [K.sb(e3, [128, 4], I32, "idxi%d" % i) for i in range(2)]
            GTa = K.sb(e3, [128, 16, 4, 128], BF16, "GTa")
            GTt = [T(None, "GTt%d" % i) for i in range(16)]
            sgt = [K.sb(e3, [128, 512], BF16, "sgt%d" % i) for i in range(2)]
            stg = [K.sb(e3, [128, 2048], F32, "stg%d" % i) for i in range(4)]
            nst = [0]

            def stage_in(src_ap, ncols):
                st = stg[nst[0] % 4]
                q = os.environ.get("WQ", "sp").split(",")
                q = q[nst[0] % len(q)]
                nst[0] += 1
                S.dma(q, st[:, 0:ncols], src_ap, writes=[st])
                return st
            wgb = [K.sb(e3, [128, 8, 256], BF16, "wgb%d" % i) for i in range(2)]
            wub = [K.sb(e3, [128, 8, 256], BF16, "wub%d" % i) for i in range(2)]
            wdb = K.sb(e3, [128, NFC, 256], BF16, "wdb")
            wdbT = [T(None, "wdbT%d" % i) for i in range(3)]
            NEX = int(os.environ.get("NEX", str(NE)))
            ng = 0
            PH = int(os.environ.get("PH", "15"))
            def route(e):
                xb = e % 2
                for j in range(16):
                    osg, blk = j // 4, j % 4
                    E_ = Ej[j % 2]
                    S.op("dve", lambda E_=E_, blk=blk, col=e * 4 + osg: dve.tensor_scalar(out=E_[:, :], in0=iot[:, :], scalar1=posT[:, blk, col:col + 1], scalar2=None, op0=ALU.is_equal), [iot, posT], [E_])
                    for sc in range(4):
                        S.op("pe", lambda sc=sc, E_=E_, j=j: pe.matmul(PB[sc][:, 0:2], lhsT=E_[:, sc * 128:(sc + 1) * 128], rhs=tidt[:, j, :], start=(j == 0), stop=(j == 15)), [E_, tidt], [PB[sc]])
                for sc in range(4):
                    S.op("dve", lambda sc=sc: dve.tensor_copy(out=idxs[:, sc * 2:(sc + 1) * 2], in_=PB[sc][:, 0:2]), [PB[sc]], [idxs])
                iv = idxs[:, :].rearrange("p (s two) -> p s two", two=2)
                S.op("dve", lambda: dve.scalar_tensor_tensor(out=idxf[:, :], in0=iv[:, :, 0], scalar=64.0, in1=iv[:, :, 1], op0=ALU.mult, op1=ALU.add), [idxs], [idxf])
                S.op("dve", lambda: dve.tensor_copy(out=idxi[xb][:, :], in_=idxf[:, :]), [idxf], [idxi[xb]])
                for sc in range(4):
                    S.dma("pool", None, None, reads=[T_HFO, idxi[xb]], writes=[xgT[xb][sc]], fn=lambda sc=sc: pool.indirect_dma_start(
                        out=xg[xb][:, sc, :], out_offset=None, in_=HFO_d, in_offset=bass.IndirectOffsetOnAxis(ap=idxi[xb][:, sc:sc + 1], axis=0)))

            if PH & 1:
                route(0)
            for e in range(NEX):
                xb = e % 2
                for c in (range(8) if PH & 1 else []):
                    bb = PB[c // 2][:, :].bitcast(BF16)
                    for sc in range(4):
                        S.op("pe", lambda c=c, sc=sc, bb=bb: pe.transpose(out=bb[:, (c % 2) * 512 + sc * 128:(c % 2) * 512 + (sc + 1) * 128], in_=xg[xb][:, sc, c * 128:(c + 1) * 128], identity=idb[:, :]), [xgT[xb][sc], idb], [PB[c // 2]])
                for c in (range(8) if PH & 1 else []):
                    bb = PB[c // 2][:, :].bitcast(BF16)
                    if (c // 2) % 2 == 0:
                        S.op("act", lambda c=c, bb=bb: act.copy(out=xsT[:, c, :], in_=bb[:, (c % 2) * 512:(c % 2 + 1) * 512]), [PB[c // 2]], [xsT])
                    else:
                        S.op("dve", lambda c=c, bb=bb: dve.tensor_copy(out=xsT[:, c, :], in_=bb[:, (c % 2) * 512:(c % 2 + 1) * 512]), [PB[c // 2]], [xsT])

                def prep(j):
                    osg, blk = j // 4, j % 4
                    col = e * 4 + osg
                    G_ = Gj[j % 2]
                    S.op("dve", lambda: dve.tensor_scalar(out=G_[:, :], in0=iot[:, :], scalar1=posT[:, blk, col:col + 1], scalar2=gateT[:, blk, col:col + 1], op0=ALU.is_equal, op1=ALU.mult), [iot, posT, gateT], [G_])
                    pt = PB[6 + j % 2]
                    ptb = pt[:, :].bitcast(BF16)
                    for sc in range(4):
                        S.op("pe", lambda sc=sc: pe.transpose(out=ptb[:, sc * 128:(sc + 1) * 128], in_=G_[:, sc * 128:(sc + 1) * 128], identity=idb[:, :]), [G_, idb], [pt])
                    S.op("act", lambda: act.copy(out=GTa[:, j, :, :], in_=ptb[:, 0:512].rearrange("p (s t) -> p s t", s=4)), [pt], [GTt[j]])

                jn = 0
                for g in (range(11) if PH & 2 else []):
                    gb = ng % 2
                    ng += 1
                    sg_st = stage_in(w_gate[e, g], 2048)
                    su_st = stage_in(w_up[e, g], 2048)
                    GUX = int(os.environ.get("GUX", "0"))
                    if GUX < 2:
                        S.op("act", lambda gb=gb, sg_st=sg_st: act.copy(out=wgb[gb][:, :, :].rearrange("p c f -> p (c f)"), in_=sg_st[:, :]), [sg_st], [wgb[gb]])
                        S.op("dve", lambda gb=gb, su_st=su_st: dve.tensor_copy(out=wub[gb][:, :, :].rearrange("p c f -> p (c f)"), in_=su_st[:, :]), [su_st], [wub[gb]])
                    for fl in (range(2) if GUX == 0 else []):
                        fc = g * 2 + fl
                        pg, pu = PB[(fc % 2) * 2], PB[(fc % 2) * 2 + 1]
                        for c in range(8):
                            S.op("pe", lambda c=c, pg=pg, gb=gb, fl=fl: pe.matmul(pg[:, :], lhsT=wgb[gb][:, c, fl * 128:(fl + 1) * 128], rhs=xsT[:, c, :], start=(c == 0), stop=(c == 7)), [wgb[gb], xsT], [pg])
                        for c in range(8):
                            S.op("pe", lambda c=c, pu=pu, gb=gb, fl=fl: pe.matmul(pu[:, :], lhsT=wub[gb][:, c, fl * 128:(fl + 1) * 128], rhs=xsT[:, c, :], start=(c == 0), stop=(c == 7)), [wub[gb], xsT], [pu])
                        sg_ = sgt[fc % 2]
                        S.op("act", lambda pg=pg, sg_=sg_: act.activation(out=sg_[:, :], in_=pg[:, :], func=AF.Silu), [pg], [sg_])
                        S.op("dve", lambda pu=pu, sg_=sg_, fc=fc: dve.tensor_tensor(out=hidT[:, fc, :], in0=sg_[:, :], in1=pu[:, :], op=ALU.mult), [sg_, pu], [hidT])
                    for _ in range(2 if g < 5 else 1):
                        if jn < 16:
                            prep(jn)
                            jn += 1
                while jn < 16 and (PH & 8):
                    prep(jn)
                    jn += 1
                if e + 1 < NEX and (PH & 1):
                    route(e + 1)
                pieces = ((0, 8), (8, 16), (16, NFC))
                for cb in (range(4) if PH & 4 else []):
                    for k, (f0, f1) in enumerate(pieces):
                        st = stage_in(w_down[e, cb, :, f0 * 256:f1 * 256], (f1 - f0) * 256)
                        S.op("act", lambda st=st, f0=f0, f1=f1: act.copy(out=wdb[:, f0:f1, :].rearrange("p c n -> p (c n)"), in_=st[:, 0:(f1 - f0) * 256]), [st], [wdbT[k]])
                    for k, (f0, f1) in enumerate(pieces):
                        for fc in range(f0, f1):
                            for sc in range(4):
                                S.op("pe", lambda fc=fc, sc=sc: pe.matmul(PB[4 + sc][:, 0:256], lhsT=hidT[:, fc, sc * 128:(sc + 1) * 128], rhs=wdb[:, fc, :], start=(fc == 0), stop=(fc == NFC - 1)), [hidT, wdbT[k]], [PB[4 + sc]])
                    for sc in range(4):
                        if sc % 2 == 0:
                            S.op("act", lambda sc=sc, cb=cb: act.copy(out=ysb[:, sc, cb * 256:(cb + 1) * 256], in_=PB[4 + sc][:, 0:256]), [PB[4 + sc]], [ysb])
                        else:
                            S.op("dve", lambda sc=sc, cb=cb: dve.tensor_copy(out=ysb[:, sc, cb * 256:(cb + 1) * 256], in_=PB[4 + sc][:, 0:256]), [PB[4 + sc]], [ysb])
                for j in (range(16) if PH & 8 else []):
                    for hlf in range(2):
                        pyc = PB[(j % 2) * 2 + hlf]
                        for sc in range(4):
                            S.op("pe", lambda sc=sc, pyc=pyc, hlf=hlf, j=j: pe.matmul(pyc[:, :], lhsT=GTa[:, j, sc, :], rhs=ysb[:, sc, hlf * 512:(hlf + 1) * 512], start=(sc == 0), stop=(sc == 3)), [GTt[j], ysb], [pyc])
                        S.op("dve", lambda pyc=pyc, j=j, hlf=hlf: dve.tensor_tensor(out=yacc[:, j, hlf * 512:(hlf + 1) * 512], in0=yacc[:, j, hlf * 512:(hlf + 1) * 512], in1=pyc[:, :], op=ALU.add), [yacc, pyc], [yacc])
            S.barrier()
            e3.close()
            gF = K.sb(es, [128, D], F32, "gF")
            S.dma("sp", gF[:, :], mods_d[:, 7, :], reads=[T_mods], writes=[gF])
            xno = [K.sb(es, [128, D], F32, "xno%d" % i) for i in range(2)]
            ob = [K.sb(es, [128, D], F32, "ob%d" % i) for i in range(2)]
            junk = K.sb(es, [128, D], BF16, "junkg")
            ss = K.sb(es, [128, 1], F32, "ssg")
            rstd = K.sb(es, [128, 1], F32, "rstdg")
            for j in range(16):
                b = j % 2
                S.dma("pool", None, None, reads=[T_XN, oi], writes=[xno[b]], fn=lambda b=b, j=j: pool.indirect_dma_start(
                    out=xno[b][:, :], out_offset=None, in_=XN_d, in_offset=bass.IndirectOffsetOnAxis(ap=oi[:, j:j + 1], axis=0)))
                S.op("act", lambda j=j: act.activation(out=junk[:, :], in_=yacc[:, j, :], func=AF.Square, accum_out=ss[:, :]), [yacc], [junk, ss])
                S.op("act", lambda: act.activation(out=rstd[:, :], in_=ss[:, :], func=AF.Sqrt, scale=1.0 / D, bias=epsb[:, :]), [ss, epsb], [rstd])
                S.op("dve", lambda: dve.reciprocal(out=rstd[:, :], in_=rstd[:, :]), [rstd], [rstd])
                S.op("dve", lambda j=j, b=b: dve.scalar_tensor_tensor(out=ob[b][:, :], in0=yacc[:, j, :], scalar=rstd[:, 0:1], in1=gF[:, :], op0=ALU.mult, op1=ALU.mult), [yacc, rstd, gF], [ob[b]])
                S.op("pool", lambda b=b: pool.tensor_tensor(out=ob[b][:, :], in0=ob[b][:, :], in1=xno[b][:, :], op=ALU.add), [ob[b], xno[b]], [ob[b]])
                S.dma("sp", out[j * 128:(j + 1) * 128, :], ob[b][:, :], reads=[ob[b]])

    stages = [("0", stage0), ("A", stageA), ("B", stageB), ("C", stageC), ("D", stageD), ("E", stageEFG)]
    if upto in ("F", "G", "all"):
        stages[-1] = (upto, stageEFG)
    for name, fn in stages:
        fn()
        S.barrier()
        if upto == name:
            break
    S.finish()
    glob.close()
    return nc


def prep_inputs(inputs, cores=range(8), with_experts=True):
    f = lambda k: np.ascontiguousarray(np.asarray(inputs[k], dtype=np.float32))
    x, c, ctx, c_ctx = f("x"), f("c"), f("ctx"), f("c_ctx")
    consts = _consts()
    cos, sin = _rope_tables()
    rpb = f("na_rpb")[0]
    nab = np.ascontiguousarray(_na_bias_tables(rpb).reshape(5, 8, 128, 640))
    a_up = f("gla_a_up")[0]
    a_bias = f("gla_a_bias")[0]
    aup_bd = np.zeros((32, 512), np.float32)
    aup_bd[0:16, 0:256] = a_up[0]
    aup_bd[16:32, 256:512] = a_up[1]
    abias = np.ascontiguousarray(a_bias.reshape(1, 512))
    norms = np.ascontiguousarray(np.stack([f("norm_mix_pre")[0], f("norm_mix_post")[0], f("norm_ffn_pre")[0], f("norm_ffn_post")[0]]))
    shared = dict(
        w_mod=f("w_mod")[0], b_mod=f("b_mod"), norms=norms, gla_norm=f("gla_norm"), w_in=f("w_in")[0],
        aup_bd=aup_bd, abias=abias, rope_cos=cos, rope_sin=sin, na_bias=nab, w_out=f("w_out")[0],
        router=f("router")[0], **consts)
    if with_experts:
        tile_gu = lambda w: np.ascontiguousarray(w.reshape(NE, 8, 128, 11, 256).transpose(0, 3, 2, 1, 4)).reshape(NE, 11, 128, 8 * 256)
        shared.update(w_gate=tile_gu(f("w_gate")[0]), w_up=tile_gu(f("w_up")[0]),
                      w_down=np.ascontiguousarray(f("w_down")[0].reshape(NE, NFC, 128, 4, 256).transpose(0, 3, 2, 1, 4)).reshape(NE, 4, 128, NFC * 256))
    maps = []
    pp = np.arange(128)
    for core in cores:
        s, p = core // 2, core % 2
        m = dict(shared)
        m["xc"] = np.ascontiguousarray(np.concatenate([ctx[s], x[s]], axis=0))
        cv = np.zeros((128, 16), np.float32)
        cv[:, 0:8] = c[s].reshape(8, 128).T
        cv[:, 8:16] = c_ctx.reshape(8, 128).T
        m["cvec"] = cv
        m["own_idx"] = np.ascontiguousarray((p * 2048 + np.arange(16)[None, :] * 128 + pp[:, None]).astype(np.int32))
        seg = pp % 8
        m["own_flag"] = ((seg // 4) == p).astype(np.float32).reshape(128, 1)
        selm = np.zeros((128, 64), np.float32)
        for e in range(16):
            for os_ in range(4):
                selm[e * 8 + p * 4 + os_, e * 4 + os_] = 1.0
        m["sel"] = selm
        maps.append(m)
    return maps


_PROGRAM = None


def kernel(**inputs):
    global _PROGRAM
    if _PROGRAM is None:
        _PROGRAM = build_program()
    maps = prep_inputs(inputs)
    res = run_bass_kernel_spmd(_PROGRAM, maps, core_ids=list(range(8)))
    out = np.zeros((4, SEQ, D), np.float32)
    for core in range(8):
        s, p = core // 2, core % 2
        out[s, p * 2048:(p + 1) * 2048] = res.results[core]["out"]
    return out
```

```python
import contextlib
import numpy as np
import ml_dtypes
import concourse.bass as bass
import concourse.mybir as mybir
from concourse.bass_utils import run_bass_kernel_spmd

F32 = mybir.dt.float32
BF16 = mybir.dt.bfloat16
I32 = mybir.dt.int32
AF = mybir.ActivationFunctionType
ALU = mybir.AluOpType

D = 1024
SEQ = 4096
CTX = 256
NTOK = SEQ + CTX
NT = NTOK // 128
INC = 3104
NE = 16
DE = 2816
NFC = DE // 128
CAP = 512
EPS = 1e-6
NEG = -30000.0


class T:
    __slots__ = ("ap", "w", "r", "name")

    def __init__(self, ap, name=""):
        self.ap = ap
        self.w = None
        self.r = {}
        self.name = name

    def __getitem__(self, idx):
        return self.ap[idx]


class Sync:
    def __init__(self, nc, n_sp=20, n_pool=8, n_act=6):
        self.nc = nc
        self.E = {"pe": nc.tensor, "act": nc.scalar, "dve": nc.vector, "pool": nc.gpsimd, "sp": nc.sync}
        self.sem = {}
        self.cnt = {}
        for k in ("pe", "act", "dve", "pool"):
            self.sem[k] = nc.alloc_semaphore("s_" + k)
            self.cnt[k] = 0
        self.seen = {k: {} for k in self.E}
        self.dq = {}
        for q, n in (("sp", n_sp), ("pool", n_pool), ("act", n_act)):
            lst = []
            for i in range(n):
                key = "d_%s%d" % (q, i)
                self.sem[key] = nc.alloc_semaphore(key)
                self.cnt[key] = 0
                lst.append(key)
            self.dq[q] = [lst, 0]

    def _wait(self, ek, key, val):
        if val <= 0:
            return
        if self.seen[ek].get(key, 0) >= val:
            return
        self.E[ek].wait_ge(self.sem[key], val)
        self.seen[ek][key] = val

    def _deps(self, ek, reads, writes):
        need = {}
        for t in reads:
            if t.w is not None:
                k, v = t.w
                need[k] = max(need.get(k, 0), v)
        for t in writes:
            if t.w is not None:
                k, v = t.w
                if not (k == "pe" and ek == "pe"):
                    need[k] = max(need.get(k, 0), v)
            for k, v in t.r.items():
                if not (k == "pe" and ek == "pe"):
                    need[k] = max(need.get(k, 0), v)
        for k, v in need.items():
            self._wait(ek, k, v)

    def op(self, ek, fn, reads=(), writes=()):
        self._deps(ek, reads, writes)
        ins = fn()
        self.cnt[ek] += 1
        ins.then_inc(self.sem[ek], 1)
        me = (ek, self.cnt[ek])
        for t in reads:
            t.r[ek] = self.cnt[ek]
        for t in writes:
            t.w = me
            t.r = {}
        return ins

    def dma(self, q, out, in_, reads=(), writes=(), fn=None, **kw):
        lst, i = self.dq[q]
        key = lst[i % len(lst)]
        self.dq[q][1] = i + 1
        self._wait(q, key, self.cnt[key])
        self._deps(q, reads, writes)
        if fn is None:
            ins = self.E[q].dma_start(out=out, in_=in_, **kw)
        else:
            ins = fn()
        self.cnt[key] += 16
        ins.then_inc(self.sem[key], 16)
        for t in reads:
            t.r[key] = self.cnt[key]
        for t in writes:
            t.w = (key, self.cnt[key])
            t.r = {}
        return ins

    def barrier(self):
        for ek in self.E:
            for k, v in self.cnt.items():
                if not (k == ek == "pe"):
                    self._wait(ek, k, v)

    def finish(self):
        for k, v in self.cnt.items():
            self._wait("sp", k, v)


class Ctx:
    def __init__(self, nc):
        self.nc = nc
        self.S = Sync(nc)
        self.uid = 0

    def sb(self, es, shape, dt, name):
        self.uid += 1
        return T(es.enter_context(self.nc.sbuf_tensor("%s_%d" % (name, self.uid), list(shape), dt)), name)

    def ps(self, es, shape, dt, name):
        self.uid += 1
        return T(es.enter_context(self.nc.psum_tensor("%s_%d" % (name, self.uid), list(shape), dt)), name)


def _rope_tables():
    half = 16
    freqs = (np.float32(10000.0) ** (-np.arange(half, dtype=np.float32) / np.float32(half))).astype(np.float32)
    t = np.arange(SEQ)
    pr = (t // 64).astype(np.float32)[:, None] * freqs
    pc = (t % 64).astype(np.float32)[:, None] * freqs
    ang = np.concatenate([pr, pc], axis=1).astype(np.float32)
    return np.cos(ang).astype(np.float32), np.sin(ang).astype(np.float32)


def _na_key_tile0(T_):
    return min(max(T_ - 2, 0), 27)


def _na_bias_tables(rpb):
    out = np.full((5, 8, 128, 5, 128), NEG, np.float32)
    for si, T_ in enumerate((0, 1, 2, 30, 31)):
        kt0 = _na_key_tile0(T_)
        for qi in range(128):
            r = 2 * T_ + qi // 64
            qc = qi % 64
            rs = min(max(r - 4, 0), 56)
            cs = min(max(qc - 8, 0), 48)
            for kr in range(rs, rs + 8):
                kt = kr // 2 - kt0
                assert 0 <= kt < 5
                dr = kr - r + 7
                kc = np.arange(cs, cs + 16)
                dc = np.clip(kc - qc + 15, 0, 30)
                j = (kr % 2) * 64 + kc
                out[si, :, j, kt, qi] = rpb[:, dr, dc].T
    return out


def _consts():
    c = {}
    c["ident_f"] = np.eye(128, dtype=np.float32)
    c["ident_b"] = np.eye(128, dtype=np.float32).astype(ml_dtypes.bfloat16)
    tt = np.arange(128)
    c["tri_f"] = np.where(tt[:, None] <= tt[None, :], -1.0 / 16.0, 0.0).astype(np.float32)
    c["tri_b"] = np.where(tt[:, None] >= tt[None, :], -1.0 / 16.0, 0.0).astype(np.float32)
    c["mask_f"] = (tt[:, None] <= tt[None, :]).astype(np.float32)
    c["mask_b"] = (tt[:, None] >= tt[None, :]).astype(np.float32)
    c["ones_col"] = np.full((128, 1), -1.0 / 16.0, np.float32)
    c["ones_row"] = np.ones((1, 128), np.float32)
    g = tt // 8
    c["blk_ones"] = (g[:, None] == g[None, :]).astype(np.float32)
    c["blk_lt"] = ((g[:, None] == g[None, :]) & (tt[:, None] < tt[None, :])).astype(np.float32)
    c["iota512"] = np.broadcast_to(np.arange(512, dtype=np.float32)[None, :], (128, 512)).copy()
    own = np.arange(16)[None, :] * 128 + np.arange(128)[:, None]
    c["tid"] = np.stack([own // 64, own % 64], axis=-1).astype(np.float32).astype(ml_dtypes.bfloat16)
    return c


def build_program(upto="all", debug=False):
    nc = bass.Bass("TRN2", target_bir_lowering=False)
    K = Ctx(nc)
    S = K.S
    pe, act, dve, pool = nc.tensor, nc.scalar, nc.vector, nc.gpsimd

    def din(name, shape, dt=F32):
        return nc.dram_tensor(name, list(shape), dt, kind="ExternalInput").ap()

    import os as _os
    _ext = set(_os.environ.get("EXTSET", "").split(","))

    def dscr(name, shape, dt=F32):
        return nc.dram_tensor(name, list(shape), dt, kind=("ExternalOutput" if (debug or name in _ext) else "Internal")).ap()

    xc = din("xc", [NTOK, D])
    cvec = din("cvec", [128, 16])
    w_mod = din("w_mod", [D, 6 * D])
    b_mod = din("b_mod", [1, 6 * D])
    norms = din("norms", [4, D])
    gla_norm = din("gla_norm", [1, 128])
    w_in = din("w_in", [D, INC])
    aup_bd = din("aup_bd", [32, 512])
    abias = din("abias", [1, 512])
    rope_cos = din("rope_cos", [SEQ, 32])
    rope_sin = din("rope_sin", [SEQ, 32])
    na_bias = din("na_bias", [5, 8, 128, 640])
    w_out = din("w_out", [D, D])
    router = din("router", [D, NE])
    if upto in ("F", "G", "all"):
        w_gate = din("w_gate", [NE, 11, 128, 8 * 256])
        w_up = din("w_up", [NE, 11, 128, 8 * 256])
        w_down = din("w_down", [NE, 4, 128, NFC * 256])
    ident_f = din("ident_f", [128, 128])
    ident_b = din("ident_b", [128, 128], BF16)
    tri_f = din("tri_f", [128, 128])
    tri_b = din("tri_b", [128, 128])
    mask_f = din("mask_f", [128, 128])
    mask_b = din("mask_b", [128, 128])
    ones_col = din("ones_col", [128, 1])
    ones_row = din("ones_row", [1, 128])
    blk_ones = din("blk_ones", [128, 128])
    blk_lt = din("blk_lt", [128, 128])
    iota512 = din("iota512", [128, 512])
    tid_in = din("tid", [128, 16, 2], BF16)
    own_idx = din("own_idx", [128, 16], I32)
    own_flag = din("own_flag", [128, 1])
    sel = din("sel", [128, 64])
    out = nc.dram_tensor("out", [SEQ // 2, D], F32, kind="ExternalOutput").ap()

    mods_d = dscr("mods_d", [128, 8, D])
    KT_d = dscr("KT_d", [512, NTOK], BF16)
    QT_d = dscr("QT_d", [512, SEQ], BF16)
    V_d = dscr("V_d", [NTOK, 512], BF16)
    GV_d = dscr("GV_d", [NTOK, 512], BF16)
    GKQ_d = dscr("GKQ_d", [NTOK, 544])
    GG_d = dscr("GG_d", [SEQ, 512])
    OF_d = dscr("OF_d", [SEQ, 512])
    MIX_d = dscr("MIX_d", [SEQ, D], BF16)
    XN_d = dscr("XN_d", [SEQ, D])
    HF_d = dscr("HF_d", [SEQ, D], BF16)
    AFFT_d = dscr("AFFT_d", [NE, SEQ])

    glob = contextlib.ExitStack()
    idf = K.sb(glob, [128, 128], F32, "idf")
    idb = K.sb(glob, [128, 128], BF16, "idb")
    onesr = K.sb(glob, [1, 128], F32, "onesr")
    S.dma("sp", idf[:, :], ident_f, writes=[idf])
    S.dma("sp", idb[:, :], ident_b, writes=[idb])
    S.dma("sp", onesr[:, :], ones_row, writes=[onesr])

    def stage0():
        with contextlib.ExitStack() as es:
            cv = K.sb(es, [128, 16], F32, "cv")
            csil = K.sb(es, [128, 16], F32, "csil")
            crep = K.sb(es, [128, 16, 128], F32, "crep")
            bm = K.sb(es, [1, 6 * D], F32, "bm")
            modA = K.sb(es, [128, 6 * D], F32, "modA")
            modC = K.sb(es, [128, 2 * D], F32, "modC")
            nbc = K.sb(es, [128, 4, D], F32, "nbc")
            wm = [K.sb(es, [128, 8, 512], F32, "wm%d" % i) for i in range(2)]
            mods = K.sb(es, [128, 8, D], F32, "mods")
            psA = [K.ps(es, [128, 512], F32, "psA%d" % i) for i in range(2)]
            psC = [K.ps(es, [128, 512], F32, "psC%d" % i) for i in range(2)]
            S.dma("sp", cv[:, :], cvec, writes=[cv])
            S.dma("sp", bm[:, :], b_mod, writes=[bm])
            S.dma("sp", nbc[:, :, :], norms.partition_broadcast(128), writes=[nbc])
            S.op("act", lambda: act.activation(out=csil[:, :], in_=cv[:, :], func=AF.Silu), [cv], [csil])
            S.op("dve", lambda: dve.tensor_copy(out=crep[:, :, :], in_=csil[:, :].unsqueeze(2).broadcast_to([128, 16, 128])), [csil], [crep])
            wmv = w_mod.rearrange("(c p) n -> p c n", p=128)
            for nb in range(12):
                w = wm[nb % 2]
                S.dma("sp", w[:, :, :], wmv[:, :, nb * 512:(nb + 1) * 512], writes=[w])
                for which in range(2 if nb < 4 else 1):
                    ps = (psA, psC)[which][nb % 2]
                    for c in range(8):
                        S.op("pe", lambda c=c, ps=ps, w=w, which=which: pe.matmul(ps[:, :], lhsT=crep[:, which * 8 + c, :], rhs=w[:, c, :], start=(c == 0), stop=False), [crep, w], [ps])
                    S.op("pe", lambda ps=ps, nb=nb: pe.matmul(ps[:, :], lhsT=onesr[:, :], rhs=bm[:, nb * 512:(nb + 1) * 512], start=False, stop=True), [onesr, bm], [ps])
                    dst = (modA, modC)[which]
                    if which == 0:
                        S.op("act", lambda ps=ps, dst=dst, nb=nb: act.copy(out=dst[:, nb * 512:(nb + 1) * 512], in_=ps[:, :]), [ps], [dst])
                    else:
                        S.op("dve", lambda ps=ps, dst=dst, nb=nb: dve.tensor_copy(out=dst[:, nb * 512:(nb + 1) * 512], in_=ps[:, :]), [ps], [dst])
            import os
            if os.environ.get("STOP0") == "1":
                S.dma("sp", mods_d[:, 0:6, :], modA[:, :].rearrange("p (a b) -> p a b", a=6), reads=[modA], writes=[T_mods])
                return
            sl = lambda t, i: t[:, i * D:(i + 1) * D]
            S.op("dve", lambda: dve.scalar_tensor_tensor(out=mods[:, 0, :], in0=sl(modA, 1), scalar=1.0, in1=nbc[:, 0, :], op0=ALU.add, op1=ALU.mult), [modA, nbc], [mods])
            S.op("dve", lambda: dve.tensor_copy(out=mods[:, 1, :], in_=sl(modA, 0)), [modA], [mods])
            S.op("dve", lambda: dve.scalar_tensor_tensor(out=mods[:, 2, :], in0=sl(modC, 1), scalar=1.0, in1=nbc[:, 0, :], op0=ALU.add, op1=ALU.mult), [modC, nbc], [mods])
            S.op("dve", lambda: dve.tensor_copy(out=mods[:, 3, :], in_=sl(modC, 0)), [modC], [mods])
            S.op("dve", lambda: dve.tensor_tensor(out=mods[:, 4, :], in0=sl(modA, 2), in1=nbc[:, 1, :], op=ALU.mult), [modA, nbc], [mods])
            S.op("dve", lambda: dve.scalar_tensor_tensor(out=mods[:, 5, :], in0=sl(modA, 4), scalar=1.0, in1=nbc[:, 2, :], op0=ALU.add, op1=ALU.mult), [modA, nbc], [mods])
            S.op("dve", lambda: dve.tensor_copy(out=mods[:, 6, :], in_=sl(modA, 3)), [modA], [mods])
            S.op("dve", lambda: dve.tensor_tensor(out=mods[:, 7, :], in0=sl(modA, 5), in1=nbc[:, 3, :], op=ALU.mult), [modA, nbc], [mods])
            S.dma("sp", mods_d, mods[:, :, :], reads=[mods], writes=[T_mods])

    T_mods = T(None, "mods_d")
    T_KT, T_QT, T_V, T_GV, T_GKQ, T_GG = (T(None, n) for n in ("KT", "QT", "V", "GV", "GKQ", "GG"))

    def rms_rstd(es_tiles, src, src_t, ss, rstd, junk):
        S.op("act", lambda: act.activation(out=junk[:, :], in_=src, func=AF.Square, accum_out=ss[:, :]), [src_t], [junk, ss])
        S.op("act", lambda: act.activation(out=rstd[:, :], in_=ss[:, :], func=AF.Sqrt, scale=1.0 / D, bias=epsb[:, :]), [ss, epsb], [rstd])
        S.op("dve", lambda: dve.reciprocal(out=rstd[:, :], in_=rstd[:, :]), [rstd], [rstd])

    epsb = K.sb(glob, [128, 1], F32, "epsb")
    S.op("pool", lambda: pool.memset(epsb[:, :], EPS), [], [epsb])

    def stageA():
        with contextlib.ExitStack() as es:
            win = K.sb(es, [128, 8, INC], BF16, "win")
            wst = [K.sb(es, [128, 8, 512], F32, "wst%d" % i) for i in range(2)]
            gs = K.sb(es, [128, 4, D], F32, "gs")
            S.dma("sp", gs[:, :, :], mods_d[:, 0:4, :], reads=[T_mods], writes=[gs])
            wiv = w_in.rearrange("(c p) n -> p c n", p=128)
            for b in range(7):
                c0 = b * 512
                w_ = min(512, INC - c0)
                st = wst[b % 2]
                S.dma("sp", st[:, :, 0:w_], wiv[:, :, c0:c0 + w_], writes=[st])
                if b % 2 == 0:
                    S.op("act", lambda st=st, c0=c0, w_=w_: act.copy(out=win[:, :, c0:c0 + w_], in_=st[:, :, 0:w_]), [st], [win])
                else:
                    S.op("dve", lambda st=st, c0=c0, w_=w_: dve.tensor_copy(out=win[:, :, c0:c0 + w_], in_=st[:, :, 0:w_]), [st], [win])
            import os
            STOPA = int(os.environ.get("STOPA", "99"))
            if STOPA <= 1:
                return
            xt = [K.sb(es, [128, D], F32, "xt%d" % i) for i in range(2)]
            junk = K.sb(es, [128, D], BF16, "junk")
            ss = [K.sb(es, [128, 1], F32, "ss%d" % i) for i in range(2)]
            rstd = [K.sb(es, [128, 1], F32, "rstd%d" % i) for i in range(2)]
            h1 = [K.sb(es, [128, D], F32, "h1_%d" % i) for i in range(2)]
            hb = [K.sb(es, [128, D], BF16, "hb%d" % i) for i in range(2)]
            hT = [K.sb(es, [128, 8, 512], BF16, "hT%d" % i) for i in range(2)]
            ktq = [K.sb(es, [128, 512], BF16, "ktq%d" % i) for i in range(2)]
            vo = [K.sb(es, [128, 512], BF16, "vo%d" % i) for i in range(2)]
            gvo = [K.sb(es, [128, 512], BF16, "gvo%d" % i) for i in range(2)]
            gkq = [K.sb(es, [128, 544], F32, "gkq%d" % i) for i in range(2)]
            ggo = [K.sb(es, [128, 512], F32, "ggo%d" % i) for i in range(2)]
            pst = [K.ps(es, [128, 8 * 128], BF16, "pst%d" % i) for i in range(2)]
            psp = [K.ps(es, [128, 512], F32, "psp%d" % i) for i in range(4)]
            npp = [0]

            def nextps():
                npp[0] += 1
                return psp[npp[0] % 4]

            ev = [0]

            def evac(dst_t, dst_ap, ps, ps_ap):
                if psp.index(ps) % 2 == 0:
                    S.op("act", lambda: act.copy(out=dst_ap, in_=ps_ap), [ps], [dst_t])
                else:
                    S.op("dve", lambda: dve.tensor_copy(out=dst_ap, in_=ps_ap), [ps], [dst_t])

            groups = [(0, 2)] + [(2 + 4 * g, 4) for g in range(8)]
            ti_glob = 0
            for gi, (t0, ntl) in enumerate(groups):
                is_ctx = gi == 0
                hTg = hT[gi % 2]
                for k in range(ntl):
                    ti = t0 + k
                    b2 = ti_glob % 2
                    ti_glob += 1
                    x_ = xt[b2]
                    S.dma("sp", x_[:, :], xc[ti * 128:(ti + 1) * 128, :], writes=[x_])
                    rms_rstd(None, x_[:, :], x_, ss[b2], rstd[b2], junk)
                    go = 2 if is_ctx else 0
                    h_ = h1[b2]
                    S.op("dve", lambda x_=x_, h_=h_, b2=b2, go=go: dve.scalar_tensor_tensor(out=h_[:, :], in0=x_[:, :], scalar=rstd[b2][:, 0:1], in1=gs[:, go, :], op0=ALU.mult, op1=ALU.mult), [x_, rstd[b2], gs], [h_])
                    S.op("pool", lambda h_=h_, b2=b2, go=go: pool.tensor_tensor(out=hb[b2][:, :], in0=h_[:, :], in1=gs[:, go + 1, :], op=ALU.add), [h_, gs], [hb[b2]])
                    pt = pst[b2]
                    for c in range(8):
                        S.op("pe", lambda c=c, pt=pt, b2=b2: pe.transpose(out=pt[:, c * 128:(c + 1) * 128], in_=hb[b2][:, c * 128:(c + 1) * 128], identity=idb[:, :]), [hb[b2], idb], [pt])
                    S.op("act", lambda pt=pt, hTg=hTg, k=k: act.copy(out=hTg[:, :, k * 128:(k + 1) * 128], in_=pt[:, :].rearrange("p (c t) -> p c t", c=8)), [pt], [hTg])
                ntok = ntl * 128
                tok0 = t0 * 128
                if STOPA <= 2:
                    return
                for which, c0 in ((0, 0), (1, 1824)):
                    if which == 1 and is_ctx:
                        continue
                    for fb in range(4):
                        ps = nextps()
                        for c in range(8):
                            S.op("pe", lambda c=c, ps=ps, fb=fb, c0=c0: pe.matmul(ps[:, 0:ntok], lhsT=win[:, c, c0 + fb * 128:c0 + (fb + 1) * 128], rhs=hTg[:, c, 0:ntok], start=(c == 0), stop=(c == 7)), [win, hTg], [ps])
                        kb = ktq[(which * 4 + fb) % 2]
                        evac(kb, kb[:, 0:ntok], ps, ps[:, 0:ntok])
                        if which == 0:
                            S.dma("sp", KT_d[fb * 128:(fb + 1) * 128, tok0:tok0 + ntok], kb[:, 0:ntok], reads=[kb], writes=[T_KT])
                        else:
                            S.dma("sp", QT_d[fb * 128:(fb + 1) * 128, tok0 - CTX:tok0 - CTX + ntok], kb[:, 0:ntok], reads=[kb], writes=[T_QT])
                if STOPA <= 3:
                    return
                for k in range(ntl):
                    ti = t0 + k
                    b2 = ti % 2
                    r0 = ti * 128

                    def mm(c0, w_, k=k):
                        ps = nextps()
                        for c in range(8):
                            S.op("pe", lambda c=c, ps=ps: pe.matmul(ps[:, 0:w_], lhsT=hTg[:, c, k * 128:(k + 1) * 128], rhs=win[:, c, c0:c0 + w_], start=(c == 0), stop=(c == 7)), [win, hTg], [ps])
                        return ps

                    ps = mm(512, 512)
                    evac(vo[b2], vo[b2][:, :], ps, ps[:, :])
                    S.dma("sp", V_d[r0:r0 + 128, :], vo[b2][:, :], reads=[vo[b2]], writes=[T_V])
                    TM = int(os.environ.get("TM", "9"))
                    if TM <= 1:
                        continue
                    ps = mm(1024, 512)
                    EV = int(os.environ.get("EV", "3"))
                    if EV & 1:
                        evac(gkq[b2], gkq[b2][:, 0:256], ps, ps[:, 0:256])
                    if EV & 2:
                        evac(gvo[b2], gvo[b2][:, 0:256], ps, ps[:, 256:512])
                    if TM <= 2:
                        continue
                    ps = mm(1536, 288)
                    evac(gvo[b2], gvo[b2][:, 256:512], ps, ps[:, 0:256])
                    evac(gkq[b2], gkq[b2][:, 256:288], ps, ps[:, 256:288])
                    S.dma("sp", GV_d[r0:r0 + 128, :], gvo[b2][:, :], reads=[gvo[b2]], writes=[T_GV])
                    if TM <= 3:
                        continue
                    if not is_ctx:
                        ps = mm(2336, 512)
                        evac(gkq[b2], gkq[b2][:, 288:544], ps, ps[:, 0:256])
                        evac(ggo[b2], ggo[b2][:, 0:256], ps, ps[:, 256:512])
                        ps = mm(2848, 256)
                        evac(ggo[b2], ggo[b2][:, 256:512], ps, ps[:, 0:256])
                        S.dma("sp", GG_d[r0 - CTX:r0 - CTX + 128, :], ggo[b2][:, :], reads=[ggo[b2]], writes=[T_GG])
                        S.dma("sp", GKQ_d[r0:r0 + 128, :], gkq[b2][:, :], reads=[gkq[b2]], writes=[T_GKQ])
                    else:
                        S.dma("sp", GKQ_d[r0:r0 + 128, 0:288], gkq[b2][:, 0:288], reads=[gkq[b2]], writes=[T_GKQ])
                if STOPA <= 4:
                    return


    T_MIXna = T(None, "MIXna")

    def stageB():
        with contextlib.ExitStack() as es:
            NB = 3
            qT = [K.sb(es, [128, 4, 128], BF16, "qT%d" % i) for i in range(2)]
            kT = [K.sb(es, [128, 4, 896], BF16, "kT%d" % i) for i in range(2)]
            va = [K.sb(es, [128, 7, 8, 65], BF16, "va%d" % i) for i in range(2)]
            bint = K.sb(es, [128, 8, 640], F32, "bint")
            bt = [K.sb(es, [128, 640], F32, "bt%d" % i) for i in range(3)]
            tmp = [K.sb(es, [128, 896], F32, "tmp%d" % i) for i in range(NB)]
            pT = [K.sb(es, [128, 896], BF16, "pT%d" % i) for i in range(NB + 1)]
            ona = [K.sb(es, [128, 512], BF16, "ona%d" % i) for i in range(2)]
            rec = [K.sb(es, [128, 1], F32, "rec%d" % i) for i in range(2)]
            sT = [K.ps(es, [128, 1024], F32, "sT%d" % i) for i in range(NB)]
            pv = [K.ps(es, [128, 512], F32, "pv%d" % i) for i in range(2)]
            for v in va:
                S.op("pool", lambda v=v: pool.memset(v[:, :, :, :], 1.0), [], [v])
            S.dma("sp", bint[:, :, :], na_bias[2].rearrange("h j c -> j h c"), writes=[bint])
            QTv = QT_d.rearrange("(c p) t -> p c t", p=128)
            KTv = KT_d.rearrange("(c p) t -> p c t", p=128)
            nbt = [0]
            tmR = [[T(None, "tmR") for _ in range(3)] for _ in range(NB)]
            onaR = [[T(None, "onaR") for _ in range(8)] for _ in range(2)]

            def loads(T_):
                b = T_ % 2
                kt0 = _na_key_tile0(T_)
                S.dma("sp", qT[b][:, :, :], QTv[:, :, T_ * 128:(T_ + 1) * 128], reads=[T_QT], writes=[qT[b]])
                S.dma("sp", kT[b][:, :, 0:640], KTv[:, :, CTX + kt0 * 128:CTX + kt0 * 128 + 640], reads=[T_KT], writes=[kT[b]])
                S.dma("sp", kT[b][:, :, 640:896], KTv[:, :, 0:CTX], reads=[T_KT], writes=[kT[b]])
                r0 = CTX + kt0 * 128
                for kk in range(5):
                    S.dma("sp", va[b][:, kk, :, 0:64], V_d[r0 + kk * 128:r0 + (kk + 1) * 128, :].rearrange("t (h d) -> t h d", h=8), reads=[T_V], writes=[va[b]])
                for kk in range(2):
                    S.dma("sp", va[b][:, 5 + kk, :, 0:64], V_d[kk * 128:(kk + 1) * 128, :].rearrange("t (h d) -> t h d", h=8), reads=[T_V], writes=[va[b]])

            def phase1(n):
                T_, h = n // 8, n % 8
                b = T_ % 2
                if h == 0:
                    loads(T_)
                si = {0: 0, 1: 1, 30: 3, 31: 4}.get(T_, 2)
                pr, P0 = h // 2, (h % 2) * 64
                s_, tm, pt = sT[n % NB], tmp[n % NB], pT[n % (NB + 1)]
                if si == 2:
                    bb_t, bb = bint, bint[:, h, :]
                else:
                    bb_t = bt[nbt[0] % 3]
                    nbt[0] += 1
                    bb = bb_t[:, :]
                    S.dma("sp", bb, na_bias[si, h], writes=[bb_t])
                for kk in range(7):
                    S.op("pe", lambda kk=kk: pe.matmul(s_[:, kk * 128:(kk + 1) * 128], lhsT=kT[b][P0:P0 + 64, pr, kk * 128:(kk + 1) * 128], rhs=qT[b][P0:P0 + 64, pr, :], start=True, stop=True), [kT[b], qT[b]], [s_])
                ta, tb, tc = tmR[n % NB]
                S.op("dve", lambda: dve.scalar_tensor_tensor(out=tm[:, 0:512], in0=s_[:, 0:512], scalar=0.125, in1=bb[:, 0:512], op0=ALU.mult, op1=ALU.add), [s_, bb_t], [ta])
                S.op("dve", lambda: dve.scalar_tensor_tensor(out=tm[:, 512:640], in0=s_[:, 512:640], scalar=0.125, in1=bb[:, 512:640], op0=ALU.mult, op1=ALU.add), [s_, bb_t], [tb])
                S.op("dve", lambda: dve.tensor_scalar(out=tm[:, 640:896], in0=s_[:, 640:896], scalar1=0.125, scalar2=None, op0=ALU.mult), [s_], [tc])
                S.op("act", lambda: act.activation(out=pt[:, :], in_=tm[:, :], func=AF.Exp), [ta, tb, tc], [pt])

            def phase2(n):
                T_, h = n // 8, n % 8
                b = T_ % 2
                pt, p_, rc = pT[n % (NB + 1)], pv[n % 2], rec[n % 2]
                for kk in range(7):
                    S.op("pe", lambda kk=kk: pe.matmul(p_[:, 0:65], lhsT=pt[:, kk * 128:(kk + 1) * 128], rhs=va[b][:, kk, h, :], start=(kk == 0), stop=(kk == 6)), [pt, va[b]], [p_])
                S.op("dve", lambda: dve.reciprocal(out=rc[:, :], in_=p_[:, 64:65]), [p_], [rc])
                S.op("dve", lambda: dve.tensor_scalar(out=ona[b][:, h * 64:(h + 1) * 64], in0=p_[:, 0:64], scalar1=rc[:, 0:1], scalar2=None, op0=ALU.mult), [p_, rc], [onaR[b][h]])
                if h == 7:
                    S.dma("sp", MIX_d[T_ * 128:(T_ + 1) * 128, 0:512], ona[b][:, :], reads=onaR[b], writes=[T_MIXna])

            NIT = 32 * 8
            LOOK = NB - 1
            for n in range(min(LOOK, NIT)):
                phase1(n)
            for n in range(NIT):
                if n + LOOK < NIT:
                    phase1(n + LOOK)
                phase2(n)

    T_OF = T(None, "OF")
    T_MIXgla = T(None, "MIXgla")

    OB_d = dscr("OB_d", [SEQ, 512])
    T_OB = T(None, "OB")

    def stageC():
        with contextlib.ExitStack() as es:
            tri = [K.sb(es, [128, 128], F32, "tri%d" % i) for i in range(2)]
            msk = [K.sb(es, [128, 512], F32, "msk%d" % i) for i in range(2)]
            onec = K.sb(es, [128, 1], F32, "onec")
            aup = K.sb(es, [32, 512], F32, "aup")
            abi = K.sb(es, [1, 512], F32, "abi")
            gnb = K.sb(es, [128, 128], F32, "gnb")
            for t_, src in ((tri[0], tri_f), (tri[1], tri_b), (onec, ones_col), (aup, aup_bd), (abi, abias)):
                S.dma("sp", t_[:, :], src, writes=[t_])
            for h in range(4):
                S.dma("sp", msk[0][:, h * 128:(h + 1) * 128], mask_f, writes=[msk[0]])
                S.dma("sp", msk[1][:, h * 128:(h + 1) * 128], mask_b, writes=[msk[1]])
            S.dma("sp", gnb[:, :], gla_norm.partition_broadcast(128).rearrange("p a b -> p (a b)"), writes=[gnb])
            ps_ad = K.ps(es, [128, 512], F32, "ps_ad")
            ps_z = K.ps(es, [128, 512], F32, "ps_z")
            ps_lam = K.ps(es, [128, 512], F32, "ps_lam")
            ps_t = K.ps(es, [128, 1024], BF16, "ps_t")
            ps_AT = K.ps(es, [128, 512], F32, "ps_AT")
            ps_o = K.ps(es, [128, 512], F32, "ps_o")
            ps_U = K.ps(es, [128, 512], F32, "ps_U")

            class B:
                pass

            def alloc(ci):
                b = B()
                n_ = lambda x: "%s_c%d" % (x, ci)
                b.gkq = [K.sb(es, [128, 544], F32, n_("gkq%d" % i)) for i in range(2)]
                b.gv = [K.sb(es, [128, 512], BF16, n_("gv%d" % i)) for i in range(2)]
                b.cs = [K.sb(es, [128, 32], F32, n_("cs%d" % i)) for i in range(2)]
                b.sn = [K.sb(es, [128, 32], F32, n_("sn%d" % i)) for i in range(2)]
                b.adT = K.sb(es, [32, 128], F32, n_("adT"))
                b.ez = K.sb(es, [128, 256], F32, n_("ez"))
                b.lz = K.sb(es, [128, 256], F32, n_("lz"))
                b.E1 = K.sb(es, [128, 256], F32, n_("E1"))
                b.E2 = K.sb(es, [128, 256], F32, n_("E2"))
                b.at = K.sb(es, [128, 2], F32, n_("at"))
                b.kr = K.sb(es, [128, 256], F32, n_("kr"))
                b.qr = K.sb(es, [128, 256], F32, n_("qr"))
                b.rt = [K.sb(es, [128, 128], F32, n_("rt%d" % i)) for i in range(4)]
                b.qk = K.sb(es, [128, 512], BF16, n_("qk"))
                b.qTz = K.sb(es, [128, 4, 128], BF16, n_("qTz"))
                S.op("pool", lambda: pool.memset(b.qTz[:, :, :], 0.0), [], [b.qTz])
                b.kTc = K.sb(es, [128, 2, 128], BF16, n_("kTc"))
                b.ATs = K.sb(es, [128, 4, 128], BF16, n_("ATs"))
                b.Sst = [K.sb(es, [128, 128], F32, n_("Sst%d" % i)) for i in range(2)]
                b.Sbf = [K.sb(es, [128, 128], BF16, n_("Sbf%d" % i)) for i in range(2)]
                b.tU = K.sb(es, [128, 128], F32, n_("tU"))
                b.osb = [K.sb(es, [128, 512], F32, n_("osb%d" % i)) for i in range(2)]
                return b

            def rope(eng, ek, src_t, src_ap, dst_t, tmps, c_, s_):
                X = src_ap.rearrange("p (h a b f) -> p h a b f", h=4, a=2, b=2)
                O = dst_t[:, :].rearrange("p (h a b f) -> p h a b f", h=4, a=2, b=2)
                C = c_[:, :].rearrange("p (a f) -> p a f", a=2).unsqueeze(1).broadcast_to([128, 4, 2, 16])
                Sn = s_[:, :].rearrange("p (a f) -> p a f", a=2).unsqueeze(1).broadcast_to([128, 4, 2, 16])
                v = lambda t_: t_[:, :].rearrange("p (h a f) -> p h a f", h=4, a=2)
                X1, X2 = X[:, :, :, 0, :], X[:, :, :, 1, :]
                t1, t2, t3, t4 = tmps
                S.op(ek, lambda: eng.tensor_tensor(out=v(t1), in0=X1, in1=C, op=ALU.mult), [src_t, c_], [t1])
                S.op(ek, lambda: eng.tensor_tensor(out=v(t2), in0=X2, in1=Sn, op=ALU.mult), [src_t, s_], [t2])
                S.op(ek, lambda: eng.tensor_tensor(out=O[:, :, :, 0, :], in0=v(t1), in1=v(t2), op=ALU.subtract), [t1, t2], [dst_t])
                S.op(ek, lambda: eng.tensor_tensor(out=v(t3), in0=X1, in1=Sn, op=ALU.mult), [src_t, s_], [t3])
                S.op(ek, lambda: eng.tensor_tensor(out=v(t4), in0=X2, in1=C, op=ALU.mult), [src_t, c_], [t4])
                S.op(ek, lambda: eng.tensor_tensor(out=O[:, :, :, 1, :], in0=v(t3), in1=v(t4), op=ALU.add), [t3, t4], [dst_t])

            def chain(dr, c):
                for pr in range(2):
                    S.op("pool", lambda pr=pr: pool.memset(c.Sst[pr][:, :], 0.0), [], [c.Sst[pr]])
                    S.op("pool", lambda pr=pr: pool.memset(c.Sbf[pr][:, :], 0.0), [], [c.Sbf[pr]])
                order = list(range(NT)) if dr == 0 else [1, 0] + list(range(NT - 1, 1, -1))
                n = 0
                for ti in order:
                    lat = ti >= 2
                    li = ti - 2
                    b = n % 2
                    n += 1
                    g_, v_ = c.gkq[b], c.gv[b]
                    gw = 544 if lat else 288
                    S.dma("sp", g_[:, 0:gw], GKQ_d[ti * 128:(ti + 1) * 128, 0:gw], reads=[T_GKQ], writes=[g_])
                    S.dma("sp", v_[:, :], GV_d[ti * 128:(ti + 1) * 128, :], reads=[T_GV], writes=[v_])
                    if lat:
                        S.dma("sp", c.cs[b][:, :], rope_cos[li * 128:(li + 1) * 128, :], writes=[c.cs[b]])
                        S.dma("sp", c.sn[b][:, :], rope_sin[li * 128:(li + 1) * 128, :], writes=[c.sn[b]])
                    yield
                    S.op("pe", lambda: pe.transpose(out=ps_ad[0:32, 0:128], in_=g_[:, 256:288], identity=idf[:, :]), [g_, idf], [ps_ad])
                    S.op("act", lambda: act.copy(out=c.adT[:, :], in_=ps_ad[0:32, 0:128]), [ps_ad], [c.adT])
                    S.op("pe", lambda: pe.matmul(ps_z[:, 0:256], lhsT=c.adT[:, :], rhs=aup[:, dr * 256:(dr + 1) * 256], start=True, stop=False), [c.adT, aup], [ps_z])
                    S.op("pe", lambda: pe.matmul(ps_z[:, 0:256], lhsT=onesr[:, :], rhs=abi[:, dr * 256:(dr + 1) * 256], start=False, stop=True), [onesr, abi], [ps_z])
                    S.op("act", lambda: act.activation(out=c.ez[:, :], in_=ps_z[:, 0:256], func=AF.Exp, scale=-1.0), [ps_z], [c.ez])
                    yield
                    S.op("act", lambda: act.activation(out=c.lz[:, :], in_=c.ez[:, :], func=AF.Ln, bias=1.0), [c.ez], [c.lz])
                    S.op("pe", lambda: pe.matmul(ps_lam[:, 0:256], lhsT=tri[dr][:, :], rhs=c.lz[:, :], start=True, stop=True), [tri[dr], c.lz], [ps_lam])
                    for pr in range(2):
                        S.op("pe", lambda pr=pr: pe.matmul(ps_lam[:, 256 + pr:257 + pr], lhsT=c.lz[:, pr * 128:(pr + 1) * 128], rhs=onec[:, :], start=True, stop=True), [c.lz, onec], [ps_lam])
                    if lat:
                        S.op("act", lambda: act.activation(out=c.E1[:, :], in_=ps_lam[:, 0:256], func=AF.Exp), [ps_lam], [c.E1])
                    S.op("act", lambda: act.activation(out=c.E2[:, :], in_=ps_lam[:, 0:256], func=AF.Exp, scale=-1.0), [ps_lam], [c.E2])
                    S.op("act", lambda: act.activation(out=c.at[:, :], in_=ps_lam[:, 256:258], func=AF.Exp), [ps_lam], [c.at])
                    yield
                    if lat:
                        rope(dve, "dve", g_, g_[:, 0:256], c.kr, c.rt, c.cs[b], c.sn[b])
                        yield
                        rope(dve, "dve", g_, g_[:, 288:544], c.qr, c.rt, c.cs[b], c.sn[b])
                        yield
                        S.op("dve", lambda: dve.scalar_tensor_tensor(out=c.qk[:, 0:256], in0=c.qr[:, :], scalar=0.125, in1=c.E1[:, :], op0=ALU.mult, op1=ALU.mult), [c.qr, c.E1], [c.qk])
                        S.op("dve", lambda: dve.tensor_tensor(out=c.qk[:, 256:512], in0=c.kr[:, :], in1=c.E2[:, :], op=ALU.mult), [c.kr, c.E2], [c.qk])
                    else:
                        S.op("dve", lambda: dve.tensor_tensor(out=c.qk[:, 256:512], in0=g_[:, 0:256], in1=c.E2[:, :], op=ALU.mult), [g_, c.E2], [c.qk])
                    for j in (range(4) if lat else range(2, 4)):
                        S.op("pe", lambda j=j: pe.transpose(out=ps_t[:, j * 128:(j + 1) * 128], in_=c.qk[:, j * 128:(j + 1) * 128], identity=idb[:, :]), [c.qk, idb], [ps_t])
                    if lat:
                        for h in range(4):
                            S.op("act", lambda h=h: act.copy(out=c.qTz[(h % 2) * 64:(h % 2) * 64 + 64, h, :], in_=ps_t[(h % 2) * 64:(h % 2) * 64 + 64, (h // 2) * 128:(h // 2 + 1) * 128]), [ps_t], [c.qTz])
                    S.op("act", lambda: act.copy(out=c.kTc[:, :, :], in_=ps_t[:, 256:512].rearrange("p (c t) -> p c t", c=2)), [ps_t], [c.kTc])
                    yield
                    if lat:
                        for h in range(4):
                            pr = h // 2
                            S.op("pe", lambda h=h, pr=pr: pe.matmul(ps_AT[:, h * 128:(h + 1) * 128], lhsT=c.kTc[:, pr, :], rhs=c.qTz[:, h, :], start=True, stop=True), [c.kTc, c.qTz], [ps_AT])
                        S.op("dve", lambda: dve.tensor_tensor(out=c.ATs[:, :, :].rearrange("p h t -> p (h t)"), in0=ps_AT[:, :], in1=msk[dr][:, :], op=ALU.mult), [ps_AT, msk[dr]], [c.ATs])
                        for h in range(4):
                            pr = h // 2
                            S.op("pe", lambda h=h: pe.matmul(ps_o[:, h * 128:(h + 1) * 128], lhsT=c.ATs[:, h, :], rhs=v_[:, h * 128:(h + 1) * 128], start=True, stop=False), [c.ATs, v_], [ps_o])
                            S.op("pe", lambda h=h, pr=pr: pe.matmul(ps_o[:, h * 128:(h + 1) * 128], lhsT=c.qTz[:, h, :], rhs=c.Sbf[pr][:, :], start=False, stop=True), [c.qTz, c.Sbf[pr]], [ps_o])
                        ob_ = c.osb[b]
                        S.op("dve", lambda: dve.tensor_copy(out=ob_[:, :], in_=ps_o[:, :]), [ps_o], [ob_])
                        if dr == 0:
                            S.dma("sp", OF_d[li * 128:(li + 1) * 128, :], ob_[:, :], reads=[ob_], writes=[T_OF])
                        else:
                            S.dma("sp", OB_d[li * 128:(li + 1) * 128, :], ob_[:, :], reads=[ob_], writes=[T_OB])
                        yield
                    for h in range(4):
                        pr = h // 2
                        S.op("pe", lambda h=h, pr=pr: pe.matmul(ps_U[:, h * 128:(h + 1) * 128], lhsT=c.qk[:, 256 + pr * 128:256 + (pr + 1) * 128], rhs=v_[:, h * 128:(h + 1) * 128], start=True, stop=True), [c.qk, v_], [ps_U])
                    for h in range(4):
                        pr, P0 = h // 2, (h % 2) * 64
                        S.op("dve", lambda h=h, pr=pr, P0=P0: dve.tensor_scalar(out=c.tU[P0:P0 + 64, :], in0=ps_U[P0:P0 + 64, h * 128:(h + 1) * 128], scalar1=c.at[P0:P0 + 64, pr:pr + 1], scalar2=None, op0=ALU.mult), [ps_U, c.at], [c.tU])
                        S.op("dve", lambda pr=pr, P0=P0: dve.scalar_tensor_tensor(out=c.Sst[pr][P0:P0 + 64, :], in0=c.Sst[pr][P0:P0 + 64, :], scalar=c.at[P0:P0 + 64, pr:pr + 1], in1=c.tU[P0:P0 + 64, :], op0=ALU.mult, op1=ALU.add), [c.Sst[pr], c.at, c.tU], [c.Sst[pr]])
                        S.op("dve", lambda pr=pr, P0=P0: dve.tensor_copy(out=c.Sbf[pr][P0:P0 + 64, :], in_=c.Sst[pr][P0:P0 + 64, :]), [c.Sst[pr]], [c.Sbf[pr]])
                    yield

            gens = [chain(0, alloc(0)), chain(1, alloc(1))]
            while gens:
                for g in list(gens):
                    try:
                        next(g)
                    except StopIteration:
                        gens.remove(g)

            gg = [K.sb(es, [128, 512], F32, "gg%d" % i) for i in range(2)]
            ofl = [K.sb(es, [128, 512], F32, "ofl%d" % i) for i in range(2)]
            obl = [K.sb(es, [128, 512], F32, "obl%d" % i) for i in range(2)]
            osm = [K.sb(es, [128, 512], F32, "osm%d" % i) for i in range(2)]
            junk = [K.sb(es, [128, 128], F32, "junkc%d" % i) for i in range(2)]
            ss4 = [K.sb(es, [128, 4], F32, "ss4_%d" % i) for i in range(2)]
            rs4 = [K.sb(es, [128, 4], F32, "rs4_%d" % i) for i in range(2)]
            sg = [K.sb(es, [128, 512], F32, "sg%d" % i) for i in range(2)]
            ogl = [K.sb(es, [128, 512], BF16, "ogl%d" % i) for i in range(2)]

            def merge(li):
                b = li % 2
                S.dma("sp", gg[b][:, :], GG_d[li * 128:(li + 1) * 128, :], reads=[T_GG], writes=[gg[b]])
                S.dma("sp", ofl[b][:, :], OF_d[li * 128:(li + 1) * 128, :], reads=[T_OF], writes=[ofl[b]])
                S.dma("sp", obl[b][:, :], OB_d[li * 128:(li + 1) * 128, :], reads=[T_OB], writes=[obl[b]])
                yield
                S.op("pool", lambda: pool.tensor_tensor(out=osm[b][:, :], in0=ofl[b][:, :], in1=obl[b][:, :], op=ALU.add), [ofl[b], obl[b]], [osm[b]])
                S.op("act", lambda: act.activation(out=sg[b][:, :], in_=gg[b][:, :], func=AF.Silu), [gg[b]], [sg[b]])
                yield
                for h in range(4):
                    S.op("act", lambda h=h: act.activation(out=junk[b][:, :], in_=osm[b][:, h * 128:(h + 1) * 128], func=AF.Square, accum_out=ss4[b][:, h:h + 1]), [osm[b]], [junk[b], ss4[b]])
                S.op("act", lambda: act.activation(out=rs4[b][:, :], in_=ss4[b][:, :], func=AF.Sqrt, scale=1.0 / 128.0, bias=epsb[:, :]), [ss4[b], epsb], [rs4[b]])
                yield
                S.op("dve", lambda: dve.reciprocal(out=rs4[b][:, :], in_=rs4[b][:, :]), [rs4[b]], [rs4[b]])
                o3 = osm[b][:, :].rearrange("p (h e) -> p h e", h=4)
                S.op("dve", lambda: dve.tensor_tensor(out=o3, in0=o3, in1=rs4[b][:, :].unsqueeze(2).broadcast_to([128, 4, 128]), op=ALU.mult), [osm[b], rs4[b]], [osm[b]])
                yield
                S.op("pool", lambda: pool.tensor_tensor(out=o3, in0=o3, in1=gnb[:, :].unsqueeze(1).broadcast_to([128, 4, 128]), op=ALU.mult), [osm[b], gnb], [osm[b]])
                S.op("dve", lambda: dve.tensor_tensor(out=ogl[b][:, :], in0=osm[b][:, :], in1=sg[b][:, :], op=ALU.mult), [osm[b], sg[b]], [ogl[b]])
                S.dma("sp", MIX_d[li * 128:(li + 1) * 128, 512:1024], ogl[b][:, :], reads=[ogl[b]], writes=[T_MIXgla])
                yield

            pend = []
            for li in range(32):
                pend.append(merge(li))
                if len(pend) == 2 or li == 31:
                    live = list(pend)
                    while live:
                        for g in list(live):
                            try:
                                next(g)
                            except StopIteration:
                                live.remove(g)
                    pend = []

    T_XN = T(None, "XN")
    T_HF = T(None, "HF")
    T_AFFT = T(None, "AFFT")
    AX = mybir.AxisListType.X

    def stageD():
        with contextlib.ExitStack() as es:
            wo = K.sb(es, [128, 8, D], BF16, "wo")
            wst = [K.sb(es, [128, 8, 512], F32, "wstd%d" % i) for i in range(2)]
            wov = w_out.rearrange("(c p) n -> p c n", p=128)
            for b in range(2):
                S.dma("sp", wst[b][:, :, :], wov[:, :, b * 512:(b + 1) * 512], writes=[wst[b]])
                S.op("act", lambda b=b: act.copy(out=wo[:, :, b * 512:(b + 1) * 512], in_=wst[b][:, :, :]), [wst[b]], [wo])
            rt = K.sb(es, [128, 8, NE], F32, "rt")
            S.dma("sp", rt[:, :, :], router.rearrange("(c p) e -> p c e", p=128), writes=[rt])
            md = K.sb(es, [128, 3, D], F32, "md")
            S.dma("sp", md[:, :, :], mods_d[:, 4:7, :], reads=[T_mods], writes=[md])
            affT = K.sb(es, [NE, SEQ], F32, "affT")
            affR = [T(None, "affR%d" % i) for i in range(32)]
            dbl = lambda shape, dt, nm: [K.sb(es, shape, dt, "%s%d" % (nm, i)) for i in range(2)]
            mixb = dbl([128, D], BF16, "mixb")
            xt = dbl([128, D], F32, "xtd")
            mixT = dbl([128, 8, 128], BF16, "mixT")
            mixs = dbl([128, D], F32, "mixs")
            junk = dbl([128, D], BF16, "junkd")
            ss = dbl([128, 1], F32, "ssd")
            rstd = dbl([128, 1], F32, "rstdd")
            t1 = dbl([128, D], F32, "t1d")
            xn = dbl([128, D], F32, "xn")
            hf32 = dbl([128, D], F32, "hf32")
            hfb = dbl([128, D], BF16, "hfb")
            hfT = dbl([128, 8, 128], F32, "hfT")
            lg = dbl([128, NE], F32, "lg")
            mx = dbl([128, 1], F32, "mx")
            ex = dbl([128, NE], F32, "ex")
            sm = dbl([128, 1], F32, "sm")
            af = dbl([128, NE], F32, "af")
            ps_t = K.ps(es, [128, 1024], BF16, "psd_t")
            ps_m = [K.ps(es, [128, 512], F32, "psd_m%d" % i) for i in range(2)]
            ps_r = [K.ps(es, [128, 512], F32, "psd_r%d" % i) for i in range(2)]
            ps_l = K.ps(es, [128, 512], F32, "psd_l")
            ps_a = K.ps(es, [128, 512], F32, "psd_a")

            def tile_prog(T_):
                b = T_ % 2
                mb, x_ = mixb[b], xt[b]
                S.dma("sp", mb[:, :], MIX_d[T_ * 128:(T_ + 1) * 128, :], reads=[T_MIXna, T_MIXgla], writes=[mb])
                S.dma("sp", x_[:, :], xc[CTX + T_ * 128:CTX + (T_ + 1) * 128, :], writes=[x_])
                yield
                for c in range(8):
                    S.op("pe", lambda c=c: pe.transpose(out=ps_t[:, c * 128:(c + 1) * 128], in_=mb[:, c * 128:(c + 1) * 128], identity=idb[:, :]), [mb, idb], [ps_t])
                S.op("act", lambda: act.copy(out=mixT[b][:, :, :], in_=ps_t[:, :].rearrange("p (c t) -> p c t", c=8)), [ps_t], [mixT[b]])
                yield
                for hlf in range(2):
                    for c in range(8):
                        S.op("pe", lambda c=c, hlf=hlf: pe.matmul(ps_m[hlf][:, :], lhsT=mixT[b][:, c, :], rhs=wo[:, c, hlf * 512:(hlf + 1) * 512], start=(c == 0), stop=(c == 7)), [mixT[b], wo], [ps_m[hlf]])
                    S.op("act", lambda hlf=hlf: act.copy(out=mixs[b][:, hlf * 512:(hlf + 1) * 512], in_=ps_m[hlf][:, :]), [ps_m[hlf]], [mixs[b]])
                yield
                rms_rstd(None, mixs[b][:, :], mixs[b], ss[b], rstd[b], junk[b])
                yield
                S.op("dve", lambda: dve.scalar_tensor_tensor(out=t1[b][:, :], in0=mixs[b][:, :], scalar=rstd[b][:, 0:1], in1=md[:, 0, :], op0=ALU.mult, op1=ALU.mult), [mixs[b], rstd[b], md], [t1[b]])
                xn_ = xn[b]
                S.op("pool", lambda: pool.tensor_tensor(out=xn_[:, :], in0=t1[b][:, :], in1=x_[:, :], op=ALU.add), [t1[b], x_], [xn_])
                S.dma("sp", XN_d[T_ * 128:(T_ + 1) * 128, :], xn_[:, :], reads=[xn_], writes=[T_XN])
                yield
                rms_rstd(None, xn_[:, :], xn_, ss[b], rstd[b], junk[b])
                yield
                S.op("dve", lambda: dve.scalar_tensor_tensor(out=t1[b][:, :], in0=xn_[:, :], scalar=rstd[b][:, 0:1], in1=md[:, 1, :], op0=ALU.mult, op1=ALU.mult), [xn_, rstd[b], md], [t1[b]])
                S.op("pool", lambda: pool.tensor_tensor(out=hf32[b][:, :], in0=t1[b][:, :], in1=md[:, 2, :], op=ALU.add), [t1[b], md], [hf32[b]])
                yield
                hb_ = hfb[b]
                S.op("act", lambda: act.copy(out=hb_[:, :], in_=hf32[b][:, :]), [hf32[b]], [hb_])
                S.dma("sp", HF_d[T_ * 128:(T_ + 1) * 128, :], hb_[:, :], reads=[hb_], writes=[T_HF])
                for c in range(8):
                    S.op("pe", lambda c=c: pe.transpose(out=ps_r[c // 4][:, (c % 4) * 128:(c % 4 + 1) * 128], in_=hf32[b][:, c * 128:(c + 1) * 128], identity=idf[:, :]), [hf32[b], idf], [ps_r[c // 4]])
                for i in range(2):
                    S.op("act", lambda i=i: act.copy(out=hfT[b][:, i * 4:(i + 1) * 4, :], in_=ps_r[i][:, :].rearrange("p (c t) -> p c t", c=4)), [ps_r[i]], [hfT[b]])
                yield
                for c in range(8):
                    S.op("pe", lambda c=c: pe.matmul(ps_l[:, 0:NE], lhsT=hfT[b][:, c, :], rhs=rt[:, c, :], start=(c == 0), stop=(c == 7)), [hfT[b], rt], [ps_l])
                S.op("dve", lambda: dve.tensor_copy(out=lg[b][:, :], in_=ps_l[:, 0:NE]), [ps_l], [lg[b]])
                yield
                S.op("dve", lambda: dve.reduce_max(out=mx[b][:, :], in_=lg[b][:, :], axis=AX), [lg[b]], [mx[b]])
                S.op("dve", lambda: dve.tensor_scalar(out=mx[b][:, :], in0=mx[b][:, :], scalar1=-1.0, scalar2=None, op0=ALU.mult), [mx[b]], [mx[b]])
                yield
                S.op("act", lambda: act.activation(out=ex[b][:, :], in_=lg[b][:, :], func=AF.Exp, bias=mx[b][:, :], accum_out=sm[b][:, :]), [lg[b], mx[b]], [ex[b], sm[b]])
                yield
                S.op("dve", lambda: dve.reciprocal(out=sm[b][:, :], in_=sm[b][:, :]), [sm[b]], [sm[b]])
                S.op("dve", lambda: dve.tensor_scalar(out=af[b][:, :], in0=ex[b][:, :], scalar1=sm[b][:, 0:1], scalar2=None, op0=ALU.mult), [ex[b], sm[b]], [af[b]])
                yield
                S.op("pe", lambda: pe.transpose(out=ps_a[0:NE, 0:128], in_=af[b][:, :], identity=idf[:, :]), [af[b], idf], [ps_a])
                S.op("act", lambda: act.copy(out=affT[:, T_ * 128:(T_ + 1) * 128], in_=ps_a[0:NE, 0:128]), [ps_a], [affR[T_]])
                yield

            for T0 in range(0, 32, 2):
                live = [tile_prog(T0), tile_prog(T0 + 1)]
                while live:
                    for g in list(live):
                        try:
                            next(g)
                        except StopIteration:
                            live.remove(g)
            S.dma("sp", AFFT_d, affT[:, :], reads=affR, writes=[T_AFFT])


    posT_d = dscr("posT_d", [128, 4, 64])
    gateT_d = dscr("gateT_d", [128, 4, 64])
    HFO_d = dscr("HFO_d", [SEQ // 2, D], BF16)
    T_HFO = T(None, "HFO")

    def stageEFG():
        import os
        with contextlib.ExitStack() as es:
            posT = K.sb(es, [128, 4, 64], F32, "posT")
            gateT = K.sb(es, [128, 4, 64], F32, "gateT")
            iot = K.sb(es, [128, 512], F32, "iot")
            S.dma("sp", iot[:, :], iota512, writes=[iot])
            PB = [K.ps(es, [128, 512], F32, "PB%d" % i) for i in range(8)]
            with contextlib.ExitStack() as e2:
                A = K.sb(e2, [128, 512], F32, "A")
                S.dma("sp", A[:, :], AFFT_d.rearrange("e (s t) -> (e s) t", s=8), reads=[T_AFFT], writes=[A])
                b1 = K.sb(e2, [128, 128], F32, "b1")
                blt = K.sb(e2, [128, 128], F32, "blt")
                ownf = K.sb(e2, [128, 1], F32, "ownf")
                selm = K.sb(e2, [128, 64], F32, "selm")
                for t_, src in ((b1, blk_ones), (blt, blk_lt), (ownf, own_flag), (selm, sel)):
                    S.dma("sp", t_[:, :], src, writes=[t_])
                lo = K.sb(e2, [128, 1], F32, "lo")
                hi = K.sb(e2, [128, 1], F32, "hi")
                mid = K.sb(e2, [128, 1], F32, "mid")
                cnt = K.sb(e2, [128, 1], F32, "cnt")
                cond = K.sb(e2, [128, 1], F32, "cond")
                d1 = K.sb(e2, [128, 1], F32, "d1")
                jk = K.sb(e2, [128, 512], F32, "jk")
                onesT = K.sb(e2, [128, 512], F32, "onesT")
                M_ = K.sb(e2, [128, 512], F32, "M_")
                posi = K.sb(e2, [128, 512], F32, "posi")
                pm = K.sb(e2, [128, 512], F32, "pm")
                offc = K.sb(e2, [128, 1], F32, "offc")
                S.op("pool", lambda: pool.memset(lo[:, :], 0.0), [], [lo])
                S.op("pool", lambda: pool.memset(hi[:, :], 1.0), [], [hi])
                S.op("pool", lambda: pool.memset(onesT[:, :], 1.0), [], [onesT])
                pc = PB[0]
                for it in range(30):
                    S.op("dve", lambda: dve.tensor_tensor(out=mid[:, :], in0=lo[:, :], in1=hi[:, :], op=ALU.add), [lo, hi], [mid])
                    S.op("dve", lambda: dve.tensor_scalar(out=mid[:, :], in0=mid[:, :], scalar1=0.5, scalar2=None, op0=ALU.mult), [mid], [mid])
                    S.op("dve", lambda: dve.tensor_scalar(out=jk[:, :], in0=A[:, :], scalar1=mid[:, 0:1], scalar2=0.0, op0=ALU.is_gt, op1=ALU.add, accum_out=cnt[:, 0:1]), [A, mid], [jk, cnt])
                    S.op("pe", lambda: pe.matmul(pc[:, 0:1], lhsT=b1[:, :], rhs=cnt[:, 0:1], start=True, stop=True), [b1, cnt], [pc])
                    S.op("dve", lambda: dve.tensor_scalar(out=cond[:, :], in0=pc[:, 0:1], scalar1=float(CAP) - 0.5, scalar2=None, op0=ALU.is_gt), [pc], [cond])
                    S.op("dve", lambda: dve.tensor_tensor(out=d1[:, :], in0=mid[:, :], in1=lo[:, :], op=ALU.subtract), [mid, lo], [d1])
                    S.op("dve", lambda: dve.scalar_tensor_tensor(out=lo[:, :], in0=d1[:, :], scalar=cond[:, 0:1], in1=lo[:, :], op0=ALU.mult, op1=ALU.add), [d1, cond, lo], [lo])
                    S.op("dve", lambda: dve.tensor_tensor(out=d1[:, :], in0=hi[:, :], in1=mid[:, :], op=ALU.subtract), [hi, mid], [d1])
                    S.op("dve", lambda: dve.scalar_tensor_tensor(out=hi[:, :], in0=d1[:, :], scalar=cond[:, 0:1], in1=mid[:, :], op0=ALU.mult, op1=ALU.add), [d1, cond, mid], [hi])
                S.op("dve", lambda: dve.tensor_scalar(out=M_[:, :], in0=A[:, :], scalar1=lo[:, 0:1], scalar2=ownf[:, 0:1], op0=ALU.is_gt, op1=ALU.mult), [A, lo, ownf], [M_])
                S.op("dve", lambda: dve.tensor_tensor_scan(out=posi[:, :], data0=onesT[:, :], data1=M_[:, :], initial=0.0, op0=ALU.mult, op1=ALU.add), [onesT, M_], [posi])
                S.op("pe", lambda: pe.matmul(pc[:, 0:1], lhsT=blt[:, :], rhs=posi[:, 511:512], start=True, stop=True), [blt, posi], [pc])
                S.op("dve", lambda: dve.tensor_scalar(out=offc[:, :], in0=pc[:, 0:1], scalar1=-10000.0, scalar2=None, op0=ALU.add), [pc], [offc])
                S.op("dve", lambda: dve.tensor_scalar(out=pm[:, :], in0=posi[:, :], scalar1=offc[:, 0:1], scalar2=None, op0=ALU.add), [posi, offc], [pm])
                S.op("dve", lambda: dve.tensor_tensor(out=pm[:, :], in0=pm[:, :], in1=M_[:, :], op=ALU.mult), [pm, M_], [pm])
                S.op("dve", lambda: dve.tensor_scalar(out=pm[:, :], in0=pm[:, :], scalar1=9999.0, scalar2=None, op0=ALU.add), [pm], [pm])
                for src, dst, pb in ((pm, posT, PB[1]), (A, gateT, PB[2])):
                    for blk in range(4):
                        S.op("pe", lambda src=src, pb=pb, blk=blk: pe.matmul(pb[:, blk * 64:(blk + 1) * 64], lhsT=src[:, blk * 128:(blk + 1) * 128], rhs=selm[:, :], start=True, stop=True), [src, selm], [pb])
                    S.op("act", lambda dst=dst, pb=pb: act.copy(out=dst[:, :, :], in_=pb[:, 0:256].rearrange("p (b c) -> p b c", b=4)), [pb], [dst])
                if debug:
                    S.dma("sp", posT_d, posT[:, :, :], reads=[posT])
                    S.dma("sp", gateT_d, gateT[:, :, :], reads=[gateT])
                S.barrier()
            S.barrier()
            if upto == "E":
                return
            oi = K.sb(es, [128, 16], I32, "oi")
            S.dma("sp", oi[:, :], own_idx, writes=[oi])
            with contextlib.ExitStack() as e4:
                hfj = [K.sb(e4, [128, D], BF16, "hfj%d" % i) for i in range(3)]
                for j in range(16):
                    hj = hfj[j % 3]
                    S.dma("pool", None, None, reads=[T_HF, oi], writes=[hj], fn=lambda hj=hj, j=j: pool.indirect_dma_start(
                        out=hj[:, :], out_offset=None, in_=HF_d, in_offset=bass.IndirectOffsetOnAxis(ap=oi[:, j:j + 1], axis=0)))
                    S.dma("sp", HFO_d[j * 128:(j + 1) * 128, :], hj[:, :], reads=[hj], writes=[T_HFO])
                S.barrier()
            yacc = K.sb(es, [128, 16, D], F32, "yacc")
            S.op("pool", lambda: pool.memset(yacc[:, :, :], 0.0), [], [yacc])
            e3 = contextlib.ExitStack()
            xsT = K.sb(e3, [128, 8, 512], BF16, "xsT")
            hidT = K.sb(e3, [128, NFC, 512], BF16, "hidT")
            ysb = K.sb(e3, [128, 4, D], BF16, "ysb")
            Ej = [K.sb(e3, [128, 512], BF16, "Ej%d" % i) for i in range(2)]
            Gj = [K.sb(e3, [128, 512], BF16, "Gj%d" % i) for i in range(2)]
            tidt = K.sb(e3, [128, 16, 2], BF16, "tidt")
            S.dma("sp", tidt[:, :, :], tid_in, writes=[tidt])
            xg = [K.sb(e3, [128, 4, D], BF16, "xg%d" % i) for i in range(2)]
            xgT = [[T(None, "xgT") for _ in range(4)] for _ in range(2)]
            idxs = K.sb(e3, [128, 8], F32, "idxs")
            idxf = K.sb(e3, [128, 4], F32, "idxf")
            idxi = [K.sb(e3, [128, 4], I32, "idxi%d" % i) for i in range(2)]
            GTa = K.sb(e3, [128, 16, 4, 128], BF16, "GTa")
            GTt = [T(None, "GTt%d" % i) for i in range(16)]
            sgt = [K.sb(e3, [128, 512], BF16, "sgt%d" % i) for i in range(2)]
            stg = [K.sb(e3, [128, 2048], F32, "stg%d" % i) for i in range(4)]
            nst = [0]

            def stage_in(src_ap, ncols):
                st = stg[nst[0] % 4]
                q = os.environ.get("WQ", "sp").split(",")
                q = q[nst[0] % len(q)]
                nst[0] += 1
                S.dma(q, st[:, 0:ncols], src_ap, writes=[st])
                return st
            wgb = [K.sb(e3, [128, 8, 256], BF16, "wgb%d" % i) for i in range(2)]
            wub = [K.sb(e3, [128, 8, 256], BF16, "wub%d" % i) for i in range(2)]
            wdb = K.sb(e3, [128, NFC, 256], BF16, "wdb")
            wdbT = [T(None, "wdbT%d" % i) for i in range(3)]
            NEX = int(os.environ.get("NEX", str(NE)))
            ng = 0
            PH = int(os.environ.get("PH", "15"))
            def route(e):
                xb = e % 2
                for j in range(16):
                    osg, blk = j // 4, j % 4
                    E_ = Ej[j % 2]
                    S.op("dve", lambda E_=E_, blk=blk, col=e * 4 + osg: dve.tensor_scalar(out=E_[:, :], in0=iot[:, :], scalar1=posT[:, blk, col:col + 1], scalar2=None, op0=ALU.is_equal), [iot, posT], [E_])
                    for sc in range(4):
                        S.op("pe", lambda sc=sc, E_=E_, j=j: pe.matmul(PB[sc][:, 0:2], lhsT=E_[:, sc * 128:(sc + 1) * 128], rhs=tidt[:, j, :], start=(j == 0), stop=(j == 15)), [E_, tidt], [PB[sc]])
                for sc in range(4):
                    S.op("dve", lambda sc=sc: dve.tensor_copy(out=idxs[:, sc * 2:(sc + 1) * 2], in_=PB[sc][:, 0:2]), [PB[sc]], [idxs])
                iv = idxs[:, :].rearrange("p (s two) -> p s two", two=2)
                S.op("dve", lambda: dve.scalar_tensor_tensor(out=idxf[:, :], in0=iv[:, :, 0], scalar=64.0, in1=iv[:, :, 1], op0=ALU.mult, op1=ALU.add), [idxs], [idxf])
                S.op("dve", lambda: dve.tensor_copy(out=idxi[xb][:, :], in_=idxf[:, :]), [idxf], [idxi[xb]])
                for sc in range(4):
                    S.dma("pool", None, None, reads=[T_HFO, idxi[xb]], writes=[xgT[xb][sc]], fn=lambda sc=sc: pool.indirect_dma_start(
                        out=xg[xb][:, sc, :], out_offset=None, in_=HFO_d, in_offset=bass.IndirectOffsetOnAxis(ap=idxi[xb][:, sc:sc + 1], axis=0)))

            if PH & 1:
                route(0)
            for e in range(NEX):
                xb = e % 2
                for c in (range(8) if PH & 1 else []):
                    bb = PB[c // 2][:, :].bitcast(BF16)
                    for sc in range(4):
                        S.op("pe", lambda c=c, sc=sc, bb=bb: pe.transpose(out=bb[:, (c % 2) * 512 + sc * 128:(c % 2) * 512 + (sc + 1) * 128], in_=xg[xb][:, sc, c * 128:(c + 1) * 128], identity=idb[:, :]), [xgT[xb][sc], idb], [PB[c // 2]])
                for c in (range(8) if PH & 1 else []):
                    bb = PB[c // 2][:, :].bitcast(BF16)
                    if (c // 2) % 2 == 0:
                        S.op("act", lambda c=c, bb=bb: act.copy(out=xsT[:, c, :], in_=bb[:, (c % 2) * 512:(c % 2 + 1) * 512]), [PB[c // 2]], [xsT])
                    else:
                        S.op("dve", lambda c=c, bb=bb: dve.tensor_copy(out=xsT[:, c, :], in_=bb[:, (c % 2) * 512:(c % 2 + 1) * 512]), [PB[c // 2]], [xsT])

                def prep(j):
                    osg, blk = j // 4, j % 4
                    col = e * 4 + osg
                    G_ = Gj[j % 2]
                    S.op("dve", lambda: dve.tensor_scalar(out=G_[:, :], in0=iot[:, :], scalar1=posT[:, blk, col:col + 1], scalar2=gateT[:, blk, col:col + 1], op0=ALU.is_equal, op1=ALU.mult), [iot, posT, gateT], [G_])
                    pt = PB[6 + j % 2]
                    ptb = pt[:, :].bitcast(BF16)
                    for sc in range(4):
                        S.op("pe", lambda sc=sc: pe.transpose(out=ptb[:, sc * 128:(sc + 1) * 128], in_=G_[:, sc * 128:(sc + 1) * 128], identity=idb[:, :]), [G_, idb], [pt])
                    S.op("act", lambda: act.copy(out=GTa[:, j, :, :], in_=ptb[:, 0:512].rearrange("p (s t) -> p s t", s=4)), [pt], [GTt[j]])

                jn = 0

                def load_gu(e_, g_):
                    gb_ = (e_ * 11 + g_) % 2
                    sg_st = stage_in(w_gate[e_, g_], 2048)
                    su_st = stage_in(w_up[e_, g_], 2048)
                    S.op("act", lambda: act.copy(out=wgb[gb_][:, :, :].rearrange("p c f -> p (c f)"), in_=sg_st[:, :]), [sg_st], [wgb[gb_]])
                    S.op("dve", lambda: dve.tensor_copy(out=wub[gb_][:, :, :].rearrange("p c f -> p (c f)"), in_=su_st[:, :]), [su_st], [wub[gb_]])

                if e == 0:
                    load_gu(0, 0)
                for g in (range(11) if PH & 2 else []):
                    gb = (e * 11 + g) % 2
                    if g + 1 < 11:
                        load_gu(e, g + 1)
                    for fl in range(2):
                        fc = g * 2 + fl
                        pg, pu = PB[(fc % 2) * 2], PB[(fc % 2) * 2 + 1]
                        for c in range(8):
                            S.op("pe", lambda c=c, pg=pg, gb=gb, fl=fl: pe.matmul(pg[:, :], lhsT=wgb[gb][:, c, fl * 128:(fl + 1) * 128], rhs=xsT[:, c, :], start=(c == 0), stop=(c == 7)), [wgb[gb], xsT], [pg])
                        for c in range(8):
                            S.op("pe", lambda c=c, pu=pu, gb=gb, fl=fl: pe.matmul(pu[:, :], lhsT=wub[gb][:, c, fl * 128:(fl + 1) * 128], rhs=xsT[:, c, :], start=(c == 0), stop=(c == 7)), [wub[gb], xsT], [pu])
                        sg_ = sgt[fc % 2]
                        S.op("act", lambda pg=pg, sg_=sg_: act.activation(out=sg_[:, :], in_=pg[:, :], func=AF.Silu), [pg], [sg_])
                        S.op("dve", lambda pu=pu, sg_=sg_, fc=fc: dve.tensor_tensor(out=hidT[:, fc, :], in0=sg_[:, :], in1=pu[:, :], op=ALU.mult), [sg_, pu], [hidT])
                    for _ in range(2 if g < 5 else 1):
                        if jn < 16:
                            prep(jn)
                            jn += 1
                while jn < 16 and (PH & 8):
                    prep(jn)
                    jn += 1
                if e + 1 < NEX and (PH & 1):
                    route(e + 1)
                pieces = ((0, 8), (8, 16), (16, NFC))
                def dma_d(cb_):
                    return [stage_in(w_down[e, cb_, :, f0 * 256:f1 * 256], (f1 - f0) * 256) for (f0, f1) in pieces]

                def cast_d(k, st):
                    f0, f1 = pieces[k]
                    S.op("act", lambda: act.copy(out=wdb[:, f0:f1, :].rearrange("p c n -> p (c n)"), in_=st[:, 0:(f1 - f0) * 256]), [st], [wdbT[k]])

                sts = dma_d(0)
                for k in range(3):
                    cast_d(k, sts[k])
                for cb in (range(4) if PH & 4 else []):
                    nxt = dma_d(cb + 1) if cb + 1 < 4 else None
                    for k, (f0, f1) in enumerate(pieces):
                        for fc in range(f0, f1):
                            for sc in range(4):
                                S.op("pe", lambda fc=fc, sc=sc: pe.matmul(PB[4 + sc][:, 0:256], lhsT=hidT[:, fc, sc * 128:(sc + 1) * 128], rhs=wdb[:, fc, :], start=(fc == 0), stop=(fc == NFC - 1)), [hidT, wdbT[k]], [PB[4 + sc]])
                        if nxt is not None:
                            cast_d(k, nxt[k])
                    for sc in range(4):
                        if sc % 2 == 0:
                            S.op("act", lambda sc=sc, cb=cb: act.copy(out=ysb[:, sc, cb * 256:(cb + 1) * 256], in_=PB[4 + sc][:, 0:256]), [PB[4 + sc]], [ysb])
                        else:
                            S.op("dve", lambda sc=sc, cb=cb: dve.tensor_copy(out=ysb[:, sc, cb * 256:(cb + 1) * 256], in_=PB[4 + sc][:, 0:256]), [PB[4 + sc]], [ysb])
                if e + 1 < NEX:
                    load_gu(e + 1, 0)
                for j in (range(16) if PH & 8 else []):
                    for hlf in range(2):
                        pyc = PB[(j % 2) * 2 + hlf]
                        for sc in range(4):
                            S.op("pe", lambda sc=sc, pyc=pyc, hlf=hlf, j=j: pe.matmul(pyc[:, :], lhsT=GTa[:, j, sc, :], rhs=ysb[:, sc, hlf * 512:(hlf + 1) * 512], start=(sc == 0), stop=(sc == 3)), [GTt[j], ysb], [pyc])
                        S.op("dve", lambda pyc=pyc, j=j, hlf=hlf: dve.tensor_tensor(out=yacc[:, j, hlf * 512:(hlf + 1) * 512], in0=yacc[:, j, hlf * 512:(hlf + 1) * 512], in1=pyc[:, :], op=ALU.add), [yacc, pyc], [yacc])
            S.barrier()
            e3.close()
            gF = K.sb(es, [128, D], F32, "gF")
            S.dma("sp", gF[:, :], mods_d[:, 7, :], reads=[T_mods], writes=[gF])
            xno = [K.sb(es, [128, D], F32, "xno%d" % i) for i in range(2)]
            ob = [K.sb(es, [128, D], F32, "ob%d" % i) for i in range(2)]
            junk = K.sb(es, [128, D], BF16, "junkg")
            ss = K.sb(es, [128, 1], F32, "ssg")
            rstd = K.sb(es, [128, 1], F32, "rstdg")
            for j in range(16):
                b = j % 2
                S.dma("pool", None, None, reads=[T_XN, oi], writes=[xno[b]], fn=lambda b=b, j=j: pool.indirect_dma_start(
                    out=xno[b][:, :], out_offset=None, in_=XN_d, in_offset=bass.IndirectOffsetOnAxis(ap=oi[:, j:j + 1], axis=0)))
                S.op("act", lambda j=j: act.activation(out=junk[:, :], in_=yacc[:, j, :], func=AF.Square, accum_out=ss[:, :]), [yacc], [junk, ss])
                S.op("act", lambda: act.activation(out=rstd[:, :], in_=ss[:, :], func=AF.Sqrt, scale=1.0 / D, bias=epsb[:, :]), [ss, epsb], [rstd])
                S.op("dve", lambda: dve.reciprocal(out=rstd[:, :], in_=rstd[:, :]), [rstd], [rstd])
                S.op("dve", lambda j=j, b=b: dve.scalar_tensor_tensor(out=ob[b][:, :], in0=yacc[:, j, :], scalar=rstd[:, 0:1], in1=gF[:, :], op0=ALU.mult, op1=ALU.mult), [yacc, rstd, gF], [ob[b]])
                S.op("pool", lambda b=b: pool.tensor_tensor(out=ob[b][:, :], in0=ob[b][:, :], in1=xno[b][:, :], op=ALU.add), [ob[b], xno[b]], [ob[b]])
                S.dma("sp", out[j * 128:(j + 1) * 128, :], ob[b][:, :], reads=[ob[b]])

    stages = [("0", stage0), ("A", stageA), ("B", stageB), ("C", stageC), ("D", stageD), ("E", stageEFG)]
    if upto in ("F", "G", "all"):
        stages[-1] = (upto, stageEFG)
    for name, fn in stages:
        fn()
        S.barrier()
        if upto == name:
            break
    S.finish()
    glob.close()
    return nc


def prep_inputs(inputs, cores=range(8), with_experts=True):
    f = lambda k: np.ascontiguousarray(np.asarray(inputs[k], dtype=np.float32))
    x, c, ctx, c_ctx = f("x"), f("c"), f("ctx"), f("c_ctx")
    consts = _consts()
    cos, sin = _rope_tables()
    rpb = f("na_rpb")[0]
    nab = np.ascontiguousarray(_na_bias_tables(rpb).reshape(5, 8, 128, 640))
    a_up = f("gla_a_up")[0]
    a_bias = f("gla_a_bias")[0]
    aup_bd = np.zeros((32, 512), np.float32)
    aup_bd[0:16, 0:256] = a_up[0]
    aup_bd[16:32, 256:512] = a_up[1]
    abias = np.ascontiguousarray(a_bias.reshape(1, 512))
    norms = np.ascontiguousarray(np.stack([f("norm_mix_pre")[0], f("norm_mix_post")[0], f("norm_ffn_pre")[0], f("norm_ffn_post")[0]]))
    shared = dict(
        w_mod=f("w_mod")[0], b_mod=f("b_mod"), norms=norms, gla_norm=f("gla_norm"), w_in=f("w_in")[0],
        aup_bd=aup_bd, abias=abias, rope_cos=cos, rope_sin=sin, na_bias=nab, w_out=f("w_out")[0],
        router=f("router")[0], **consts)
    if with_experts:
        tile_gu = lambda w: np.ascontiguousarray(w.reshape(NE, 8, 128, 11, 256).transpose(0, 3, 2, 1, 4)).reshape(NE, 11, 128, 8 * 256)
        shared.update(w_gate=tile_gu(f("w_gate")[0]), w_up=tile_gu(f("w_up")[0]),
                      w_down=np.ascontiguousarray(f("w_down")[0].reshape(NE, NFC, 128, 4, 256).transpose(0, 3, 2, 1, 4)).reshape(NE, 4, 128, NFC * 256))
    maps = []
    pp = np.arange(128)
    for core in cores:
        s, p = core // 2, core % 2
        m = dict(shared)
        m["xc"] = np.ascontiguousarray(np.concatenate([ctx[s], x[s]], axis=0))
        cv = np.zeros((128, 16), np.float32)
        cv[:, 0:8] = c[s].reshape(8, 128).T
        cv[:, 8:16] = c_ctx.reshape(8, 128).T
        m["cvec"] = cv
        m["own_idx"] = np.ascontiguousarray((p * 2048 + np.arange(16)[None, :] * 128 + pp[:, None]).astype(np.int32))
        seg = pp % 8
        m["own_flag"] = ((seg // 4) == p).astype(np.float32).reshape(128, 1)
        selm = np.zeros((128, 64), np.float32)
        for e in range(16):
            for os_ in range(4):
                selm[e * 8 + p * 4 + os_, e * 4 + os_] = 1.0
        m["sel"] = selm
        maps.append(m)
    return maps


_PROGRAM = None


def kernel(**inputs):
    global _PROGRAM
    if _PROGRAM is None:
        _PROGRAM = build_program()
    maps = prep_inputs(inputs)
    res = run_bass_kernel_spmd(_PROGRAM, maps, core_ids=list(range(8)))
    out = np.zeros((4, SEQ, D), np.float32)
    for core in range(8):
        s, p = core // 2, core % 2
        out[s, p * 2048:(p + 1) * 2048] = res.results[core]["out"]
    return out
```

```python
import contextlib
import numpy as np
import ml_dtypes
import concourse.bass as bass
import concourse.mybir as mybir
from concourse.bass_utils import run_bass_kernel_spmd

F32 = mybir.dt.float32
BF16 = mybir.dt.bfloat16
I32 = mybir.dt.int32
AF = mybir.ActivationFunctionType
ALU = mybir.AluOpType

D = 1024
SEQ = 4096
CTX = 256
NTOK = SEQ + CTX
NT = NTOK // 128
INC = 3104
NE = 16
DE = 2816
NFC = DE // 128
CAP = 512
EPS = 1e-6
NEG = -30000.0


class T:
    __slots__ = ("ap", "w", "r", "name")

    def __init__(self, ap, name=""):
        self.ap = ap
        self.w = None
        self.r = {}
        self.name = name

    def __getitem__(self, idx):
        return self.ap[idx]


class Sync:
    def __init__(self, nc, n_sp=20, n_pool=8, n_act=6):
        self.nc = nc
        self.E = {"pe": nc.tensor, "act": nc.scalar, "dve": nc.vector, "pool": nc.gpsimd, "sp": nc.sync}
        self.sem = {}
        self.cnt = {}
        for k in ("pe", "act", "dve", "pool"):
            self.sem[k] = nc.alloc_semaphore("s_" + k)
            self.cnt[k] = 0
        self.seen = {k: {} for k in self.E}
        self.dq = {}
        for q, n in (("sp", n_sp), ("pool", n_pool), ("act", n_act)):
            lst = []
            for i in range(n):
                key = "d_%s%d" % (q, i)
                self.sem[key] = nc.alloc_semaphore(key)
                self.cnt[key] = 0
                lst.append(key)
            self.dq[q] = [lst, 0]

    def _wait(self, ek, key, val):
        if val <= 0:
            return
        if self.seen[ek].get(key, 0) >= val:
            return
        self.E[ek].wait_ge(self.sem[key], val)
        self.seen[ek][key] = val

    def _deps(self, ek, reads, writes):
        need = {}
        for t in reads:
            if t.w is not None:
                k, v = t.w
                need[k] = max(need.get(k, 0), v)
        for t in writes:
            if t.w is not None:
                k, v = t.w
                if not (k == "pe" and ek == "pe"):
                    need[k] = max(need.get(k, 0), v)
            for k, v in t.r.items():
                if not (k == "pe" and ek == "pe"):
                    need[k] = max(need.get(k, 0), v)
        for k, v in need.items():
            self._wait(ek, k, v)

    def op(self, ek, fn, reads=(), writes=()):
        self._deps(ek, reads, writes)
        ins = fn()
        self.cnt[ek] += 1
        ins.then_inc(self.sem[ek], 1)
        me = (ek, self.cnt[ek])
        for t in reads:
            t.r[ek] = self.cnt[ek]
        for t in writes:
            t.w = me
            t.r = {}
        return ins

    def dma(self, q, out, in_, reads=(), writes=(), fn=None, **kw):
        lst, i = self.dq[q]
        key = lst[i % len(lst)]
        self.dq[q][1] = i + 1
        self._wait(q, key, self.cnt[key])
        self._deps(q, reads, writes)
        if fn is None:
            ins = self.E[q].dma_start(out=out, in_=in_, **kw)
        else:
            ins = fn()
        self.cnt[key] += 16
        ins.then_inc(self.sem[key], 16)
        for t in reads:
            t.r[key] = self.cnt[key]
        for t in writes:
            t.w = (key, self.cnt[key])
            t.r = {}
        return ins

    def barrier(self):
        for ek in self.E:
            for k, v in self.cnt.items():
                if not (k == ek == "pe"):
                    self._wait(ek, k, v)

    def finish(self):
        for k, v in self.cnt.items():
            self._wait("sp", k, v)


class Ctx:
    def __init__(self, nc):
        self.nc = nc
        self.S = Sync(nc)
        self.uid = 0

    def sb(self, es, shape, dt, name):
        self.uid += 1
        return T(es.enter_context(self.nc.sbuf_tensor("%s_%d" % (name, self.uid), list(shape), dt)), name)

    def ps(self, es, shape, dt, name):
        self.uid += 1
        return T(es.enter_context(self.nc.psum_tensor("%s_%d" % (name, self.uid), list(shape), dt)), name)


def _rope_tables():
    half = 16
    freqs = (np.float32(10000.0) ** (-np.arange(half, dtype=np.float32) / np.float32(half))).astype(np.float32)
    t = np.arange(SEQ)
    pr = (t // 64).astype(np.float32)[:, None] * freqs
    pc = (t % 64).astype(np.float32)[:, None] * freqs
    ang = np.concatenate([pr, pc], axis=1).astype(np.float32)
    return np.cos(ang).astype(np.float32), np.sin(ang).astype(np.float32)


def _na_key_tile0(T_):
    return min(max(T_ - 2, 0), 27)


def _na_bias_tables(rpb):
    out = np.full((5, 8, 128, 5, 128), NEG, np.float32)
    for si, T_ in enumerate((0, 1, 2, 30, 31)):
        kt0 = _na_key_tile0(T_)
        for qi in range(128):
            r = 2 * T_ + qi // 64
            qc = qi % 64
            rs = min(max(r - 4, 0), 56)
            cs = min(max(qc - 8, 0), 48)
            for kr in range(rs, rs + 8):
                kt = kr // 2 - kt0
                assert 0 <= kt < 5
                dr = kr - r + 7
                kc = np.arange(cs, cs + 16)
                dc = np.clip(kc - qc + 15, 0, 30)
                j = (kr % 2) * 64 + kc
                out[si, :, j, kt, qi] = rpb[:, dr, dc].T
    return out


def _consts():
    c = {}
    c["ident_f"] = np.eye(128, dtype=np.float32)
    c["ident_b"] = np.eye(128, dtype=np.float32).astype(ml_dtypes.bfloat16)
    tt = np.arange(128)
    c["tri_f"] = np.where(tt[:, None] <= tt[None, :], -1.0 / 16.0, 0.0).astype(np.float32)
    c["tri_b"] = np.where(tt[:, None] >= tt[None, :], -1.0 / 16.0, 0.0).astype(np.float32)
    c["mask_f"] = (tt[:, None] <= tt[None, :]).astype(np.float32)
    c["mask_b"] = (tt[:, None] >= tt[None, :]).astype(np.float32)
    c["ones_col"] = np.full((128, 1), -1.0 / 16.0, np.float32)
    c["ones_row"] = np.ones((1, 128), np.float32)
    g = tt // 8
    c["blk_ones"] = (g[:, None] == g[None, :]).astype(np.float32)
    c["blk_lt"] = ((g[:, None] == g[None, :]) & (tt[:, None] < tt[None, :])).astype(np.float32)
    c["iota512"] = np.broadcast_to(np.arange(512, dtype=np.float32)[None, :], (128, 512)).copy()
    own = np.arange(16)[None, :] * 128 + np.arange(128)[:, None]
    c["tid"] = np.stack([own // 64, own % 64], axis=-1).astype(np.float32).astype(ml_dtypes.bfloat16)
    return c


def build_program(upto="all", debug=False):
    nc = bass.Bass("TRN2", target_bir_lowering=False)
    K = Ctx(nc)
    S = K.S
    pe, act, dve, pool = nc.tensor, nc.scalar, nc.vector, nc.gpsimd

    def din(name, shape, dt=F32):
        return nc.dram_tensor(name, list(shape), dt, kind="ExternalInput").ap()

    import os as _os
    _ext = set(_os.environ.get("EXTSET", "").split(","))

    def dscr(name, shape, dt=F32):
        return nc.dram_tensor(name, list(shape), dt, kind=("ExternalOutput" if (debug or name in _ext) else "Internal")).ap()

    xc = din("xc", [NTOK, D])
    cvec = din("cvec", [128, 16])
    w_mod = din("w_mod", [D, 6 * D])
    b_mod = din("b_mod", [1, 6 * D])
    norms = din("norms", [4, D])
    gla_norm = din("gla_norm", [1, 128])
    w_in = din("w_in", [D, INC])
    aup_bd = din("aup_bd", [32, 512])
    abias = din("abias", [1, 512])
    rope_cos = din("rope_cos", [SEQ, 32])
    rope_sin = din("rope_sin", [SEQ, 32])
    na_bias = din("na_bias", [5, 8, 128, 640])
    w_out = din("w_out", [D, D])
    router = din("router", [D, NE])
    if upto in ("F", "G", "all"):
        w_gate = din("w_gate", [NE, 11, 128, 8 * 256])
        w_up = din("w_up", [NE, 11, 128, 8 * 256])
        w_down = din("w_down", [NE, 4, 128, NFC * 256])
    ident_f = din("ident_f", [128, 128])
    ident_b = din("ident_b", [128, 128], BF16)
    tri_f = din("tri_f", [128, 128])
    tri_b = din("tri_b", [128, 128])
    mask_f = din("mask_f", [128, 128])
    mask_b = din("mask_b", [128, 128])
    ones_col = din("ones_col", [128, 1])
    ones_row = din("ones_row", [1, 128])
    blk_ones = din("blk_ones", [128, 128])
    blk_lt = din("blk_lt", [128, 128])
    iota512 = din("iota512", [128, 512])
    tid_in = din("tid", [128, 16, 2], BF16)
    own_idx = din("own_idx", [128, 16], I32)
    own_flag = din("own_flag", [128, 1])
    sel = din("sel", [128, 64])
    out = nc.dram_tensor("out", [SEQ // 2, D], F32, kind="ExternalOutput").ap()

    mods_d = dscr("mods_d", [128, 8, D])
    KT_d = dscr("KT_d", [512, NTOK], BF16)
    QT_d = dscr("QT_d", [512, SEQ], BF16)
    V_d = dscr("V_d", [NTOK, 512], BF16)
    GV_d = dscr("GV_d", [NTOK, 512], BF16)
    GKQ_d = dscr("GKQ_d", [NTOK, 544])
    GG_d = dscr("GG_d", [SEQ, 512])
    OF_d = dscr("OF_d", [SEQ, 512])
    MIX_d = dscr("MIX_d", [SEQ, D], BF16)
    XN_d = dscr("XN_d", [SEQ, D])
    HF_d = dscr("HF_d", [SEQ, D], BF16)
    AFFT_d = dscr("AFFT_d", [NE, SEQ])

    glob = contextlib.ExitStack()
    idf = K.sb(glob, [128, 128], F32, "idf")
    idb = K.sb(glob, [128, 128], BF16, "idb")
    onesr = K.sb(glob, [1, 128], F32, "onesr")
    S.dma("sp", idf[:, :], ident_f, writes=[idf])
    S.dma("sp", idb[:, :], ident_b, writes=[idb])
    S.dma("sp", onesr[:, :], ones_row, writes=[onesr])

    def stage0():
        with contextlib.ExitStack() as es:
            cv = K.sb(es, [128, 16], F32, "cv")
            csil = K.sb(es, [128, 16], F32, "csil")
            crep = K.sb(es, [128, 16, 128], F32, "crep")
            bm = K.sb(es, [1, 6 * D], F32, "bm")
            modA = K.sb(es, [128, 6 * D], F32, "modA")
            modC = K.sb(es, [128, 2 * D], F32, "modC")
            nbc = K.sb(es, [128, 4, D], F32, "nbc")
            wm = [K.sb(es, [128, 8, 512], F32, "wm%d" % i) for i in range(2)]
            mods = K.sb(es, [128, 8, D], F32, "mods")
            psA = [K.ps(es, [128, 512], F32, "psA%d" % i) for i in range(2)]
            psC = [K.ps(es, [128, 512], F32, "psC%d" % i) for i in range(2)]
            S.dma("sp", cv[:, :], cvec, writes=[cv])
            S.dma("sp", bm[:, :], b_mod, writes=[bm])
            S.dma("sp", nbc[:, :, :], norms.partition_broadcast(128), writes=[nbc])
            S.op("act", lambda: act.activation(out=csil[:, :], in_=cv[:, :], func=AF.Silu), [cv], [csil])
            S.op("dve", lambda: dve.tensor_copy(out=crep[:, :, :], in_=csil[:, :].unsqueeze(2).broadcast_to([128, 16, 128])), [csil], [crep])
            wmv = w_mod.rearrange("(c p) n -> p c n", p=128)
            for nb in range(12):
                w = wm[nb % 2]
                S.dma("sp", w[:, :, :], wmv[:, :, nb * 512:(nb + 1) * 512], writes=[w])
                for which in range(2 if nb < 4 else 1):
                    ps = (psA, psC)[which][nb % 2]
                    for c in range(8):
                        S.op("pe", lambda c=c, ps=ps, w=w, which=which: pe.matmul(ps[:, :], lhsT=crep[:, which * 8 + c, :], rhs=w[:, c, :], start=(c == 0), stop=False), [crep, w], [ps])
                    S.op("pe", lambda ps=ps, nb=nb: pe.matmul(ps[:, :], lhsT=onesr[:, :], rhs=bm[:, nb * 512:(nb + 1) * 512], start=False, stop=True), [onesr, bm], [ps])
                    dst = (modA, modC)[which]
                    if which == 0:
                        S.op("act", lambda ps=ps, dst=dst, nb=nb: act.copy(out=dst[:, nb * 512:(nb + 1) * 512], in_=ps[:, :]), [ps], [dst])
                    else:
                        S.op("dve", lambda ps=ps, dst=dst, nb=nb: dve.tensor_copy(out=dst[:, nb * 512:(nb + 1) * 512], in_=ps[:, :]), [ps], [dst])
            import os
            if os.environ.get("STOP0") == "1":
                S.dma("sp", mods_d[:, 0:6, :], modA[:, :].rearrange("p (a b) -> p a b", a=6), reads=[modA], writes=[T_mods])
                return
            sl = lambda t, i: t[:, i * D:(i + 1) * D]
            S.op("dve", lambda: dve.scalar_tensor_tensor(out=mods[:, 0, :], in0=sl(modA, 1), scalar=1.0, in1=nbc[:, 0, :], op0=ALU.add, op1=ALU.mult), [modA, nbc], [mods])
            S.op("dve", lambda: dve.tensor_copy(out=mods[:, 1, :], in_=sl(modA, 0)), [modA], [mods])
            S.op("dve", lambda: dve.scalar_tensor_tensor(out=mods[:, 2, :], in0=sl(modC, 1), scalar=1.0, in1=nbc[:, 0, :], op0=ALU.add, op1=ALU.mult), [modC, nbc], [mods])
            S.op("dve", lambda: dve.tensor_copy(out=mods[:, 3, :], in_=sl(modC, 0)), [modC], [mods])
            S.op("dve", lambda: dve.tensor_tensor(out=mods[:, 4, :], in0=sl(modA, 2), in1=nbc[:, 1, :], op=ALU.mult), [modA, nbc], [mods])
            S.op("dve", lambda: dve.scalar_tensor_tensor(out=mods[:, 5, :], in0=sl(modA, 4), scalar=1.0, in1=nbc[:, 2, :], op0=ALU.add, op1=ALU.mult), [modA, nbc], [mods])
            S.op("dve", lambda: dve.tensor_copy(out=mods[:, 6, :], in_=sl(modA, 3)), [modA], [mods])
            S.op("dve", lambda: dve.tensor_tensor(out=mods[:, 7, :], in0=sl(modA, 5), in1=nbc[:, 3, :], op=ALU.mult), [modA, nbc], [mods])
            S.dma("sp", mods_d, mods[:, :, :], reads=[mods], writes=[T_mods])

    T_mods = T(None, "mods_d")
    T_KT, T_QT, T_V, T_GV, T_GKQ, T_GG = (T(None, n) for n in ("KT", "QT", "V", "GV", "GKQ", "GG"))

    def rms_rstd(es_tiles, src, src_t, ss, rstd, junk):
        S.op("act", lambda: act.activation(out=junk[:, :], in_=src, func=AF.Square, accum_out=ss[:, :]), [src_t], [junk, ss])
        S.op("act", lambda: act.activation(out=rstd[:, :], in_=ss[:, :], func=AF.Sqrt, scale=1.0 / D, bias=epsb[:, :]), [ss, epsb], [rstd])
        S.op("dve", lambda: dve.reciprocal(out=rstd[:, :], in_=rstd[:, :]), [rstd], [rstd])

    epsb = K.sb(glob, [128, 1], F32, "epsb")
    S.op("pool", lambda: pool.memset(epsb[:, :], EPS), [], [epsb])

    def stageA():
        with contextlib.ExitStack() as es:
            win = K.sb(es, [128, 8, INC], BF16, "win")
            wst = [K.sb(es, [128, 8, 512], F32, "wst%d" % i) for i in range(2)]
            gs = K.sb(es, [128, 4, D], F32, "gs")
            S.dma("sp", gs[:, :, :], mods_d[:, 0:4, :], reads=[T_mods], writes=[gs])
            wiv = w_in.rearrange("(c p) n -> p c n", p=128)
            for b in range(7):
                c0 = b * 512
                w_ = min(512, INC - c0)
                st = wst[b % 2]
                S.dma("sp", st[:, :, 0:w_], wiv[:, :, c0:c0 + w_], writes=[st])
                if b % 2 == 0:
                    S.op("act", lambda st=st, c0=c0, w_=w_: act.copy(out=win[:, :, c0:c0 + w_], in_=st[:, :, 0:w_]), [st], [win])
                else:
                    S.op("dve", lambda st=st, c0=c0, w_=w_: dve.tensor_copy(out=win[:, :, c0:c0 + w_], in_=st[:, :, 0:w_]), [st], [win])
            import os
            STOPA = int(os.environ.get("STOPA", "99"))
            if STOPA <= 1:
                return
            xt = [K.sb(es, [128, D], F32, "xt%d" % i) for i in range(2)]
            junk = K.sb(es, [128, D], BF16, "junk")
            ss = [K.sb(es, [128, 1], F32, "ss%d" % i) for i in range(2)]
            rstd = [K.sb(es, [128, 1], F32, "rstd%d" % i) for i in range(2)]
            h1 = [K.sb(es, [128, D], F32, "h1_%d" % i) for i in range(2)]
            hb = [K.sb(es, [128, D], BF16, "hb%d" % i) for i in range(2)]
            hT = [K.sb(es, [128, 8, 512], BF16, "hT%d" % i) for i in range(2)]
            ktq = [K.sb(es, [128, 512], BF16, "ktq%d" % i) for i in range(2)]
            vo = [K.sb(es, [128, 512], BF16, "vo%d" % i) for i in range(2)]
            gvo = [K.sb(es, [128, 512], BF16, "gvo%d" % i) for i in range(2)]
            gkq = [K.sb(es, [128, 544], F32, "gkq%d" % i) for i in range(2)]
            ggo = [K.sb(es, [128, 512], F32, "ggo%d" % i) for i in range(2)]
            pst = [K.ps(es, [128, 8 * 128], BF16, "pst%d" % i) for i in range(2)]
            psp = [K.ps(es, [128, 512], F32, "psp%d" % i) for i in range(4)]
            npp = [0]

            def nextps():
                npp[0] += 1
                return psp[npp[0] % 4]

            ev = [0]

            def evac(dst_t, dst_ap, ps, ps_ap):
                if psp.index(ps) % 2 == 0:
                    S.op("act", lambda: act.copy(out=dst_ap, in_=ps_ap), [ps], [dst_t])
                else:
                    S.op("dve", lambda: dve.tensor_copy(out=dst_ap, in_=ps_ap), [ps], [dst_t])

            groups = [(0, 2)] + [(2 + 4 * g, 4) for g in range(8)]
            hb4 = hb + [K.sb(es, [128, D], BF16, "hbx%d" % i) for i in range(2)]
            tcnt = [0]

            def modulate(gi):
                t0, ntl = groups[gi]
                for k in range(ntl):
                    ti = t0 + k
                    b2 = tcnt[0] % 2
                    tcnt[0] += 1
                    x_ = xt[b2]
                    S.dma("sp", x_[:, :], xc[ti * 128:(ti + 1) * 128, :], writes=[x_])
                    rms_rstd(None, x_[:, :], x_, ss[b2], rstd[b2], junk)
                    go = 2 if gi == 0 else 0
                    h_ = h1[b2]
                    S.op("dve", lambda: dve.scalar_tensor_tensor(out=h_[:, :], in0=x_[:, :], scalar=rstd[b2][:, 0:1], in1=gs[:, go, :], op0=ALU.mult, op1=ALU.mult), [x_, rstd[b2], gs], [h_])
                    S.op("pool", lambda: pool.tensor_tensor(out=hb4[k][:, :], in0=h_[:, :], in1=gs[:, go + 1, :], op=ALU.add), [h_, gs], [hb4[k]])

            def transposes(gi):
                t0, ntl = groups[gi]
                hTg_ = hT[gi % 2]
                for k in range(ntl):
                    pt = pst[k % 2]
                    for c in range(8):
                        S.op("pe", lambda c=c: pe.transpose(out=pt[:, c * 128:(c + 1) * 128], in_=hb4[k][:, c * 128:(c + 1) * 128], identity=idb[:, :]), [hb4[k], idb], [pt])
                    S.op("act", lambda: act.copy(out=hTg_[:, :, k * 128:(k + 1) * 128], in_=pt[:, :].rearrange("p (c t) -> p c t", c=8)), [pt], [hTg_])

            modulate(0)
            transposes(0)
            for gi, (t0, ntl) in enumerate(groups):
                is_ctx = gi == 0
                hTg = hT[gi % 2]
                if gi + 1 < len(groups):
                    modulate(gi + 1)
                ntok = ntl * 128
                tok0 = t0 * 128
                for which, c0 in ((0, 0), (1, 1824)):
                    if which == 1 and is_ctx:
                        continue
                    for fb in range(4):
                        ps = nextps()
                        for c in range(8):
                            S.op("pe", lambda c=c, ps=ps, fb=fb, c0=c0: pe.matmul(ps[:, 0:ntok], lhsT=win[:, c, c0 + fb * 128:c0 + (fb + 1) * 128], rhs=hTg[:, c, 0:ntok], start=(c == 0), stop=(c == 7)), [win, hTg], [ps])
                        kb = ktq[(which * 4 + fb) % 2]
                        evac(kb, kb[:, 0:ntok], ps, ps[:, 0:ntok])
                        if which == 0:
                            S.dma("sp", KT_d[fb * 128:(fb + 1) * 128, tok0:tok0 + ntok], kb[:, 0:ntok], reads=[kb], writes=[T_KT])
                        else:
                            S.dma("sp", QT_d[fb * 128:(fb + 1) * 128, tok0 - CTX:tok0 - CTX + ntok], kb[:, 0:ntok], reads=[kb], writes=[T_QT])
                for k in range(ntl):
                    ti = t0 + k
                    b2 = ti % 2
                    r0 = ti * 128

                    def mm(c0, w_, k=k):
                        ps = nextps()
                        for c in range(8):
                            S.op("pe", lambda c=c, ps=ps: pe.matmul(ps[:, 0:w_], lhsT=hTg[:, c, k * 128:(k + 1) * 128], rhs=win[:, c, c0:c0 + w_], start=(c == 0), stop=(c == 7)), [win, hTg], [ps])
                        return ps

                    ps = mm(512, 512)
                    evac(vo[b2], vo[b2][:, :], ps, ps[:, :])
                    S.dma("sp", V_d[r0:r0 + 128, :], vo[b2][:, :], reads=[vo[b2]], writes=[T_V])
                    TM = int(os.environ.get("TM", "9"))
                    if TM <= 1:
                        continue
                    ps = mm(1024, 512)
                    EV = int(os.environ.get("EV", "3"))
                    if EV & 1:
                        evac(gkq[b2], gkq[b2][:, 0:256], ps, ps[:, 0:256])
                    if EV & 2:
                        evac(gvo[b2], gvo[b2][:, 0:256], ps, ps[:, 256:512])
                    if TM <= 2:
                        continue
                    ps = mm(1536, 288)
                    evac(gvo[b2], gvo[b2][:, 256:512], ps, ps[:, 0:256])
                    evac(gkq[b2], gkq[b2][:, 256:288], ps, ps[:, 256:288])
                    S.dma("sp", GV_d[r0:r0 + 128, :], gvo[b2][:, :], reads=[gvo[b2]], writes=[T_GV])
                    if TM <= 3:
                        continue
                    if not is_ctx:
                        ps = mm(2336, 512)
                        evac(gkq[b2], gkq[b2][:, 288:544], ps, ps[:, 0:256])
                        evac(ggo[b2], ggo[b2][:, 0:256], ps, ps[:, 256:512])
                        ps = mm(2848, 256)
                        evac(ggo[b2], ggo[b2][:, 256:512], ps, ps[:, 0:256])
                        S.dma("sp", GG_d[r0 - CTX:r0 - CTX + 128, :], ggo[b2][:, :], reads=[ggo[b2]], writes=[T_GG])
                        S.dma("sp", GKQ_d[r0:r0 + 128, :], gkq[b2][:, :], reads=[gkq[b2]], writes=[T_GKQ])
                    else:
                        S.dma("sp", GKQ_d[r0:r0 + 128, 0:288], gkq[b2][:, 0:288], reads=[gkq[b2]], writes=[T_GKQ])
                if gi + 1 < len(groups):
                    transposes(gi + 1)


    T_MIXna = T(None, "MIXna")

    def stageB():
        with contextlib.ExitStack() as es:
            NB = 3
            qT = [K.sb(es, [128, 4, 128], BF16, "qT%d" % i) for i in range(2)]
            kT = [K.sb(es, [128, 4, 896], BF16, "kT%d" % i) for i in range(2)]
            va = [K.sb(es, [128, 7, 8, 65], BF16, "va%d" % i) for i in range(2)]
            bint = K.sb(es, [128, 8, 640], F32, "bint")
            bt = [K.sb(es, [128, 640], F32, "bt%d" % i) for i in range(3)]
            tmp = [K.sb(es, [128, 896], F32, "tmp%d" % i) for i in range(NB)]
            pT = [K.sb(es, [128, 896], BF16, "pT%d" % i) for i in range(NB + 1)]
            ona = [K.sb(es, [128, 512], BF16, "ona%d" % i) for i in range(2)]
            rec = [K.sb(es, [128, 1], F32, "rec%d" % i) for i in range(2)]
            sT = [K.ps(es, [128, 1024], F32, "sT%d" % i) for i in range(NB)]
            pv = [K.ps(es, [128, 512], F32, "pv%d" % i) for i in range(2)]
            for v in va:
                S.op("pool", lambda v=v: pool.memset(v[:, :, :, :], 1.0), [], [v])
            S.dma("sp", bint[:, :, :], na_bias[2].rearrange("h j c -> j h c"), writes=[bint])
            QTv = QT_d.rearrange("(c p) t -> p c t", p=128)
            KTv = KT_d.rearrange("(c p) t -> p c t", p=128)
            nbt = [0]
            tmR = [[T(None, "tmR") for _ in range(3)] for _ in range(NB)]
            onaR = [[T(None, "onaR") for _ in range(8)] for _ in range(2)]

            def loads(T_):
                b = T_ % 2
                kt0 = _na_key_tile0(T_)
                S.dma("sp", qT[b][:, :, :], QTv[:, :, T_ * 128:(T_ + 1) * 128], reads=[T_QT], writes=[qT[b]])
                S.dma("sp", kT[b][:, :, 0:640], KTv[:, :, CTX + kt0 * 128:CTX + kt0 * 128 + 640], reads=[T_KT], writes=[kT[b]])
                S.dma("sp", kT[b][:, :, 640:896], KTv[:, :, 0:CTX], reads=[T_KT], writes=[kT[b]])
                r0 = CTX + kt0 * 128
                for kk in range(5):
                    S.dma("sp", va[b][:, kk, :, 0:64], V_d[r0 + kk * 128:r0 + (kk + 1) * 128, :].rearrange("t (h d) -> t h d", h=8), reads=[T_V], writes=[va[b]])
                for kk in range(2):
                    S.dma("sp", va[b][:, 5 + kk, :, 0:64], V_d[kk * 128:(kk + 1) * 128, :].rearrange("t (h d) -> t h d", h=8), reads=[T_V], writes=[va[b]])

            def phase1(n):
                T_, h = n // 8, n % 8
                b = T_ % 2
                if h == 0:
                    loads(T_)
                si = {0: 0, 1: 1, 30: 3, 31: 4}.get(T_, 2)
                pr, P0 = h // 2, (h % 2) * 64
                s_, tm, pt = sT[n % NB], tmp[n % NB], pT[n % (NB + 1)]
                if si == 2:
                    bb_t, bb = bint, bint[:, h, :]
                else:
                    bb_t = bt[nbt[0] % 3]
                    nbt[0] += 1
                    bb = bb_t[:, :]
                    S.dma("sp", bb, na_bias[si, h], writes=[bb_t])
                for kk in range(7):
                    S.op("pe", lambda kk=kk: pe.matmul(s_[:, kk * 128:(kk + 1) * 128], lhsT=kT[b][P0:P0 + 64, pr, kk * 128:(kk + 1) * 128], rhs=qT[b][P0:P0 + 64, pr, :], start=True, stop=True), [kT[b], qT[b]], [s_])
                ta, tb, tc = tmR[n % NB]
                S.op("dve", lambda: dve.scalar_tensor_tensor(out=tm[:, 0:512], in0=s_[:, 0:512], scalar=0.125, in1=bb[:, 0:512], op0=ALU.mult, op1=ALU.add), [s_, bb_t], [ta])
                S.op("dve", lambda: dve.scalar_tensor_tensor(out=tm[:, 512:640], in0=s_[:, 512:640], scalar=0.125, in1=bb[:, 512:640], op0=ALU.mult, op1=ALU.add), [s_, bb_t], [tb])
                S.op("dve", lambda: dve.tensor_scalar(out=tm[:, 640:896], in0=s_[:, 640:896], scalar1=0.125, scalar2=None, op0=ALU.mult), [s_], [tc])
                S.op("act", lambda: act.activation(out=pt[:, :], in_=tm[:, :], func=AF.Exp), [ta, tb, tc], [pt])

            def phase2(n):
                T_, h = n // 8, n % 8
                b = T_ % 2
                pt, p_, rc = pT[n % (NB + 1)], pv[n % 2], rec[n % 2]
                for kk in range(7):
                    S.op("pe", lambda kk=kk: pe.matmul(p_[:, 0:65], lhsT=pt[:, kk * 128:(kk + 1) * 128], rhs=va[b][:, kk, h, :], start=(kk == 0), stop=(kk == 6)), [pt, va[b]], [p_])
                S.op("dve", lambda: dve.reciprocal(out=rc[:, :], in_=p_[:, 64:65]), [p_], [rc])
                S.op("dve", lambda: dve.tensor_scalar(out=ona[b][:, h * 64:(h + 1) * 64], in0=p_[:, 0:64], scalar1=rc[:, 0:1], scalar2=None, op0=ALU.mult), [p_, rc], [onaR[b][h]])
                if h == 7:
                    S.dma("sp", MIX_d[T_ * 128:(T_ + 1) * 128, 0:512], ona[b][:, :], reads=onaR[b], writes=[T_MIXna])

            NIT = 32 * 8
            LOOK = NB - 1
            for n in range(min(LOOK, NIT)):
                phase1(n)
            for n in range(NIT):
                if n + LOOK < NIT:
                    phase1(n + LOOK)
                phase2(n)

    T_OF = T(None, "OF")
    T_MIXgla = T(None, "MIXgla")

    OB_d = dscr("OB_d", [SEQ, 512])
    T_OB = T(None, "OB")

    def stageC():
        with contextlib.ExitStack() as es:
            tri = [K.sb(es, [128, 128], F32, "tri%d" % i) for i in range(2)]
            msk = [K.sb(es, [128, 512], F32, "msk%d" % i) for i in range(2)]
            onec = K.sb(es, [128, 1], F32, "onec")
            aup = K.sb(es, [32, 512], F32, "aup")
            abi = K.sb(es, [1, 512], F32, "abi")
            gnb = K.sb(es, [128, 128], F32, "gnb")
            for t_, src in ((tri[0], tri_f), (tri[1], tri_b), (onec, ones_col), (aup, aup_bd), (abi, abias)):
                S.dma("sp", t_[:, :], src, writes=[t_])
            for h in range(4):
                S.dma("sp", msk[0][:, h * 128:(h + 1) * 128], mask_f, writes=[msk[0]])
                S.dma("sp", msk[1][:, h * 128:(h + 1) * 128], mask_b, writes=[msk[1]])
            S.dma("sp", gnb[:, :], gla_norm.partition_broadcast(128).rearrange("p a b -> p (a b)"), writes=[gnb])
            ps_ad = K.ps(es, [128, 512], F32, "ps_ad")
            ps_z = K.ps(es, [128, 512], F32, "ps_z")
            ps_lam = K.ps(es, [128, 512], F32, "ps_lam")
            ps_t = K.ps(es, [128, 1024], BF16, "ps_t")
            ps_AT = K.ps(es, [128, 512], F32, "ps_AT")
            ps_o = K.ps(es, [128, 512], F32, "ps_o")
            ps_U = K.ps(es, [128, 512], F32, "ps_U")

            class B:
                pass

            def alloc(ci):
                b = B()
                n_ = lambda x: "%s_c%d" % (x, ci)
                b.gkq = [K.sb(es, [128, 544], F32, n_("gkq%d" % i)) for i in range(2)]
                b.gv = [K.sb(es, [128, 512], BF16, n_("gv%d" % i)) for i in range(2)]
                b.cs = [K.sb(es, [128, 32], F32, n_("cs%d" % i)) for i in range(2)]
                b.sn = [K.sb(es, [128, 32], F32, n_("sn%d" % i)) for i in range(2)]
                b.adT = K.sb(es, [32, 128], F32, n_("adT"))
                b.ez = K.sb(es, [128, 256], F32, n_("ez"))
                b.lz = K.sb(es, [128, 256], F32, n_("lz"))
                b.E1 = K.sb(es, [128, 256], F32, n_("E1"))
                b.E2 = K.sb(es, [128, 256], F32, n_("E2"))
                b.at = K.sb(es, [128, 2], F32, n_("at"))
                b.kr = K.sb(es, [128, 256], F32, n_("kr"))
                b.qr = K.sb(es, [128, 256], F32, n_("qr"))
                b.rt = [K.sb(es, [128, 128], F32, n_("rt%d" % i)) for i in range(4)]
                b.qk = K.sb(es, [128, 512], BF16, n_("qk"))
                b.qTz = K.sb(es, [128, 4, 128], BF16, n_("qTz"))
                S.op("pool", lambda: pool.memset(b.qTz[:, :, :], 0.0), [], [b.qTz])
                b.kTc = K.sb(es, [128, 2, 128], BF16, n_("kTc"))
                b.ATs = K.sb(es, [128, 4, 128], BF16, n_("ATs"))
                b.Sst = [K.sb(es, [128, 128], F32, n_("Sst%d" % i)) for i in range(2)]
                b.Sbf = [K.sb(es, [128, 128], BF16, n_("Sbf%d" % i)) for i in range(2)]
                b.tU = K.sb(es, [128, 128], F32, n_("tU"))
                b.osb = [K.sb(es, [128, 512], F32, n_("osb%d" % i)) for i in range(2)]
                return b

            def rope(eng, ek, src_t, src_ap, dst_t, tmps, c_, s_):
                X = src_ap.rearrange("p (h a b f) -> p h a b f", h=4, a=2, b=2)
                O = dst_t[:, :].rearrange("p (h a b f) -> p h a b f", h=4, a=2, b=2)
                C = c_[:, :].rearrange("p (a f) -> p a f", a=2).unsqueeze(1).broadcast_to([128, 4, 2, 16])
                Sn = s_[:, :].rearrange("p (a f) -> p a f", a=2).unsqueeze(1).broadcast_to([128, 4, 2, 16])
                v = lambda t_: t_[:, :].rearrange("p (h a f) -> p h a f", h=4, a=2)
                X1, X2 = X[:, :, :, 0, :], X[:, :, :, 1, :]
                t1, t2, t3, t4 = tmps
                S.op(ek, lambda: eng.tensor_tensor(out=v(t1), in0=X1, in1=C, op=ALU.mult), [src_t, c_], [t1])
                S.op(ek, lambda: eng.tensor_tensor(out=v(t2), in0=X2, in1=Sn, op=ALU.mult), [src_t, s_], [t2])
                S.op(ek, lambda: eng.tensor_tensor(out=O[:, :, :, 0, :], in0=v(t1), in1=v(t2), op=ALU.subtract), [t1, t2], [dst_t])
                S.op(ek, lambda: eng.tensor_tensor(out=v(t3), in0=X1, in1=Sn, op=ALU.mult), [src_t, s_], [t3])
                S.op(ek, lambda: eng.tensor_tensor(out=v(t4), in0=X2, in1=C, op=ALU.mult), [src_t, c_], [t4])
                S.op(ek, lambda: eng.tensor_tensor(out=O[:, :, :, 1, :], in0=v(t3), in1=v(t4), op=ALU.add), [t3, t4], [dst_t])

            def chain(dr, c):
                for pr in range(2):
                    S.op("pool", lambda pr=pr: pool.memset(c.Sst[pr][:, :], 0.0), [], [c.Sst[pr]])
                    S.op("pool", lambda pr=pr: pool.memset(c.Sbf[pr][:, :], 0.0), [], [c.Sbf[pr]])
                order = list(range(NT)) if dr == 0 else [1, 0] + list(range(NT - 1, 1, -1))
                n = 0
                for ti in order:
                    lat = ti >= 2
                    li = ti - 2
                    b = n % 2
                    n += 1
                    g_, v_ = c.gkq[b], c.gv[b]
                    gw = 544 if lat else 288
                    S.dma("sp", g_[:, 0:gw], GKQ_d[ti * 128:(ti + 1) * 128, 0:gw], reads=[T_GKQ], writes=[g_])
                    S.dma("sp", v_[:, :], GV_d[ti * 128:(ti + 1) * 128, :], reads=[T_GV], writes=[v_])
                    if lat:
                        S.dma("sp", c.cs[b][:, :], rope_cos[li * 128:(li + 1) * 128, :], writes=[c.cs[b]])
                        S.dma("sp", c.sn[b][:, :], rope_sin[li * 128:(li + 1) * 128, :], writes=[c.sn[b]])
                    yield
                    S.op("pe", lambda: pe.transpose(out=ps_ad[0:32, 0:128], in_=g_[:, 256:288], identity=idf[:, :]), [g_, idf], [ps_ad])
                    S.op("act", lambda: act.copy(out=c.adT[:, :], in_=ps_ad[0:32, 0:128]), [ps_ad], [c.adT])
                    S.op("pe", lambda: pe.matmul(ps_z[:, 0:256], lhsT=c.adT[:, :], rhs=aup[:, dr * 256:(dr + 1) * 256], start=True, stop=False), [c.adT, aup], [ps_z])
                    S.op("pe", lambda: pe.matmul(ps_z[:, 0:256], lhsT=onesr[:, :], rhs=abi[:, dr * 256:(dr + 1) * 256], start=False, stop=True), [onesr, abi], [ps_z])
                    S.op("act", lambda: act.activation(out=c.ez[:, :], in_=ps_z[:, 0:256], func=AF.Exp, scale=-1.0), [ps_z], [c.ez])
                    yield
                    S.op("act", lambda: act.activation(out=c.lz[:, :], in_=c.ez[:, :], func=AF.Ln, bias=1.0), [c.ez], [c.lz])
                    S.op("pe", lambda: pe.matmul(ps_lam[:, 0:256], lhsT=tri[dr][:, :], rhs=c.lz[:, :], start=True, stop=True), [tri[dr], c.lz], [ps_lam])
                    for pr in range(2):
                        S.op("pe", lambda pr=pr: pe.matmul(ps_lam[:, 256 + pr:257 + pr], lhsT=c.lz[:, pr * 128:(pr + 1) * 128], rhs=onec[:, :], start=True, stop=True), [c.lz, onec], [ps_lam])
                    if lat:
                        S.op("act", lambda: act.activation(out=c.E1[:, :], in_=ps_lam[:, 0:256], func=AF.Exp), [ps_lam], [c.E1])
                    S.op("act", lambda: act.activation(out=c.E2[:, :], in_=ps_lam[:, 0:256], func=AF.Exp, scale=-1.0), [ps_lam], [c.E2])
                    S.op("act", lambda: act.activation(out=c.at[:, :], in_=ps_lam[:, 256:258], func=AF.Exp), [ps_lam], [c.at])
                    yield
                    if lat:
                        rope(dve, "dve", g_, g_[:, 0:256], c.kr, c.rt, c.cs[b], c.sn[b])
                        yield
                        rope(dve, "dve", g_, g_[:, 288:544], c.qr, c.rt, c.cs[b], c.sn[b])
                        yield
                        S.op("dve", lambda: dve.scalar_tensor_tensor(out=c.qk[:, 0:256], in0=c.qr[:, :], scalar=0.125, in1=c.E1[:, :], op0=ALU.mult, op1=ALU.mult), [c.qr, c.E1], [c.qk])
                        S.op("dve", lambda: dve.tensor_tensor(out=c.qk[:, 256:512], in0=c.kr[:, :], in1=c.E2[:, :], op=ALU.mult), [c.kr, c.E2], [c.qk])
                    else:
                        S.op("dve", lambda: dve.tensor_tensor(out=c.qk[:, 256:512], in0=g_[:, 0:256], in1=c.E2[:, :], op=ALU.mult), [g_, c.E2], [c.qk])
                    for j in (range(4) if lat else range(2, 4)):
                        S.op("pe", lambda j=j: pe.transpose(out=ps_t[:, j * 128:(j + 1) * 128], in_=c.qk[:, j * 128:(j + 1) * 128], identity=idb[:, :]), [c.qk, idb], [ps_t])
                    if lat:
                        for h in range(4):
                            S.op("act", lambda h=h: act.copy(out=c.qTz[(h % 2) * 64:(h % 2) * 64 + 64, h, :], in_=ps_t[(h % 2) * 64:(h % 2) * 64 + 64, (h // 2) * 128:(h // 2 + 1) * 128]), [ps_t], [c.qTz])
                    S.op("act", lambda: act.copy(out=c.kTc[:, :, :], in_=ps_t[:, 256:512].rearrange("p (c t) -> p c t", c=2)), [ps_t], [c.kTc])
                    yield
                    if lat:
                        for h in range(4):
                            pr = h // 2
                            S.op("pe", lambda h=h, pr=pr: pe.matmul(ps_AT[:, h * 128:(h + 1) * 128], lhsT=c.kTc[:, pr, :], rhs=c.qTz[:, h, :], start=True, stop=True), [c.kTc, c.qTz], [ps_AT])
                        S.op("dve", lambda: dve.tensor_tensor(out=c.ATs[:, :, :].rearrange("p h t -> p (h t)"), in0=ps_AT[:, :], in1=msk[dr][:, :], op=ALU.mult), [ps_AT, msk[dr]], [c.ATs])
                        for h in range(4):
                            pr = h // 2
                            S.op("pe", lambda h=h: pe.matmul(ps_o[:, h * 128:(h + 1) * 128], lhsT=c.ATs[:, h, :], rhs=v_[:, h * 128:(h + 1) * 128], start=True, stop=False), [c.ATs, v_], [ps_o])
                            S.op("pe", lambda h=h, pr=pr: pe.matmul(ps_o[:, h * 128:(h + 1) * 128], lhsT=c.qTz[:, h, :], rhs=c.Sbf[pr][:, :], start=False, stop=True), [c.qTz, c.Sbf[pr]], [ps_o])
                        ob_ = c.osb[b]
                        S.op("dve", lambda: dve.tensor_copy(out=ob_[:, :], in_=ps_o[:, :]), [ps_o], [ob_])
                        if dr == 0:
                            S.dma("sp", OF_d[li * 128:(li + 1) * 128, :], ob_[:, :], reads=[ob_], writes=[T_OF])
                        else:
                            S.dma("sp", OB_d[li * 128:(li + 1) * 128, :], ob_[:, :], reads=[ob_], writes=[T_OB])
                        yield
                    for h in range(4):
                        pr = h // 2
                        S.op("pe", lambda h=h, pr=pr: pe.matmul(ps_U[:, h * 128:(h + 1) * 128], lhsT=c.qk[:, 256 + pr * 128:256 + (pr + 1) * 128], rhs=v_[:, h * 128:(h + 1) * 128], start=True, stop=True), [c.qk, v_], [ps_U])
                    for h in range(4):
                        pr, P0 = h // 2, (h % 2) * 64
                        S.op("dve", lambda h=h, pr=pr, P0=P0: dve.tensor_scalar(out=c.tU[P0:P0 + 64, :], in0=ps_U[P0:P0 + 64, h * 128:(h + 1) * 128], scalar1=c.at[P0:P0 + 64, pr:pr + 1], scalar2=None, op0=ALU.mult), [ps_U, c.at], [c.tU])
                        S.op("dve", lambda pr=pr, P0=P0: dve.scalar_tensor_tensor(out=c.Sst[pr][P0:P0 + 64, :], in0=c.Sst[pr][P0:P0 + 64, :], scalar=c.at[P0:P0 + 64, pr:pr + 1], in1=c.tU[P0:P0 + 64, :], op0=ALU.mult, op1=ALU.add), [c.Sst[pr], c.at, c.tU], [c.Sst[pr]])
                        S.op("dve", lambda pr=pr, P0=P0: dve.tensor_copy(out=c.Sbf[pr][P0:P0 + 64, :], in_=c.Sst[pr][P0:P0 + 64, :]), [c.Sst[pr]], [c.Sbf[pr]])
                    yield

            gens = [chain(0, alloc(0)), chain(1, alloc(1))]
            while gens:
                for g in list(gens):
                    try:
                        next(g)
                    except StopIteration:
                        gens.remove(g)

            gg = [K.sb(es, [128, 512], F32, "gg%d" % i) for i in range(2)]
            ofl = [K.sb(es, [128, 512], F32, "ofl%d" % i) for i in range(2)]
            obl = [K.sb(es, [128, 512], F32, "obl%d" % i) for i in range(2)]
            osm = [K.sb(es, [128, 512], F32, "osm%d" % i) for i in range(2)]
            junk = [K.sb(es, [128, 128], F32, "junkc%d" % i) for i in range(2)]
            ss4 = [K.sb(es, [128, 4], F32, "ss4_%d" % i) for i in range(2)]
            rs4 = [K.sb(es, [128, 4], F32, "rs4_%d" % i) for i in range(2)]
            sg = [K.sb(es, [128, 512], F32, "sg%d" % i) for i in range(2)]
            ogl = [K.sb(es, [128, 512], BF16, "ogl%d" % i) for i in range(2)]

            def merge(li):
                b = li % 2
                S.dma("sp", gg[b][:, :], GG_d[li * 128:(li + 1) * 128, :], reads=[T_GG], writes=[gg[b]])
                S.dma("sp", ofl[b][:, :], OF_d[li * 128:(li + 1) * 128, :], reads=[T_OF], writes=[ofl[b]])
                S.dma("sp", obl[b][:, :], OB_d[li * 128:(li + 1) * 128, :], reads=[T_OB], writes=[obl[b]])
                yield
                S.op("pool", lambda: pool.tensor_tensor(out=osm[b][:, :], in0=ofl[b][:, :], in1=obl[b][:, :], op=ALU.add), [ofl[b], obl[b]], [osm[b]])
                S.op("act", lambda: act.activation(out=sg[b][:, :], in_=gg[b][:, :], func=AF.Silu), [gg[b]], [sg[b]])
                yield
                for h in range(4):
                    S.op("act", lambda h=h: act.activation(out=junk[b][:, :], in_=osm[b][:, h * 128:(h + 1) * 128], func=AF.Square, accum_out=ss4[b][:, h:h + 1]), [osm[b]], [junk[b], ss4[b]])
                S.op("act", lambda: act.activation(out=rs4[b][:, :], in_=ss4[b][:, :], func=AF.Sqrt, scale=1.0 / 128.0, bias=epsb[:, :]), [ss4[b], epsb], [rs4[b]])
                yield
                S.op("dve", lambda: dve.reciprocal(out=rs4[b][:, :], in_=rs4[b][:, :]), [rs4[b]], [rs4[b]])
                o3 = osm[b][:, :].rearrange("p (h e) -> p h e", h=4)
                S.op("dve", lambda: dve.tensor_tensor(out=o3, in0=o3, in1=rs4[b][:, :].unsqueeze(2).broadcast_to([128, 4, 128]), op=ALU.mult), [osm[b], rs4[b]], [osm[b]])
                yield
                S.op("pool", lambda: pool.tensor_tensor(out=o3, in0=o3, in1=gnb[:, :].unsqueeze(1).broadcast_to([128, 4, 128]), op=ALU.mult), [osm[b], gnb], [osm[b]])
                S.op("dve", lambda: dve.tensor_tensor(out=ogl[b][:, :], in0=osm[b][:, :], in1=sg[b][:, :], op=ALU.mult), [osm[b], sg[b]], [ogl[b]])
                S.dma("sp", MIX_d[li * 128:(li + 1) * 128, 512:1024], ogl[b][:, :], reads=[ogl[b]], writes=[T_MIXgla])
                yield

            pend = []
            for li in range(32):
                pend.append(merge(li))
                if len(pend) == 2 or li == 31:
                    live = list(pend)
                    while live:
                        for g in list(live):
                            try:
                                next(g)
                            except StopIteration:
                                live.remove(g)
                    pend = []

    T_XN = T(None, "XN")
    T_HF = T(None, "HF")
    T_AFFT = T(None, "AFFT")
    AX = mybir.AxisListType.X

    def stageD():
        with contextlib.ExitStack() as es:
            wo = K.sb(es, [128, 8, D], BF16, "wo")
            wst = [K.sb(es, [128, 8, 512], F32, "wstd%d" % i) for i in range(2)]
            wov = w_out.rearrange("(c p) n -> p c n", p=128)
            for b in range(2):
                S.dma("sp", wst[b][:, :, :], wov[:, :, b * 512:(b + 1) * 512], writes=[wst[b]])
                S.op("act", lambda b=b: act.copy(out=wo[:, :, b * 512:(b + 1) * 512], in_=wst[b][:, :, :]), [wst[b]], [wo])
            rt = K.sb(es, [128, 8, NE], F32, "rt")
            S.dma("sp", rt[:, :, :], router.rearrange("(c p) e -> p c e", p=128), writes=[rt])
            md = K.sb(es, [128, 3, D], F32, "md")
            S.dma("sp", md[:, :, :], mods_d[:, 4:7, :], reads=[T_mods], writes=[md])
            affT = K.sb(es, [NE, SEQ], F32, "affT")
            affR = [T(None, "affR%d" % i) for i in range(32)]
            NL = 3
            dbl = lambda shape, dt, nm: [K.sb(es, shape, dt, "%s%d" % (nm, i)) for i in range(NL)]
            mixb = dbl([128, D], BF16, "mixb")
            xt = dbl([128, D], F32, "xtd")
            mixT = dbl([128, 8, 128], BF16, "mixT")
            mixs = dbl([128, D], F32, "mixs")
            junk = dbl([128, D], BF16, "junkd")
            ss = dbl([128, 1], F32, "ssd")
            rstd = dbl([128, 1], F32, "rstdd")
            t1 = dbl([128, D], F32, "t1d")
            xn = dbl([128, D], F32, "xn")
            hf32 = dbl([128, D], F32, "hf32")
            hfb = dbl([128, D], BF16, "hfb")
            hfT = dbl([128, 8, 128], F32, "hfT")
            lg = dbl([128, NE], F32, "lg")
            mx = dbl([128, 1], F32, "mx")
            ex = dbl([128, NE], F32, "ex")
            sm = dbl([128, 1], F32, "sm")
            af = dbl([128, NE], F32, "af")
            ps_t = K.ps(es, [128, 1024], BF16, "psd_t")
            ps_m = [K.ps(es, [128, 512], F32, "psd_m%d" % i) for i in range(2)]
            ps_r = [K.ps(es, [128, 512], F32, "psd_r%d" % i) for i in range(2)]
            ps_l = K.ps(es, [128, 512], F32, "psd_l")
            ps_a = K.ps(es, [128, 512], F32, "psd_a")

            def tile_prog(T_):
                b = T_ % NL
                mb, x_ = mixb[b], xt[b]
                S.dma("sp", mb[:, :], MIX_d[T_ * 128:(T_ + 1) * 128, :], reads=[T_MIXna, T_MIXgla], writes=[mb])
                S.dma("sp", x_[:, :], xc[CTX + T_ * 128:CTX + (T_ + 1) * 128, :], writes=[x_])
                yield
                for c in range(8):
                    S.op("pe", lambda c=c: pe.transpose(out=ps_t[:, c * 128:(c + 1) * 128], in_=mb[:, c * 128:(c + 1) * 128], identity=idb[:, :]), [mb, idb], [ps_t])
                S.op("act", lambda: act.copy(out=mixT[b][:, :, :], in_=ps_t[:, :].rearrange("p (c t) -> p c t", c=8)), [ps_t], [mixT[b]])
                yield
                for hlf in range(2):
                    for c in range(8):
                        S.op("pe", lambda c=c, hlf=hlf: pe.matmul(ps_m[hlf][:, :], lhsT=mixT[b][:, c, :], rhs=wo[:, c, hlf * 512:(hlf + 1) * 512], start=(c == 0), stop=(c == 7)), [mixT[b], wo], [ps_m[hlf]])
                    S.op("act", lambda hlf=hlf: act.copy(out=mixs[b][:, hlf * 512:(hlf + 1) * 512], in_=ps_m[hlf][:, :]), [ps_m[hlf]], [mixs[b]])
                yield
                rms_rstd(None, mixs[b][:, :], mixs[b], ss[b], rstd[b], junk[b])
                yield
                S.op("dve", lambda: dve.scalar_tensor_tensor(out=t1[b][:, :], in0=mixs[b][:, :], scalar=rstd[b][:, 0:1], in1=md[:, 0, :], op0=ALU.mult, op1=ALU.mult), [mixs[b], rstd[b], md], [t1[b]])
                xn_ = xn[b]
                S.op("pool", lambda: pool.tensor_tensor(out=xn_[:, :], in0=t1[b][:, :], in1=x_[:, :], op=ALU.add), [t1[b], x_], [xn_])
                S.dma("sp", XN_d[T_ * 128:(T_ + 1) * 128, :], xn_[:, :], reads=[xn_], writes=[T_XN])
                yield
                rms_rstd(None, xn_[:, :], xn_, ss[b], rstd[b], junk[b])
                yield
                S.op("dve", lambda: dve.scalar_tensor_tensor(out=t1[b][:, :], in0=xn_[:, :], scalar=rstd[b][:, 0:1], in1=md[:, 1, :], op0=ALU.mult, op1=ALU.mult), [xn_, rstd[b], md], [t1[b]])
                S.op("pool", lambda: pool.tensor_tensor(out=hf32[b][:, :], in0=t1[b][:, :], in1=md[:, 2, :], op=ALU.add), [t1[b], md], [hf32[b]])
                yield
                hb_ = hfb[b]
                S.op("act", lambda: act.copy(out=hb_[:, :], in_=hf32[b][:, :]), [hf32[b]], [hb_])
                S.dma("sp", HF_d[T_ * 128:(T_ + 1) * 128, :], hb_[:, :], reads=[hb_], writes=[T_HF])
                for c in range(8):
                    S.op("pe", lambda c=c: pe.transpose(out=ps_r[c // 4][:, (c % 4) * 128:(c % 4 + 1) * 128], in_=hf32[b][:, c * 128:(c + 1) * 128], identity=idf[:, :]), [hf32[b], idf], [ps_r[c // 4]])
                for i in range(2):
                    S.op("act", lambda i=i: act.copy(out=hfT[b][:, i * 4:(i + 1) * 4, :], in_=ps_r[i][:, :].rearrange("p (c t) -> p c t", c=4)), [ps_r[i]], [hfT[b]])
                yield
                for c in range(8):
                    S.op("pe", lambda c=c: pe.matmul(ps_l[:, 0:NE], lhsT=hfT[b][:, c, :], rhs=rt[:, c, :], start=(c == 0), stop=(c == 7)), [hfT[b], rt], [ps_l])
                S.op("dve", lambda: dve.tensor_copy(out=lg[b][:, :], in_=ps_l[:, 0:NE]), [ps_l], [lg[b]])
                yield
                S.op("dve", lambda: dve.reduce_max(out=mx[b][:, :], in_=lg[b][:, :], axis=AX), [lg[b]], [mx[b]])
                S.op("dve", lambda: dve.tensor_scalar(out=mx[b][:, :], in0=mx[b][:, :], scalar1=-1.0, scalar2=None, op0=ALU.mult), [mx[b]], [mx[b]])
                yield
                S.op("act", lambda: act.activation(out=ex[b][:, :], in_=lg[b][:, :], func=AF.Exp, bias=mx[b][:, :], accum_out=sm[b][:, :]), [lg[b], mx[b]], [ex[b], sm[b]])
                yield
                S.op("dve", lambda: dve.reciprocal(out=sm[b][:, :], in_=sm[b][:, :]), [sm[b]], [sm[b]])
                S.op("dve", lambda: dve.tensor_scalar(out=af[b][:, :], in0=ex[b][:, :], scalar1=sm[b][:, 0:1], scalar2=None, op0=ALU.mult), [ex[b], sm[b]], [af[b]])
                yield
                S.op("pe", lambda: pe.transpose(out=ps_a[0:NE, 0:128], in_=af[b][:, :], identity=idf[:, :]), [af[b], idf], [ps_a])
                S.op("act", lambda: act.copy(out=affT[:, T_ * 128:(T_ + 1) * 128], in_=ps_a[0:NE, 0:128]), [ps_a], [affR[T_]])
                yield

            for T0 in range(0, 32, NL):
                live = [tile_prog(T_) for T_ in range(T0, min(T0 + NL, 32))]
                while live:
                    for g in list(live):
                        try:
                            next(g)
                        except StopIteration:
                            live.remove(g)
            S.dma("sp", AFFT_d, affT[:, :], reads=affR, writes=[T_AFFT])


    posT_d = dscr("posT_d", [128, 4, 64])
    gateT_d = dscr("gateT_d", [128, 4, 64])
    HFO_d = dscr("HFO_d", [SEQ // 2, D], BF16)
    T_HFO = T(None, "HFO")

    def stageEFG():
        import os
        with contextlib.ExitStack() as es:
            posT = K.sb(es, [128, 4, 64], F32, "posT")
            gateT = K.sb(es, [128, 4, 64], F32, "gateT")
            iot = K.sb(es, [128, 512], F32, "iot")
            S.dma("sp", iot[:, :], iota512, writes=[iot])
            PB = [K.ps(es, [128, 512], F32, "PB%d" % i) for i in range(8)]
            with contextlib.ExitStack() as e2:
                A = K.sb(e2, [128, 512], F32, "A")
                S.dma("sp", A[:, :], AFFT_d.rearrange("e (s t) -> (e s) t", s=8), reads=[T_AFFT], writes=[A])
                b1 = K.sb(e2, [128, 128], F32, "b1")
                blt = K.sb(e2, [128, 128], F32, "blt")
                ownf = K.sb(e2, [128, 1], F32, "ownf")
                selm = K.sb(e2, [128, 64], F32, "selm")
                for t_, src in ((b1, blk_ones), (blt, blk_lt), (ownf, own_flag), (selm, sel)):
                    S.dma("sp", t_[:, :], src, writes=[t_])
                lo = K.sb(e2, [128, 1], F32, "lo")
                hi = K.sb(e2, [128, 1], F32, "hi")
                mid = K.sb(e2, [128, 1], F32, "mid")
                cnt = K.sb(e2, [128, 1], F32, "cnt")
                cond = K.sb(e2, [128, 1], F32, "cond")
                d1 = K.sb(e2, [128, 1], F32, "d1")
                jk = K.sb(e2, [128, 512], F32, "jk")
                onesT = K.sb(e2, [128, 512], F32, "onesT")
                M_ = K.sb(e2, [128, 512], F32, "M_")
                posi = K.sb(e2, [128, 512], F32, "posi")
                pm = K.sb(e2, [128, 512], F32, "pm")
                offc = K.sb(e2, [128, 1], F32, "offc")
                S.op("pool", lambda: pool.memset(lo[:, :], 0.0), [], [lo])
                S.op("pool", lambda: pool.memset(hi[:, :], 1.0), [], [hi])
                S.op("pool", lambda: pool.memset(onesT[:, :], 1.0), [], [onesT])
                pc = PB[0]
                for it in range(30):
                    S.op("dve", lambda: dve.tensor_tensor(out=mid[:, :], in0=lo[:, :], in1=hi[:, :], op=ALU.add), [lo, hi], [mid])
                    S.op("dve", lambda: dve.tensor_scalar(out=mid[:, :], in0=mid[:, :], scalar1=0.5, scalar2=None, op0=ALU.mult), [mid], [mid])
                    S.op("dve", lambda: dve.tensor_scalar(out=jk[:, :], in0=A[:, :], scalar1=mid[:, 0:1], scalar2=0.0, op0=ALU.is_gt, op1=ALU.add, accum_out=cnt[:, 0:1]), [A, mid], [jk, cnt])
                    S.op("pe", lambda: pe.matmul(pc[:, 0:1], lhsT=b1[:, :], rhs=cnt[:, 0:1], start=True, stop=True), [b1, cnt], [pc])
                    S.op("dve", lambda: dve.tensor_scalar(out=cond[:, :], in0=pc[:, 0:1], scalar1=float(CAP) - 0.5, scalar2=None, op0=ALU.is_gt), [pc], [cond])
                    S.op("dve", lambda: dve.tensor_tensor(out=d1[:, :], in0=mid[:, :], in1=lo[:, :], op=ALU.subtract), [mid, lo], [d1])
                    S.op("dve", lambda: dve.scalar_tensor_tensor(out=lo[:, :], in0=d1[:, :], scalar=cond[:, 0:1], in1=lo[:, :], op0=ALU.mult, op1=ALU.add), [d1, cond, lo], [lo])
                    S.op("dve", lambda: dve.tensor_tensor(out=d1[:, :], in0=hi[:, :], in1=mid[:, :], op=ALU.subtract), [hi, mid], [d1])
                    S.op("dve", lambda: dve.scalar_tensor_tensor(out=hi[:, :], in0=d1[:, :], scalar=cond[:, 0:1], in1=mid[:, :], op0=ALU.mult, op1=ALU.add), [d1, cond, mid], [hi])
                S.op("dve", lambda: dve.tensor_scalar(out=M_[:, :], in0=A[:, :], scalar1=lo[:, 0:1], scalar2=ownf[:, 0:1], op0=ALU.is_gt, op1=ALU.mult), [A, lo, ownf], [M_])
                S.op("dve", lambda: dve.tensor_tensor_scan(out=posi[:, :], data0=onesT[:, :], data1=M_[:, :], initial=0.0, op0=ALU.mult, op1=ALU.add), [onesT, M_], [posi])
                S.op("pe", lambda: pe.matmul(pc[:, 0:1], lhsT=blt[:, :], rhs=posi[:, 511:512], start=True, stop=True), [blt, posi], [pc])
                S.op("dve", lambda: dve.tensor_scalar(out=offc[:, :], in0=pc[:, 0:1], scalar1=-10000.0, scalar2=None, op0=ALU.add), [pc], [offc])
                S.op("dve", lambda: dve.tensor_scalar(out=pm[:, :], in0=posi[:, :], scalar1=offc[:, 0:1], scalar2=None, op0=ALU.add), [posi, offc], [pm])
                S.op("dve", lambda: dve.tensor_tensor(out=pm[:, :], in0=pm[:, :], in1=M_[:, :], op=ALU.mult), [pm, M_], [pm])
                S.op("dve", lambda: dve.tensor_scalar(out=pm[:, :], in0=pm[:, :], scalar1=9999.0, scalar2=None, op0=ALU.add), [pm], [pm])
                for src, dst, pb in ((pm, posT, PB[1]), (A, gateT, PB[2])):
                    for blk in range(4):
                        S.op("pe", lambda src=src, pb=pb, blk=blk: pe.matmul(pb[:, blk * 64:(blk + 1) * 64], lhsT=src[:, blk * 128:(blk + 1) * 128], rhs=selm[:, :], start=True, stop=True), [src, selm], [pb])
                    S.op("act", lambda dst=dst, pb=pb: act.copy(out=dst[:, :, :], in_=pb[:, 0:256].rearrange("p (b c) -> p b c", b=4)), [pb], [dst])
                if debug:
                    S.dma("sp", posT_d, posT[:, :, :], reads=[posT])
                    S.dma("sp", gateT_d, gateT[:, :, :], reads=[gateT])
                S.barrier()
            S.barrier()
            if upto == "E":
                return
            oi = K.sb(es, [128, 16], I32, "oi")
            S.dma("sp", oi[:, :], own_idx, writes=[oi])
            with contextlib.ExitStack() as e4:
                hfj = [K.sb(e4, [128, D], BF16, "hfj%d" % i) for i in range(3)]
                for j in range(16):
                    hj = hfj[j % 3]
                    S.dma("pool", None, None, reads=[T_HF, oi], writes=[hj], fn=lambda hj=hj, j=j: pool.indirect_dma_start(
                        out=hj[:, :], out_offset=None, in_=HF_d, in_offset=bass.IndirectOffsetOnAxis(ap=oi[:, j:j + 1], axis=0)))
                    S.dma("sp", HFO_d[j * 128:(j + 1) * 128, :], hj[:, :], reads=[hj], writes=[T_HFO])
                S.barrier()
            yacc = K.sb(es, [128, 16, D], F32, "yacc")
            S.op("pool", lambda: pool.memset(yacc[:, :, :], 0.0), [], [yacc])
            e3 = contextlib.ExitStack()
            xsT = K.sb(e3, [128, 8, 512], BF16, "xsT")
            hidT = K.sb(e3, [128, NFC, 512], BF16, "hidT")
            ysb = K.sb(e3, [128, 4, D], BF16, "ysb")
            Ej = [K.sb(e3, [128, 512], BF16, "Ej%d" % i) for i in range(2)]
            Gj = [K.sb(e3, [128, 512], BF16, "Gj%d" % i) for i in range(2)]
            tidt = K.sb(e3, [128, 16, 2], BF16, "tidt")
            S.dma("sp", tidt[:, :, :], tid_in, writes=[tidt])
            xg = [K.sb(e3, [128, 4, D], BF16, "xg%d" % i) for i in range(2)]
            xgT = [[T(None, "xgT") for _ in range(4)] for _ in range(2)]
            idxs = K.sb(e3, [128, 8], F32, "idxs")
            idxf = K.sb(e3, [128, 4], F32, "idxf")
            idxi = [K.sb(e3, [128, 4], I32, "idxi%d" % i) for i in range(2)]
            GTa = K.sb(e3, [128, 16, 4, 128], BF16, "GTa")
            GTt = [T(None, "GTt%d" % i) for i in range(16)]
            sgt = [K.sb(e3, [128, 512], BF16, "sgt%d" % i) for i in range(2)]
            stg = [K.sb(e3, [128, 2048], F32, "stg%d" % i) for i in range(4)]
            nst = [0]

            def stage_in(src_ap, ncols):
                st = stg[nst[0] % 4]
                q = os.environ.get("WQ", "sp").split(",")
                q = q[nst[0] % len(q)]
                nst[0] += 1
                S.dma(q, st[:, 0:ncols], src_ap, writes=[st])
                return st
            wgb = [K.sb(e3, [128, 8, 256], BF16, "wgb%d" % i) for i in range(2)]
            wub = [K.sb(e3, [128, 8, 256], BF16, "wub%d" % i) for i in range(2)]
            wdb = K.sb(e3, [128, NFC, 256], BF16, "wdb")
            wdbT = [T(None, "wdbT%d" % i) for i in range(3)]
            NEX = int(os.environ.get("NEX", str(NE)))
            ng = 0
            PH = int(os.environ.get("PH", "15"))
            def route(e):
                xb = e % 2
                for j in range(16):
                    osg, blk = j // 4, j % 4
                    E_ = Ej[j % 2]
                    S.op("dve", lambda E_=E_, blk=blk, col=e * 4 + osg: dve.tensor_scalar(out=E_[:, :], in0=iot[:, :], scalar1=posT[:, blk, col:col + 1], scalar2=None, op0=ALU.is_equal), [iot, posT], [E_])
                    for sc in range(4):
                        S.op("pe", lambda sc=sc, E_=E_, j=j: pe.matmul(PB[sc][:, 0:2], lhsT=E_[:, sc * 128:(sc + 1) * 128], rhs=tidt[:, j, :], start=(j == 0), stop=(j == 15)), [E_, tidt], [PB[sc]])
                for sc in range(4):
                    S.op("dve", lambda sc=sc: dve.tensor_copy(out=idxs[:, sc * 2:(sc + 1) * 2], in_=PB[sc][:, 0:2]), [PB[sc]], [idxs])
                iv = idxs[:, :].rearrange("p (s two) -> p s two", two=2)
                S.op("dve", lambda: dve.scalar_tensor_tensor(out=idxf[:, :], in0=iv[:, :, 0], scalar=64.0, in1=iv[:, :, 1], op0=ALU.mult, op1=ALU.add), [idxs], [idxf])
                S.op("dve", lambda: dve.tensor_copy(out=idxi[xb][:, :], in_=idxf[:, :]), [idxf], [idxi[xb]])
                for sc in range(4):
                    S.dma("pool", None, None, reads=[T_HFO, idxi[xb]], writes=[xgT[xb][sc]], fn=lambda sc=sc: pool.indirect_dma_start(
                        out=xg[xb][:, sc, :], out_offset=None, in_=HFO_d, in_offset=bass.IndirectOffsetOnAxis(ap=idxi[xb][:, sc:sc + 1], axis=0)))

            if PH & 1:
                route(0)
            for e in range(NEX):
                xb = e % 2
                for c in (range(8) if PH & 1 else []):
                    bb = PB[c // 2][:, :].bitcast(BF16)
                    for sc in range(4):
                        S.op("pe", lambda c=c, sc=sc, bb=bb: pe.transpose(out=bb[:, (c % 2) * 512 + sc * 128:(c % 2) * 512 + (sc + 1) * 128], in_=xg[xb][:, sc, c * 128:(c + 1) * 128], identity=idb[:, :]), [xgT[xb][sc], idb], [PB[c // 2]])
                for c in (range(8) if PH & 1 else []):
                    bb = PB[c // 2][:, :].bitcast(BF16)
                    if (c // 2) % 2 == 0:
                        S.op("act", lambda c=c, bb=bb: act.copy(out=xsT[:, c, :], in_=bb[:, (c % 2) * 512:(c % 2 + 1) * 512]), [PB[c // 2]], [xsT])
                    else:
                        S.op("dve", lambda c=c, bb=bb: dve.tensor_copy(out=xsT[:, c, :], in_=bb[:, (c % 2) * 512:(c % 2 + 1) * 512]), [PB[c // 2]], [xsT])

                def prep(j):
                    osg, blk = j // 4, j % 4
                    col = e * 4 + osg
                    G_ = Gj[j % 2]
                    S.op("dve", lambda: dve.tensor_scalar(out=G_[:, :], in0=iot[:, :], scalar1=posT[:, blk, col:col + 1], scalar2=gateT[:, blk, col:col + 1], op0=ALU.is_equal, op1=ALU.mult), [iot, posT, gateT], [G_])
                    pt = PB[6 + j % 2]
                    ptb = pt[:, :].bitcast(BF16)
                    for sc in range(4):
                        S.op("pe", lambda sc=sc: pe.transpose(out=ptb[:, sc * 128:(sc + 1) * 128], in_=G_[:, sc * 128:(sc + 1) * 128], identity=idb[:, :]), [G_, idb], [pt])
                    S.op("act", lambda: act.copy(out=GTa[:, j, :, :], in_=ptb[:, 0:512].rearrange("p (s t) -> p s t", s=4)), [pt], [GTt[j]])

                jn = 0

                def load_gu(e_, g_):
                    gb_ = (e_ * 11 + g_) % 2
                    sg_st = stage_in(w_gate[e_, g_], 2048)
                    su_st = stage_in(w_up[e_, g_], 2048)
                    S.op("act", lambda: act.copy(out=wgb[gb_][:, :, :].rearrange("p c f -> p (c f)"), in_=sg_st[:, :]), [sg_st], [wgb[gb_]])
                    S.op("dve", lambda: dve.tensor_copy(out=wub[gb_][:, :, :].rearrange("p c f -> p (c f)"), in_=su_st[:, :]), [su_st], [wub[gb_]])

                if e == 0:
                    load_gu(0, 0)
                for g in (range(11) if PH & 2 else []):
                    gb = (e * 11 + g) % 2
                    if g + 1 < 11:
                        load_gu(e, g + 1)
                    for fl in range(2):
                        fc = g * 2 + fl
                        pg, pu = PB[(fc % 2) * 2], PB[(fc % 2) * 2 + 1]
                        for c in range(8):
                            S.op("pe", lambda c=c, pg=pg, gb=gb, fl=fl: pe.matmul(pg[:, :], lhsT=wgb[gb][:, c, fl * 128:(fl + 1) * 128], rhs=xsT[:, c, :], start=(c == 0), stop=(c == 7)), [wgb[gb], xsT], [pg])
                        for c in range(8):
                            S.op("pe", lambda c=c, pu=pu, gb=gb, fl=fl: pe.matmul(pu[:, :], lhsT=wub[gb][:, c, fl * 128:(fl + 1) * 128], rhs=xsT[:, c, :], start=(c == 0), stop=(c == 7)), [wub[gb], xsT], [pu])
                        sg_ = sgt[fc % 2]
                        S.op("act", lambda pg=pg, sg_=sg_: act.activation(out=sg_[:, :], in_=pg[:, :], func=AF.Silu), [pg], [sg_])
                        S.op("dve", lambda pu=pu, sg_=sg_, fc=fc: dve.tensor_tensor(out=hidT[:, fc, :], in0=sg_[:, :], in1=pu[:, :], op=ALU.mult), [sg_, pu], [hidT])
                    for _ in range(2 if g < 5 else 1):
                        if jn < 16:
                            prep(jn)
                            jn += 1
                while jn < 16 and (PH & 8):
                    prep(jn)
                    jn += 1
                if e + 1 < NEX and (PH & 1):
                    route(e + 1)
                pieces = ((0, 8), (8, 16), (16, NFC))
                def dma_d(cb_):
                    return [stage_in(w_down[e, cb_, :, f0 * 256:f1 * 256], (f1 - f0) * 256) for (f0, f1) in pieces]

                def cast_d(k, st):
                    f0, f1 = pieces[k]
                    S.op("act", lambda: act.copy(out=wdb[:, f0:f1, :].rearrange("p c n -> p (c n)"), in_=st[:, 0:(f1 - f0) * 256]), [st], [wdbT[k]])

                sts = dma_d(0)
                for k in range(3):
                    cast_d(k, sts[k])
                for cb in (range(4) if PH & 4 else []):
                    nxt = dma_d(cb + 1) if cb + 1 < 4 else None
                    for k, (f0, f1) in enumerate(pieces):
                        for fc in range(f0, f1):
                            for sc in range(4):
                                S.op("pe", lambda fc=fc, sc=sc: pe.matmul(PB[4 + sc][:, 0:256], lhsT=hidT[:, fc, sc * 128:(sc + 1) * 128], rhs=wdb[:, fc, :], start=(fc == 0), stop=(fc == NFC - 1)), [hidT, wdbT[k]], [PB[4 + sc]])
                        if nxt is not None:
                            cast_d(k, nxt[k])
                    for sc in range(4):
                        if sc % 2 == 0:
                            S.op("act", lambda sc=sc, cb=cb: act.copy(out=ysb[:, sc, cb * 256:(cb + 1) * 256], in_=PB[4 + sc][:, 0:256]), [PB[4 + sc]], [ysb])
                        else:
                            S.op("dve", lambda sc=sc, cb=cb: dve.tensor_copy(out=ysb[:, sc, cb * 256:(cb + 1) * 256], in_=PB[4 + sc][:, 0:256]), [PB[4 + sc]], [ysb])
                if e + 1 < NEX:
                    load_gu(e + 1, 0)
                for j in (range(16) if PH & 8 else []):
                    for hlf in range(2):
                        pyc = PB[(j % 2) * 2 + hlf]
                        for sc in range(4):
                            S.op("pe", lambda sc=sc, pyc=pyc, hlf=hlf, j=j: pe.matmul(pyc[:, :], lhsT=GTa[:, j, sc, :], rhs=ysb[:, sc, hlf * 512:(hlf + 1) * 512], start=(sc == 0), stop=(sc == 3)), [GTt[j], ysb], [pyc])
                        S.op("dve", lambda pyc=pyc, j=j, hlf=hlf: dve.tensor_tensor(out=yacc[:, j, hlf * 512:(hlf + 1) * 512], in0=yacc[:, j, hlf * 512:(hlf + 1) * 512], in1=pyc[:, :], op=ALU.add), [yacc, pyc], [yacc])
            S.barrier()
            e3.close()
            gF = K.sb(es, [128, D], F32, "gF")
            S.dma("sp", gF[:, :], mods_d[:, 7, :], reads=[T_mods], writes=[gF])
            xno = [K.sb(es, [128, D], F32, "xno%d" % i) for i in range(2)]
            ob = [K.sb(es, [128, D], F32, "ob%d" % i) for i in range(2)]
            junk = K.sb(es, [128, D], BF16, "junkg")
            ss = K.sb(es, [128, 1], F32, "ssg")
            rstd = K.sb(es, [128, 1], F32, "rstdg")
            for j in range(16):
                b = j % 2
                S.dma("pool", None, None, reads=[T_XN, oi], writes=[xno[b]], fn=lambda b=b, j=j: pool.indirect_dma_start(
                    out=xno[b][:, :], out_offset=None, in_=XN_d, in_offset=bass.IndirectOffsetOnAxis(ap=oi[:, j:j + 1], axis=0)))
                S.op("act", lambda j=j: act.activation(out=junk[:, :], in_=yacc[:, j, :], func=AF.Square, accum_out=ss[:, :]), [yacc], [junk, ss])
                S.op("act", lambda: act.activation(out=rstd[:, :], in_=ss[:, :], func=AF.Sqrt, scale=1.0 / D, bias=epsb[:, :]), [ss, epsb], [rstd])
                S.op("dve", lambda: dve.reciprocal(out=rstd[:, :], in_=rstd[:, :]), [rstd], [rstd])
                S.op("dve", lambda j=j, b=b: dve.scalar_tensor_tensor(out=ob[b][:, :], in0=yacc[:, j, :], scalar=rstd[:, 0:1], in1=gF[:, :], op0=ALU.mult, op1=ALU.mult), [yacc, rstd, gF], [ob[b]])
                S.op("pool", lambda b=b: pool.tensor_tensor(out=ob[b][:, :], in0=ob[b][:, :], in1=xno[b][:, :], op=ALU.add), [ob[b], xno[b]], [ob[b]])
                S.dma("sp", out[j * 128:(j + 1) * 128, :], ob[b][:, :], reads=[ob[b]])

    stages = [("0", stage0), ("A", stageA), ("B", stageB), ("C", stageC), ("D", stageD), ("E", stageEFG)]
    if upto in ("F", "G", "all"):
        stages[-1] = (upto, stageEFG)
    for name, fn in stages:
        fn()
        S.barrier()
        if upto == name:
            break
    S.finish()
    glob.close()
    return nc


def prep_inputs(inputs, cores=range(8), with_experts=True):
    f = lambda k: np.ascontiguousarray(np.asarray(inputs[k], dtype=np.float32))
    x, c, ctx, c_ctx = f("x"), f("c"), f("ctx"), f("c_ctx")
    consts = _consts()
    cos, sin = _rope_tables()
    rpb = f("na_rpb")[0]
    nab = np.ascontiguousarray(_na_bias_tables(rpb).reshape(5, 8, 128, 640))
    a_up = f("gla_a_up")[0]
    a_bias = f("gla_a_bias")[0]
    aup_bd = np.zeros((32, 512), np.float32)
    aup_bd[0:16, 0:256] = a_up[0]
    aup_bd[16:32, 256:512] = a_up[1]
    abias = np.ascontiguousarray(a_bias.reshape(1, 512))
    norms = np.ascontiguousarray(np.stack([f("norm_mix_pre")[0], f("norm_mix_post")[0], f("norm_ffn_pre")[0], f("norm_ffn_post")[0]]))
    shared = dict(
        w_mod=f("w_mod")[0], b_mod=f("b_mod"), norms=norms, gla_norm=f("gla_norm"), w_in=f("w_in")[0],
        aup_bd=aup_bd, abias=abias, rope_cos=cos, rope_sin=sin, na_bias=nab, w_out=f("w_out")[0],
        router=f("router")[0], **consts)
    if with_experts:
        tile_gu = lambda w: np.ascontiguousarray(w.reshape(NE, 8, 128, 11, 256).transpose(0, 3, 2, 1, 4)).reshape(NE, 11, 128, 8 * 256)
        shared.update(w_gate=tile_gu(f("w_gate")[0]), w_up=tile_gu(f("w_up")[0]),
                      w_down=np.ascontiguousarray(f("w_down")[0].reshape(NE, NFC, 128, 4, 256).transpose(0, 3, 2, 1, 4)).reshape(NE, 4, 128, NFC * 256))
    maps = []
    pp = np.arange(128)
    for core in cores:
        s, p = core // 2, core % 2
        m = dict(shared)
        m["xc"] = np.ascontiguousarray(np.concatenate([ctx[s], x[s]], axis=0))
        cv = np.zeros((128, 16), np.float32)
        cv[:, 0:8] = c[s].reshape(8, 128).T
        cv[:, 8:16] = c_ctx.reshape(8, 128).T
        m["cvec"] = cv
        m["own_idx"] = np.ascontiguousarray((p * 2048 + np.arange(16)[None, :] * 128 + pp[:, None]).astype(np.int32))
        seg = pp % 8
        m["own_flag"] = ((seg // 4) == p).astype(np.float32).reshape(128, 1)
        selm = np.zeros((128, 64), np.float32)
        for e in range(16):
            for os_ in range(4):
                selm[e * 8 + p * 4 + os_, e * 4 + os_] = 1.0
        m["sel"] = selm
        maps.append(m)
    return maps


_PROGRAM = None


def kernel(**inputs):
    global _PROGRAM
    if _PROGRAM is None:
        _PROGRAM = build_program()
    maps = prep_inputs(inputs)
    res = run_bass_kernel_spmd(_PROGRAM, maps, core_ids=list(range(8)))
    out = np.zeros((4, SEQ, D), np.float32)
    for core in range(8):
        s, p = core // 2, core % 2
        out[s, p * 2048:(p + 1) * 2048] = res.results[core]["out"]
    return out
```

```python
import contextlib
import numpy as np
import ml_dtypes
import concourse.bass as bass
import concourse.mybir as mybir
from concourse.bass_utils import run_bass_kernel_spmd

F32 = mybir.dt.float32
BF16 = mybir.dt.bfloat16
I32 = mybir.dt.int32
AF = mybir.ActivationFunctionType
ALU = mybir.AluOpType

D = 1024
SEQ = 4096
CTX = 256
NTOK = SEQ + CTX
NT = NTOK // 128
INC = 3104
NE = 16
DE = 2816
NFC = DE // 128
CAP = 512
EPS = 1e-6
NEG = -30000.0


class T:
    __slots__ = ("ap", "w", "r", "name")

    def __init__(self, ap, name=""):
        self.ap = ap
        self.w = None
        self.r = {}
        self.name = name

    def __getitem__(self, idx):
        return self.ap[idx]


class Sync:
    def __init__(self, nc, n_sp=20, n_pool=8, n_act=6):
        self.nc = nc
        self.E = {"pe": nc.tensor, "act": nc.scalar, "dve": nc.vector, "pool": nc.gpsimd, "sp": nc.sync}
        self.sem = {}
        self.cnt = {}
        for k in ("pe", "act", "dve", "pool"):
            self.sem[k] = nc.alloc_semaphore("s_" + k)
            self.cnt[k] = 0
        self.seen = {k: {} for k in self.E}
        self.dq = {}
        for q, n in (("sp", n_sp), ("pool", n_pool), ("act", n_act)):
            lst = []
            for i in range(n):
                key = "d_%s%d" % (q, i)
                self.sem[key] = nc.alloc_semaphore(key)
                self.cnt[key] = 0
                lst.append(key)
            self.dq[q] = [lst, 0]

    def _wait(self, ek, key, val):
        if val <= 0:
            return
        if self.seen[ek].get(key, 0) >= val:
            return
        self.E[ek].wait_ge(self.sem[key], val)
        self.seen[ek][key] = val

    def _deps(self, ek, reads, writes):
        need = {}
        for t in reads:
            if t.w is not None:
                k, v = t.w
                need[k] = max(need.get(k, 0), v)
        for t in writes:
            if t.w is not None:
                k, v = t.w
                if not (k == "pe" and ek == "pe"):
                    need[k] = max(need.get(k, 0), v)
            for k, v in t.r.items():
                if not (k == "pe" and ek == "pe"):
                    need[k] = max(need.get(k, 0), v)
        for k, v in need.items():
            self._wait(ek, k, v)

    def op(self, ek, fn, reads=(), writes=()):
        self._deps(ek, reads, writes)
        ins = fn()
        self.cnt[ek] += 1
        ins.then_inc(self.sem[ek], 1)
        me = (ek, self.cnt[ek])
        for t in reads:
            t.r[ek] = self.cnt[ek]
        for t in writes:
            t.w = me
            t.r = {}
        return ins

    def dma(self, q, out, in_, reads=(), writes=(), fn=None, **kw):
        lst, i = self.dq[q]
        key = lst[i % len(lst)]
        self.dq[q][1] = i + 1
        self._wait(q, key, self.cnt[key])
        self._deps(q, reads, writes)
        if fn is None:
            ins = self.E[q].dma_start(out=out, in_=in_, **kw)
        else:
            ins = fn()
        self.cnt[key] += 16
        ins.then_inc(self.sem[key], 16)
        for t in reads:
            t.r[key] = self.cnt[key]
        for t in writes:
            t.w = (key, self.cnt[key])
            t.r = {}
        return ins

    def barrier(self):
        for ek in self.E:
            for k, v in self.cnt.items():
                if not (k == ek == "pe"):
                    self._wait(ek, k, v)

    def finish(self):
        for k, v in self.cnt.items():
            self._wait("sp", k, v)


class Ctx:
    def __init__(self, nc):
        self.nc = nc
        self.S = Sync(nc)
        self.uid = 0

    def sb(self, es, shape, dt, name):
        self.uid += 1
        return T(es.enter_context(self.nc.sbuf_tensor("%s_%d" % (name, self.uid), list(shape), dt)), name)

    def ps(self, es, shape, dt, name):
        self.uid += 1
        return T(es.enter_context(self.nc.psum_tensor("%s_%d" % (name, self.uid), list(shape), dt)), name)


def _rope_tables():
    half = 16
    freqs = (np.float32(10000.0) ** (-np.arange(half, dtype=np.float32) / np.float32(half))).astype(np.float32)
    t = np.arange(SEQ)
    pr = (t // 64).astype(np.float32)[:, None] * freqs
    pc = (t % 64).astype(np.float32)[:, None] * freqs
    ang = np.concatenate([pr, pc], axis=1).astype(np.float32)
    return np.cos(ang).astype(np.float32), np.sin(ang).astype(np.float32)


def _na_key_tile0(T_):
    return min(max(T_ - 2, 0), 27)


def _na_bias_tables(rpb):
    out = np.full((5, 8, 128, 5, 128), NEG, np.float32)
    for si, T_ in enumerate((0, 1, 2, 30, 31)):
        kt0 = _na_key_tile0(T_)
        for qi in range(128):
            r = 2 * T_ + qi // 64
            qc = qi % 64
            rs = min(max(r - 4, 0), 56)
            cs = min(max(qc - 8, 0), 48)
            for kr in range(rs, rs + 8):
                kt = kr // 2 - kt0
                assert 0 <= kt < 5
                dr = kr - r + 7
                kc = np.arange(cs, cs + 16)
                dc = np.clip(kc - qc + 15, 0, 30)
                j = (kr % 2) * 64 + kc
                out[si, :, j, kt, qi] = rpb[:, dr, dc].T
    return out


def _consts():
    c = {}
    c["ident_f"] = np.eye(128, dtype=np.float32)
    c["ident_b"] = np.eye(128, dtype=np.float32).astype(ml_dtypes.bfloat16)
    tt = np.arange(128)
    c["tri_f"] = np.where(tt[:, None] <= tt[None, :], -1.0 / 16.0, 0.0).astype(np.float32)
    c["tri_b"] = np.where(tt[:, None] >= tt[None, :], -1.0 / 16.0, 0.0).astype(np.float32)
    c["mask_f"] = (tt[:, None] <= tt[None, :]).astype(np.float32)
    c["mask_b"] = (tt[:, None] >= tt[None, :]).astype(np.float32)
    c["ones_col"] = np.full((128, 1), -1.0 / 16.0, np.float32)
    c["ones_row"] = np.ones((1, 128), np.float32)
    g = tt // 8
    c["blk_ones"] = (g[:, None] == g[None, :]).astype(np.float32)
    c["blk_lt"] = ((g[:, None] == g[None, :]) & (tt[:, None] < tt[None, :])).astype(np.float32)
    c["iota512"] = np.broadcast_to(np.arange(512, dtype=np.float32)[None, :], (128, 512)).copy()
    own = np.arange(16)[None, :] * 128 + np.arange(128)[:, None]
    c["tid"] = np.stack([own // 64, own % 64], axis=-1).astype(np.float32).astype(ml_dtypes.bfloat16)
    return c


def build_program(upto="all", debug=False):
    nc = bass.Bass("TRN2", target_bir_lowering=False)
    K = Ctx(nc)
    S = K.S
    pe, act, dve, pool = nc.tensor, nc.scalar, nc.vector, nc.gpsimd

    def din(name, shape, dt=F32):
        return nc.dram_tensor(name, list(shape), dt, kind="ExternalInput").ap()

    import os as _os
    _ext = set(_os.environ.get("EXTSET", "").split(","))

    def dscr(name, shape, dt=F32):
        return nc.dram_tensor(name, list(shape), dt, kind=("ExternalOutput" if (debug or name in _ext) else "Internal")).ap()

    xc = din("xc", [NTOK, D])
    cvec = din("cvec", [128, 16])
    w_mod = din("w_mod", [D, 6 * D])
    b_mod = din("b_mod", [1, 6 * D])
    norms = din("norms", [4, D])
    gla_norm = din("gla_norm", [1, 128])
    w_in = din("w_in", [D, INC])
    aup_bd = din("aup_bd", [32, 512])
    abias = din("abias", [1, 512])
    rope_cos = din("rope_cos", [SEQ, 32])
    rope_sin = din("rope_sin", [SEQ, 32])
    na_bias = din("na_bias", [5, 8, 128, 640])
    w_out = din("w_out", [D, D])
    router = din("router", [D, NE])
    if upto in ("F", "G", "all"):
        w_gate = din("w_gate", [NE, 11, 128, 8 * 256])
        w_up = din("w_up", [NE, 11, 128, 8 * 256])
        w_down = din("w_down", [NE, 4, 128, NFC * 256])
    ident_f = din("ident_f", [128, 128])
    ident_b = din("ident_b", [128, 128], BF16)
    tri_f = din("tri_f", [128, 128])
    tri_b = din("tri_b", [128, 128])
    mask_f = din("mask_f", [128, 128])
    mask_b = din("mask_b", [128, 128])
    ones_col = din("ones_col", [128, 1])
    ones_row = din("ones_row", [1, 128])
    blk_ones = din("blk_ones", [128, 128])
    blk_lt = din("blk_lt", [128, 128])
    iota512 = din("iota512", [128, 512])
    tid_in = din("tid", [128, 16, 2], BF16)
    own_idx = din("own_idx", [128, 16], I32)
    own_flag = din("own_flag", [128, 1])
    sel = din("sel", [128, 64])
    out = nc.dram_tensor("out", [SEQ // 2, D], F32, kind="ExternalOutput").ap()

    mods_d = dscr("mods_d", [128, 8, D])
    KT_d = dscr("KT_d", [512, NTOK], BF16)
    QT_d = dscr("QT_d", [512, SEQ], BF16)
    V_d = dscr("V_d", [NTOK, 512], BF16)
    GV_d = dscr("GV_d", [NTOK, 512], BF16)
    GKQ_d = dscr("GKQ_d", [NTOK, 544])
    GG_d = dscr("GG_d", [SEQ, 512])
    OF_d = dscr("OF_d", [SEQ, 512])
    MIX_d = dscr("MIX_d", [SEQ, D], BF16)
    XN_d = dscr("XN_d", [SEQ, D])
    HF_d = dscr("HF_d", [SEQ, D], BF16)
    AFFT_d = dscr("AFFT_d", [NE, SEQ])

    glob = contextlib.ExitStack()
    idf = K.sb(glob, [128, 128], F32, "idf")
    idb = K.sb(glob, [128, 128], BF16, "idb")
    onesr = K.sb(glob, [1, 128], F32, "onesr")
    S.dma("sp", idf[:, :], ident_f, writes=[idf])
    S.dma("sp", idb[:, :], ident_b, writes=[idb])
    S.dma("sp", onesr[:, :], ones_row, writes=[onesr])

    def stage0():
        with contextlib.ExitStack() as es:
            cv = K.sb(es, [128, 16], F32, "cv")
            csil = K.sb(es, [128, 16], F32, "csil")
            crep = K.sb(es, [128, 16, 128], F32, "crep")
            bm = K.sb(es, [1, 6 * D], F32, "bm")
            modA = K.sb(es, [128, 6 * D], F32, "modA")
            modC = K.sb(es, [128, 2 * D], F32, "modC")
            nbc = K.sb(es, [128, 4, D], F32, "nbc")
            wm = [K.sb(es, [128, 8, 512], F32, "wm%d" % i) for i in range(2)]
            mods = K.sb(es, [128, 8, D], F32, "mods")
            psA = [K.ps(es, [128, 512], F32, "psA%d" % i) for i in range(2)]
            psC = [K.ps(es, [128, 512], F32, "psC%d" % i) for i in range(2)]
            S.dma("sp", cv[:, :], cvec, writes=[cv])
            S.dma("sp", bm[:, :], b_mod, writes=[bm])
            S.dma("sp", nbc[:, :, :], norms.partition_broadcast(128), writes=[nbc])
            S.op("act", lambda: act.activation(out=csil[:, :], in_=cv[:, :], func=AF.Silu), [cv], [csil])
            S.op("dve", lambda: dve.tensor_copy(out=crep[:, :, :], in_=csil[:, :].unsqueeze(2).broadcast_to([128, 16, 128])), [csil], [crep])
            wmv = w_mod.rearrange("(c p) n -> p c n", p=128)
            for nb in range(12):
                w = wm[nb % 2]
                S.dma("sp", w[:, :, :], wmv[:, :, nb * 512:(nb + 1) * 512], writes=[w])
                for which in range(2 if nb < 4 else 1):
                    ps = (psA, psC)[which][nb % 2]
                    for c in range(8):
                        S.op("pe", lambda c=c, ps=ps, w=w, which=which: pe.matmul(ps[:, :], lhsT=crep[:, which * 8 + c, :], rhs=w[:, c, :], start=(c == 0), stop=False), [crep, w], [ps])
                    S.op("pe", lambda ps=ps, nb=nb: pe.matmul(ps[:, :], lhsT=onesr[:, :], rhs=bm[:, nb * 512:(nb + 1) * 512], start=False, stop=True), [onesr, bm], [ps])
                    dst = (modA, modC)[which]
                    if which == 0:
                        S.op("act", lambda ps=ps, dst=dst, nb=nb: act.copy(out=dst[:, nb * 512:(nb + 1) * 512], in_=ps[:, :]), [ps], [dst])
                    else:
                        S.op("dve", lambda ps=ps, dst=dst, nb=nb: dve.tensor_copy(out=dst[:, nb * 512:(nb + 1) * 512], in_=ps[:, :]), [ps], [dst])
            import os
            if os.environ.get("STOP0") == "1":
                S.dma("sp", mods_d[:, 0:6, :], modA[:, :].rearrange("p (a b) -> p a b", a=6), reads=[modA], writes=[T_mods])
                return
            sl = lambda t, i: t[:, i * D:(i + 1) * D]
            S.op("dve", lambda: dve.scalar_tensor_tensor(out=mods[:, 0, :], in0=sl(modA, 1), scalar=1.0, in1=nbc[:, 0, :], op0=ALU.add, op1=ALU.mult), [modA, nbc], [mods])
            S.op("dve", lambda: dve.tensor_copy(out=mods[:, 1, :], in_=sl(modA, 0)), [modA], [mods])
            S.op("dve", lambda: dve.scalar_tensor_tensor(out=mods[:, 2, :], in0=sl(modC, 1), scalar=1.0, in1=nbc[:, 0, :], op0=ALU.add, op1=ALU.mult), [modC, nbc], [mods])
            S.op("dve", lambda: dve.tensor_copy(out=mods[:, 3, :], in_=sl(modC, 0)), [modC], [mods])
            S.op("dve", lambda: dve.tensor_tensor(out=mods[:, 4, :], in0=sl(modA, 2), in1=nbc[:, 1, :], op=ALU.mult), [modA, nbc], [mods])
            S.op("dve", lambda: dve.scalar_tensor_tensor(out=mods[:, 5, :], in0=sl(modA, 4), scalar=1.0, in1=nbc[:, 2, :], op0=ALU.add, op1=ALU.mult), [modA, nbc], [mods])
            S.op("dve", lambda: dve.tensor_copy(out=mods[:, 6, :], in_=sl(modA, 3)), [modA], [mods])
            S.op("dve", lambda: dve.tensor_tensor(out=mods[:, 7, :], in0=sl(modA, 5), in1=nbc[:, 3, :], op=ALU.mult), [modA, nbc], [mods])
            S.dma("sp", mods_d, mods[:, :, :], reads=[mods], writes=[T_mods])

    T_mods = T(None, "mods_d")
    T_KT, T_QT, T_V, T_GV, T_GKQ, T_GG = (T(None, n) for n in ("KT", "QT", "V", "GV", "GKQ", "GG"))

    def rms_rstd(es_tiles, src, src_t, ss, rstd, junk):
        S.op("act", lambda: act.activation(out=junk[:, :], in_=src, func=AF.Square, accum_out=ss[:, :]), [src_t], [junk, ss])
        S.op("act", lambda: act.activation(out=rstd[:, :], in_=ss[:, :], func=AF.Sqrt, scale=1.0 / D, bias=epsb[:, :]), [ss, epsb], [rstd])
        S.op("dve", lambda: dve.reciprocal(out=rstd[:, :], in_=rstd[:, :]), [rstd], [rstd])

    epsb = K.sb(glob, [128, 1], F32, "epsb")
    S.op("pool", lambda: pool.memset(epsb[:, :], EPS), [], [epsb])

    def stageA():
        with contextlib.ExitStack() as es:
            win = K.sb(es, [128, 8, INC], BF16, "win")
            wst = [K.sb(es, [128, 8, 512], F32, "wst%d" % i) for i in range(2)]
            gs = K.sb(es, [128, 4, D], F32, "gs")
            S.dma("sp", gs[:, :, :], mods_d[:, 0:4, :], reads=[T_mods], writes=[gs])
            wiv = w_in.rearrange("(c p) n -> p c n", p=128)
            for b in range(7):
                c0 = b * 512
                w_ = min(512, INC - c0)
                st = wst[b % 2]
                S.dma("sp", st[:, :, 0:w_], wiv[:, :, c0:c0 + w_], writes=[st])
                if b % 2 == 0:
                    S.op("act", lambda st=st, c0=c0, w_=w_: act.copy(out=win[:, :, c0:c0 + w_], in_=st[:, :, 0:w_]), [st], [win])
                else:
                    S.op("dve", lambda st=st, c0=c0, w_=w_: dve.tensor_copy(out=win[:, :, c0:c0 + w_], in_=st[:, :, 0:w_]), [st], [win])
            import os
            STOPA = int(os.environ.get("STOPA", "99"))
            if STOPA <= 1:
                return
            xt = [K.sb(es, [128, D], F32, "xt%d" % i) for i in range(2)]
            junk = K.sb(es, [128, D], BF16, "junk")
            ss = [K.sb(es, [128, 1], F32, "ss%d" % i) for i in range(2)]
            rstd = [K.sb(es, [128, 1], F32, "rstd%d" % i) for i in range(2)]
            h1 = [K.sb(es, [128, D], F32, "h1_%d" % i) for i in range(2)]
            hb = [K.sb(es, [128, D], BF16, "hb%d" % i) for i in range(2)]
            hT = [K.sb(es, [128, 8, 512], BF16, "hT%d" % i) for i in range(2)]
            ktq = [K.sb(es, [128, 512], BF16, "ktq%d" % i) for i in range(2)]
            vo = [K.sb(es, [128, 512], BF16, "vo%d" % i) for i in range(2)]
            gvo = [K.sb(es, [128, 512], BF16, "gvo%d" % i) for i in range(2)]
            gkq = [K.sb(es, [128, 544], F32, "gkq%d" % i) for i in range(2)]
            ggo = [K.sb(es, [128, 512], F32, "ggo%d" % i) for i in range(2)]
            pst = [K.ps(es, [128, 8 * 128], BF16, "pst%d" % i) for i in range(2)]
            psp = [K.ps(es, [128, 512], F32, "psp%d" % i) for i in range(4)]
            npp = [0]

            def nextps():
                npp[0] += 1
                return psp[npp[0] % 4]

            ev = [0]

            def evac(dst_t, dst_ap, ps, ps_ap):
                if psp.index(ps) % 2 == 0:
                    S.op("act", lambda: act.copy(out=dst_ap, in_=ps_ap), [ps], [dst_t])
                else:
                    S.op("dve", lambda: dve.tensor_copy(out=dst_ap, in_=ps_ap), [ps], [dst_t])

            groups = [(0, 2)] + [(2 + 4 * g, 4) for g in range(8)]
            hb4 = hb + [K.sb(es, [128, D], BF16, "hbx%d" % i) for i in range(2)]
            tcnt = [0]

            def modulate(gi):
                t0, ntl = groups[gi]
                for k in range(ntl):
                    ti = t0 + k
                    b2 = tcnt[0] % 2
                    tcnt[0] += 1
                    x_ = xt[b2]
                    S.dma("sp", x_[:, :], xc[ti * 128:(ti + 1) * 128, :], writes=[x_])
                    rms_rstd(None, x_[:, :], x_, ss[b2], rstd[b2], junk)
                    go = 2 if gi == 0 else 0
                    h_ = h1[b2]
                    S.op("dve", lambda: dve.scalar_tensor_tensor(out=h_[:, :], in0=x_[:, :], scalar=rstd[b2][:, 0:1], in1=gs[:, go, :], op0=ALU.mult, op1=ALU.mult), [x_, rstd[b2], gs], [h_])
                    S.op("pool", lambda: pool.tensor_tensor(out=hb4[k][:, :], in0=h_[:, :], in1=gs[:, go + 1, :], op=ALU.add), [h_, gs], [hb4[k]])

            def transposes(gi):
                t0, ntl = groups[gi]
                hTg_ = hT[gi % 2]
                for k in range(ntl):
                    pt = pst[k % 2]
                    for c in range(8):
                        S.op("pe", lambda c=c: pe.transpose(out=pt[:, c * 128:(c + 1) * 128], in_=hb4[k][:, c * 128:(c + 1) * 128], identity=idb[:, :]), [hb4[k], idb], [pt])
                    S.op("act", lambda: act.copy(out=hTg_[:, :, k * 128:(k + 1) * 128], in_=pt[:, :].rearrange("p (c t) -> p c t", c=8)), [pt], [hTg_])

            modulate(0)
            transposes(0)
            for gi, (t0, ntl) in enumerate(groups):
                is_ctx = gi == 0
                hTg = hT[gi % 2]
                if gi + 1 < len(groups):
                    modulate(gi + 1)
                ntok = ntl * 128
                tok0 = t0 * 128
                for which, c0 in ((0, 0), (1, 1824)):
                    if which == 1 and is_ctx:
                        continue
                    for fb in range(4):
                        ps = nextps()
                        for c in range(8):
                            S.op("pe", lambda c=c, ps=ps, fb=fb, c0=c0: pe.matmul(ps[:, 0:ntok], lhsT=win[:, c, c0 + fb * 128:c0 + (fb + 1) * 128], rhs=hTg[:, c, 0:ntok], start=(c == 0), stop=(c == 7)), [win, hTg], [ps])
                        kb = ktq[(which * 4 + fb) % 2]
                        evac(kb, kb[:, 0:ntok], ps, ps[:, 0:ntok])
                        if which == 0:
                            S.dma("sp", KT_d[fb * 128:(fb + 1) * 128, tok0:tok0 + ntok], kb[:, 0:ntok], reads=[kb], writes=[T_KT])
                        else:
                            S.dma("sp", QT_d[fb * 128:(fb + 1) * 128, tok0 - CTX:tok0 - CTX + ntok], kb[:, 0:ntok], reads=[kb], writes=[T_QT])
                for k in range(ntl):
                    ti = t0 + k
                    b2 = ti % 2
                    r0 = ti * 128

                    def mm(c0, w_, k=k):
                        ps = nextps()
                        for c in range(8):
                            S.op("pe", lambda c=c, ps=ps: pe.matmul(ps[:, 0:w_], lhsT=hTg[:, c, k * 128:(k + 1) * 128], rhs=win[:, c, c0:c0 + w_], start=(c == 0), stop=(c == 7)), [win, hTg], [ps])
                        return ps

                    ps = mm(512, 512)
                    evac(vo[b2], vo[b2][:, :], ps, ps[:, :])
                    S.dma("sp", V_d[r0:r0 + 128, :], vo[b2][:, :], reads=[vo[b2]], writes=[T_V])
                    TM = int(os.environ.get("TM", "9"))
                    if TM <= 1:
                        continue
                    ps = mm(1024, 512)
                    EV = int(os.environ.get("EV", "3"))
                    if EV & 1:
                        evac(gkq[b2], gkq[b2][:, 0:256], ps, ps[:, 0:256])
                    if EV & 2:
                        evac(gvo[b2], gvo[b2][:, 0:256], ps, ps[:, 256:512])
                    if TM <= 2:
                        continue
                    ps = mm(1536, 288)
                    evac(gvo[b2], gvo[b2][:, 256:512], ps, ps[:, 0:256])
                    evac(gkq[b2], gkq[b2][:, 256:288], ps, ps[:, 256:288])
                    S.dma("sp", GV_d[r0:r0 + 128, :], gvo[b2][:, :], reads=[gvo[b2]], writes=[T_GV])
                    if TM <= 3:
                        continue
                    if not is_ctx:
                        ps = mm(2336, 512)
                        evac(gkq[b2], gkq[b2][:, 288:544], ps, ps[:, 0:256])
                        evac(ggo[b2], ggo[b2][:, 0:256], ps, ps[:, 256:512])
                        ps = mm(2848, 256)
                        evac(ggo[b2], ggo[b2][:, 256:512], ps, ps[:, 0:256])
                        S.dma("sp", GG_d[r0 - CTX:r0 - CTX + 128, :], ggo[b2][:, :], reads=[ggo[b2]], writes=[T_GG])
                        S.dma("sp", GKQ_d[r0:r0 + 128, :], gkq[b2][:, :], reads=[gkq[b2]], writes=[T_GKQ])
                    else:
                        S.dma("sp", GKQ_d[r0:r0 + 128, 0:288], gkq[b2][:, 0:288], reads=[gkq[b2]], writes=[T_GKQ])
                if gi + 1 < len(groups):
                    transposes(gi + 1)


    T_MIXna = T(None, "MIXna")

    def stageB():
        with contextlib.ExitStack() as es:
            NB = 3
            qT = [K.sb(es, [128, 4, 128], BF16, "qT%d" % i) for i in range(2)]
            kT = [K.sb(es, [128, 4, 896], BF16, "kT%d" % i) for i in range(2)]
            va = [K.sb(es, [128, 7, 8, 65], BF16, "va%d" % i) for i in range(2)]
            bint = K.sb(es, [128, 8, 640], F32, "bint")
            bt = [K.sb(es, [128, 640], F32, "bt%d" % i) for i in range(3)]
            tmp = [K.sb(es, [128, 896], F32, "tmp%d" % i) for i in range(NB)]
            pT = [K.sb(es, [128, 896], BF16, "pT%d" % i) for i in range(NB + 1)]
            ona = [K.sb(es, [128, 512], BF16, "ona%d" % i) for i in range(2)]
            rec = [K.sb(es, [128, 1], F32, "rec%d" % i) for i in range(2)]
            sT = [K.ps(es, [128, 1024], F32, "sT%d" % i) for i in range(NB)]
            pv = [K.ps(es, [128, 512], F32, "pv%d" % i) for i in range(2)]
            for v in va:
                S.op("pool", lambda v=v: pool.memset(v[:, :, :, :], 1.0), [], [v])
            S.dma("sp", bint[:, :, :], na_bias[2].rearrange("h j c -> j h c"), writes=[bint])
            QTv = QT_d.rearrange("(c p) t -> p c t", p=128)
            KTv = KT_d.rearrange("(c p) t -> p c t", p=128)
            nbt = [0]
            tmR = [[T(None, "tmR") for _ in range(3)] for _ in range(NB)]
            onaR = [[T(None, "onaR") for _ in range(8)] for _ in range(2)]

            def loads(T_):
                b = T_ % 2
                kt0 = _na_key_tile0(T_)
                S.dma("sp", qT[b][:, :, :], QTv[:, :, T_ * 128:(T_ + 1) * 128], reads=[T_QT], writes=[qT[b]])
                S.dma("sp", kT[b][:, :, 0:640], KTv[:, :, CTX + kt0 * 128:CTX + kt0 * 128 + 640], reads=[T_KT], writes=[kT[b]])
                S.dma("sp", kT[b][:, :, 640:896], KTv[:, :, 0:CTX], reads=[T_KT], writes=[kT[b]])
                r0 = CTX + kt0 * 128
                for kk in range(5):
                    S.dma("sp", va[b][:, kk, :, 0:64], V_d[r0 + kk * 128:r0 + (kk + 1) * 128, :].rearrange("t (h d) -> t h d", h=8), reads=[T_V], writes=[va[b]])
                for kk in range(2):
                    S.dma("sp", va[b][:, 5 + kk, :, 0:64], V_d[kk * 128:(kk + 1) * 128, :].rearrange("t (h d) -> t h d", h=8), reads=[T_V], writes=[va[b]])

            def phase1(n):
                T_, h = n // 8, n % 8
                b = T_ % 2
                if h == 0:
                    loads(T_)
                si = {0: 0, 1: 1, 30: 3, 31: 4}.get(T_, 2)
                pr, P0 = h // 2, (h % 2) * 64
                s_, tm, pt = sT[n % NB], tmp[n % NB], pT[n % (NB + 1)]
                if si == 2:
                    bb_t, bb = bint, bint[:, h, :]
                else:
                    bb_t = bt[nbt[0] % 3]
                    nbt[0] += 1
                    bb = bb_t[:, :]
                    S.dma("sp", bb, na_bias[si, h], writes=[bb_t])
                for kk in range(7):
                    S.op("pe", lambda kk=kk: pe.matmul(s_[:, kk * 128:(kk + 1) * 128], lhsT=kT[b][P0:P0 + 64, pr, kk * 128:(kk + 1) * 128], rhs=qT[b][P0:P0 + 64, pr, :], start=True, stop=True), [kT[b], qT[b]], [s_])
                ta, tb, tc = tmR[n % NB]
                S.op("dve", lambda: dve.scalar_tensor_tensor(out=tm[:, 0:512], in0=s_[:, 0:512], scalar=0.125, in1=bb[:, 0:512], op0=ALU.mult, op1=ALU.add), [s_, bb_t], [ta])
                S.op("dve", lambda: dve.scalar_tensor_tensor(out=tm[:, 512:640], in0=s_[:, 512:640], scalar=0.125, in1=bb[:, 512:640], op0=ALU.mult, op1=ALU.add), [s_, bb_t], [tb])
                S.op("dve", lambda: dve.tensor_scalar(out=tm[:, 640:896], in0=s_[:, 640:896], scalar1=0.125, scalar2=None, op0=ALU.mult), [s_], [tc])
                S.op("act", lambda: act.activation(out=pt[:, :], in_=tm[:, :], func=AF.Exp), [ta, tb, tc], [pt])

            def phase2(n):
                T_, h = n // 8, n % 8
                b = T_ % 2
                pt, p_, rc = pT[n % (NB + 1)], pv[n % 2], rec[n % 2]
                for kk in range(7):
                    S.op("pe", lambda kk=kk: pe.matmul(p_[:, 0:65], lhsT=pt[:, kk * 128:(kk + 1) * 128], rhs=va[b][:, kk, h, :], start=(kk == 0), stop=(kk == 6)), [pt, va[b]], [p_])
                S.op("dve", lambda: dve.reciprocal(out=rc[:, :], in_=p_[:, 64:65]), [p_], [rc])
                S.op("dve", lambda: dve.tensor_scalar(out=ona[b][:, h * 64:(h + 1) * 64], in0=p_[:, 0:64], scalar1=rc[:, 0:1], scalar2=None, op0=ALU.mult), [p_, rc], [onaR[b][h]])
                if h == 7:
                    S.dma("sp", MIX_d[T_ * 128:(T_ + 1) * 128, 0:512], ona[b][:, :], reads=onaR[b], writes=[T_MIXna])

            NIT = 32 * 8
            LOOK = NB - 1
            for n in range(min(LOOK, NIT)):
                phase1(n)
            for n in range(NIT):
                if n + LOOK < NIT:
                    phase1(n + LOOK)
                phase2(n)

    T_OF = T(None, "OF")
    T_MIXgla = T(None, "MIXgla")

    OB_d = dscr("OB_d", [SEQ, 512])
    T_OB = T(None, "OB")

    def stageC():
        with contextlib.ExitStack() as es:
            tri = [K.sb(es, [128, 128], F32, "tri%d" % i) for i in range(2)]
            msk = [K.sb(es, [128, 512], F32, "msk%d" % i) for i in range(2)]
            onec = K.sb(es, [128, 1], F32, "onec")
            aup = K.sb(es, [32, 512], F32, "aup")
            abi = K.sb(es, [1, 512], F32, "abi")
            gnb = K.sb(es, [128, 128], F32, "gnb")
            for t_, src in ((tri[0], tri_f), (tri[1], tri_b), (onec, ones_col), (aup, aup_bd), (abi, abias)):
                S.dma("sp", t_[:, :], src, writes=[t_])
            for h in range(4):
                S.dma("sp", msk[0][:, h * 128:(h + 1) * 128], mask_f, writes=[msk[0]])
                S.dma("sp", msk[1][:, h * 128:(h + 1) * 128], mask_b, writes=[msk[1]])
            S.dma("sp", gnb[:, :], gla_norm.partition_broadcast(128).rearrange("p a b -> p (a b)"), writes=[gnb])
            ps_ad = K.ps(es, [128, 512], F32, "ps_ad")
            ps_z = K.ps(es, [128, 512], F32, "ps_z")
            ps_lam = K.ps(es, [128, 512], F32, "ps_lam")
            ps_t = K.ps(es, [128, 1024], BF16, "ps_t")
            ps_AT = K.ps(es, [128, 512], F32, "ps_AT")
            ps_o = K.ps(es, [128, 512], F32, "ps_o")
            ps_U = K.ps(es, [128, 512], F32, "ps_U")

            class B:
                pass

            def alloc(ci):
                b = B()
                n_ = lambda x: "%s_c%d" % (x, ci)
                b.gkq = [K.sb(es, [128, 544], F32, n_("gkq%d" % i)) for i in range(2)]
                b.gv = [K.sb(es, [128, 512], BF16, n_("gv%d" % i)) for i in range(2)]
                b.cs = [K.sb(es, [128, 32], F32, n_("cs%d" % i)) for i in range(2)]
                b.sn = [K.sb(es, [128, 32], F32, n_("sn%d" % i)) for i in range(2)]
                b.adT = K.sb(es, [32, 128], F32, n_("adT"))
                b.ez = K.sb(es, [128, 256], F32, n_("ez"))
                b.lz = K.sb(es, [128, 256], F32, n_("lz"))
                b.E1 = K.sb(es, [128, 256], F32, n_("E1"))
                b.E2 = K.sb(es, [128, 256], F32, n_("E2"))
                b.at = K.sb(es, [128, 2], F32, n_("at"))
                b.kr = K.sb(es, [128, 256], F32, n_("kr"))
                b.qr = K.sb(es, [128, 256], F32, n_("qr"))
                b.rt = [K.sb(es, [128, 128], F32, n_("rt%d" % i)) for i in range(4)]
                b.qk = K.sb(es, [128, 512], BF16, n_("qk"))
                b.qTz = K.sb(es, [128, 4, 128], BF16, n_("qTz"))
                S.op("pool", lambda: pool.memset(b.qTz[:, :, :], 0.0), [], [b.qTz])
                b.kTc = K.sb(es, [128, 2, 128], BF16, n_("kTc"))
                b.ATs = K.sb(es, [128, 4, 128], BF16, n_("ATs"))
                b.Sst = [K.sb(es, [128, 128], F32, n_("Sst%d" % i)) for i in range(2)]
                b.Sbf = [K.sb(es, [128, 128], BF16, n_("Sbf%d" % i)) for i in range(2)]
                b.tU = K.sb(es, [128, 128], F32, n_("tU"))
                b.osb = [K.sb(es, [128, 512], F32, n_("osb%d" % i)) for i in range(2)]
                return b

            def rope(eng, ek, src_t, src_ap, dst_t, tmps, c_, s_):
                X = src_ap.rearrange("p (h a b f) -> p h a b f", h=4, a=2, b=2)
                O = dst_t[:, :].rearrange("p (h a b f) -> p h a b f", h=4, a=2, b=2)
                C = c_[:, :].rearrange("p (a f) -> p a f", a=2).unsqueeze(1).broadcast_to([128, 4, 2, 16])
                Sn = s_[:, :].rearrange("p (a f) -> p a f", a=2).unsqueeze(1).broadcast_to([128, 4, 2, 16])
                v = lambda t_: t_[:, :].rearrange("p (h a f) -> p h a f", h=4, a=2)
                X1, X2 = X[:, :, :, 0, :], X[:, :, :, 1, :]
                t1, t2, t3, t4 = tmps
                S.op(ek, lambda: eng.tensor_tensor(out=v(t1), in0=X1, in1=C, op=ALU.mult), [src_t, c_], [t1])
                S.op(ek, lambda: eng.tensor_tensor(out=v(t2), in0=X2, in1=Sn, op=ALU.mult), [src_t, s_], [t2])
                S.op(ek, lambda: eng.tensor_tensor(out=O[:, :, :, 0, :], in0=v(t1), in1=v(t2), op=ALU.subtract), [t1, t2], [dst_t])
                S.op(ek, lambda: eng.tensor_tensor(out=v(t3), in0=X1, in1=Sn, op=ALU.mult), [src_t, s_], [t3])
                S.op(ek, lambda: eng.tensor_tensor(out=v(t4), in0=X2, in1=C, op=ALU.mult), [src_t, c_], [t4])
                S.op(ek, lambda: eng.tensor_tensor(out=O[:, :, :, 1, :], in0=v(t3), in1=v(t4), op=ALU.add), [t3, t4], [dst_t])

            def chain(dr, c):
                for pr in range(2):
                    S.op("pool", lambda pr=pr: pool.memset(c.Sst[pr][:, :], 0.0), [], [c.Sst[pr]])
                    S.op("pool", lambda pr=pr: pool.memset(c.Sbf[pr][:, :], 0.0), [], [c.Sbf[pr]])
                order = list(range(NT)) if dr == 0 else [1, 0] + list(range(NT - 1, 1, -1))
                n = 0
                for ti in order:
                    lat = ti >= 2
                    li = ti - 2
                    b = n % 2
                    n += 1
                    g_, v_ = c.gkq[b], c.gv[b]
                    gw = 544 if lat else 288
                    S.dma("sp", g_[:, 0:gw], GKQ_d[ti * 128:(ti + 1) * 128, 0:gw], reads=[T_GKQ], writes=[g_])
                    S.dma("sp", v_[:, :], GV_d[ti * 128:(ti + 1) * 128, :], reads=[T_GV], writes=[v_])
                    if lat:
                        S.dma("sp", c.cs[b][:, :], rope_cos[li * 128:(li + 1) * 128, :], writes=[c.cs[b]])
                        S.dma("sp", c.sn[b][:, :], rope_sin[li * 128:(li + 1) * 128, :], writes=[c.sn[b]])
                    yield
                    S.op("pe", lambda: pe.transpose(out=ps_ad[0:32, 0:128], in_=g_[:, 256:288], identity=idf[:, :]), [g_, idf], [ps_ad])
                    S.op("act", lambda: act.copy(out=c.adT[:, :], in_=ps_ad[0:32, 0:128]), [ps_ad], [c.adT])
                    S.op("pe", lambda: pe.matmul(ps_z[:, 0:256], lhsT=c.adT[:, :], rhs=aup[:, dr * 256:(dr + 1) * 256], start=True, stop=False), [c.adT, aup], [ps_z])
                    S.op("pe", lambda: pe.matmul(ps_z[:, 0:256], lhsT=onesr[:, :], rhs=abi[:, dr * 256:(dr + 1) * 256], start=False, stop=True), [onesr, abi], [ps_z])
                    S.op("act", lambda: act.activation(out=c.ez[:, :], in_=ps_z[:, 0:256], func=AF.Exp, scale=-1.0), [ps_z], [c.ez])
                    yield
                    S.op("act", lambda: act.activation(out=c.lz[:, :], in_=c.ez[:, :], func=AF.Ln, bias=1.0), [c.ez], [c.lz])
                    S.op("pe", lambda: pe.matmul(ps_lam[:, 0:256], lhsT=tri[dr][:, :], rhs=c.lz[:, :], start=True, stop=True), [tri[dr], c.lz], [ps_lam])
                    for pr in range(2):
                        S.op("pe", lambda pr=pr: pe.matmul(ps_lam[:, 256 + pr:257 + pr], lhsT=c.lz[:, pr * 128:(pr + 1) * 128], rhs=onec[:, :], start=True, stop=True), [c.lz, onec], [ps_lam])
                    if lat:
                        S.op("act", lambda: act.activation(out=c.E1[:, :], in_=ps_lam[:, 0:256], func=AF.Exp), [ps_lam], [c.E1])
                    S.op("act", lambda: act.activation(out=c.E2[:, :], in_=ps_lam[:, 0:256], func=AF.Exp, scale=-1.0), [ps_lam], [c.E2])
                    S.op("act", lambda: act.activation(out=c.at[:, :], in_=ps_lam[:, 256:258], func=AF.Exp), [ps_lam], [c.at])
                    yield
                    if lat:
                        rope(dve, "dve", g_, g_[:, 0:256], c.kr, c.rt, c.cs[b], c.sn[b])
                        yield
                        rope(dve, "dve", g_, g_[:, 288:544], c.qr, c.rt, c.cs[b], c.sn[b])
                        yield
                        S.op("dve", lambda: dve.scalar_tensor_tensor(out=c.qk[:, 0:256], in0=c.qr[:, :], scalar=0.125, in1=c.E1[:, :], op0=ALU.mult, op1=ALU.mult), [c.qr, c.E1], [c.qk])
                        S.op("dve", lambda: dve.tensor_tensor(out=c.qk[:, 256:512], in0=c.kr[:, :], in1=c.E2[:, :], op=ALU.mult), [c.kr, c.E2], [c.qk])
                    else:
                        S.op("dve", lambda: dve.tensor_tensor(out=c.qk[:, 256:512], in0=g_[:, 0:256], in1=c.E2[:, :], op=ALU.mult), [g_, c.E2], [c.qk])
                    for j in (range(4) if lat else range(2, 4)):
                        S.op("pe", lambda j=j: pe.transpose(out=ps_t[:, j * 128:(j + 1) * 128], in_=c.qk[:, j * 128:(j + 1) * 128], identity=idb[:, :]), [c.qk, idb], [ps_t])
                    if lat:
                        for h in range(4):
                            S.op("act", lambda h=h: act.copy(out=c.qTz[(h % 2) * 64:(h % 2) * 64 + 64, h, :], in_=ps_t[(h % 2) * 64:(h % 2) * 64 + 64, (h // 2) * 128:(h // 2 + 1) * 128]), [ps_t], [c.qTz])
                    S.op("act", lambda: act.copy(out=c.kTc[:, :, :], in_=ps_t[:, 256:512].rearrange("p (c t) -> p c t", c=2)), [ps_t], [c.kTc])
                    yield
                    if lat:
                        for h in range(4):
                            pr = h // 2
                            S.op("pe", lambda h=h, pr=pr: pe.matmul(ps_AT[:, h * 128:(h + 1) * 128], lhsT=c.kTc[:, pr, :], rhs=c.qTz[:, h, :], start=True, stop=True), [c.kTc, c.qTz], [ps_AT])
                        S.op("dve", lambda: dve.tensor_tensor(out=c.ATs[:, :, :].rearrange("p h t -> p (h t)"), in0=ps_AT[:, :], in1=msk[dr][:, :], op=ALU.mult), [ps_AT, msk[dr]], [c.ATs])
                        for h in range(4):
                            pr = h // 2
                            S.op("pe", lambda h=h: pe.matmul(ps_o[:, h * 128:(h + 1) * 128], lhsT=c.ATs[:, h, :], rhs=v_[:, h * 128:(h + 1) * 128], start=True, stop=False), [c.ATs, v_], [ps_o])
                            S.op("pe", lambda h=h, pr=pr: pe.matmul(ps_o[:, h * 128:(h + 1) * 128], lhsT=c.qTz[:, h, :], rhs=c.Sbf[pr][:, :], start=False, stop=True), [c.qTz, c.Sbf[pr]], [ps_o])
                        ob_ = c.osb[b]
                        S.op("dve", lambda: dve.tensor_copy(out=ob_[:, :], in_=ps_o[:, :]), [ps_o], [ob_])
                        if dr == 0:
                            S.dma("sp", OF_d[li * 128:(li + 1) * 128, :], ob_[:, :], reads=[ob_], writes=[T_OF])
                        else:
                            S.dma("sp", OB_d[li * 128:(li + 1) * 128, :], ob_[:, :], reads=[ob_], writes=[T_OB])
                        yield
                    for h in range(4):
                        pr = h // 2
                        S.op("pe", lambda h=h, pr=pr: pe.matmul(ps_U[:, h * 128:(h + 1) * 128], lhsT=c.qk[:, 256 + pr * 128:256 + (pr + 1) * 128], rhs=v_[:, h * 128:(h + 1) * 128], start=True, stop=True), [c.qk, v_], [ps_U])
                    for h in range(4):
                        pr, P0 = h // 2, (h % 2) * 64
                        S.op("dve", lambda h=h, pr=pr, P0=P0: dve.tensor_scalar(out=c.tU[P0:P0 + 64, :], in0=ps_U[P0:P0 + 64, h * 128:(h + 1) * 128], scalar1=c.at[P0:P0 + 64, pr:pr + 1], scalar2=None, op0=ALU.mult), [ps_U, c.at], [c.tU])
                        S.op("dve", lambda pr=pr, P0=P0: dve.scalar_tensor_tensor(out=c.Sst[pr][P0:P0 + 64, :], in0=c.Sst[pr][P0:P0 + 64, :], scalar=c.at[P0:P0 + 64, pr:pr + 1], in1=c.tU[P0:P0 + 64, :], op0=ALU.mult, op1=ALU.add), [c.Sst[pr], c.at, c.tU], [c.Sst[pr]])
                        S.op("dve", lambda pr=pr, P0=P0: dve.tensor_copy(out=c.Sbf[pr][P0:P0 + 64, :], in_=c.Sst[pr][P0:P0 + 64, :]), [c.Sst[pr]], [c.Sbf[pr]])
                    yield

            gens = [chain(0, alloc(0)), chain(1, alloc(1))]
            while gens:
                for g in list(gens):
                    try:
                        next(g)
                    except StopIteration:
                        gens.remove(g)

            gg = [K.sb(es, [128, 512], F32, "gg%d" % i) for i in range(2)]
            ofl = [K.sb(es, [128, 512], F32, "ofl%d" % i) for i in range(2)]
            obl = [K.sb(es, [128, 512], F32, "obl%d" % i) for i in range(2)]
            osm = [K.sb(es, [128, 512], F32, "osm%d" % i) for i in range(2)]
            junk = [K.sb(es, [128, 128], F32, "junkc%d" % i) for i in range(2)]
            ss4 = [K.sb(es, [128, 4], F32, "ss4_%d" % i) for i in range(2)]
            rs4 = [K.sb(es, [128, 4], F32, "rs4_%d" % i) for i in range(2)]
            sg = [K.sb(es, [128, 512], F32, "sg%d" % i) for i in range(2)]
            ogl = [K.sb(es, [128, 512], BF16, "ogl%d" % i) for i in range(2)]

            def merge(li):
                b = li % 2
                S.dma("sp", gg[b][:, :], GG_d[li * 128:(li + 1) * 128, :], reads=[T_GG], writes=[gg[b]])
                S.dma("sp", ofl[b][:, :], OF_d[li * 128:(li + 1) * 128, :], reads=[T_OF], writes=[ofl[b]])
                S.dma("sp", obl[b][:, :], OB_d[li * 128:(li + 1) * 128, :], reads=[T_OB], writes=[obl[b]])
                yield
                S.op("pool", lambda: pool.tensor_tensor(out=osm[b][:, :], in0=ofl[b][:, :], in1=obl[b][:, :], op=ALU.add), [ofl[b], obl[b]], [osm[b]])
                S.op("act", lambda: act.activation(out=sg[b][:, :], in_=gg[b][:, :], func=AF.Silu), [gg[b]], [sg[b]])
                yield
                for h in range(4):
                    S.op("act", lambda h=h: act.activation(out=junk[b][:, :], in_=osm[b][:, h * 128:(h + 1) * 128], func=AF.Square, accum_out=ss4[b][:, h:h + 1]), [osm[b]], [junk[b], ss4[b]])
                S.op("act", lambda: act.activation(out=rs4[b][:, :], in_=ss4[b][:, :], func=AF.Sqrt, scale=1.0 / 128.0, bias=epsb[:, :]), [ss4[b], epsb], [rs4[b]])
                yield
                S.op("dve", lambda: dve.reciprocal(out=rs4[b][:, :], in_=rs4[b][:, :]), [rs4[b]], [rs4[b]])
                o3 = osm[b][:, :].rearrange("p (h e) -> p h e", h=4)
                S.op("dve", lambda: dve.tensor_tensor(out=o3, in0=o3, in1=rs4[b][:, :].unsqueeze(2).broadcast_to([128, 4, 128]), op=ALU.mult), [osm[b], rs4[b]], [osm[b]])
                yield
                S.op("pool", lambda: pool.tensor_tensor(out=o3, in0=o3, in1=gnb[:, :].unsqueeze(1).broadcast_to([128, 4, 128]), op=ALU.mult), [osm[b], gnb], [osm[b]])
                S.op("dve", lambda: dve.tensor_tensor(out=ogl[b][:, :], in0=osm[b][:, :], in1=sg[b][:, :], op=ALU.mult), [osm[b], sg[b]], [ogl[b]])
                S.dma("sp", MIX_d[li * 128:(li + 1) * 128, 512:1024], ogl[b][:, :], reads=[ogl[b]], writes=[T_MIXgla])
                yield

            pend = []
            for li in range(32):
                pend.append(merge(li))
                if len(pend) == 2 or li == 31:
                    live = list(pend)
                    while live:
                        for g in list(live):
                            try:
                                next(g)
                            except StopIteration:
                                live.remove(g)
                    pend = []

    T_XN = T(None, "XN")
    T_HF = T(None, "HF")
    T_AFFT = T(None, "AFFT")
    AX = mybir.AxisListType.X

    def stageD():
        with contextlib.ExitStack() as es:
            wo = K.sb(es, [128, 8, D], BF16, "wo")
            wst = [K.sb(es, [128, 8, 512], F32, "wstd%d" % i) for i in range(2)]
            wov = w_out.rearrange("(c p) n -> p c n", p=128)
            for b in range(2):
                S.dma("sp", wst[b][:, :, :], wov[:, :, b * 512:(b + 1) * 512], writes=[wst[b]])
                S.op("act", lambda b=b: act.copy(out=wo[:, :, b * 512:(b + 1) * 512], in_=wst[b][:, :, :]), [wst[b]], [wo])
            rt = K.sb(es, [128, 8, NE], F32, "rt")
            S.dma("sp", rt[:, :, :], router.rearrange("(c p) e -> p c e", p=128), writes=[rt])
            md = K.sb(es, [128, 3, D], F32, "md")
            S.dma("sp", md[:, :, :], mods_d[:, 4:7, :], reads=[T_mods], writes=[md])
            affT = K.sb(es, [NE, SEQ], F32, "affT")
            affR = [T(None, "affR%d" % i) for i in range(32)]
            NL = 3
            dbl = lambda shape, dt, nm: [K.sb(es, shape, dt, "%s%d" % (nm, i)) for i in range(NL)]
            mixb = dbl([128, D], BF16, "mixb")
            xt = dbl([128, D], F32, "xtd")
            mixT = dbl([128, 8, 128], BF16, "mixT")
            mixs = dbl([128, D], F32, "mixs")
            junk = dbl([128, D], BF16, "junkd")
            ss = dbl([128, 1], F32, "ssd")
            rstd = dbl([128, 1], F32, "rstdd")
            t1 = dbl([128, D], F32, "t1d")
            xn = dbl([128, D], F32, "xn")
            hf32 = dbl([128, D], F32, "hf32")
            hfb = dbl([128, D], BF16, "hfb")
            hfT = dbl([128, 8, 128], F32, "hfT")
            lg = dbl([128, NE], F32, "lg")
            mx = dbl([128, 1], F32, "mx")
            ex = dbl([128, NE], F32, "ex")
            sm = dbl([128, 1], F32, "sm")
            af = dbl([128, NE], F32, "af")
            ps_t = K.ps(es, [128, 1024], BF16, "psd_t")
            ps_m = [K.ps(es, [128, 512], F32, "psd_m%d" % i) for i in range(2)]
            ps_r = [K.ps(es, [128, 512], F32, "psd_r%d" % i) for i in range(2)]
            ps_l = K.ps(es, [128, 512], F32, "psd_l")
            ps_a = K.ps(es, [128, 512], F32, "psd_a")

            def tile_prog(T_):
                b = T_ % NL
                mb, x_ = mixb[b], xt[b]
                S.dma("sp", mb[:, :], MIX_d[T_ * 128:(T_ + 1) * 128, :], reads=[T_MIXna, T_MIXgla], writes=[mb])
                S.dma("sp", x_[:, :], xc[CTX + T_ * 128:CTX + (T_ + 1) * 128, :], writes=[x_])
                yield
                for c in range(8):
                    S.op("pe", lambda c=c: pe.transpose(out=ps_t[:, c * 128:(c + 1) * 128], in_=mb[:, c * 128:(c + 1) * 128], identity=idb[:, :]), [mb, idb], [ps_t])
                S.op("act", lambda: act.copy(out=mixT[b][:, :, :], in_=ps_t[:, :].rearrange("p (c t) -> p c t", c=8)), [ps_t], [mixT[b]])
                yield
                for hlf in range(2):
                    for c in range(8):
                        S.op("pe", lambda c=c, hlf=hlf: pe.matmul(ps_m[hlf][:, :], lhsT=mixT[b][:, c, :], rhs=wo[:, c, hlf * 512:(hlf + 1) * 512], start=(c == 0), stop=(c == 7)), [mixT[b], wo], [ps_m[hlf]])
                    S.op("act", lambda hlf=hlf: act.copy(out=mixs[b][:, hlf * 512:(hlf + 1) * 512], in_=ps_m[hlf][:, :]), [ps_m[hlf]], [mixs[b]])
                yield
                rms_rstd(None, mixs[b][:, :], mixs[b], ss[b], rstd[b], junk[b])
                yield
                S.op("dve", lambda: dve.scalar_tensor_tensor(out=t1[b][:, :], in0=mixs[b][:, :], scalar=rstd[b][:, 0:1], in1=md[:, 0, :], op0=ALU.mult, op1=ALU.mult), [mixs[b], rstd[b], md], [t1[b]])
                xn_ = xn[b]
                S.op("pool", lambda: pool.tensor_tensor(out=xn_[:, :], in0=t1[b][:, :], in1=x_[:, :], op=ALU.add), [t1[b], x_], [xn_])
                S.dma("sp", XN_d[T_ * 128:(T_ + 1) * 128, :], xn_[:, :], reads=[xn_], writes=[T_XN])
                yield
                rms_rstd(None, xn_[:, :], xn_, ss[b], rstd[b], junk[b])
                yield
                S.op("dve", lambda: dve.scalar_tensor_tensor(out=t1[b][:, :], in0=xn_[:, :], scalar=rstd[b][:, 0:1], in1=md[:, 1, :], op0=ALU.mult, op1=ALU.mult), [xn_, rstd[b], md], [t1[b]])
                S.op("pool", lambda: pool.tensor_tensor(out=hf32[b][:, :], in0=t1[b][:, :], in1=md[:, 2, :], op=ALU.add), [t1[b], md], [hf32[b]])
                yield
                hb_ = hfb[b]
                S.op("act", lambda: act.copy(out=hb_[:, :], in_=hf32[b][:, :]), [hf32[b]], [hb_])
                S.dma("sp", HF_d[T_ * 128:(T_ + 1) * 128, :], hb_[:, :], reads=[hb_], writes=[T_HF])
                for c in range(8):
                    S.op("pe", lambda c=c: pe.transpose(out=ps_r[c // 4][:, (c % 4) * 128:(c % 4 + 1) * 128], in_=hf32[b][:, c * 128:(c + 1) * 128], identity=idf[:, :]), [hf32[b], idf], [ps_r[c // 4]])
                for i in range(2):
                    S.op("act", lambda i=i: act.copy(out=hfT[b][:, i * 4:(i + 1) * 4, :], in_=ps_r[i][:, :].rearrange("p (c t) -> p c t", c=4)), [ps_r[i]], [hfT[b]])
                yield
                for c in range(8):
                    S.op("pe", lambda c=c: pe.matmul(ps_l[:, 0:NE], lhsT=hfT[b][:, c, :], rhs=rt[:, c, :], start=(c == 0), stop=(c == 7)), [hfT[b], rt], [ps_l])
                S.op("dve", lambda: dve.tensor_copy(out=lg[b][:, :], in_=ps_l[:, 0:NE]), [ps_l], [lg[b]])
                yield
                S.op("dve", lambda: dve.reduce_max(out=mx[b][:, :], in_=lg[b][:, :], axis=AX), [lg[b]], [mx[b]])
                S.op("dve", lambda: dve.tensor_scalar(out=mx[b][:, :], in0=mx[b][:, :], scalar1=-1.0, scalar2=None, op0=ALU.mult), [mx[b]], [mx[b]])
                yield
                S.op("act", lambda: act.activation(out=ex[b][:, :], in_=lg[b][:, :], func=AF.Exp, bias=mx[b][:, :], accum_out=sm[b][:, :]), [lg[b], mx[b]], [ex[b], sm[b]])
                yield
                S.op("dve", lambda: dve.reciprocal(out=sm[b][:, :], in_=sm[b][:, :]), [sm[b]], [sm[b]])
                S.op("dve", lambda: dve.tensor_scalar(out=af[b][:, :], in0=ex[b][:, :], scalar1=sm[b][:, 0:1], scalar2=None, op0=ALU.mult), [ex[b], sm[b]], [af[b]])
                yield
                S.op("pe", lambda: pe.transpose(out=ps_a[0:NE, 0:128], in_=af[b][:, :], identity=idf[:, :]), [af[b], idf], [ps_a])
                S.op("act", lambda: act.copy(out=affT[:, T_ * 128:(T_ + 1) * 128], in_=ps_a[0:NE, 0:128]), [ps_a], [affR[T_]])
                yield

            for T0 in range(0, 32, NL):
                live = [tile_prog(T_) for T_ in range(T0, min(T0 + NL, 32))]
                while live:
                    for g in list(live):
                        try:
                            next(g)
                        except StopIteration:
                            live.remove(g)
            S.dma("sp", AFFT_d, affT[:, :], reads=affR, writes=[T_AFFT])


    posT_d = dscr("posT_d", [128, 4, 64])
    gateT_d = dscr("gateT_d", [128, 4, 64])
    HFO_d = dscr("HFO_d", [SEQ // 2, D], BF16)
    T_HFO = T(None, "HFO")

    def stageEFG():
        import os
        with contextlib.ExitStack() as es:
            posT = K.sb(es, [128, 4, 64], F32, "posT")
            gateT = K.sb(es, [128, 4, 64], F32, "gateT")
            iot = K.sb(es, [128, 512], F32, "iot")
            S.dma("sp", iot[:, :], iota512, writes=[iot])
            PB = [K.ps(es, [128, 512], F32, "PB%d" % i) for i in range(8)]
            oi = K.sb(es, [128, 16], I32, "oi")
            S.dma("sp", oi[:, :], own_idx, writes=[oi])
            e4 = contextlib.ExitStack()
            hfj = [K.sb(e4, [128, D], BF16, "hfj%d" % i) for i in range(3)]
            for j in range(16):
                hj = hfj[j % 3]
                S.dma("pool", None, None, reads=[T_HF, oi], writes=[hj], fn=lambda hj=hj, j=j: pool.indirect_dma_start(
                    out=hj[:, :], out_offset=None, in_=HF_d, in_offset=bass.IndirectOffsetOnAxis(ap=oi[:, j:j + 1], axis=0)))
                S.dma("sp", HFO_d[j * 128:(j + 1) * 128, :], hj[:, :], reads=[hj], writes=[T_HFO])
            with contextlib.ExitStack() as e2:
                A = K.sb(e2, [128, 512], F32, "A")
                S.dma("sp", A[:, :], AFFT_d.rearrange("e (s t) -> (e s) t", s=8), reads=[T_AFFT], writes=[A])
                b1 = K.sb(e2, [128, 128], F32, "b1")
                blt = K.sb(e2, [128, 128], F32, "blt")
                ownf = K.sb(e2, [128, 1], F32, "ownf")
                selm = K.sb(e2, [128, 64], F32, "selm")
                for t_, src in ((b1, blk_ones), (blt, blk_lt), (ownf, own_flag), (selm, sel)):
                    S.dma("sp", t_[:, :], src, writes=[t_])
                lo = K.sb(e2, [128, 1], F32, "lo")
                hi = K.sb(e2, [128, 1], F32, "hi")
                mid = K.sb(e2, [128, 1], F32, "mid")
                cnt = K.sb(e2, [128, 1], F32, "cnt")
                cond = K.sb(e2, [128, 1], F32, "cond")
                d1 = K.sb(e2, [128, 1], F32, "d1")
                jk = K.sb(e2, [128, 512], F32, "jk")
                onesT = K.sb(e2, [128, 512], F32, "onesT")
                M_ = K.sb(e2, [128, 512], F32, "M_")
                posi = K.sb(e2, [128, 512], F32, "posi")
                pm = K.sb(e2, [128, 512], F32, "pm")
                offc = K.sb(e2, [128, 1], F32, "offc")
                S.op("pool", lambda: pool.memset(lo[:, :], 0.0), [], [lo])
                S.op("pool", lambda: pool.memset(hi[:, :], 1.0), [], [hi])
                S.op("pool", lambda: pool.memset(onesT[:, :], 1.0), [], [onesT])
                pc = PB[0]
                for it in range(30):
                    S.op("dve", lambda: dve.tensor_tensor(out=mid[:, :], in0=lo[:, :], in1=hi[:, :], op=ALU.add), [lo, hi], [mid])
                    S.op("dve", lambda: dve.tensor_scalar(out=mid[:, :], in0=mid[:, :], scalar1=0.5, scalar2=None, op0=ALU.mult), [mid], [mid])
                    S.op("dve", lambda: dve.tensor_scalar(out=jk[:, :], in0=A[:, :], scalar1=mid[:, 0:1], scalar2=0.0, op0=ALU.is_gt, op1=ALU.add, accum_out=cnt[:, 0:1]), [A, mid], [jk, cnt])
                    S.op("pe", lambda: pe.matmul(pc[:, 0:1], lhsT=b1[:, :], rhs=cnt[:, 0:1], start=True, stop=True), [b1, cnt], [pc])
                    S.op("dve", lambda: dve.tensor_scalar(out=cond[:, :], in0=pc[:, 0:1], scalar1=float(CAP) - 0.5, scalar2=None, op0=ALU.is_gt), [pc], [cond])
                    S.op("dve", lambda: dve.tensor_tensor(out=d1[:, :], in0=mid[:, :], in1=lo[:, :], op=ALU.subtract), [mid, lo], [d1])
                    S.op("dve", lambda: dve.scalar_tensor_tensor(out=lo[:, :], in0=d1[:, :], scalar=cond[:, 0:1], in1=lo[:, :], op0=ALU.mult, op1=ALU.add), [d1, cond, lo], [lo])
                    S.op("dve", lambda: dve.tensor_tensor(out=d1[:, :], in0=hi[:, :], in1=mid[:, :], op=ALU.subtract), [hi, mid], [d1])
                    S.op("dve", lambda: dve.scalar_tensor_tensor(out=hi[:, :], in0=d1[:, :], scalar=cond[:, 0:1], in1=mid[:, :], op0=ALU.mult, op1=ALU.add), [d1, cond, mid], [hi])
                S.op("dve", lambda: dve.tensor_scalar(out=M_[:, :], in0=A[:, :], scalar1=lo[:, 0:1], scalar2=ownf[:, 0:1], op0=ALU.is_gt, op1=ALU.mult), [A, lo, ownf], [M_])
                S.op("dve", lambda: dve.tensor_tensor_scan(out=posi[:, :], data0=onesT[:, :], data1=M_[:, :], initial=0.0, op0=ALU.mult, op1=ALU.add), [onesT, M_], [posi])
                S.op("pe", lambda: pe.matmul(pc[:, 0:1], lhsT=blt[:, :], rhs=posi[:, 511:512], start=True, stop=True), [blt, posi], [pc])
                S.op("dve", lambda: dve.tensor_scalar(out=offc[:, :], in0=pc[:, 0:1], scalar1=-10000.0, scalar2=None, op0=ALU.add), [pc], [offc])
                S.op("dve", lambda: dve.tensor_scalar(out=pm[:, :], in0=posi[:, :], scalar1=offc[:, 0:1], scalar2=None, op0=ALU.add), [posi, offc], [pm])
                S.op("dve", lambda: dve.tensor_tensor(out=pm[:, :], in0=pm[:, :], in1=M_[:, :], op=ALU.mult), [pm, M_], [pm])
                S.op("dve", lambda: dve.tensor_scalar(out=pm[:, :], in0=pm[:, :], scalar1=9999.0, scalar2=None, op0=ALU.add), [pm], [pm])
                for src, dst, pb in ((pm, posT, PB[1]), (A, gateT, PB[2])):
                    for blk in range(4):
                        S.op("pe", lambda src=src, pb=pb, blk=blk: pe.matmul(pb[:, blk * 64:(blk + 1) * 64], lhsT=src[:, blk * 128:(blk + 1) * 128], rhs=selm[:, :], start=True, stop=True), [src, selm], [pb])
                    S.op("act", lambda dst=dst, pb=pb: act.copy(out=dst[:, :, :], in_=pb[:, 0:256].rearrange("p (b c) -> p b c", b=4)), [pb], [dst])
                if debug:
                    S.dma("sp", posT_d, posT[:, :, :], reads=[posT])
                    S.dma("sp", gateT_d, gateT[:, :, :], reads=[gateT])
                S.barrier()
            S.barrier()
            e4.close()
            if upto == "E":
                return
            yacc = K.sb(es, [128, 16, D], F32, "yacc")
            S.op("pool", lambda: pool.memset(yacc[:, :, :], 0.0), [], [yacc])
            e3 = contextlib.ExitStack()
            xsT = K.sb(e3, [128, 8, 512], BF16, "xsT")
            hidT = K.sb(e3, [128, NFC, 512], BF16, "hidT")
            ysb = K.sb(e3, [128, 4, D], BF16, "ysb")
            Ej = [K.sb(e3, [128, 512], BF16, "Ej%d" % i) for i in range(2)]
            Gj = [K.sb(e3, [128, 512], BF16, "Gj%d" % i) for i in range(2)]
            tidt = K.sb(e3, [128, 16, 2], BF16, "tidt")
            S.dma("sp", tidt[:, :, :], tid_in, writes=[tidt])
            xg = [K.sb(e3, [128, 4, D], BF16, "xg%d" % i) for i in range(2)]
            xgT = [[T(None, "xgT") for _ in range(4)] for _ in range(2)]
            idxs = K.sb(e3, [128, 8], F32, "idxs")
            idxf = K.sb(e3, [128, 4], F32, "idxf")
            idxi = [K.sb(e3, [128, 4], I32, "idxi%d" % i) for i in range(2)]
            GTa = K.sb(e3, [128, 16, 4, 128], BF16, "GTa")
            GTt = [T(None, "GTt%d" % i) for i in range(16)]
            sgt = [K.sb(e3, [128, 512], BF16, "sgt%d" % i) for i in range(2)]
            stg = [K.sb(e3, [128, 2048], F32, "stg%d" % i) for i in range(4)]
            nst = [0]

            def stage_in(src_ap, ncols):
                st = stg[nst[0] % 4]
                q = os.environ.get("WQ", "sp").split(",")
                q = q[nst[0] % len(q)]
                nst[0] += 1
                S.dma(q, st[:, 0:ncols], src_ap, writes=[st])
                return st
            wgb = [K.sb(e3, [128, 8, 256], BF16, "wgb%d" % i) for i in range(2)]
            wub = [K.sb(e3, [128, 8, 256], BF16, "wub%d" % i) for i in range(2)]
            wdb = K.sb(e3, [128, NFC, 256], BF16, "wdb")
            wdbT = [T(None, "wdbT%d" % i) for i in range(3)]
            NEX = int(os.environ.get("NEX", str(NE)))
            ng = 0
            PH = int(os.environ.get("PH", "15"))
            def route(e):
                xb = e % 2
                for j in range(16):
                    osg, blk = j // 4, j % 4
                    E_ = Ej[j % 2]
                    S.op("dve", lambda E_=E_, blk=blk, col=e * 4 + osg: dve.tensor_scalar(out=E_[:, :], in0=iot[:, :], scalar1=posT[:, blk, col:col + 1], scalar2=None, op0=ALU.is_equal), [iot, posT], [E_])
                    for sc in range(4):
                        S.op("pe", lambda sc=sc, E_=E_, j=j: pe.matmul(PB[sc][:, 0:2], lhsT=E_[:, sc * 128:(sc + 1) * 128], rhs=tidt[:, j, :], start=(j == 0), stop=(j == 15)), [E_, tidt], [PB[sc]])
                for sc in range(4):
                    S.op("dve", lambda sc=sc: dve.tensor_copy(out=idxs[:, sc * 2:(sc + 1) * 2], in_=PB[sc][:, 0:2]), [PB[sc]], [idxs])
                iv = idxs[:, :].rearrange("p (s two) -> p s two", two=2)
                S.op("dve", lambda: dve.scalar_tensor_tensor(out=idxf[:, :], in0=iv[:, :, 0], scalar=64.0, in1=iv[:, :, 1], op0=ALU.mult, op1=ALU.add), [idxs], [idxf])
                S.op("dve", lambda: dve.tensor_copy(out=idxi[xb][:, :], in_=idxf[:, :]), [idxf], [idxi[xb]])
                for sc in range(4):
                    S.dma("pool", None, None, reads=[T_HFO, idxi[xb]], writes=[xgT[xb][sc]], fn=lambda sc=sc: pool.indirect_dma_start(
                        out=xg[xb][:, sc, :], out_offset=None, in_=HFO_d, in_offset=bass.IndirectOffsetOnAxis(ap=idxi[xb][:, sc:sc + 1], axis=0)))

            if PH & 1:
                route(0)
            for e in range(NEX):
                xb = e % 2
                for c in (range(8) if PH & 1 else []):
                    bb = PB[c // 2][:, :].bitcast(BF16)
                    for sc in range(4):
                        S.op("pe", lambda c=c, sc=sc, bb=bb: pe.transpose(out=bb[:, (c % 2) * 512 + sc * 128:(c % 2) * 512 + (sc + 1) * 128], in_=xg[xb][:, sc, c * 128:(c + 1) * 128], identity=idb[:, :]), [xgT[xb][sc], idb], [PB[c // 2]])
                for c in (range(8) if PH & 1 else []):
                    bb = PB[c // 2][:, :].bitcast(BF16)
                    if (c // 2) % 2 == 0:
                        S.op("act", lambda c=c, bb=bb: act.copy(out=xsT[:, c, :], in_=bb[:, (c % 2) * 512:(c % 2 + 1) * 512]), [PB[c // 2]], [xsT])
                    else:
                        S.op("dve", lambda c=c, bb=bb: dve.tensor_copy(out=xsT[:, c, :], in_=bb[:, (c % 2) * 512:(c % 2 + 1) * 512]), [PB[c // 2]], [xsT])

                def prep(j):
                    osg, blk = j // 4, j % 4
                    col = e * 4 + osg
                    G_ = Gj[j % 2]
                    S.op("dve", lambda: dve.tensor_scalar(out=G_[:, :], in0=iot[:, :], scalar1=posT[:, blk, col:col + 1], scalar2=gateT[:, blk, col:col + 1], op0=ALU.is_equal, op1=ALU.mult), [iot, posT, gateT], [G_])
                    pt = PB[6 + j % 2]
                    ptb = pt[:, :].bitcast(BF16)
                    for sc in range(4):
                        S.op("pe", lambda sc=sc: pe.transpose(out=ptb[:, sc * 128:(sc + 1) * 128], in_=G_[:, sc * 128:(sc + 1) * 128], identity=idb[:, :]), [G_, idb], [pt])
                    S.op("act", lambda: act.copy(out=GTa[:, j, :, :], in_=ptb[:, 0:512].rearrange("p (s t) -> p s t", s=4)), [pt], [GTt[j]])

                jn = 0

                def load_gu(e_, g_):
                    gb_ = (e_ * 11 + g_) % 2
                    sg_st = stage_in(w_gate[e_, g_], 2048)
                    su_st = stage_in(w_up[e_, g_], 2048)
                    S.op("act", lambda: act.copy(out=wgb[gb_][:, :, :].rearrange("p c f -> p (c f)"), in_=sg_st[:, :]), [sg_st], [wgb[gb_]])
                    S.op("dve", lambda: dve.tensor_copy(out=wub[gb_][:, :, :].rearrange("p c f -> p (c f)"), in_=su_st[:, :]), [su_st], [wub[gb_]])

                if e == 0:
                    load_gu(0, 0)
                for g in (range(11) if PH & 2 else []):
                    gb = (e * 11 + g) % 2
                    if g + 1 < 11:
                        load_gu(e, g + 1)
                    for fl in range(2):
                        fc = g * 2 + fl
                        pg, pu = PB[(fc % 2) * 2], PB[(fc % 2) * 2 + 1]
                        for c in range(8):
                            S.op("pe", lambda c=c, pg=pg, gb=gb, fl=fl: pe.matmul(pg[:, :], lhsT=wgb[gb][:, c, fl * 128:(fl + 1) * 128], rhs=xsT[:, c, :], start=(c == 0), stop=(c == 7)), [wgb[gb], xsT], [pg])
                        for c in range(8):
                            S.op("pe", lambda c=c, pu=pu, gb=gb, fl=fl: pe.matmul(pu[:, :], lhsT=wub[gb][:, c, fl * 128:(fl + 1) * 128], rhs=xsT[:, c, :], start=(c == 0), stop=(c == 7)), [wub[gb], xsT], [pu])
                        sg_ = sgt[fc % 2]
                        S.op("act", lambda pg=pg, sg_=sg_: act.activation(out=sg_[:, :], in_=pg[:, :], func=AF.Silu), [pg], [sg_])
                        S.op("dve", lambda pu=pu, sg_=sg_, fc=fc: dve.tensor_tensor(out=hidT[:, fc, :], in0=sg_[:, :], in1=pu[:, :], op=ALU.mult), [sg_, pu], [hidT])
                    for _ in range(2 if g < 5 else 1):
                        if jn < 16:
                            prep(jn)
                            jn += 1
                while jn < 16 and (PH & 8):
                    prep(jn)
                    jn += 1
                if e + 1 < NEX and (PH & 1):
                    route(e + 1)
                pieces = ((0, 8), (8, 16), (16, NFC))
                def dma_d(cb_):
                    return [stage_in(w_down[e, cb_, :, f0 * 256:f1 * 256], (f1 - f0) * 256) for (f0, f1) in pieces]

                def cast_d(k, st):
                    f0, f1 = pieces[k]
                    S.op("act", lambda: act.copy(out=wdb[:, f0:f1, :].rearrange("p c n -> p (c n)"), in_=st[:, 0:(f1 - f0) * 256]), [st], [wdbT[k]])

                sts = dma_d(0)
                for k in range(3):
                    cast_d(k, sts[k])
                for cb in (range(4) if PH & 4 else []):
                    nxt = dma_d(cb + 1) if cb + 1 < 4 else None
                    for k, (f0, f1) in enumerate(pieces):
                        for fc in range(f0, f1):
                            for sc in range(4):
                                S.op("pe", lambda fc=fc, sc=sc: pe.matmul(PB[4 + sc][:, 0:256], lhsT=hidT[:, fc, sc * 128:(sc + 1) * 128], rhs=wdb[:, fc, :], start=(fc == 0), stop=(fc == NFC - 1)), [hidT, wdbT[k]], [PB[4 + sc]])
                        if nxt is not None:
                            cast_d(k, nxt[k])
                    for sc in range(4):
                        if sc % 2 == 0:
                            S.op("act", lambda sc=sc, cb=cb: act.copy(out=ysb[:, sc, cb * 256:(cb + 1) * 256], in_=PB[4 + sc][:, 0:256]), [PB[4 + sc]], [ysb])
                        else:
                            S.op("dve", lambda sc=sc, cb=cb: dve.tensor_copy(out=ysb[:, sc, cb * 256:(cb + 1) * 256], in_=PB[4 + sc][:, 0:256]), [PB[4 + sc]], [ysb])
                if e + 1 < NEX:
                    load_gu(e + 1, 0)
                for j in (range(16) if PH & 8 else []):
                    for hlf in range(2):
                        pyc = PB[(j % 2) * 2 + hlf]
                        for sc in range(4):
                            S.op("pe", lambda sc=sc, pyc=pyc, hlf=hlf, j=j: pe.matmul(pyc[:, :], lhsT=GTa[:, j, sc, :], rhs=ysb[:, sc, hlf * 512:(hlf + 1) * 512], start=(sc == 0), stop=(sc == 3)), [GTt[j], ysb], [pyc])
                        S.op("dve", lambda pyc=pyc, j=j, hlf=hlf: dve.tensor_tensor(out=yacc[:, j, hlf * 512:(hlf + 1) * 512], in0=yacc[:, j, hlf * 512:(hlf + 1) * 512], in1=pyc[:, :], op=ALU.add), [yacc, pyc], [yacc])
            S.barrier()
            e3.close()
            gF = K.sb(es, [128, D], F32, "gF")
            S.dma("sp", gF[:, :], mods_d[:, 7, :], reads=[T_mods], writes=[gF])
            xno = [K.sb(es, [128, D], F32, "xno%d" % i) for i in range(2)]
            ob = [K.sb(es, [128, D], F32, "ob%d" % i) for i in range(2)]
            junk = K.sb(es, [128, D], BF16, "junkg")
            ss = K.sb(es, [128, 1], F32, "ssg")
            rstd = K.sb(es, [128, 1], F32, "rstdg")
            for j in range(16):
                b = j % 2
                S.dma("pool", None, None, reads=[T_XN, oi], writes=[xno[b]], fn=lambda b=b, j=j: pool.indirect_dma_start(
                    out=xno[b][:, :], out_offset=None, in_=XN_d, in_offset=bass.IndirectOffsetOnAxis(ap=oi[:, j:j + 1], axis=0)))
                S.op("act", lambda j=j: act.activation(out=junk[:, :], in_=yacc[:, j, :], func=AF.Square, accum_out=ss[:, :]), [yacc], [junk, ss])
                S.op("act", lambda: act.activation(out=rstd[:, :], in_=ss[:, :], func=AF.Sqrt, scale=1.0 / D, bias=epsb[:, :]), [ss, epsb], [rstd])
                S.op("dve", lambda: dve.reciprocal(out=rstd[:, :], in_=rstd[:, :]), [rstd], [rstd])
                S.op("dve", lambda j=j, b=b: dve.scalar_tensor_tensor(out=ob[b][:, :], in0=yacc[:, j, :], scalar=rstd[:, 0:1], in1=gF[:, :], op0=ALU.mult, op1=ALU.mult), [yacc, rstd, gF], [ob[b]])
                S.op("pool", lambda b=b: pool.tensor_tensor(out=ob[b][:, :], in0=ob[b][:, :], in1=xno[b][:, :], op=ALU.add), [ob[b], xno[b]], [ob[b]])
                S.dma("sp", out[j * 128:(j + 1) * 128, :], ob[b][:, :], reads=[ob[b]])

    stages = [("0", stage0), ("A", stageA), ("B", stageB), ("C", stageC), ("D", stageD), ("E", stageEFG)]
    if upto in ("F", "G", "all"):
        stages[-1] = (upto, stageEFG)
    for name, fn in stages:
        fn()
        S.barrier()
        if upto == name:
            break
    S.finish()
    glob.close()
    return nc


def prep_inputs(inputs, cores=range(8), with_experts=True):
    f = lambda k: np.ascontiguousarray(np.asarray(inputs[k], dtype=np.float32))
    x, c, ctx, c_ctx = f("x"), f("c"), f("ctx"), f("c_ctx")
    consts = _consts()
    cos, sin = _rope_tables()
    rpb = f("na_rpb")[0]
    nab = np.ascontiguousarray(_na_bias_tables(rpb).reshape(5, 8, 128, 640))
    a_up = f("gla_a_up")[0]
    a_bias = f("gla_a_bias")[0]
    aup_bd = np.zeros((32, 512), np.float32)
    aup_bd[0:16, 0:256] = a_up[0]
    aup_bd[16:32, 256:512] = a_up[1]
    abias = np.ascontiguousarray(a_bias.reshape(1, 512))
    norms = np.ascontiguousarray(np.stack([f("norm_mix_pre")[0], f("norm_mix_post")[0], f("norm_ffn_pre")[0], f("norm_ffn_post")[0]]))
    shared = dict(
        w_mod=f("w_mod")[0], b_mod=f("b_mod"), norms=norms, gla_norm=f("gla_norm"), w_in=f("w_in")[0],
        aup_bd=aup_bd, abias=abias, rope_cos=cos, rope_sin=sin, na_bias=nab, w_out=f("w_out")[0],
        router=f("router")[0], **consts)
    if with_experts:
        tile_gu = lambda w: np.ascontiguousarray(w.reshape(NE, 8, 128, 11, 256).transpose(0, 3, 2, 1, 4)).reshape(NE, 11, 128, 8 * 256)
        shared.update(w_gate=tile_gu(f("w_gate")[0]), w_up=tile_gu(f("w_up")[0]),
                      w_down=np.ascontiguousarray(f("w_down")[0].reshape(NE, NFC, 128, 4, 256).transpose(0, 3, 2, 1, 4)).reshape(NE, 4, 128, NFC * 256))
    maps = []
    pp = np.arange(128)
    for core in cores:
        s, p = core // 2, core % 2
        m = dict(shared)
        m["xc"] = np.ascontiguousarray(np.concatenate([ctx[s], x[s]], axis=0))
        cv = np.zeros((128, 16), np.float32)
        cv[:, 0:8] = c[s].reshape(8, 128).T
        cv[:, 8:16] = c_ctx.reshape(8, 128).T
        m["cvec"] = cv
        m["own_idx"] = np.ascontiguousarray((p * 2048 + np.arange(16)[None, :] * 128 + pp[:, None]).astype(np.int32))
        seg = pp % 8
        m["own_flag"] = ((seg // 4) == p).astype(np.float32).reshape(128, 1)
        selm = np.zeros((128, 64), np.float32)
        for e in range(16):
            for os_ in range(4):
                selm[e * 8 + p * 4 + os_, e * 4 + os_] = 1.0
        m["sel"] = selm
        maps.append(m)
    return maps


_PROGRAM = None


def kernel(**inputs):
    global _PROGRAM
    if _PROGRAM is None:
        _PROGRAM = build_program()
    maps = prep_inputs(inputs)
    res = run_bass_kernel_spmd(_PROGRAM, maps, core_ids=list(range(8)))
    out = np.zeros((4, SEQ, D), np.float32)
    for core in range(8):
        s, p = core // 2, core % 2
        out[s, p * 2048:(p + 1) * 2048] = res.results[core]["out"]
    return out
```

```python
import contextlib
import numpy as np
import ml_dtypes
import concourse.bass as bass
import concourse.mybir as mybir
from concourse.bass_utils import run_bass_kernel_spmd

F32 = mybir.dt.float32
BF16 = mybir.dt.bfloat16
I32 = mybir.dt.int32
AF = mybir.ActivationFunctionType
ALU = mybir.AluOpType

D = 1024
SEQ = 4096
CTX = 256
NTOK = SEQ + CTX
NT = NTOK // 128
INC = 3104
NE = 16
DE = 2816
NFC = DE // 128
CAP = 512
EPS = 1e-6
NEG = -30000.0


class T:
    __slots__ = ("ap", "w", "r", "name")

    def __init__(self, ap, name=""):
        self.ap = ap
        self.w = None
        self.r = {}
        self.name = name

    def __getitem__(self, idx):
        return self.ap[idx]


class Sync:
    def __init__(self, nc, n_sp=20, n_pool=8, n_act=6):
        self.nc = nc
        self.E = {"pe": nc.tensor, "act": nc.scalar, "dve": nc.vector, "pool": nc.gpsimd, "sp": nc.sync}
        self.sem = {}
        self.cnt = {}
        for k in ("pe", "act", "dve", "pool"):
            self.sem[k] = nc.alloc_semaphore("s_" + k)
            self.cnt[k] = 0
        self.seen = {k: {} for k in self.E}
        self.dq = {}
        for q, n in (("sp", n_sp), ("pool", n_pool), ("act", n_act)):
            lst = []
            for i in range(n):
                key = "d_%s%d" % (q, i)
                self.sem[key] = nc.alloc_semaphore(key)
                self.cnt[key] = 0
                lst.append(key)
            self.dq[q] = [lst, 0]

    def _wait(self, ek, key, val):
        if val <= 0:
            return
        if self.seen[ek].get(key, 0) >= val:
            return
        self.E[ek].wait_ge(self.sem[key], val)
        self.seen[ek][key] = val

    def _deps(self, ek, reads, writes):
        need = {}
        for t in reads:
            if t.w is not None:
                k, v = t.w
                need[k] = max(need.get(k, 0), v)
        for t in writes:
            if t.w is not None:
                k, v = t.w
                if not (k == "pe" and ek == "pe"):
                    need[k] = max(need.get(k, 0), v)
            for k, v in t.r.items():
                if not (k == "pe" and ek == "pe"):
                    need[k] = max(need.get(k, 0), v)
        for k, v in need.items():
            self._wait(ek, k, v)

    def op(self, ek, fn, reads=(), writes=()):
        self._deps(ek, reads, writes)
        ins = fn()
        self.cnt[ek] += 1
        ins.then_inc(self.sem[ek], 1)
        me = (ek, self.cnt[ek])
        for t in reads:
            t.r[ek] = self.cnt[ek]
        for t in writes:
            t.w = me
            t.r = {}
        return ins

    def dma(self, q, out, in_, reads=(), writes=(), fn=None, **kw):
        lst, i = self.dq[q]
        key = lst[i % len(lst)]
        self.dq[q][1] = i + 1
        self._wait(q, key, self.cnt[key])
        self._deps(q, reads, writes)
        if fn is None:
            ins = self.E[q].dma_start(out=out, in_=in_, **kw)
        else:
            ins = fn()
        self.cnt[key] += 16
        ins.then_inc(self.sem[key], 16)
        for t in reads:
            t.r[key] = self.cnt[key]
        for t in writes:
            t.w = (key, self.cnt[key])
            t.r = {}
        return ins

    def barrier(self):
        for ek in self.E:
            for k, v in self.cnt.items():
                if not (k == ek == "pe"):
                    self._wait(ek, k, v)

    def finish(self):
        for k, v in self.cnt.items():
            self._wait("sp", k, v)


class Ctx:
    def __init__(self, nc):
        self.nc = nc
        self.S = Sync(nc)
        self.uid = 0

    def sb(self, es, shape, dt, name):
        self.uid += 1
        return T(es.enter_context(self.nc.sbuf_tensor("%s_%d" % (name, self.uid), list(shape), dt)), name)

    def ps(self, es, shape, dt, name):
        self.uid += 1
        return T(es.enter_context(self.nc.psum_tensor("%s_%d" % (name, self.uid), list(shape), dt)), name)


def _env(key, default=None):
    import os
    return os.environ.get(key, default) if os.environ.get("KERNEL_DEV") == "1" else default


def _rope_tables():
    half = 16
    freqs = (np.float32(10000.0) ** (-np.arange(half, dtype=np.float32) / np.float32(half))).astype(np.float32)
    t = np.arange(SEQ)
    pr = (t // 64).astype(np.float32)[:, None] * freqs
    pc = (t % 64).astype(np.float32)[:, None] * freqs
    ang = np.concatenate([pr, pc], axis=1).astype(np.float32)
    return np.cos(ang).astype(np.float32), np.sin(ang).astype(np.float32)


def _na_key_tile0(T_):
    return min(max(T_ - 2, 0), 27)


def _na_bias_tables(rpb):
    out = np.full((5, 8, 128, 5, 128), NEG, np.float32)
    for si, T_ in enumerate((0, 1, 2, 30, 31)):
        kt0 = _na_key_tile0(T_)
        for qi in range(128):
            r = 2 * T_ + qi // 64
            qc = qi % 64
            rs = min(max(r - 4, 0), 56)
            cs = min(max(qc - 8, 0), 48)
            for kr in range(rs, rs + 8):
                kt = kr // 2 - kt0
                assert 0 <= kt < 5
                dr = kr - r + 7
                kc = np.arange(cs, cs + 16)
                dc = np.clip(kc - qc + 15, 0, 30)
                j = (kr % 2) * 64 + kc
                out[si, :, j, kt, qi] = rpb[:, dr, dc].T
    return out


def _consts():
    c = {}
    c["ident_f"] = np.eye(128, dtype=np.float32)
    c["ident_b"] = np.eye(128, dtype=np.float32).astype(ml_dtypes.bfloat16)
    tt = np.arange(128)
    c["tri_f"] = np.where(tt[:, None] <= tt[None, :], -1.0 / 16.0, 0.0).astype(np.float32)
    c["tri_b"] = np.where(tt[:, None] >= tt[None, :], -1.0 / 16.0, 0.0).astype(np.float32)
    c["mask_f"] = (tt[:, None] <= tt[None, :]).astype(np.float32)
    c["mask_b"] = (tt[:, None] >= tt[None, :]).astype(np.float32)
    c["ones_col"] = np.full((128, 1), -1.0 / 16.0, np.float32)
    c["ones_row"] = np.ones((1, 128), np.float32)
    g = tt // 8
    c["blk_ones"] = (g[:, None] == g[None, :]).astype(np.float32)
    c["blk_lt"] = ((g[:, None] == g[None, :]) & (tt[:, None] < tt[None, :])).astype(np.float32)
    c["iota512"] = np.broadcast_to(np.arange(512, dtype=np.float32)[None, :], (128, 512)).copy()
    own = np.arange(16)[None, :] * 128 + np.arange(128)[:, None]
    c["tid"] = np.stack([own // 64, own % 64], axis=-1).astype(np.float32).astype(ml_dtypes.bfloat16)
    return c


def build_program(upto="all", debug=False):
    nc = bass.Bass("TRN2", target_bir_lowering=False)
    K = Ctx(nc)
    S = K.S
    pe, act, dve, pool = nc.tensor, nc.scalar, nc.vector, nc.gpsimd

    def din(name, shape, dt=F32):
        return nc.dram_tensor(name, list(shape), dt, kind="ExternalInput").ap()

    import os as _os
    _ext = set(_env("EXTSET", "").split(","))

    def dscr(name, shape, dt=F32):
        return nc.dram_tensor(name, list(shape), dt, kind=("ExternalOutput" if (debug or name in _ext) else "Internal")).ap()

    xc = din("xc", [NTOK, D])
    cvec = din("cvec", [128, 16])
    w_mod = din("w_mod", [D, 6 * D])
    b_mod = din("b_mod", [1, 6 * D])
    norms = din("norms", [4, D])
    gla_norm = din("gla_norm", [1, 128])
    w_in = din("w_in", [D, INC])
    aup_bd = din("aup_bd", [32, 512])
    abias = din("abias", [1, 512])
    rope_cos = din("rope_cos", [SEQ, 32])
    rope_sin = din("rope_sin", [SEQ, 32])
    na_bias = din("na_bias", [5, 8, 128, 640])
    w_out = din("w_out", [D, D])
    router = din("router", [D, NE])
    if upto in ("F", "G", "all"):
        w_gate = din("w_gate", [NE, 11, 128, 8 * 256])
        w_up = din("w_up", [NE, 11, 128, 8 * 256])
        w_down = din("w_down", [NE, 4, 128, NFC * 256])
    ident_f = din("ident_f", [128, 128])
    ident_b = din("ident_b", [128, 128], BF16)
    tri_f = din("tri_f", [128, 128])
    tri_b = din("tri_b", [128, 128])
    mask_f = din("mask_f", [128, 128])
    mask_b = din("mask_b", [128, 128])
    ones_col = din("ones_col", [128, 1])
    ones_row = din("ones_row", [1, 128])
    blk_ones = din("blk_ones", [128, 128])
    blk_lt = din("blk_lt", [128, 128])
    iota512 = din("iota512", [128, 512])
    tid_in = din("tid", [128, 16, 2], BF16)
    own_idx = din("own_idx", [128, 16], I32)
    own_flag = din("own_flag", [128, 1])
    sel = din("sel", [128, 64])
    out = nc.dram_tensor("out", [SEQ // 2, D], F32, kind="ExternalOutput").ap()

    mods_d = dscr("mods_d", [128, 8, D])
    KT_d = dscr("KT_d", [512, NTOK], BF16)
    QT_d = dscr("QT_d", [512, SEQ], BF16)
    V_d = dscr("V_d", [NTOK, 512], BF16)
    GV_d = dscr("GV_d", [NTOK, 512], BF16)
    GKQ_d = dscr("GKQ_d", [NTOK, 544])
    GG_d = dscr("GG_d", [SEQ, 512])
    OF_d = dscr("OF_d", [SEQ, 512])
    MIX_d = dscr("MIX_d", [SEQ, D], BF16)
    XN_d = dscr("XN_d", [SEQ, D])
    HF_d = dscr("HF_d", [SEQ, D], BF16)
    AFFT_d = dscr("AFFT_d", [NE, SEQ])

    glob = contextlib.ExitStack()
    idf = K.sb(glob, [128, 128], F32, "idf")
    idb = K.sb(glob, [128, 128], BF16, "idb")
    onesr = K.sb(glob, [1, 128], F32, "onesr")
    S.dma("sp", idf[:, :], ident_f, writes=[idf])
    S.dma("sp", idb[:, :], ident_b, writes=[idb])
    S.dma("sp", onesr[:, :], ones_row, writes=[onesr])

    def stage0():
        with contextlib.ExitStack() as es:
            cv = K.sb(es, [128, 16], F32, "cv")
            csil = K.sb(es, [128, 16], F32, "csil")
            crep = K.sb(es, [128, 16, 128], F32, "crep")
            bm = K.sb(es, [1, 6 * D], F32, "bm")
            modA = K.sb(es, [128, 6 * D], F32, "modA")
            modC = K.sb(es, [128, 2 * D], F32, "modC")
            nbc = K.sb(es, [128, 4, D], F32, "nbc")
            wm = [K.sb(es, [128, 8, 512], F32, "wm%d" % i) for i in range(2)]
            mods = K.sb(es, [128, 8, D], F32, "mods")
            psA = [K.ps(es, [128, 512], F32, "psA%d" % i) for i in range(2)]
            psC = [K.ps(es, [128, 512], F32, "psC%d" % i) for i in range(2)]
            S.dma("sp", cv[:, :], cvec, writes=[cv])
            S.dma("sp", bm[:, :], b_mod, writes=[bm])
            S.dma("sp", nbc[:, :, :], norms.partition_broadcast(128), writes=[nbc])
            S.op("act", lambda: act.activation(out=csil[:, :], in_=cv[:, :], func=AF.Silu), [cv], [csil])
            S.op("dve", lambda: dve.tensor_copy(out=crep[:, :, :], in_=csil[:, :].unsqueeze(2).broadcast_to([128, 16, 128])), [csil], [crep])
            wmv = w_mod.rearrange("(c p) n -> p c n", p=128)
            for nb in range(12):
                w = wm[nb % 2]
                S.dma("sp", w[:, :, :], wmv[:, :, nb * 512:(nb + 1) * 512], writes=[w])
                for which in range(2 if nb < 4 else 1):
                    ps = (psA, psC)[which][nb % 2]
                    for c in range(8):
                        S.op("pe", lambda c=c, ps=ps, w=w, which=which: pe.matmul(ps[:, :], lhsT=crep[:, which * 8 + c, :], rhs=w[:, c, :], start=(c == 0), stop=False), [crep, w], [ps])
                    S.op("pe", lambda ps=ps, nb=nb: pe.matmul(ps[:, :], lhsT=onesr[:, :], rhs=bm[:, nb * 512:(nb + 1) * 512], start=False, stop=True), [onesr, bm], [ps])
                    dst = (modA, modC)[which]
                    if which == 0:
                        S.op("act", lambda ps=ps, dst=dst, nb=nb: act.copy(out=dst[:, nb * 512:(nb + 1) * 512], in_=ps[:, :]), [ps], [dst])
                    else:
                        S.op("dve", lambda ps=ps, dst=dst, nb=nb: dve.tensor_copy(out=dst[:, nb * 512:(nb + 1) * 512], in_=ps[:, :]), [ps], [dst])
            import os
            if _env("STOP0") == "1":
                S.dma("sp", mods_d[:, 0:6, :], modA[:, :].rearrange("p (a b) -> p a b", a=6), reads=[modA], writes=[T_mods])
                return
            sl = lambda t, i: t[:, i * D:(i + 1) * D]
            S.op("dve", lambda: dve.scalar_tensor_tensor(out=mods[:, 0, :], in0=sl(modA, 1), scalar=1.0, in1=nbc[:, 0, :], op0=ALU.add, op1=ALU.mult), [modA, nbc], [mods])
            S.op("dve", lambda: dve.tensor_copy(out=mods[:, 1, :], in_=sl(modA, 0)), [modA], [mods])
            S.op("dve", lambda: dve.scalar_tensor_tensor(out=mods[:, 2, :], in0=sl(modC, 1), scalar=1.0, in1=nbc[:, 0, :], op0=ALU.add, op1=ALU.mult), [modC, nbc], [mods])
            S.op("dve", lambda: dve.tensor_copy(out=mods[:, 3, :], in_=sl(modC, 0)), [modC], [mods])
            S.op("dve", lambda: dve.tensor_tensor(out=mods[:, 4, :], in0=sl(modA, 2), in1=nbc[:, 1, :], op=ALU.mult), [modA, nbc], [mods])
            S.op("dve", lambda: dve.scalar_tensor_tensor(out=mods[:, 5, :], in0=sl(modA, 4), scalar=1.0, in1=nbc[:, 2, :], op0=ALU.add, op1=ALU.mult), [modA, nbc], [mods])
            S.op("dve", lambda: dve.tensor_copy(out=mods[:, 6, :], in_=sl(modA, 3)), [modA], [mods])
            S.op("dve", lambda: dve.tensor_tensor(out=mods[:, 7, :], in0=sl(modA, 5), in1=nbc[:, 3, :], op=ALU.mult), [modA, nbc], [mods])
            S.dma("sp", mods_d, mods[:, :, :], reads=[mods], writes=[T_mods])

    T_mods = T(None, "mods_d")
    T_KT, T_QT, T_V, T_GV, T_GKQ, T_GG = (T(None, n) for n in ("KT", "QT", "V", "GV", "GKQ", "GG"))

    def rms_rstd(es_tiles, src, src_t, ss, rstd, junk):
        S.op("act", lambda: act.activation(out=junk[:, :], in_=src, func=AF.Square, accum_out=ss[:, :]), [src_t], [junk, ss])
        S.op("act", lambda: act.activation(out=rstd[:, :], in_=ss[:, :], func=AF.Sqrt, scale=1.0 / D, bias=epsb[:, :]), [ss, epsb], [rstd])
        S.op("dve", lambda: dve.reciprocal(out=rstd[:, :], in_=rstd[:, :]), [rstd], [rstd])

    epsb = K.sb(glob, [128, 1], F32, "epsb")
    S.op("pool", lambda: pool.memset(epsb[:, :], EPS), [], [epsb])

    def stageA():
        with contextlib.ExitStack() as es:
            win = K.sb(es, [128, 8, INC], BF16, "win")
            wst = [K.sb(es, [128, 8, 512], F32, "wst%d" % i) for i in range(2)]
            gs = K.sb(es, [128, 4, D], F32, "gs")
            S.dma("sp", gs[:, :, :], mods_d[:, 0:4, :], reads=[T_mods], writes=[gs])
            wiv = w_in.rearrange("(c p) n -> p c n", p=128)
            for b in range(7):
                c0 = b * 512
                w_ = min(512, INC - c0)
                st = wst[b % 2]
                S.dma("sp", st[:, :, 0:w_], wiv[:, :, c0:c0 + w_], writes=[st])
                if b % 2 == 0:
                    S.op("act", lambda st=st, c0=c0, w_=w_: act.copy(out=win[:, :, c0:c0 + w_], in_=st[:, :, 0:w_]), [st], [win])
                else:
                    S.op("dve", lambda st=st, c0=c0, w_=w_: dve.tensor_copy(out=win[:, :, c0:c0 + w_], in_=st[:, :, 0:w_]), [st], [win])
            import os
            STOPA = int(_env("STOPA", "99"))
            if STOPA <= 1:
                return
            xt = [K.sb(es, [128, D], F32, "xt%d" % i) for i in range(2)]
            junk = K.sb(es, [128, D], BF16, "junk")
            ss = [K.sb(es, [128, 1], F32, "ss%d" % i) for i in range(2)]
            rstd = [K.sb(es, [128, 1], F32, "rstd%d" % i) for i in range(2)]
            h1 = [K.sb(es, [128, D], F32, "h1_%d" % i) for i in range(2)]
            hb = [K.sb(es, [128, D], BF16, "hb%d" % i) for i in range(2)]
            hT = [K.sb(es, [128, 8, 512], BF16, "hT%d" % i) for i in range(2)]
            ktq = [K.sb(es, [128, 512], BF16, "ktq%d" % i) for i in range(2)]
            vo = [K.sb(es, [128, 512], BF16, "vo%d" % i) for i in range(2)]
            gvo = [K.sb(es, [128, 512], BF16, "gvo%d" % i) for i in range(2)]
            gkq = [K.sb(es, [128, 544], F32, "gkq%d" % i) for i in range(2)]
            ggo = [K.sb(es, [128, 512], F32, "ggo%d" % i) for i in range(2)]
            pst = [K.ps(es, [128, 8 * 128], BF16, "pst%d" % i) for i in range(2)]
            psp = [K.ps(es, [128, 512], F32, "psp%d" % i) for i in range(4)]
            npp = [0]

            def nextps():
                npp[0] += 1
                return psp[npp[0] % 4]

            ev = [0]

            def evac(dst_t, dst_ap, ps, ps_ap):
                if psp.index(ps) % 2 == 0:
                    S.op("act", lambda: act.copy(out=dst_ap, in_=ps_ap), [ps], [dst_t])
                else:
                    S.op("dve", lambda: dve.tensor_copy(out=dst_ap, in_=ps_ap), [ps], [dst_t])

            groups = [(0, 2)] + [(2 + 4 * g, 4) for g in range(8)]
            hb4 = hb + [K.sb(es, [128, D], BF16, "hbx%d" % i) for i in range(2)]
            tcnt = [0]

            def modulate(gi):
                t0, ntl = groups[gi]
                for k in range(ntl):
                    ti = t0 + k
                    b2 = tcnt[0] % 2
                    tcnt[0] += 1
                    x_ = xt[b2]
                    S.dma("sp", x_[:, :], xc[ti * 128:(ti + 1) * 128, :], writes=[x_])
                    rms_rstd(None, x_[:, :], x_, ss[b2], rstd[b2], junk)
                    go = 2 if gi == 0 else 0
                    h_ = h1[b2]
                    S.op("dve", lambda: dve.scalar_tensor_tensor(out=h_[:, :], in0=x_[:, :], scalar=rstd[b2][:, 0:1], in1=gs[:, go, :], op0=ALU.mult, op1=ALU.mult), [x_, rstd[b2], gs], [h_])
                    S.op("pool", lambda: pool.tensor_tensor(out=hb4[k][:, :], in0=h_[:, :], in1=gs[:, go + 1, :], op=ALU.add), [h_, gs], [hb4[k]])

            def transposes(gi):
                t0, ntl = groups[gi]
                hTg_ = hT[gi % 2]
                for k in range(ntl):
                    pt = pst[k % 2]
                    for c in range(8):
                        S.op("pe", lambda c=c: pe.transpose(out=pt[:, c * 128:(c + 1) * 128], in_=hb4[k][:, c * 128:(c + 1) * 128], identity=idb[:, :]), [hb4[k], idb], [pt])
                    S.op("act", lambda: act.copy(out=hTg_[:, :, k * 128:(k + 1) * 128], in_=pt[:, :].rearrange("p (c t) -> p c t", c=8)), [pt], [hTg_])

            modulate(0)
            transposes(0)
            for gi, (t0, ntl) in enumerate(groups):
                is_ctx = gi == 0
                hTg = hT[gi % 2]
                if gi + 1 < len(groups):
                    modulate(gi + 1)
                ntok = ntl * 128
                tok0 = t0 * 128
                for which, c0 in ((0, 0), (1, 1824)):
                    if which == 1 and is_ctx:
                        continue
                    for fb in range(4):
                        ps = nextps()
                        for c in range(8):
                            S.op("pe", lambda c=c, ps=ps, fb=fb, c0=c0: pe.matmul(ps[:, 0:ntok], lhsT=win[:, c, c0 + fb * 128:c0 + (fb + 1) * 128], rhs=hTg[:, c, 0:ntok], start=(c == 0), stop=(c == 7)), [win, hTg], [ps])
                        kb = ktq[(which * 4 + fb) % 2]
                        evac(kb, kb[:, 0:ntok], ps, ps[:, 0:ntok])
                        if which == 0:
                            S.dma("sp", KT_d[fb * 128:(fb + 1) * 128, tok0:tok0 + ntok], kb[:, 0:ntok], reads=[kb], writes=[T_KT])
                        else:
                            S.dma("sp", QT_d[fb * 128:(fb + 1) * 128, tok0 - CTX:tok0 - CTX + ntok], kb[:, 0:ntok], reads=[kb], writes=[T_QT])
                for k in range(ntl):
                    ti = t0 + k
                    b2 = ti % 2
                    r0 = ti * 128

                    def mm(c0, w_, k=k):
                        ps = nextps()
                        for c in range(8):
                            S.op("pe", lambda c=c, ps=ps: pe.matmul(ps[:, 0:w_], lhsT=hTg[:, c, k * 128:(k + 1) * 128], rhs=win[:, c, c0:c0 + w_], start=(c == 0), stop=(c == 7)), [win, hTg], [ps])
                        return ps

                    ps = mm(512, 512)
                    evac(vo[b2], vo[b2][:, :], ps, ps[:, :])
                    S.dma("sp", V_d[r0:r0 + 128, :], vo[b2][:, :], reads=[vo[b2]], writes=[T_V])
                    TM = int(_env("TM", "9"))
                    if TM <= 1:
                        continue
                    ps = mm(1024, 512)
                    EV = int(_env("EV", "3"))
                    if EV & 1:
                        evac(gkq[b2], gkq[b2][:, 0:256], ps, ps[:, 0:256])
                    if EV & 2:
                        evac(gvo[b2], gvo[b2][:, 0:256], ps, ps[:, 256:512])
                    if TM <= 2:
                        continue
                    ps = mm(1536, 288)
                    evac(gvo[b2], gvo[b2][:, 256:512], ps, ps[:, 0:256])
                    evac(gkq[b2], gkq[b2][:, 256:288], ps, ps[:, 256:288])
                    S.dma("sp", GV_d[r0:r0 + 128, :], gvo[b2][:, :], reads=[gvo[b2]], writes=[T_GV])
                    if TM <= 3:
                        continue
                    if not is_ctx:
                        ps = mm(2336, 512)
                        evac(gkq[b2], gkq[b2][:, 288:544], ps, ps[:, 0:256])
                        evac(ggo[b2], ggo[b2][:, 0:256], ps, ps[:, 256:512])
                        ps = mm(2848, 256)
                        evac(ggo[b2], ggo[b2][:, 256:512], ps, ps[:, 0:256])
                        S.dma("sp", GG_d[r0 - CTX:r0 - CTX + 128, :], ggo[b2][:, :], reads=[ggo[b2]], writes=[T_GG])
                        S.dma("sp", GKQ_d[r0:r0 + 128, :], gkq[b2][:, :], reads=[gkq[b2]], writes=[T_GKQ])
                    else:
                        S.dma("sp", GKQ_d[r0:r0 + 128, 0:288], gkq[b2][:, 0:288], reads=[gkq[b2]], writes=[T_GKQ])
                if gi + 1 < len(groups):
                    transposes(gi + 1)


    T_MIXna = T(None, "MIXna")

    def stageB():
        with contextlib.ExitStack() as es:
            NB = 3
            qT = [K.sb(es, [128, 4, 128], BF16, "qT%d" % i) for i in range(2)]
            kT = [K.sb(es, [128, 4, 896], BF16, "kT%d" % i) for i in range(2)]
            va = [K.sb(es, [128, 7, 8, 65], BF16, "va%d" % i) for i in range(2)]
            bint = K.sb(es, [128, 8, 640], F32, "bint")
            bt = [K.sb(es, [128, 640], F32, "bt%d" % i) for i in range(3)]
            tmp = [K.sb(es, [128, 896], F32, "tmp%d" % i) for i in range(NB)]
            pT = [K.sb(es, [128, 896], BF16, "pT%d" % i) for i in range(NB + 1)]
            ona = [K.sb(es, [128, 512], BF16, "ona%d" % i) for i in range(2)]
            rec = [K.sb(es, [128, 1], F32, "rec%d" % i) for i in range(2)]
            sT = [K.ps(es, [128, 1024], F32, "sT%d" % i) for i in range(NB)]
            pv = [K.ps(es, [128, 512], F32, "pv%d" % i) for i in range(2)]
            for v in va:
                S.op("pool", lambda v=v: pool.memset(v[:, :, :, :], 1.0), [], [v])
            S.dma("sp", bint[:, :, :], na_bias[2].rearrange("h j c -> j h c"), writes=[bint])
            QTv = QT_d.rearrange("(c p) t -> p c t", p=128)
            KTv = KT_d.rearrange("(c p) t -> p c t", p=128)
            nbt = [0]
            tmR = [[T(None, "tmR") for _ in range(3)] for _ in range(NB)]
            onaR = [[T(None, "onaR") for _ in range(8)] for _ in range(2)]

            def loads(T_):
                b = T_ % 2
                kt0 = _na_key_tile0(T_)
                S.dma("sp", qT[b][:, :, :], QTv[:, :, T_ * 128:(T_ + 1) * 128], reads=[T_QT], writes=[qT[b]])
                S.dma("sp", kT[b][:, :, 0:640], KTv[:, :, CTX + kt0 * 128:CTX + kt0 * 128 + 640], reads=[T_KT], writes=[kT[b]])
                S.dma("sp", kT[b][:, :, 640:896], KTv[:, :, 0:CTX], reads=[T_KT], writes=[kT[b]])
                r0 = CTX + kt0 * 128
                for kk in range(5):
                    S.dma("sp", va[b][:, kk, :, 0:64], V_d[r0 + kk * 128:r0 + (kk + 1) * 128, :].rearrange("t (h d) -> t h d", h=8), reads=[T_V], writes=[va[b]])
                for kk in range(2):
                    S.dma("sp", va[b][:, 5 + kk, :, 0:64], V_d[kk * 128:(kk + 1) * 128, :].rearrange("t (h d) -> t h d", h=8), reads=[T_V], writes=[va[b]])

            def phase1(n):
                T_, h = n // 8, n % 8
                b = T_ % 2
                if h == 0:
                    loads(T_)
                si = {0: 0, 1: 1, 30: 3, 31: 4}.get(T_, 2)
                pr, P0 = h // 2, (h % 2) * 64
                s_, tm, pt = sT[n % NB], tmp[n % NB], pT[n % (NB + 1)]
                if si == 2:
                    bb_t, bb = bint, bint[:, h, :]
                else:
                    bb_t = bt[nbt[0] % 3]
                    nbt[0] += 1
                    bb = bb_t[:, :]
                    S.dma("sp", bb, na_bias[si, h], writes=[bb_t])
                for kk in range(7):
                    S.op("pe", lambda kk=kk: pe.matmul(s_[:, kk * 128:(kk + 1) * 128], lhsT=kT[b][P0:P0 + 64, pr, kk * 128:(kk + 1) * 128], rhs=qT[b][P0:P0 + 64, pr, :], start=True, stop=True), [kT[b], qT[b]], [s_])
                ta, tb, tc = tmR[n % NB]
                S.op("dve", lambda: dve.scalar_tensor_tensor(out=tm[:, 0:512], in0=s_[:, 0:512], scalar=0.125, in1=bb[:, 0:512], op0=ALU.mult, op1=ALU.add), [s_, bb_t], [ta])
                S.op("dve", lambda: dve.scalar_tensor_tensor(out=tm[:, 512:640], in0=s_[:, 512:640], scalar=0.125, in1=bb[:, 512:640], op0=ALU.mult, op1=ALU.add), [s_, bb_t], [tb])
                S.op("dve", lambda: dve.tensor_scalar(out=tm[:, 640:896], in0=s_[:, 640:896], scalar1=0.125, scalar2=None, op0=ALU.mult), [s_], [tc])
                S.op("act", lambda: act.activation(out=pt[:, :], in_=tm[:, :], func=AF.Exp), [ta, tb, tc], [pt])

            def phase2(n):
                T_, h = n // 8, n % 8
                b = T_ % 2
                pt, p_, rc = pT[n % (NB + 1)], pv[n % 2], rec[n % 2]
                for kk in range(7):
                    S.op("pe", lambda kk=kk: pe.matmul(p_[:, 0:65], lhsT=pt[:, kk * 128:(kk + 1) * 128], rhs=va[b][:, kk, h, :], start=(kk == 0), stop=(kk == 6)), [pt, va[b]], [p_])
                S.op("dve", lambda: dve.reciprocal(out=rc[:, :], in_=p_[:, 64:65]), [p_], [rc])
                S.op("dve", lambda: dve.tensor_scalar(out=ona[b][:, h * 64:(h + 1) * 64], in0=p_[:, 0:64], scalar1=rc[:, 0:1], scalar2=None, op0=ALU.mult), [p_, rc], [onaR[b][h]])
                if h == 7:
                    S.dma("sp", MIX_d[T_ * 128:(T_ + 1) * 128, 0:512], ona[b][:, :], reads=onaR[b], writes=[T_MIXna])

            NIT = 32 * 8
            LOOK = NB - 1
            for n in range(min(LOOK, NIT)):
                phase1(n)
            for n in range(NIT):
                if n + LOOK < NIT:
                    phase1(n + LOOK)
                phase2(n)

    T_OF = T(None, "OF")
    T_MIXgla = T(None, "MIXgla")

    OB_d = dscr("OB_d", [SEQ, 512])
    T_OB = T(None, "OB")

    def stageC():
        with contextlib.ExitStack() as es:
            tri = [K.sb(es, [128, 128], F32, "tri%d" % i) for i in range(2)]
            msk = [K.sb(es, [128, 512], F32, "msk%d" % i) for i in range(2)]
            onec = K.sb(es, [128, 1], F32, "onec")
            aup = K.sb(es, [32, 512], F32, "aup")
            abi = K.sb(es, [1, 512], F32, "abi")
            gnb = K.sb(es, [128, 128], F32, "gnb")
            for t_, src in ((tri[0], tri_f), (tri[1], tri_b), (onec, ones_col), (aup, aup_bd), (abi, abias)):
                S.dma("sp", t_[:, :], src, writes=[t_])
            for h in range(4):
                S.dma("sp", msk[0][:, h * 128:(h + 1) * 128], mask_f, writes=[msk[0]])
                S.dma("sp", msk[1][:, h * 128:(h + 1) * 128], mask_b, writes=[msk[1]])
            S.dma("sp", gnb[:, :], gla_norm.partition_broadcast(128).rearrange("p a b -> p (a b)"), writes=[gnb])
            ps_ad = K.ps(es, [128, 512], F32, "ps_ad")
            ps_z = K.ps(es, [128, 512], F32, "ps_z")
            ps_lam = K.ps(es, [128, 512], F32, "ps_lam")
            ps_t = K.ps(es, [128, 1024], BF16, "ps_t")
            ps_AT = K.ps(es, [128, 512], F32, "ps_AT")
            ps_o = K.ps(es, [128, 512], F32, "ps_o")
            ps_U = K.ps(es, [128, 512], F32, "ps_U")

            class B:
                pass

            def alloc(ci):
                b = B()
                n_ = lambda x: "%s_c%d" % (x, ci)
                b.gkq = [K.sb(es, [128, 544], F32, n_("gkq%d" % i)) for i in range(2)]
                b.gv = [K.sb(es, [128, 512], BF16, n_("gv%d" % i)) for i in range(2)]
                b.cs = [K.sb(es, [128, 32], F32, n_("cs%d" % i)) for i in range(2)]
                b.sn = [K.sb(es, [128, 32], F32, n_("sn%d" % i)) for i in range(2)]
                b.adT = K.sb(es, [32, 128], F32, n_("adT"))
                b.ez = K.sb(es, [128, 256], F32, n_("ez"))
                b.lz = K.sb(es, [128, 256], F32, n_("lz"))
                b.E1 = K.sb(es, [128, 256], F32, n_("E1"))
                b.E2 = K.sb(es, [128, 256], F32, n_("E2"))
                b.at = K.sb(es, [128, 2], F32, n_("at"))
                b.kr = K.sb(es, [128, 256], F32, n_("kr"))
                b.qr = K.sb(es, [128, 256], F32, n_("qr"))
                b.rt = [K.sb(es, [128, 128], F32, n_("rt%d" % i)) for i in range(4)]
                b.qk = K.sb(es, [128, 512], BF16, n_("qk"))
                b.qTz = K.sb(es, [128, 4, 128], BF16, n_("qTz"))
                S.op("pool", lambda: pool.memset(b.qTz[:, :, :], 0.0), [], [b.qTz])
                b.kTc = K.sb(es, [128, 2, 128], BF16, n_("kTc"))
                b.ATs = K.sb(es, [128, 4, 128], BF16, n_("ATs"))
                b.Sst = [K.sb(es, [128, 128], F32, n_("Sst%d" % i)) for i in range(2)]
                b.Sbf = [K.sb(es, [128, 128], BF16, n_("Sbf%d" % i)) for i in range(2)]
                b.tU = K.sb(es, [128, 128], F32, n_("tU"))
                b.osb = [K.sb(es, [128, 512], F32, n_("osb%d" % i)) for i in range(2)]
                return b

            def rope(eng, ek, src_t, src_ap, dst_t, tmps, c_, s_):
                X = src_ap.rearrange("p (h a b f) -> p h a b f", h=4, a=2, b=2)
                O = dst_t[:, :].rearrange("p (h a b f) -> p h a b f", h=4, a=2, b=2)
                C = c_[:, :].rearrange("p (a f) -> p a f", a=2).unsqueeze(1).broadcast_to([128, 4, 2, 16])
                Sn = s_[:, :].rearrange("p (a f) -> p a f", a=2).unsqueeze(1).broadcast_to([128, 4, 2, 16])
                v = lambda t_: t_[:, :].rearrange("p (h a f) -> p h a f", h=4, a=2)
                X1, X2 = X[:, :, :, 0, :], X[:, :, :, 1, :]
                t1, t2, t3, t4 = tmps
                S.op(ek, lambda: eng.tensor_tensor(out=v(t1), in0=X1, in1=C, op=ALU.mult), [src_t, c_], [t1])
                S.op(ek, lambda: eng.tensor_tensor(out=v(t2), in0=X2, in1=Sn, op=ALU.mult), [src_t, s_], [t2])
                S.op(ek, lambda: eng.tensor_tensor(out=O[:, :, :, 0, :], in0=v(t1), in1=v(t2), op=ALU.subtract), [t1, t2], [dst_t])
                S.op(ek, lambda: eng.tensor_tensor(out=v(t3), in0=X1, in1=Sn, op=ALU.mult), [src_t, s_], [t3])
                S.op(ek, lambda: eng.tensor_tensor(out=v(t4), in0=X2, in1=C, op=ALU.mult), [src_t, c_], [t4])
                S.op(ek, lambda: eng.tensor_tensor(out=O[:, :, :, 1, :], in0=v(t3), in1=v(t4), op=ALU.add), [t3, t4], [dst_t])

            def chain(dr, c):
                for pr in range(2):
                    S.op("pool", lambda pr=pr: pool.memset(c.Sst[pr][:, :], 0.0), [], [c.Sst[pr]])
                    S.op("pool", lambda pr=pr: pool.memset(c.Sbf[pr][:, :], 0.0), [], [c.Sbf[pr]])
                order = list(range(NT)) if dr == 0 else [1, 0] + list(range(NT - 1, 1, -1))
                n = 0
                for ti in order:
                    lat = ti >= 2
                    li = ti - 2
                    b = n % 2
                    n += 1
                    g_, v_ = c.gkq[b], c.gv[b]
                    gw = 544 if lat else 288
                    S.dma("sp", g_[:, 0:gw], GKQ_d[ti * 128:(ti + 1) * 128, 0:gw], reads=[T_GKQ], writes=[g_])
                    S.dma("sp", v_[:, :], GV_d[ti * 128:(ti + 1) * 128, :], reads=[T_GV], writes=[v_])
                    if lat:
                        S.dma("sp", c.cs[b][:, :], rope_cos[li * 128:(li + 1) * 128, :], writes=[c.cs[b]])
                        S.dma("sp", c.sn[b][:, :], rope_sin[li * 128:(li + 1) * 128, :], writes=[c.sn[b]])
                    yield
                    S.op("pe", lambda: pe.transpose(out=ps_ad[0:32, 0:128], in_=g_[:, 256:288], identity=idf[:, :]), [g_, idf], [ps_ad])
                    S.op("act", lambda: act.copy(out=c.adT[:, :], in_=ps_ad[0:32, 0:128]), [ps_ad], [c.adT])
                    S.op("pe", lambda: pe.matmul(ps_z[:, 0:256], lhsT=c.adT[:, :], rhs=aup[:, dr * 256:(dr + 1) * 256], start=True, stop=False), [c.adT, aup], [ps_z])
                    S.op("pe", lambda: pe.matmul(ps_z[:, 0:256], lhsT=onesr[:, :], rhs=abi[:, dr * 256:(dr + 1) * 256], start=False, stop=True), [onesr, abi], [ps_z])
                    S.op("act", lambda: act.activation(out=c.ez[:, :], in_=ps_z[:, 0:256], func=AF.Exp, scale=-1.0), [ps_z], [c.ez])
                    yield
                    S.op("act", lambda: act.activation(out=c.lz[:, :], in_=c.ez[:, :], func=AF.Ln, bias=1.0), [c.ez], [c.lz])
                    S.op("pe", lambda: pe.matmul(ps_lam[:, 0:256], lhsT=tri[dr][:, :], rhs=c.lz[:, :], start=True, stop=True), [tri[dr], c.lz], [ps_lam])
                    for pr in range(2):
                        S.op("pe", lambda pr=pr: pe.matmul(ps_lam[:, 256 + pr:257 + pr], lhsT=c.lz[:, pr * 128:(pr + 1) * 128], rhs=onec[:, :], start=True, stop=True), [c.lz, onec], [ps_lam])
                    if lat:
                        S.op("act", lambda: act.activation(out=c.E1[:, :], in_=ps_lam[:, 0:256], func=AF.Exp), [ps_lam], [c.E1])
                    S.op("act", lambda: act.activation(out=c.E2[:, :], in_=ps_lam[:, 0:256], func=AF.Exp, scale=-1.0), [ps_lam], [c.E2])
                    S.op("act", lambda: act.activation(out=c.at[:, :], in_=ps_lam[:, 256:258], func=AF.Exp), [ps_lam], [c.at])
                    yield
                    if lat:
                        rope(dve, "dve", g_, g_[:, 0:256], c.kr, c.rt, c.cs[b], c.sn[b])
                        yield
                        rope(dve, "dve", g_, g_[:, 288:544], c.qr, c.rt, c.cs[b], c.sn[b])
                        yield
                        S.op("dve", lambda: dve.scalar_tensor_tensor(out=c.qk[:, 0:256], in0=c.qr[:, :], scalar=0.125, in1=c.E1[:, :], op0=ALU.mult, op1=ALU.mult), [c.qr, c.E1], [c.qk])
                        S.op("dve", lambda: dve.tensor_tensor(out=c.qk[:, 256:512], in0=c.kr[:, :], in1=c.E2[:, :], op=ALU.mult), [c.kr, c.E2], [c.qk])
                    else:
                        S.op("dve", lambda: dve.tensor_tensor(out=c.qk[:, 256:512], in0=g_[:, 0:256], in1=c.E2[:, :], op=ALU.mult), [g_, c.E2], [c.qk])
                    for j in (range(4) if lat else range(2, 4)):
                        S.op("pe", lambda j=j: pe.transpose(out=ps_t[:, j * 128:(j + 1) * 128], in_=c.qk[:, j * 128:(j + 1) * 128], identity=idb[:, :]), [c.qk, idb], [ps_t])
                    if lat:
                        for h in range(4):
                            S.op("act", lambda h=h: act.copy(out=c.qTz[(h % 2) * 64:(h % 2) * 64 + 64, h, :], in_=ps_t[(h % 2) * 64:(h % 2) * 64 + 64, (h // 2) * 128:(h // 2 + 1) * 128]), [ps_t], [c.qTz])
                    S.op("act", lambda: act.copy(out=c.kTc[:, :, :], in_=ps_t[:, 256:512].rearrange("p (c t) -> p c t", c=2)), [ps_t], [c.kTc])
                    yield
                    if lat:
                        for h in range(4):
                            pr = h // 2
                            S.op("pe", lambda h=h, pr=pr: pe.matmul(ps_AT[:, h * 128:(h + 1) * 128], lhsT=c.kTc[:, pr, :], rhs=c.qTz[:, h, :], start=True, stop=True), [c.kTc, c.qTz], [ps_AT])
                        S.op("dve", lambda: dve.tensor_tensor(out=c.ATs[:, :, :].rearrange("p h t -> p (h t)"), in0=ps_AT[:, :], in1=msk[dr][:, :], op=ALU.mult), [ps_AT, msk[dr]], [c.ATs])
                        for h in range(4):
                            pr = h // 2
                            S.op("pe", lambda h=h: pe.matmul(ps_o[:, h * 128:(h + 1) * 128], lhsT=c.ATs[:, h, :], rhs=v_[:, h * 128:(h + 1) * 128], start=True, stop=False), [c.ATs, v_], [ps_o])
                            S.op("pe", lambda h=h, pr=pr: pe.matmul(ps_o[:, h * 128:(h + 1) * 128], lhsT=c.qTz[:, h, :], rhs=c.Sbf[pr][:, :], start=False, stop=True), [c.qTz, c.Sbf[pr]], [ps_o])
                        ob_ = c.osb[b]
                        S.op("dve", lambda: dve.tensor_copy(out=ob_[:, :], in_=ps_o[:, :]), [ps_o], [ob_])
                        if dr == 0:
                            S.dma("sp", OF_d[li * 128:(li + 1) * 128, :], ob_[:, :], reads=[ob_], writes=[T_OF])
                        else:
                            S.dma("sp", OB_d[li * 128:(li + 1) * 128, :], ob_[:, :], reads=[ob_], writes=[T_OB])
                        yield
                    for h in range(4):
                        pr = h // 2
                        S.op("pe", lambda h=h, pr=pr: pe.matmul(ps_U[:, h * 128:(h + 1) * 128], lhsT=c.qk[:, 256 + pr * 128:256 + (pr + 1) * 128], rhs=v_[:, h * 128:(h + 1) * 128], start=True, stop=True), [c.qk, v_], [ps_U])
                    for h in range(4):
                        pr, P0 = h // 2, (h % 2) * 64
                        S.op("dve", lambda h=h, pr=pr, P0=P0: dve.tensor_scalar(out=c.tU[P0:P0 + 64, :], in0=ps_U[P0:P0 + 64, h * 128:(h + 1) * 128], scalar1=c.at[P0:P0 + 64, pr:pr + 1], scalar2=None, op0=ALU.mult), [ps_U, c.at], [c.tU])
                        S.op("dve", lambda pr=pr, P0=P0: dve.scalar_tensor_tensor(out=c.Sst[pr][P0:P0 + 64, :], in0=c.Sst[pr][P0:P0 + 64, :], scalar=c.at[P0:P0 + 64, pr:pr + 1], in1=c.tU[P0:P0 + 64, :], op0=ALU.mult, op1=ALU.add), [c.Sst[pr], c.at, c.tU], [c.Sst[pr]])
                        S.op("dve", lambda pr=pr, P0=P0: dve.tensor_copy(out=c.Sbf[pr][P0:P0 + 64, :], in_=c.Sst[pr][P0:P0 + 64, :]), [c.Sst[pr]], [c.Sbf[pr]])
                    yield

            gens = [chain(0, alloc(0)), chain(1, alloc(1))]
            while gens:
                for g in list(gens):
                    try:
                        next(g)
                    except StopIteration:
                        gens.remove(g)

            gg = [K.sb(es, [128, 512], F32, "gg%d" % i) for i in range(2)]
            ofl = [K.sb(es, [128, 512], F32, "ofl%d" % i) for i in range(2)]
            obl = [K.sb(es, [128, 512], F32, "obl%d" % i) for i in range(2)]
            osm = [K.sb(es, [128, 512], F32, "osm%d" % i) for i in range(2)]
            junk = [K.sb(es, [128, 128], F32, "junkc%d" % i) for i in range(2)]
            ss4 = [K.sb(es, [128, 4], F32, "ss4_%d" % i) for i in range(2)]
            rs4 = [K.sb(es, [128, 4], F32, "rs4_%d" % i) for i in range(2)]
            sg = [K.sb(es, [128, 512], F32, "sg%d" % i) for i in range(2)]
            ogl = [K.sb(es, [128, 512], BF16, "ogl%d" % i) for i in range(2)]

            def merge(li):
                b = li % 2
                S.dma("sp", gg[b][:, :], GG_d[li * 128:(li + 1) * 128, :], reads=[T_GG], writes=[gg[b]])
                S.dma("sp", ofl[b][:, :], OF_d[li * 128:(li + 1) * 128, :], reads=[T_OF], writes=[ofl[b]])
                S.dma("sp", obl[b][:, :], OB_d[li * 128:(li + 1) * 128, :], reads=[T_OB], writes=[obl[b]])
                yield
                S.op("pool", lambda: pool.tensor_tensor(out=osm[b][:, :], in0=ofl[b][:, :], in1=obl[b][:, :], op=ALU.add), [ofl[b], obl[b]], [osm[b]])
                S.op("act", lambda: act.activation(out=sg[b][:, :], in_=gg[b][:, :], func=AF.Silu), [gg[b]], [sg[b]])
                yield
                for h in range(4):
                    S.op("act", lambda h=h: act.activation(out=junk[b][:, :], in_=osm[b][:, h * 128:(h + 1) * 128], func=AF.Square, accum_out=ss4[b][:, h:h + 1]), [osm[b]], [junk[b], ss4[b]])
                S.op("act", lambda: act.activation(out=rs4[b][:, :], in_=ss4[b][:, :], func=AF.Sqrt, scale=1.0 / 128.0, bias=epsb[:, :]), [ss4[b], epsb], [rs4[b]])
                yield
                S.op("dve", lambda: dve.reciprocal(out=rs4[b][:, :], in_=rs4[b][:, :]), [rs4[b]], [rs4[b]])
                o3 = osm[b][:, :].rearrange("p (h e) -> p h e", h=4)
                S.op("dve", lambda: dve.tensor_tensor(out=o3, in0=o3, in1=rs4[b][:, :].unsqueeze(2).broadcast_to([128, 4, 128]), op=ALU.mult), [osm[b], rs4[b]], [osm[b]])
                yield
                S.op("pool", lambda: pool.tensor_tensor(out=o3, in0=o3, in1=gnb[:, :].unsqueeze(1).broadcast_to([128, 4, 128]), op=ALU.mult), [osm[b], gnb], [osm[b]])
                S.op("dve", lambda: dve.tensor_tensor(out=ogl[b][:, :], in0=osm[b][:, :], in1=sg[b][:, :], op=ALU.mult), [osm[b], sg[b]], [ogl[b]])
                S.dma("sp", MIX_d[li * 128:(li + 1) * 128, 512:1024], ogl[b][:, :], reads=[ogl[b]], writes=[T_MIXgla])
                yield

            pend = []
            for li in range(32):
                pend.append(merge(li))
                if len(pend) == 2 or li == 31:
                    live = list(pend)
                    while live:
                        for g in list(live):
                            try:
                                next(g)
                            except StopIteration:
                                live.remove(g)
                    pend = []

    T_XN = T(None, "XN")
    T_HF = T(None, "HF")
    T_AFFT = T(None, "AFFT")
    AX = mybir.AxisListType.X

    def stageD():
        with contextlib.ExitStack() as es:
            wo = K.sb(es, [128, 8, D], BF16, "wo")
            wst = [K.sb(es, [128, 8, 512], F32, "wstd%d" % i) for i in range(2)]
            wov = w_out.rearrange("(c p) n -> p c n", p=128)
            for b in range(2):
                S.dma("sp", wst[b][:, :, :], wov[:, :, b * 512:(b + 1) * 512], writes=[wst[b]])
                S.op("act", lambda b=b: act.copy(out=wo[:, :, b * 512:(b + 1) * 512], in_=wst[b][:, :, :]), [wst[b]], [wo])
            rt = K.sb(es, [128, 8, NE], F32, "rt")
            S.dma("sp", rt[:, :, :], router.rearrange("(c p) e -> p c e", p=128), writes=[rt])
            md = K.sb(es, [128, 3, D], F32, "md")
            S.dma("sp", md[:, :, :], mods_d[:, 4:7, :], reads=[T_mods], writes=[md])
            affT = K.sb(es, [NE, SEQ], F32, "affT")
            affR = [T(None, "affR%d" % i) for i in range(32)]
            NL = 3
            dbl = lambda shape, dt, nm: [K.sb(es, shape, dt, "%s%d" % (nm, i)) for i in range(NL)]
            mixb = dbl([128, D], BF16, "mixb")
            xt = dbl([128, D], F32, "xtd")
            mixT = dbl([128, 8, 128], BF16, "mixT")
            mixs = dbl([128, D], F32, "mixs")
            junk = dbl([128, D], BF16, "junkd")
            ss = dbl([128, 1], F32, "ssd")
            rstd = dbl([128, 1], F32, "rstdd")
            t1 = dbl([128, D], F32, "t1d")
            xn = dbl([128, D], F32, "xn")
            hf32 = dbl([128, D], F32, "hf32")
            hfb = dbl([128, D], BF16, "hfb")
            hfT = dbl([128, 8, 128], F32, "hfT")
            lg = dbl([128, NE], F32, "lg")
            mx = dbl([128, 1], F32, "mx")
            ex = dbl([128, NE], F32, "ex")
            sm = dbl([128, 1], F32, "sm")
            af = dbl([128, NE], F32, "af")
            ps_t = K.ps(es, [128, 1024], BF16, "psd_t")
            ps_m = [K.ps(es, [128, 512], F32, "psd_m%d" % i) for i in range(2)]
            ps_r = [K.ps(es, [128, 512], F32, "psd_r%d" % i) for i in range(2)]
            ps_l = K.ps(es, [128, 512], F32, "psd_l")
            ps_a = K.ps(es, [128, 512], F32, "psd_a")

            def tile_prog(T_):
                b = T_ % NL
                mb, x_ = mixb[b], xt[b]
                S.dma("sp", mb[:, :], MIX_d[T_ * 128:(T_ + 1) * 128, :], reads=[T_MIXna, T_MIXgla], writes=[mb])
                S.dma("sp", x_[:, :], xc[CTX + T_ * 128:CTX + (T_ + 1) * 128, :], writes=[x_])
                yield
                for c in range(8):
                    S.op("pe", lambda c=c: pe.transpose(out=ps_t[:, c * 128:(c + 1) * 128], in_=mb[:, c * 128:(c + 1) * 128], identity=idb[:, :]), [mb, idb], [ps_t])
                S.op("act", lambda: act.copy(out=mixT[b][:, :, :], in_=ps_t[:, :].rearrange("p (c t) -> p c t", c=8)), [ps_t], [mixT[b]])
                yield
                for hlf in range(2):
                    for c in range(8):
                        S.op("pe", lambda c=c, hlf=hlf: pe.matmul(ps_m[hlf][:, :], lhsT=mixT[b][:, c, :], rhs=wo[:, c, hlf * 512:(hlf + 1) * 512], start=(c == 0), stop=(c == 7)), [mixT[b], wo], [ps_m[hlf]])
                    S.op("act", lambda hlf=hlf: act.copy(out=mixs[b][:, hlf * 512:(hlf + 1) * 512], in_=ps_m[hlf][:, :]), [ps_m[hlf]], [mixs[b]])
                yield
                rms_rstd(None, mixs[b][:, :], mixs[b], ss[b], rstd[b], junk[b])
                yield
                S.op("dve", lambda: dve.scalar_tensor_tensor(out=t1[b][:, :], in0=mixs[b][:, :], scalar=rstd[b][:, 0:1], in1=md[:, 0, :], op0=ALU.mult, op1=ALU.mult), [mixs[b], rstd[b], md], [t1[b]])
                xn_ = xn[b]
                S.op("pool", lambda: pool.tensor_tensor(out=xn_[:, :], in0=t1[b][:, :], in1=x_[:, :], op=ALU.add), [t1[b], x_], [xn_])
                S.dma("sp", XN_d[T_ * 128:(T_ + 1) * 128, :], xn_[:, :], reads=[xn_], writes=[T_XN])
                yield
                rms_rstd(None, xn_[:, :], xn_, ss[b], rstd[b], junk[b])
                yield
                S.op("dve", lambda: dve.scalar_tensor_tensor(out=t1[b][:, :], in0=xn_[:, :], scalar=rstd[b][:, 0:1], in1=md[:, 1, :], op0=ALU.mult, op1=ALU.mult), [xn_, rstd[b], md], [t1[b]])
                S.op("pool", lambda: pool.tensor_tensor(out=hf32[b][:, :], in0=t1[b][:, :], in1=md[:, 2, :], op=ALU.add), [t1[b], md], [hf32[b]])
                yield
                hb_ = hfb[b]
                S.op("act", lambda: act.copy(out=hb_[:, :], in_=hf32[b][:, :]), [hf32[b]], [hb_])
                S.dma("sp", HF_d[T_ * 128:(T_ + 1) * 128, :], hb_[:, :], reads=[hb_], writes=[T_HF])
                for c in range(8):
                    S.op("pe", lambda c=c: pe.transpose(out=ps_r[c // 4][:, (c % 4) * 128:(c % 4 + 1) * 128], in_=hf32[b][:, c * 128:(c + 1) * 128], identity=idf[:, :]), [hf32[b], idf], [ps_r[c // 4]])
                for i in range(2):
                    S.op("act", lambda i=i: act.copy(out=hfT[b][:, i * 4:(i + 1) * 4, :], in_=ps_r[i][:, :].rearrange("p (c t) -> p c t", c=4)), [ps_r[i]], [hfT[b]])
                yield
                for c in range(8):
                    S.op("pe", lambda c=c: pe.matmul(ps_l[:, 0:NE], lhsT=hfT[b][:, c, :], rhs=rt[:, c, :], start=(c == 0), stop=(c == 7)), [hfT[b], rt], [ps_l])
                S.op("dve", lambda: dve.tensor_copy(out=lg[b][:, :], in_=ps_l[:, 0:NE]), [ps_l], [lg[b]])
                yield
                S.op("dve", lambda: dve.reduce_max(out=mx[b][:, :], in_=lg[b][:, :], axis=AX), [lg[b]], [mx[b]])
                S.op("dve", lambda: dve.tensor_scalar(out=mx[b][:, :], in0=mx[b][:, :], scalar1=-1.0, scalar2=None, op0=ALU.mult), [mx[b]], [mx[b]])
                yield
                S.op("act", lambda: act.activation(out=ex[b][:, :], in_=lg[b][:, :], func=AF.Exp, bias=mx[b][:, :], accum_out=sm[b][:, :]), [lg[b], mx[b]], [ex[b], sm[b]])
                yield
                S.op("dve", lambda: dve.reciprocal(out=sm[b][:, :], in_=sm[b][:, :]), [sm[b]], [sm[b]])
                S.op("dve", lambda: dve.tensor_scalar(out=af[b][:, :], in0=ex[b][:, :], scalar1=sm[b][:, 0:1], scalar2=None, op0=ALU.mult), [ex[b], sm[b]], [af[b]])
                yield
                S.op("pe", lambda: pe.transpose(out=ps_a[0:NE, 0:128], in_=af[b][:, :], identity=idf[:, :]), [af[b], idf], [ps_a])
                S.op("act", lambda: act.copy(out=affT[:, T_ * 128:(T_ + 1) * 128], in_=ps_a[0:NE, 0:128]), [ps_a], [affR[T_]])
                yield

            for T0 in range(0, 32, NL):
                live = [tile_prog(T_) for T_ in range(T0, min(T0 + NL, 32))]
                while live:
                    for g in list(live):
                        try:
                            next(g)
                        except StopIteration:
                            live.remove(g)
            S.dma("sp", AFFT_d, affT[:, :], reads=affR, writes=[T_AFFT])


    posT_d = dscr("posT_d", [128, 4, 64])
    gateT_d = dscr("gateT_d", [128, 4, 64])
    HFO_d = dscr("HFO_d", [SEQ // 2, D], BF16)
    T_HFO = T(None, "HFO")

    def stageEFG():
        import os
        with contextlib.ExitStack() as es:
            posT = K.sb(es, [128, 4, 64], F32, "posT")
            gateT = K.sb(es, [128, 4, 64], F32, "gateT")
            iot = K.sb(es, [128, 512], F32, "iot")
            S.dma("sp", iot[:, :], iota512, writes=[iot])
            PB = [K.ps(es, [128, 512], F32, "PB%d" % i) for i in range(8)]
            oi = K.sb(es, [128, 16], I32, "oi")
            S.dma("sp", oi[:, :], own_idx, writes=[oi])
            e4 = contextlib.ExitStack()
            hfj = [K.sb(e4, [128, D], BF16, "hfj%d" % i) for i in range(3)]
            for j in range(16):
                hj = hfj[j % 3]
                S.dma("pool", None, None, reads=[T_HF, oi], writes=[hj], fn=lambda hj=hj, j=j: pool.indirect_dma_start(
                    out=hj[:, :], out_offset=None, in_=HF_d, in_offset=bass.IndirectOffsetOnAxis(ap=oi[:, j:j + 1], axis=0)))
                S.dma("sp", HFO_d[j * 128:(j + 1) * 128, :], hj[:, :], reads=[hj], writes=[T_HFO])
            with contextlib.ExitStack() as e2:
                A = K.sb(e2, [128, 512], F32, "A")
                S.dma("sp", A[:, :], AFFT_d.rearrange("e (s t) -> (e s) t", s=8), reads=[T_AFFT], writes=[A])
                b1 = K.sb(e2, [128, 128], F32, "b1")
                blt = K.sb(e2, [128, 128], F32, "blt")
                ownf = K.sb(e2, [128, 1], F32, "ownf")
                selm = K.sb(e2, [128, 64], F32, "selm")
                for t_, src in ((b1, blk_ones), (blt, blk_lt), (ownf, own_flag), (selm, sel)):
                    S.dma("sp", t_[:, :], src, writes=[t_])
                lo = K.sb(e2, [128, 1], F32, "lo")
                hi = K.sb(e2, [128, 1], F32, "hi")
                mid = K.sb(e2, [128, 1], F32, "mid")
                cnt = K.sb(e2, [128, 1], F32, "cnt")
                cond = K.sb(e2, [128, 1], F32, "cond")
                d1 = K.sb(e2, [128, 1], F32, "d1")
                jk = K.sb(e2, [128, 512], F32, "jk")
                onesT = K.sb(e2, [128, 512], F32, "onesT")
                M_ = K.sb(e2, [128, 512], F32, "M_")
                posi = K.sb(e2, [128, 512], F32, "posi")
                pm = K.sb(e2, [128, 512], F32, "pm")
                offc = K.sb(e2, [128, 1], F32, "offc")
                S.op("pool", lambda: pool.memset(lo[:, :], 0.0), [], [lo])
                S.op("pool", lambda: pool.memset(hi[:, :], 1.0), [], [hi])
                S.op("pool", lambda: pool.memset(onesT[:, :], 1.0), [], [onesT])
                pc = PB[0]
                for it in range(30):
                    S.op("dve", lambda: dve.tensor_tensor(out=mid[:, :], in0=lo[:, :], in1=hi[:, :], op=ALU.add), [lo, hi], [mid])
                    S.op("dve", lambda: dve.tensor_scalar(out=mid[:, :], in0=mid[:, :], scalar1=0.5, scalar2=None, op0=ALU.mult), [mid], [mid])
                    S.op("dve", lambda: dve.tensor_scalar(out=jk[:, :], in0=A[:, :], scalar1=mid[:, 0:1], scalar2=0.0, op0=ALU.is_gt, op1=ALU.add, accum_out=cnt[:, 0:1]), [A, mid], [jk, cnt])
                    S.op("pe", lambda: pe.matmul(pc[:, 0:1], lhsT=b1[:, :], rhs=cnt[:, 0:1], start=True, stop=True), [b1, cnt], [pc])
                    S.op("dve", lambda: dve.tensor_scalar(out=cond[:, :], in0=pc[:, 0:1], scalar1=float(CAP) - 0.5, scalar2=None, op0=ALU.is_gt), [pc], [cond])
                    S.op("dve", lambda: dve.tensor_tensor(out=d1[:, :], in0=mid[:, :], in1=lo[:, :], op=ALU.subtract), [mid, lo], [d1])
                    S.op("dve", lambda: dve.scalar_tensor_tensor(out=lo[:, :], in0=d1[:, :], scalar=cond[:, 0:1], in1=lo[:, :], op0=ALU.mult, op1=ALU.add), [d1, cond, lo], [lo])
                    S.op("dve", lambda: dve.tensor_tensor(out=d1[:, :], in0=hi[:, :], in1=mid[:, :], op=ALU.subtract), [hi, mid], [d1])
                    S.op("dve", lambda: dve.scalar_tensor_tensor(out=hi[:, :], in0=d1[:, :], scalar=cond[:, 0:1], in1=mid[:, :], op0=ALU.mult, op1=ALU.add), [d1, cond, mid], [hi])
                S.op("dve", lambda: dve.tensor_scalar(out=M_[:, :], in0=A[:, :], scalar1=lo[:, 0:1], scalar2=ownf[:, 0:1], op0=ALU.is_gt, op1=ALU.mult), [A, lo, ownf], [M_])
                S.op("dve", lambda: dve.tensor_tensor_scan(out=posi[:, :], data0=onesT[:, :], data1=M_[:, :], initial=0.0, op0=ALU.mult, op1=ALU.add), [onesT, M_], [posi])
                S.op("pe", lambda: pe.matmul(pc[:, 0:1], lhsT=blt[:, :], rhs=posi[:, 511:512], start=True, stop=True), [blt, posi], [pc])
                S.op("dve", lambda: dve.tensor_scalar(out=offc[:, :], in0=pc[:, 0:1], scalar1=-10000.0, scalar2=None, op0=ALU.add), [pc], [offc])
                S.op("dve", lambda: dve.tensor_scalar(out=pm[:, :], in0=posi[:, :], scalar1=offc[:, 0:1], scalar2=None, op0=ALU.add), [posi, offc], [pm])
                S.op("dve", lambda: dve.tensor_tensor(out=pm[:, :], in0=pm[:, :], in1=M_[:, :], op=ALU.mult), [pm, M_], [pm])
                S.op("dve", lambda: dve.tensor_scalar(out=pm[:, :], in0=pm[:, :], scalar1=9999.0, scalar2=None, op0=ALU.add), [pm], [pm])
                for src, dst, pb in ((pm, posT, PB[1]), (A, gateT, PB[2])):
                    for blk in range(4):
                        S.op("pe", lambda src=src, pb=pb, blk=blk: pe.matmul(pb[:, blk * 64:(blk + 1) * 64], lhsT=src[:, blk * 128:(blk + 1) * 128], rhs=selm[:, :], start=True, stop=True), [src, selm], [pb])
                    S.op("act", lambda dst=dst, pb=pb: act.copy(out=dst[:, :, :], in_=pb[:, 0:256].rearrange("p (b c) -> p b c", b=4)), [pb], [dst])
                if debug:
                    S.dma("sp", posT_d, posT[:, :, :], reads=[posT])
                    S.dma("sp", gateT_d, gateT[:, :, :], reads=[gateT])
                S.barrier()
            S.barrier()
            e4.close()
            if upto == "E":
                return
            yacc = K.sb(es, [128, 16, D], F32, "yacc")
            S.op("pool", lambda: pool.memset(yacc[:, :, :], 0.0), [], [yacc])
            e3 = contextlib.ExitStack()
            xsT = K.sb(e3, [128, 8, 512], BF16, "xsT")
            hidT = K.sb(e3, [128, NFC, 512], BF16, "hidT")
            ysb = K.sb(e3, [128, 4, D], BF16, "ysb")
            Ej = [K.sb(e3, [128, 512], BF16, "Ej%d" % i) for i in range(2)]
            Gj = [K.sb(e3, [128, 512], BF16, "Gj%d" % i) for i in range(2)]
            tidt = K.sb(e3, [128, 16, 2], BF16, "tidt")
            S.dma("sp", tidt[:, :, :], tid_in, writes=[tidt])
            xg = [K.sb(e3, [128, 4, D], BF16, "xg%d" % i) for i in range(2)]
            xgT = [[T(None, "xgT") for _ in range(4)] for _ in range(2)]
            idxs = K.sb(e3, [128, 8], F32, "idxs")
            idxf = K.sb(e3, [128, 4], F32, "idxf")
            idxi = [K.sb(e3, [128, 4], I32, "idxi%d" % i) for i in range(2)]
            GTa = K.sb(e3, [128, 16, 4, 128], BF16, "GTa")
            GTt = [T(None, "GTt%d" % i) for i in range(16)]
            sgt = [K.sb(e3, [128, 512], BF16, "sgt%d" % i) for i in range(2)]
            stg = [K.sb(e3, [128, 2048], F32, "stg%d" % i) for i in range(4)]
            nst = [0]

            def stage_in(src_ap, ncols):
                st = stg[nst[0] % 4]
                q = _env("WQ", "sp").split(",")
                q = q[nst[0] % len(q)]
                nst[0] += 1
                S.dma(q, st[:, 0:ncols], src_ap, writes=[st])
                return st
            wgb = [K.sb(e3, [128, 8, 256], BF16, "wgb%d" % i) for i in range(2)]
            wub = [K.sb(e3, [128, 8, 256], BF16, "wub%d" % i) for i in range(2)]
            wdb = K.sb(e3, [128, NFC, 256], BF16, "wdb")
            wdbT = [T(None, "wdbT%d" % i) for i in range(3)]
            NEX = int(_env("NEX", str(NE)))
            ng = 0
            PH = int(_env("PH", "15"))
            def route(e):
                xb = e % 2
                for j in range(16):
                    osg, blk = j // 4, j % 4
                    E_ = Ej[j % 2]
                    S.op("dve", lambda E_=E_, blk=blk, col=e * 4 + osg: dve.tensor_scalar(out=E_[:, :], in0=iot[:, :], scalar1=posT[:, blk, col:col + 1], scalar2=None, op0=ALU.is_equal), [iot, posT], [E_])
                    for sc in range(4):
                        S.op("pe", lambda sc=sc, E_=E_, j=j: pe.matmul(PB[sc][:, 0:2], lhsT=E_[:, sc * 128:(sc + 1) * 128], rhs=tidt[:, j, :], start=(j == 0), stop=(j == 15)), [E_, tidt], [PB[sc]])
                for sc in range(4):
                    S.op("dve", lambda sc=sc: dve.tensor_copy(out=idxs[:, sc * 2:(sc + 1) * 2], in_=PB[sc][:, 0:2]), [PB[sc]], [idxs])
                iv = idxs[:, :].rearrange("p (s two) -> p s two", two=2)
                S.op("dve", lambda: dve.scalar_tensor_tensor(out=idxf[:, :], in0=iv[:, :, 0], scalar=64.0, in1=iv[:, :, 1], op0=ALU.mult, op1=ALU.add), [idxs], [idxf])
                S.op("dve", lambda: dve.tensor_copy(out=idxi[xb][:, :], in_=idxf[:, :]), [idxf], [idxi[xb]])
                for sc in range(4):
                    S.dma("pool", None, None, reads=[T_HFO, idxi[xb]], writes=[xgT[xb][sc]], fn=lambda sc=sc: pool.indirect_dma_start(
                        out=xg[xb][:, sc, :], out_offset=None, in_=HFO_d, in_offset=bass.IndirectOffsetOnAxis(ap=idxi[xb][:, sc:sc + 1], axis=0)))

            if PH & 1:
                route(0)
            for e in range(NEX):
                xb = e % 2
                for c in (range(8) if PH & 1 else []):
                    bb = PB[c // 2][:, :].bitcast(BF16)
                    for sc in range(4):
                        S.op("pe", lambda c=c, sc=sc, bb=bb: pe.transpose(out=bb[:, (c % 2) * 512 + sc * 128:(c % 2) * 512 + (sc + 1) * 128], in_=xg[xb][:, sc, c * 128:(c + 1) * 128], identity=idb[:, :]), [xgT[xb][sc], idb], [PB[c // 2]])
                for c in (range(8) if PH & 1 else []):
                    bb = PB[c // 2][:, :].bitcast(BF16)
                    if (c // 2) % 2 == 0:
                        S.op("act", lambda c=c, bb=bb: act.copy(out=xsT[:, c, :], in_=bb[:, (c % 2) * 512:(c % 2 + 1) * 512]), [PB[c // 2]], [xsT])
                    else:
                        S.op("dve", lambda c=c, bb=bb: dve.tensor_copy(out=xsT[:, c, :], in_=bb[:, (c % 2) * 512:(c % 2 + 1) * 512]), [PB[c // 2]], [xsT])

                def prep(j):
                    osg, blk = j // 4, j % 4
                    col = e * 4 + osg
                    G_ = Gj[j % 2]
                    S.op("dve", lambda: dve.tensor_scalar(out=G_[:, :], in0=iot[:, :], scalar1=posT[:, blk, col:col + 1], scalar2=gateT[:, blk, col:col + 1], op0=ALU.is_equal, op1=ALU.mult), [iot, posT, gateT], [G_])
                    pt = PB[6 + j % 2]
                    ptb = pt[:, :].bitcast(BF16)
                    for sc in range(4):
                        S.op("pe", lambda sc=sc: pe.transpose(out=ptb[:, sc * 128:(sc + 1) * 128], in_=G_[:, sc * 128:(sc + 1) * 128], identity=idb[:, :]), [G_, idb], [pt])
                    S.op("act", lambda: act.copy(out=GTa[:, j, :, :], in_=ptb[:, 0:512].rearrange("p (s t) -> p s t", s=4)), [pt], [GTt[j]])

                jn = 0

                def load_gu(e_, g_):
                    gb_ = (e_ * 11 + g_) % 2
                    sg_st = stage_in(w_gate[e_, g_], 2048)
                    su_st = stage_in(w_up[e_, g_], 2048)
                    S.op("act", lambda: act.copy(out=wgb[gb_][:, :, :].rearrange("p c f -> p (c f)"), in_=sg_st[:, :]), [sg_st], [wgb[gb_]])
                    S.op("dve", lambda: dve.tensor_copy(out=wub[gb_][:, :, :].rearrange("p c f -> p (c f)"), in_=su_st[:, :]), [su_st], [wub[gb_]])

                if e == 0:
                    load_gu(0, 0)
                for g in (range(11) if PH & 2 else []):
                    gb = (e * 11 + g) % 2
                    if g + 1 < 11:
                        load_gu(e, g + 1)
                    for fl in range(2):
                        fc = g * 2 + fl
                        pg, pu = PB[(fc % 2) * 2], PB[(fc % 2) * 2 + 1]
                        for c in range(8):
                            S.op("pe", lambda c=c, pg=pg, gb=gb, fl=fl: pe.matmul(pg[:, :], lhsT=wgb[gb][:, c, fl * 128:(fl + 1) * 128], rhs=xsT[:, c, :], start=(c == 0), stop=(c == 7)), [wgb[gb], xsT], [pg])
                        for c in range(8):
                            S.op("pe", lambda c=c, pu=pu, gb=gb, fl=fl: pe.matmul(pu[:, :], lhsT=wub[gb][:, c, fl * 128:(fl + 1) * 128], rhs=xsT[:, c, :], start=(c == 0), stop=(c == 7)), [wub[gb], xsT], [pu])
                        sg_ = sgt[fc % 2]
                        S.op("act", lambda pg=pg, sg_=sg_: act.activation(out=sg_[:, :], in_=pg[:, :], func=AF.Silu), [pg], [sg_])
                        S.op("dve", lambda pu=pu, sg_=sg_, fc=fc: dve.tensor_tensor(out=hidT[:, fc, :], in0=sg_[:, :], in1=pu[:, :], op=ALU.mult), [sg_, pu], [hidT])
                    for _ in range(2 if g < 5 else 1):
                        if jn < 16:
                            prep(jn)
                            jn += 1
                while jn < 16 and (PH & 8):
                    prep(jn)
                    jn += 1
                if e + 1 < NEX and (PH & 1):
                    route(e + 1)
                pieces = ((0, 8), (8, 16), (16, NFC))
                def dma_d(cb_):
                    return [stage_in(w_down[e, cb_, :, f0 * 256:f1 * 256], (f1 - f0) * 256) for (f0, f1) in pieces]

                def cast_d(k, st):
                    f0, f1 = pieces[k]
                    S.op("act", lambda: act.copy(out=wdb[:, f0:f1, :].rearrange("p c n -> p (c n)"), in_=st[:, 0:(f1 - f0) * 256]), [st], [wdbT[k]])

                sts = dma_d(0)
                for k in range(3):
                    cast_d(k, sts[k])
                for cb in (range(4) if PH & 4 else []):
                    nxt = dma_d(cb + 1) if cb + 1 < 4 else None
                    for k, (f0, f1) in enumerate(pieces):
                        for fc in range(f0, f1):
                            for sc in range(4):
                                S.op("pe", lambda fc=fc, sc=sc: pe.matmul(PB[4 + sc][:, 0:256], lhsT=hidT[:, fc, sc * 128:(sc + 1) * 128], rhs=wdb[:, fc, :], start=(fc == 0), stop=(fc == NFC - 1)), [hidT, wdbT[k]], [PB[4 + sc]])
                        if nxt is not None:
                            cast_d(k, nxt[k])
                    for sc in range(4):
                        if sc % 2 == 0:
                            S.op("act", lambda sc=sc, cb=cb: act.copy(out=ysb[:, sc, cb * 256:(cb + 1) * 256], in_=PB[4 + sc][:, 0:256]), [PB[4 + sc]], [ysb])
                        else:
                            S.op("dve", lambda sc=sc, cb=cb: dve.tensor_copy(out=ysb[:, sc, cb * 256:(cb + 1) * 256], in_=PB[4 + sc][:, 0:256]), [PB[4 + sc]], [ysb])
                if e + 1 < NEX:
                    load_gu(e + 1, 0)
                for j in (range(16) if PH & 8 else []):
                    for hlf in range(2):
                        pyc = PB[(j % 2) * 2 + hlf]
                        for sc in range(4):
                            S.op("pe", lambda sc=sc, pyc=pyc, hlf=hlf, j=j: pe.matmul(pyc[:, :], lhsT=GTa[:, j, sc, :], rhs=ysb[:, sc, hlf * 512:(hlf + 1) * 512], start=(sc == 0), stop=(sc == 3)), [GTt[j], ysb], [pyc])
                        S.op("dve", lambda pyc=pyc, j=j, hlf=hlf: dve.tensor_tensor(out=yacc[:, j, hlf * 512:(hlf + 1) * 512], in0=yacc[:, j, hlf * 512:(hlf + 1) * 512], in1=pyc[:, :], op=ALU.add), [yacc, pyc], [yacc])
            S.barrier()
            e3.close()
            gF = K.sb(es, [128, D], F32, "gF")
            S.dma("sp", gF[:, :], mods_d[:, 7, :], reads=[T_mods], writes=[gF])
            xno = [K.sb(es, [128, D], F32, "xno%d" % i) for i in range(2)]
            ob = [K.sb(es, [128, D], F32, "ob%d" % i) for i in range(2)]
            junk = K.sb(es, [128, D], BF16, "junkg")
            ss = K.sb(es, [128, 1], F32, "ssg")
            rstd = K.sb(es, [128, 1], F32, "rstdg")
            for j in range(16):
                b = j % 2
                S.dma("pool", None, None, reads=[T_XN, oi], writes=[xno[b]], fn=lambda b=b, j=j: pool.indirect_dma_start(
                    out=xno[b][:, :], out_offset=None, in_=XN_d, in_offset=bass.IndirectOffsetOnAxis(ap=oi[:, j:j + 1], axis=0)))
                S.op("act", lambda j=j: act.activation(out=junk[:, :], in_=yacc[:, j, :], func=AF.Square, accum_out=ss[:, :]), [yacc], [junk, ss])
                S.op("act", lambda: act.activation(out=rstd[:, :], in_=ss[:, :], func=AF.Sqrt, scale=1.0 / D, bias=epsb[:, :]), [ss, epsb], [rstd])
                S.op("dve", lambda: dve.reciprocal(out=rstd[:, :], in_=rstd[:, :]), [rstd], [rstd])
                S.op("dve", lambda j=j, b=b: dve.scalar_tensor_tensor(out=ob[b][:, :], in0=yacc[:, j, :], scalar=rstd[:, 0:1], in1=gF[:, :], op0=ALU.mult, op1=ALU.mult), [yacc, rstd, gF], [ob[b]])
                S.op("pool", lambda b=b: pool.tensor_tensor(out=ob[b][:, :], in0=ob[b][:, :], in1=xno[b][:, :], op=ALU.add), [ob[b], xno[b]], [ob[b]])
                S.dma("sp", out[j * 128:(j + 1) * 128, :], ob[b][:, :], reads=[ob[b]])

    stages = [("0", stage0), ("A", stageA), ("B", stageB), ("C", stageC), ("D", stageD), ("E", stageEFG)]
    if upto in ("F", "G", "all"):
        stages[-1] = (upto, stageEFG)
    for name, fn in stages:
        fn()
        S.barrier()
        if upto == name:
            break
    S.finish()
    glob.close()
    return nc


def prep_inputs(inputs, cores=range(8), with_experts=True):
    f = lambda k: np.ascontiguousarray(np.asarray(inputs[k], dtype=np.float32))
    x, c, ctx, c_ctx = f("x"), f("c"), f("ctx"), f("c_ctx")
    consts = _consts()
    cos, sin = _rope_tables()
    rpb = f("na_rpb")[0]
    nab = np.ascontiguousarray(_na_bias_tables(rpb).reshape(5, 8, 128, 640))
    a_up = f("gla_a_up")[0]
    a_bias = f("gla_a_bias")[0]
    aup_bd = np.zeros((32, 512), np.float32)
    aup_bd[0:16, 0:256] = a_up[0]
    aup_bd[16:32, 256:512] = a_up[1]
    abias = np.ascontiguousarray(a_bias.reshape(1, 512))
    norms = np.ascontiguousarray(np.stack([f("norm_mix_pre")[0], f("norm_mix_post")[0], f("norm_ffn_pre")[0], f("norm_ffn_post")[0]]))
    shared = dict(
        w_mod=f("w_mod")[0], b_mod=f("b_mod"), norms=norms, gla_norm=f("gla_norm"), w_in=f("w_in")[0],
        aup_bd=aup_bd, abias=abias, rope_cos=cos, rope_sin=sin, na_bias=nab, w_out=f("w_out")[0],
        router=f("router")[0], **consts)
    if with_experts:
        tile_gu = lambda w: np.ascontiguousarray(w.reshape(NE, 8, 128, 11, 256).transpose(0, 3, 2, 1, 4)).reshape(NE, 11, 128, 8 * 256)
        shared.update(w_gate=tile_gu(f("w_gate")[0]), w_up=tile_gu(f("w_up")[0]),
                      w_down=np.ascontiguousarray(f("w_down")[0].reshape(NE, NFC, 128, 4, 256).transpose(0, 3, 2, 1, 4)).reshape(NE, 4, 128, NFC * 256))
    maps = []
    pp = np.arange(128)
    for core in cores:
        s, p = core // 2, core % 2
        m = dict(shared)
        m["xc"] = np.ascontiguousarray(np.concatenate([ctx[s], x[s]], axis=0))
        cv = np.zeros((128, 16), np.float32)
        cv[:, 0:8] = c[s].reshape(8, 128).T
        cv[:, 8:16] = c_ctx.reshape(8, 128).T
        m["cvec"] = cv
        m["own_idx"] = np.ascontiguousarray((p * 2048 + np.arange(16)[None, :] * 128 + pp[:, None]).astype(np.int32))
        seg = pp % 8
        m["own_flag"] = ((seg // 4) == p).astype(np.float32).reshape(128, 1)
        selm = np.zeros((128, 64), np.float32)
        for e in range(16):
            for os_ in range(4):
                selm[e * 8 + p * 4 + os_, e * 4 + os_] = 1.0
        m["sel"] = selm
        maps.append(m)
    return maps


_PROGRAM = None


def kernel(**inputs):
    global _PROGRAM
    if _PROGRAM is None:
        _PROGRAM = build_program()
    maps = prep_inputs(inputs)
    res = run_bass_kernel_spmd(_PROGRAM, maps, core_ids=list(range(8)))
    out = np.zeros((4, SEQ, D), np.float32)
    for core in range(8):
        s, p = core // 2, core % 2
        out[s, p * 2048:(p + 1) * 2048] = res.results[core]["out"]
    return out
```
